# Optimizing a Trainium2 kernel written in Bass

```python
import math
import jax, jax.numpy as jnp
from jax import lax
import numpy as np

D_MODEL = 1024
BATCH = 8
SEQ = 2048
DEPTH = 4

GRID_W = 64
CTX_LEN = 256
ROPE_BASE = 10000.0
LN_EPS = 1e-5
DEEPNORM_ALPHA = (2 * DEPTH) ** 0.25
DEEPNORM_BETA = (8 * DEPTH) ** -0.25
N_EVEN = (DEPTH + 1) // 2
N_ODD = DEPTH // 2
H_A = 4
DK_A = D_MODEL // 8
DV_A = D_MODEL // 8
CHUNK_A = 128
RET_EXP_FWD = 5.0
RET_EXP_BWD = 5.5
H_B = 4
DK_B = D_MODEL // 16
DV_B = D_MODEL // 8
GLA_RANK = 16
GLA_TAU = 16.0
CHUNK_B = 64
AB_SPLITS = (H_A * DK_A, H_A * DK_A, H_A * DV_A, H_A * DV_A,
             H_B * DK_B, H_B * DK_B, H_B * DV_B, H_B * DV_B, GLA_RANK, GLA_RANK)
AB_IN = sum(AB_SPLITS)
AB_OUT = H_A * DV_A + H_B * DV_B
H_C = 8
DH_C = D_MODEL // 16
DV_C = 2 * DH_C
Q_BLOCK = 128
C_SPLITS = (H_C * 2 * DH_C, H_C * 2 * DH_C, H_C * DV_C)
C_IN = sum(C_SPLITS)
C_OUT = H_C * DV_C
LAMBDA_STD = 0.1
N_GROUPS = 4
EXPERTS_PER_GROUP = 8
N_EXPERTS = N_GROUPS * EXPERTS_PER_GROUP
TOP_K_EXPERT = 2
D_EXPERT = D_MODEL // 2
MOE_BLOCK = 128

kernel_name = 'hybrid_retention_gla_diffattn_hmoe_dit'


def layer_norm(x, g, b):
    xf = x.astype(jnp.float32)
    mu = jnp.mean(xf, axis=-1, keepdims=True)
    var = jnp.mean(jnp.square(xf - mu), axis=-1, keepdims=True)
    return ((xf - mu) * lax.rsqrt(var + LN_EPS) * g + b).astype(x.dtype)


def head_norm(o, center):
    of = o.astype(jnp.float32)
    if center:
        of = of - jnp.mean(of, axis=-1, keepdims=True)
    return of * lax.rsqrt(jnp.mean(jnp.square(of), axis=-1, keepdims=True) + LN_EPS)


def split_sizes(t, sizes):
    cuts = [int(s) for s in np.cumsum(sizes)[:-1]]
    return jnp.split(t, cuts, axis=-1)


def axial_rope(rows, head_dim):
    row = jnp.repeat(jnp.arange(rows, dtype=jnp.float32), GRID_W)
    col = jnp.tile(jnp.arange(GRID_W, dtype=jnp.float32), rows)
    quarter = head_dim // 4
    inv = ROPE_BASE ** (-jnp.arange(quarter, dtype=jnp.float32) / quarter)
    ang_r = row[:, None] * inv
    ang_c = col[:, None] * inv
    ang = jnp.concatenate([ang_r, ang_r, ang_c, ang_c], axis=-1)
    return jnp.cos(ang), jnp.sin(ang)


def rope(x, cos, sin):
    a, b, c, d = jnp.split(x, 4, axis=-1)
    rot = jnp.concatenate([-b, a, -d, c], axis=-1)
    return (x * cos + rot * sin).astype(x.dtype)


def chunk_scan(q, k, v, log_a, s0, chunk, inclusive):
    bn, h, t, dk = q.shape
    dv = v.shape[-1]
    n = t // chunk

    def to_chunks(z):
        return jnp.moveaxis(z.reshape(bn, h, n, chunk, z.shape[-1]), 2, 0)

    mask = jnp.tril(jnp.ones((chunk, chunk), dtype=bool), 0 if inclusive else -1)

    def step(s, inp):
        qi, ki, vi, li = inp
        b = jnp.cumsum(li, axis=-2)
        bq = b if inclusive else b - li
        btot = b[..., -1:, :]
        qd = qi * jnp.exp(bq)
        kd = ki * jnp.exp(-b)
        att = jnp.where(mask, jnp.einsum('bhid,bhjd->bhij', qd, kd), 0.0)
        o = jnp.einsum('bhij,bhje->bhie', att, vi) + jnp.einsum('bhid,bhde->bhie', qd, s)
        kt = ki * jnp.exp(btot - b)
        s = jnp.exp(btot)[..., 0, :, None] * s + jnp.einsum('bhjd,bhje->bhde', kt, vi)
        return s, o

    s_fin, o = lax.scan(step, s0, (to_chunks(q), to_chunks(k), to_chunks(v), to_chunks(log_a)))
    return jnp.moveaxis(o, 0, 2).reshape(bn, h, t, dv), s_fin


def bidir_scan(q, k, v, la_f, la_b, s0_f, s0_b, chunk):
    q, k, v = (z.astype(jnp.float32) for z in (q, k, v))
    o_f, s_f = chunk_scan(q, k, v, la_f, s0_f, chunk, True)
    flip = lambda z: jnp.flip(z, axis=2)
    o_b, s_b = chunk_scan(flip(q), flip(k), flip(v), flip(la_b), s0_b, chunk, False)
    return o_f + flip(o_b), s_f, s_b


def ret_logdecay(exp0, like):
    gam = jnp.log1p(-jnp.exp2(-(exp0 + jnp.arange(H_A, dtype=jnp.float32))))
    return jnp.broadcast_to(gam[None, :, None, None], like.shape)


def ab_heads(z, w_in, w_lr_f, b_lr_f, w_lr_b, b_lr_b, cs):
    bn, t, _ = z.shape
    qa, ka, va, ga, qb, kb, vb, gb, lr_f, lr_b = split_sizes(z @ w_in, AB_SPLITS)
    qa = qa.reshape(bn, t, H_A, DK_A)
    ka = ka.reshape(bn, t, H_A, DK_A)
    if cs is not None:
        qa = rope(qa, *cs)
        ka = rope(ka, *cs)
    qa = qa * (DK_A ** -0.5)
    va = va.reshape(bn, t, H_A, DV_A)
    qb = qb.reshape(bn, t, H_B, DK_B) * (DK_B ** -0.5)
    kb = kb.reshape(bn, t, H_B, DK_B)
    vb = vb.reshape(bn, t, H_B, DV_B)

    def gla_logdecay(lr, w, b):
        g = (lr @ w + b).astype(jnp.float32)
        return (jax.nn.log_sigmoid(g) / GLA_TAU).reshape(bn, t, H_B, DK_B)

    la_f = gla_logdecay(lr_f, w_lr_f, b_lr_f)
    la_b = gla_logdecay(lr_b, w_lr_b, b_lr_b)
    bhtd = lambda u: jnp.swapaxes(u, 1, 2)
    return (bhtd(qa), bhtd(ka), bhtd(va), ga, bhtd(qb), bhtd(kb), bhtd(vb), gb, bhtd(la_f), bhtd(la_b))


def mixer_ab(ux, uc, w_in, w_lr_f, b_lr_f, w_lr_b, b_lr_b, gn_a, gn_b, w_out, cs, need_ctx):
    fx = ab_heads(ux, w_in, w_lr_f, b_lr_f, w_lr_b, b_lr_b, cs)
    fc = ab_heads(uc, w_in, w_lr_f, b_lr_f, w_lr_b, b_lr_b, None)
    bn = ux.shape[0]
    za = jnp.zeros((bn, H_A, DK_A, DV_A), jnp.float32)
    zb = jnp.zeros((bn, H_B, DK_B, DV_B), jnp.float32)

    def mix(f, st):
        qa, ka, va, ga, qb, kb, vb, gb, laf, lab = f
        oa, saf, sab = bidir_scan(qa, ka, va, ret_logdecay(RET_EXP_FWD, qa),
                                  ret_logdecay(RET_EXP_BWD, qa), st[0], st[1], CHUNK_A)
        ob, sbf, sbb = bidir_scan(qb, kb, vb, laf, lab, st[2], st[3], CHUNK_B)
        return (oa, ga, ob, gb), (saf, sab, sbf, sbb)

    out_c, st_c = mix(fc, (za, za, zb, zb))
    out_x, _ = mix(fx, st_c)

    def merge(oa, ga, ob, gb):
        b_, _, t_, _ = oa.shape
        ya = jnp.swapaxes(head_norm(oa, True), 1, 2).reshape(b_, t_, H_A * DV_A) * gn_a
        yb = jnp.swapaxes(head_norm(ob, False), 1, 2).reshape(b_, t_, H_B * DV_B) * gn_b
        y = jnp.concatenate([jax.nn.silu(ga.astype(jnp.float32)) * ya,
                             jax.nn.silu(gb.astype(jnp.float32)) * yb], axis=-1)
        return y.astype(ux.dtype) @ w_out

    yx = merge(*out_x)
    yc = merge(*out_c) if need_ctx else None
    return yx, yc


def c_heads(z, w_qkv, cs):
    bn, t, _ = z.shape
    q, k, v = split_sizes(z @ w_qkv, C_SPLITS)
    q = q.reshape(bn, t, H_C, 2, DH_C)
    k = k.reshape(bn, t, H_C, 2, DH_C)
    if cs is not None:
        q = rope(q, *cs)
        k = rope(k, *cs)
    return q * (DH_C ** -0.5), k, v.reshape(bn, t, H_C, DV_C)


def diff_attend(q, k, v, lam):
    s = jnp.einsum('bqhcd,bkhcd->bchqk', q, k).astype(jnp.float32)
    p = jax.nn.softmax(s, axis=-1)
    a = p[:, 0] - lam[:, None, None] * p[:, 1]
    return jnp.einsum('bhqk,bkhe->bqhe', a.astype(v.dtype), v)


def mixer_c(ux, uc, w_qkv, lq1, lk1, lq2, lk2, subln_g, w_out, lam_init, cs, need_ctx):
    qx, kx, vx = c_heads(ux, w_qkv, cs)
    qc, kc, vc = c_heads(uc, w_qkv, None)
    lam = (jnp.exp(jnp.sum(lq1 * lk1, axis=-1)) - jnp.exp(jnp.sum(lq2 * lk2, axis=-1))).astype(jnp.float32) + lam_init
    k_all = jnp.concatenate([kc, kx], axis=1)
    v_all = jnp.concatenate([vc, vx], axis=1)
    bn, t = qx.shape[:2]
    qb = jnp.moveaxis(qx.reshape(bn, t // Q_BLOCK, Q_BLOCK, H_C, 2, DH_C), 1, 0)
    ox = lax.map(lambda qi: diff_attend(qi, k_all, v_all, lam), qb)
    ox = jnp.moveaxis(ox, 0, 1).reshape(bn, t, H_C, DV_C)

    def finish(o):
        b_, t_ = o.shape[:2]
        y = head_norm(o, False).reshape(b_, t_, C_OUT) * subln_g * (1.0 - lam_init)
        return y.astype(ux.dtype) @ w_out

    yx = finish(ox)
    yc = finish(diff_attend(qc, kc, vc, lam)) if need_ctx else None
    return yx, yc


def hier_moe(z, w_grp, b_grp, w_rexp, b_rexp, w_gate, w_up, w_down):
    n, d = z.shape
    zf = z.astype(jnp.float32)
    g_logits = zf @ w_grp.astype(jnp.float32) + b_grp
    g_prob = jax.nn.softmax(g_logits, axis=-1)
    grp = jnp.argmax(g_logits, axis=-1).astype(jnp.int32)
    g_oh = jax.nn.one_hot(grp, N_GROUPS, dtype=jnp.float32)
    g_w = jnp.sum(g_prob * g_oh, axis=-1)
    e_logits = (zf @ w_rexp.astype(jnp.float32) + b_rexp).reshape(n, N_GROUPS, EXPERTS_PER_GROUP)
    e_sel = jnp.einsum('nge,ng->ne', e_logits, g_oh)
    top_v, top_i = lax.top_k(e_sel, TOP_K_EXPERT)
    comb = g_w[:, None] * jax.nn.softmax(top_v, axis=-1)
    eid = grp[:, None] * EXPERTS_PER_GROUP + top_i.astype(jnp.int32)
    na = n * TOP_K_EXPERT
    e_flat = eid.reshape(-1)
    t_flat = jnp.repeat(jnp.arange(n, dtype=jnp.int32), TOP_K_EXPERT)
    w_flat = comb.reshape(-1)
    order = jnp.argsort(e_flat)
    e_sorted = e_flat[order]
    counts = jnp.bincount(e_flat, length=N_EXPERTS)
    padded = (counts + MOE_BLOCK - 1) // MOE_BLOCK * MOE_BLOCK
    pad_end = jnp.cumsum(padded)
    pad_start = pad_end - padded
    raw_start = jnp.cumsum(counts) - counts
    dest = pad_start[e_sorted] + jnp.arange(na) - raw_start[e_sorted]
    p_rows = -(-na // MOE_BLOCK) * MOE_BLOCK + N_EXPERTS * MOE_BLOCK
    n_blocks = p_rows // MOE_BLOCK
    row_tok = jnp.full((p_rows,), n, jnp.int32).at[dest].set(t_flat[order])
    row_w = jnp.zeros((p_rows,), jnp.float32).at[dest].set(w_flat[order])
    blk_exp = jnp.minimum(jnp.searchsorted(pad_end, jnp.arange(n_blocks) * MOE_BLOCK, side='right'),
                          N_EXPERTS - 1)
    z_pad = jnp.concatenate([z, jnp.zeros((1, d), z.dtype)], axis=0)
    xs = z_pad[row_tok].reshape(n_blocks, MOE_BLOCK, d)

    def run(args):
        xb, e = args
        h = jax.nn.silu(xb @ w_gate[e]) * (xb @ w_up[e])
        return h @ w_down[e]

    ys = lax.map(run, (xs, blk_exp)).reshape(p_rows, d)
    out = jnp.zeros((n + 1, d), z.dtype).at[row_tok].add(ys * row_w[:, None].astype(z.dtype))
    return out[:n]


def setup_inputs(seed: int = 0) -> dict:
    key = jax.random.key(seed)
    ks = iter(jax.random.split(key, 40))
    nrm = lambda shape, scale: jax.random.normal(next(ks), shape, jnp.float32) * scale
    gain = lambda shape: 1.0 + nrm(shape, 0.05)
    d = D_MODEL
    return {
        'x': nrm((BATCH, SEQ, d), 1.0),
        'c': nrm((BATCH, d), 1.0),
        'ctx': nrm((BATCH, CTX_LEN, d), 1.0),
        'c_ctx': nrm((d,), 1.0),
        'ada_w': nrm((DEPTH, d, 6 * d), 0.5 * d ** -0.5),
        'ada_b': nrm((DEPTH, 6 * d), 0.01),
        'ln1_g': gain((DEPTH, d)),
        'ln1_b': nrm((DEPTH, d), 0.01),
        'ln2_g': gain((DEPTH, d)),
        'ln2_b': nrm((DEPTH, d), 0.01),
        'ab_w_in': nrm((N_EVEN, d, AB_IN), d ** -0.5),
        'ab_w_lr_f': nrm((N_EVEN, GLA_RANK, H_B * DK_B), GLA_RANK ** -0.5),
        'ab_b_lr_f': nrm((N_EVEN, H_B * DK_B), 0.01),
        'ab_w_lr_b': nrm((N_EVEN, GLA_RANK, H_B * DK_B), GLA_RANK ** -0.5),
        'ab_b_lr_b': nrm((N_EVEN, H_B * DK_B), 0.01),
        'ab_gn_a': gain((N_EVEN, H_A * DV_A)),
        'ab_gn_b': gain((N_EVEN, H_B * DV_B)),
        'ab_w_out': nrm((N_EVEN, AB_OUT, d), DEEPNORM_BETA * AB_OUT ** -0.5),
        'c_w_qkv': nrm((N_ODD, d, C_IN), d ** -0.5),
        'c_lq1': nrm((N_ODD, H_C, DH_C), LAMBDA_STD),
        'c_lk1': nrm((N_ODD, H_C, DH_C), LAMBDA_STD),
        'c_lq2': nrm((N_ODD, H_C, DH_C), LAMBDA_STD),
        'c_lk2': nrm((N_ODD, H_C, DH_C), LAMBDA_STD),
        'c_subln_g': gain((N_ODD, C_OUT)),
        'c_w_out': nrm((N_ODD, C_OUT, d), DEEPNORM_BETA * C_OUT ** -0.5),
        'moe_w_grp': nrm((DEPTH, d, N_GROUPS), d ** -0.5),
        'moe_b_grp': nrm((DEPTH, N_GROUPS), 0.01),
        'moe_w_rexp': nrm((DEPTH, d, N_EXPERTS), d ** -0.5),
        'moe_b_rexp': nrm((DEPTH, N_EXPERTS), 0.01),
        'moe_w_gate': nrm((DEPTH, N_EXPERTS, d, D_EXPERT), d ** -0.5),
        'moe_w_up': nrm((DEPTH, N_EXPERTS, d, D_EXPERT), d ** -0.5),
        'moe_w_down': nrm((DEPTH, N_EXPERTS, D_EXPERT, d), DEEPNORM_BETA * D_EXPERT ** -0.5),
    }


def reference(x, c, ctx, c_ctx, ada_w, ada_b, ln1_g, ln1_b, ln2_g, ln2_b,
              ab_w_in, ab_w_lr_f, ab_b_lr_f, ab_w_lr_b, ab_b_lr_b, ab_gn_a, ab_gn_b, ab_w_out,
              c_w_qkv, c_lq1, c_lk1, c_lq2, c_lk2, c_subln_g, c_w_out,
              moe_w_grp, moe_b_grp, moe_w_rexp, moe_b_rexp, moe_w_gate, moe_w_up, moe_w_down):
    bn, t, d = x.shape
    n_ctx = ctx.shape[1]
    rows = t // GRID_W
    cos_a, sin_a = axial_rope(rows, DK_A)
    cs_a = (cos_a[:, None, :], sin_a[:, None, :])
    cos_c, sin_c = axial_rope(rows, DH_C)
    cs_c = (cos_c[:, None, None, :], sin_c[:, None, None, :])
    sc = jax.nn.silu(c)
    scc = jax.nn.silu(c_ctx)
    for l in range(DEPTH):
        last = l == DEPTH - 1
        i = l // 2
        sh1x, s1x, g1x, sh2x, s2x, g2x = jnp.split(sc @ ada_w[l] + ada_b[l], 6, axis=-1)
        sh1c, s1c, g1c, sh2c, s2c, g2c = jnp.split(scc @ ada_w[l] + ada_b[l], 6, axis=-1)
        ux = x * (1.0 + s1x[:, None, :]) + sh1x[:, None, :]
        uc = ctx * (1.0 + s1c) + sh1c
        if l % 2 == 0:
            yx, yc = mixer_ab(ux, uc, ab_w_in[i], ab_w_lr_f[i], ab_b_lr_f[i], ab_w_lr_b[i], ab_b_lr_b[i],
                              ab_gn_a[i], ab_gn_b[i], ab_w_out[i], cs_a, not last)
        else:
            lam_init = 0.8 - 0.6 * math.exp(-0.3 * l)
            yx, yc = mixer_c(ux, uc, c_w_qkv[i], c_lq1[i], c_lk1[i], c_lq2[i], c_lk2[i], c_subln_g[i],
                             c_w_out[i], lam_init, cs_c, not last)
        x = layer_norm(DEEPNORM_ALPHA * x + g1x[:, None, :] * yx, ln1_g[l], ln1_b[l])
        if not last:
            ctx = layer_norm(DEEPNORM_ALPHA * ctx + g1c * yc, ln1_g[l], ln1_b[l])
        ux = x * (1.0 + s2x[:, None, :]) + sh2x[:, None, :]
        tok = ux.reshape(bn * t, d)
        if not last:
            uc = ctx * (1.0 + s2c) + sh2c
            tok = jnp.concatenate([tok, uc.reshape(bn * n_ctx, d)], axis=0)
        y = hier_moe(tok, moe_w_grp[l], moe_b_grp[l], moe_w_rexp[l], moe_b_rexp[l],
                     moe_w_gate[l], moe_w_up[l], moe_w_down[l])
        x = layer_norm(DEEPNORM_ALPHA * x + g2x[:, None, :] * y[:bn * t].reshape(bn, t, d), ln2_g[l], ln2_b[l])
        if not last:
            ctx = layer_norm(DEEPNORM_ALPHA * ctx + g2c * y[bn * t:].reshape(bn, n_ctx, d), ln2_g[l], ln2_b[l])
    return x
```

```python
import math
import numpy as np
import ml_dtypes
from contextlib import ExitStack
import concourse.bass as bass
import concourse.mybir as mybir
from concourse.bass_utils import run_bass_kernel_spmd

F32 = mybir.dt.float32
BF16 = mybir.dt.bfloat16
U8 = mybir.dt.uint8
AF = mybir.ActivationFunctionType
ALU = mybir.AluOpType
AX = mybir.AxisListType

D = 1024
T = 2048
TC = 256
NT = T + TC
DEPTH = 4
ALPHA = (2 * DEPTH) ** 0.25
EPS = 1e-5
BLOCKS = [(0, 512, 0), (512, 512, 0), (1024, 512, 0), (1536, 512, 0), (2048, 256, 1)]
ORDER_F = [16, 17] + list(range(16))
ORDER_B = [17, 16] + list(range(15, -1, -1))
AB_IN = 3616
import os as _os
DBG_HEADS = int(_os.environ.get("DBG_HEADS", "8"))
DBG_B = int(_os.environ.get("DBG_B", "9"))
DBG_C = _os.environ.get("DBG_C", "z")


class Prog:
    def __init__(self, nc, es, ndma=36):
        self.nc = nc
        self.e = dict(pe=nc.tensor, act=nc.scalar, dve=nc.vector, pool=nc.gpsimd, sp=nc.sync)
        self.sem = {k: es.enter_context(nc.semaphore("s_" + k)) for k in self.e}
        self.cnt = {k: 0 for k in self.e}
        self.seen = {k: {} for k in self.e}
        self.st = {}
        self.dq = {}
        for q in ("sp", "pool"):
            self.dq[q] = dict(sems=[es.enter_context(nc.semaphore(f"d_{q}{i}")) for i in range(ndma)],
                              vals=[0] * ndma, idx=0)
        self.nops = 0
        self.bar = es.enter_context(nc.sbuf_tensor("barrier_t", [128, 1], F32))

    def _wait(self, eng, ticks):
        need = {}
        for t in ticks:
            if t is None:
                continue
            key, sem, val = t
            if key == "pe" and eng == "pe":
                continue
            if need.get(key, (None, 0))[1] < val:
                need[key] = (sem, val)
        for key, (sem, val) in need.items():
            if self.seen[eng].get(key, 0) >= val:
                continue
            self.e[eng].wait_ge(sem, val)
            self.seen[eng][key] = val

    def _deps(self, R, W):
        ticks = []
        for k in R:
            s = self.st.get(k)
            if s is not None:
                ticks.append(s[0])
                if k.startswith("ps"):
                    ticks.extend(s[1].values())
        for k in W:
            s = self.st.get(k)
            if s is not None:
                ticks.append(s[0])
                ticks.extend(s[1].values())
        return ticks

    def _update(self, tick, R, W):
        for k in W:
            self.st[k] = [tick, {}]
        for k in R:
            s = self.st.get(k)
            if s is None:
                s = self.st[k] = [None, {}]
            s[1][tick[0]] = tick

    def op(self, eng, fn, R, W):
        self._wait(eng, self._deps(R, W))
        inst = fn()
        inst.then_inc(self.sem[eng], 1)
        self.cnt[eng] += 1
        self.nops += 1
        self._update((eng, self.sem[eng], self.cnt[eng]), R, W)

    def dma(self, q, out, in_, R, W):
        d = self.dq[q]
        i = d["idx"]
        d["idx"] = (i + 1) % len(d["sems"])
        key = ("d", q, i)
        ticks = self._deps(R, W)
        if d["vals"][i] > 0:
            ticks.append((key, d["sems"][i], d["vals"][i]))
        self._wait(q, ticks)
        self.e[q].dma_start(out=out, in_=in_).then_inc(d["sems"][i], 16)
        d["vals"][i] += 16
        self.nops += 1
        self._update((key, d["sems"][i], d["vals"][i]), R, W)

    def barrier(self):
        ticks = []
        for e in self.e:
            if self.cnt[e] > 0:
                ticks.append((e, self.sem[e], self.cnt[e]))
        for q, d in self.dq.items():
            for i, v in enumerate(d["vals"]):
                if v > 0:
                    ticks.append((("d", q, i), d["sems"][i], v))
        self._wait("dve", ticks)
        inst = self.nc.vector.memset(self.bar[:], 0.0)
        inst.then_inc(self.sem["dve"], 1)
        self.cnt["dve"] += 1
        t = ("dve", self.sem["dve"], self.cnt["dve"])
        for e in ("pe", "act", "pool", "sp"):
            self._wait(e, [t])

    def finish(self, eng, keys):
        self._wait(eng, self._deps(keys, []))


def build(n_layers=DEPTH, stop=None):
    nc = bass.Bass("TRN2", target_bir_lowering=False)

    def din(name, shape, dt=F32):
        return nc.dram_tensor(name, list(shape), dt, kind="ExternalInput").ap()

    xT_d = din("xT", [8, 128, NT])
    cc_d = din("cc", [128, 8, 2])
    adab_d = din("adab", [128, 4, 48])
    lnp_d = din("lnp", [128, 4, 4, 8])
    gn_d = din("gn", [128, 2, 8])
    subg_d = din("subg", [128, 2, 8])
    wlr_d = din("wlr", [32, 2, 2, 256])
    wr_d = din("wr", [128, 4, 8, 36])
    rb_d = din("rb", [1, 4, 36])
    lqk_d = din("lqk", [64, 2, 4, 8])
    ret_d = din("ret", [128, 4, 6, 128])
    ropeA_d = din("ropeA", [4, 128, T])
    ropeC_d = din("ropeC", [4, 128, T])
    cst_d = din("cst", [128, 1024])
    cst2_d = din("cst2", [128, 256])
    cstb_d = din("cstb", [128, 256], BF16)
    mask_d = din("mask", [128, 512], U8)
    sele_d = din("sele", [32, 32 * 128], BF16)
    ada_w_d = din("ada_w", [4, D, 6 * D])
    ab_w_in_d = din("ab_w_in", [2, D, AB_IN])
    ab_w_out_d = din("ab_w_out", [2, D, D])
    c_w_qkv_d = din("c_w_qkv", [2, D, 3 * D])
    c_w_out_d = din("c_w_out", [2, D, D])
    use_moe = not (stop in ("ada", "mix", "ln1", "y") and n_layers <= 1)
    if use_moe:
        wg_d = din("moe_w_gate", [4, 32, D, 512])
        wu_d = din("moe_w_up", [4, 32, D, 512])
        wd_d = din("moe_w_down", [4, 32, 512, D])
    out_d = nc.dram_tensor("outT", [8, 128, NT], F32, kind="ExternalOutput").ap()

    es = ExitStack()
    with es:
        P = Prog(nc, es)

        uid = [0]

        def sb(name, shape, dt=F32, stack=es):
            uid[0] += 1
            return stack.enter_context(nc.sbuf_tensor(f"{name}_{uid[0]}", list(shape), dt))

        PS = [es.enter_context(nc.psum_tensor(f"ps{i}", [128, 512], F32)) for i in range(8)]
        psc = [0]

        def nb():
            i = psc[0]
            psc[0] = (i + 1) % 8
            return PS[i], f"ps{i}"

        def mm(out, lhsT, rhs, start, stop, R, W):
            P.op("pe", lambda: nc.tensor.matmul(out, lhsT=lhsT, rhs=rhs, start=start, stop=stop), R, W)

        def act(out, in_, func, R, W, scale=None, bias=None, accum_out=None):
            kw = {}
            if scale is not None:
                kw["scale"] = scale
            if bias is None and func != AF.Copy:
                sp_, np_ = in_.start_partition(), in_.partition_size()
                bias = ZEROC[sp_:sp_ + np_, 0:1]
                R = list(R) + ["ZEROC"]
            if bias is not None:
                kw["bias"] = bias
            if accum_out is not None:
                kw["accum_out"] = accum_out
            P.op("act", lambda: nc.scalar.activation(out=out, in_=in_, func=func, **kw), R, W)

        def tt(out, a, b, op, R, W, eng="dve"):
            e = nc.vector if eng == "dve" else nc.gpsimd
            P.op(eng, lambda: e.tensor_tensor(out=out, in0=a, in1=b, op=op), R, W)

        def ts(out, a, s1, op0, R, W, s2=None, op1=None, eng="dve"):
            e = nc.vector if eng == "dve" else nc.gpsimd
            if op1 is None:
                P.op(eng, lambda: e.tensor_scalar(out=out, in0=a, scalar1=s1, scalar2=None, op0=op0), R, W)
            else:
                P.op(eng, lambda: e.tensor_scalar(out=out, in0=a, scalar1=s1, scalar2=s2, op0=op0, op1=op1), R, W)

        def stt(out, a, s, b, op0, op1, R, W):
            P.op("dve", lambda: nc.vector.scalar_tensor_tensor(out=out, in0=a, scalar=s, in1=b, op0=op0, op1=op1), R, W)

        def cp(out, in_, R, W, eng="dve"):
            if eng == "act":
                act(out, in_, AF.Copy, R, W)
            else:
                e = nc.vector if eng == "dve" else nc.gpsimd
                P.op(eng, lambda: e.tensor_copy(out=out, in_=in_), R, W)

        XT = sb("XT", [128, 8, NT])
        UY = sb("UY", [128, 8, NT], BF16)
        MOD = sb("MOD", [128, 4, 6, 8, 2])
        LNP = sb("LNP", [128, 4, 4, 8])
        GN = sb("GN", [128, 2, 8])
        SUBG = sb("SUBG", [128, 2, 8])
        RB = sb("RB", [1, 4, 36])
        LAM = sb("LAM", [128, 2, 8])
        CST = sb("CST", [128, 1024])
        CST2 = sb("CST2", [128, 256])
        CSTB = sb("CSTB", [128, 256], BF16)
        MASK = sb("MASK", [128, 512], U8)
        EPSC = sb("EPSC", [128, 1])
        ONEC = sb("ONEC", [128, 1])
        ZEROC = sb("ZEROC", [128, 1])
        IDF = CST[:, 0:128]
        ONESD = CST[:, 128:256]
        ONESH = CST[:, 256:384]
        TRIF = CST[:, 384:512]
        ONES1 = CST[0:1, 512:640]
        TRIB = CST2[:, 0:128]
        TRIBP = CST2[:, 128:256]
        IDB = CSTB[:, 0:128]
        ONESB = CSTB[:, 128:256]

        def xkeys(blocks=range(5), ks=range(8)):
            return [f"x{k}b{b}" for k in ks for b in blocks]

        def ukeys(blocks=range(5), ks=range(8)):
            return [f"u{k}b{b}" for k in ks for b in blocks]

        for k in range(8):
            P.dma("sp", XT[:, k, :], xT_d[k], [], xkeys(ks=[k]))
        for (t_, d_, kname) in [(LNP, lnp_d, "LNP"), (GN, gn_d, "GN"), (SUBG, subg_d, "SUBG"),
                                (RB, rb_d, "RB"), (CST, cst_d, "CST"), (CST2, cst2_d, "CST2"), (CSTB, cstb_d, "CSTB"),
                                (MASK, mask_d, "MASK")]:
            P.dma("sp", t_[:], d_, [], [kname])
        P.op("dve", lambda: nc.vector.memset(EPSC[:], EPS), [], ["EPSC"])
        P.op("dve", lambda: nc.vector.memset(ONEC[:], 1.0), [], ["ONEC"])
        P.op("dve", lambda: nc.vector.memset(ZEROC[:], 0.0), [], ["ZEROC"])

        with ExitStack() as s1:
            CC = sb("CC", [128, 8, 2], F32, s1)
            SCB = sb("SCB", [128, 8, 2], BF16, s1)
            ADAB = sb("ADAB", [128, 4, 48], F32, s1)
            WA = [sb(f"WA{i}", [128, 8, 1024], BF16, s1) for i in range(2)]
            LQK = sb("LQK", [64, 2, 4, 8], F32, s1)
            LQP = sb("LQP", [64, 2, 2, 8], F32, s1)
            P.dma("sp", CC[:], cc_d, [], ["CC"])
            P.dma("sp", ADAB[:], adab_d, [], ["ADAB"])
            P.dma("sp", LQK[:], lqk_d, [], ["LQK"])
            act(SCB[:], CC[:], AF.Silu, ["CC"], ["SCB"])
            it = 0
            for l in range(n_layers):
                for j6 in range(6):
                    w = WA[it % 2]
                    wk = f"WA{it % 2}"
                    it += 1
                    src = ada_w_d[l].rearrange("(k p) n -> p k n", p=128)[:, :, j6 * 1024:(j6 + 1) * 1024]
                    P.dma("pool", w[:], src, [], [wk])
                    ps, pk = nb()
                    for jj in range(8):
                        for k in range(8):
                            mm(ps[:, jj * 2:jj * 2 + 2], w[:, k, jj * 128:(jj + 1) * 128], SCB[:, k, :],
                               k == 0, k == 7, [wk, "SCB"], [pk])
                    tt(MOD[:, l, j6, :, :], ps[:, 0:16].rearrange("p (j g) -> p j g", g=2),
                       ADAB[:, l, j6 * 8:(j6 + 1) * 8].unsqueeze(2).broadcast_to([128, 8, 2]), ALU.add,
                       [pk, "ADAB"], ["MOD"])
                for j6 in (1, 4):
                    ts(MOD[:, l, j6, :, :], MOD[:, l, j6, :, :], 1.0, ALU.add, ["MOD"], ["MOD"])
            tt(LQP[:, :, 0, :], LQK[:, :, 0, :], LQK[:, :, 1, :], ALU.mult, ["LQK"], ["LQP"])
            tt(LQP[:, :, 1, :], LQK[:, :, 2, :], LQK[:, :, 3, :], ALU.mult, ["LQP", "LQK"], ["LQP"])
            ps, pk = nb()
            mm(ps[:, 0:32], CST[0:64, 512:640], LQP[:].rearrange("p a b c -> p (a b c)"), True, True,
               ["CST", "LQP"], [pk])
            LE = sb("LE", [128, 32], F32, s1)
            act(LE[:], ps[:, 0:32], AF.Exp, [pk], ["LE"])
            lev = LE[:].rearrange("p (a b c) -> p a b c", a=2, b=2)
            for i in range(2):
                lam_init = 0.8 - 0.6 * math.exp(-0.3 * (2 * i + 1))
                tt(LAM[:, i, :], lev[:, i, 1, :], lev[:, i, 0, :], ALU.subtract, ["LE", "LAM"], ["LAM"])
                ts(LAM[:, i, :], LAM[:, i, :], -lam_init, ALU.add, ["LAM"], ["LAM"])
                ts(SUBG[:, i, :], SUBG[:, i, :], 1.0 - lam_init, ALU.mult, ["SUBG"], ["SUBG"])

        P.barrier()

        def mod(l, which, k, grp):
            return MOD[:, l, which, k, grp:grp + 1]

        def ln_block(l, which_ln, b, scr):
            t0, n, grp = BLOCKS[b]
            SQ, MEAN, RSTD = scr
            psm, pkm = nb()
            pss, pks = nb()
            for k in range(8):
                xk = XT[:, k, t0:t0 + n]
                mm(psm[:, :n], ONESD, xk, k == 0, k == 7, [f"x{k}b{b}", "CST"], [pkm])
                sq = SQ[k % 2]
                act(sq[:, :n], xk, AF.Square, [f"x{k}b{b}"], [f"SQ{k % 2}"])
                mm(pss[:, :n], ONESD, sq[:, :n], k == 0, k == 7, [f"SQ{k % 2}", "CST"], [pks])
            cp(MEAN[:, :n], psm[:, :n], [pkm], ["MEAN"], eng="act")
            act(RSTD[:, :n], psm[:, :n], AF.Square, [pkm], ["RSTD"])
            tt(RSTD[:, :n], pss[:, :n], RSTD[:, :n], ALU.subtract, [pks, "RSTD"], ["RSTD"])
            act(RSTD[:, :n], RSTD[:, :n], AF.Ln, ["RSTD"], ["RSTD"], bias=EPSC[:, 0:1])
            act(RSTD[:, :n], RSTD[:, :n], AF.Exp, ["RSTD"], ["RSTD"], scale=-0.5)
            for k in range(8):
                xk = XT[:, k, t0:t0 + n]
                kk = [f"x{k}b{b}"]
                tt(xk, xk, MEAN[:, :n], ALU.subtract, kk + ["MEAN"], kk)
                tt(xk, xk, RSTD[:, :n], ALU.mult, kk + ["RSTD"], kk)
                act(xk, xk, AF.Identity, kk + ["LNP"], kk, scale=LNP[:, l, 2 * which_ln, k:k + 1],
                    bias=LNP[:, l, 2 * which_ln + 1, k:k + 1])

        def proj_ln(l, w_dram, nblocks, raw):
            with ExitStack() as s2:
                WO = sb("WO", [128, 8, 1024], BF16, s2)
                TMP = [sb(f"TMPO{i}", [128, 512], F32, s2) for i in range(2)]
                SQ = [sb(f"SQ{i}", [128, 512], F32, s2) for i in range(2)]
                MEAN = sb("MEAN", [128, 512], F32, s2)
                RSTD = sb("RSTD", [128, 512], F32, s2)
                P.dma("pool", WO[:], w_dram.rearrange("(k p) n -> p k n", p=128), [], ["WO"])
                for b in range(nblocks):
                    t0, n, grp = BLOCKS[b]
                    for c in range(8):
                        ps, pk = nb()
                        for h in range(8):
                            mm(ps[:, :n], WO[:, h, c * 128:(c + 1) * 128], UY[:, h, t0:t0 + n], h == 0, h == 7,
                               ["WO", f"u{h}b{b}"], [pk])
                        xk = XT[:, c, t0:t0 + n]
                        kk = [f"x{c}b{b}"]
                        if raw:
                            cp(xk, ps[:, :n], [pk], kk, eng="act")
                        else:
                            tmp = TMP[c % 2]
                            act(tmp[:, :n], ps[:, :n], AF.Identity, [pk, "MOD"], [f"TMPO{c % 2}"], scale=mod(l, 2, c, grp))
                            stt(xk, xk, ALPHA, tmp[:, :n], ALU.mult, ALU.add, kk + [f"TMPO{c % 2}"], kk)
                    if not raw:
                        ln_block(l, 0, b, (SQ, MEAN, RSTD))
            P.barrier()

        def make_u(l, b, UB, ub_i, which_s, which_sh):
            t0, n, grp = BLOCKS[b]
            for k in range(8):
                src = XT[:, k, t0:t0 + n]
                dst = UB[ub_i][:, k, :n]
                R = [f"x{k}b{b}", "MOD"]
                W = [f"UB{ub_i}k{k}"]
                if k % 2 == 0:
                    act(dst, src, AF.Identity, R, W, scale=mod(l, which_s, k, grp), bias=mod(l, which_sh, k, grp))
                else:
                    ts(dst, src, mod(l, which_s, k, grp), ALU.mult, R, W, s2=mod(l, which_sh, k, grp), op1=ALU.add)

        def fm_group(UBt, ub_i, n, Wt, wkey, c0, M):
            ps, pk = nb()
            for k in range(8):
                mm(ps[0:M, :n], Wt[:, k, c0:c0 + M], UBt[:, k, :n], k == 0, k == 7, [wkey, f"UB{ub_i}k{k}"], [pk])
            return ps, pk

        def rope_evac(dst, dkey, ps_a, pk_a, ps_b, pk_b, cos_t, sin_t, tkey, n, TR, ti):
            t1 = TR[0]
            t2 = TR[1]
            tt(t1[:, :n], ps_a[:, :n], cos_t, ALU.mult, [pk_a, tkey], ["TR0"])
            tt(t2[:, :n], ps_b[:, :n], sin_t, ALU.mult, [pk_b, tkey], ["TR1"])
            tt(dst, t1[:, :n], t2[:, :n], ALU.add, ["TR0", "TR1"], [dkey])

        def in_proj_head(l, hs, kind, wsrc, cols, Qf, Kf, V, SGN, gn_ap, rope_d, qscale, LRT=None, lr_cols=None,
                         after_block=None, nblocks=5):
            dk = 64 if kind == "B" else 128
            has_rope = kind in ("A", "C")
            has_g = kind in ("A", "B")
            wv = wsrc.rearrange("(k p) n -> p k n", p=128)
            W = hs["W"]
            ofs = {}
            o = 0
            names = ["q", "k", "v"] + (["g"] if has_g else [])
            for nm in names:
                w_ = dk if nm in ("q", "k") else 128
                P.dma("pool", W[:, :, o:o + w_], wv[:, :, cols[nm]:cols[nm] + w_], [], [f"W_{nm}"])
                ofs[nm] = o
                o += w_
            if LRT is not None:
                P.dma("pool", W[:, :, o:o + 32], wv[:, :, lr_cols:lr_cols + 32], [], ["W_lr"])
                ofs["lr"] = o
                o += 32
            if has_rope:
                blk = 32 if kind == "A" else 16
                for nm in ("q", "k"):
                    s_ = W[:, :, ofs[nm]:ofs[nm] + 128].rearrange("p k (a two b) -> p k a two b", two=2, b=blk)
                    d_ = W[:, :, o:o + 128].rearrange("p k (a two b) -> p k a two b", two=2, b=blk)
                    cp(d_[:, :, :, 0, :], s_[:, :, :, 1, :], [f"W_{nm}"], [f"W_{nm}p"])
                    cp(d_[:, :, :, 1, :], s_[:, :, :, 0, :], [f"W_{nm}", f"W_{nm}p"], [f"W_{nm}p"])
                    ofs[nm + "p"] = o
                    o += 128
            UB = hs["UB"]
            TR = hs["TR"]
            ROPE = hs["ROPE"]
            ti = 0
            for b in range(nblocks):
                t0, n, grp = BLOCKS[b]
                ub_i = b % len(UB)
                make_u(l, b, UB, ub_i, 1, 0)
                UBt = UB[ub_i]
                if has_rope and grp == 0:
                    P.dma("sp", ROPE[:, :, :], rope_d[:, :, t0:t0 + n].rearrange("f p t -> p f t"), [], ["ROPE"])
                for nm, dst_f in (("q", Qf), ("k", Kf)):
                    ps, pk = fm_group(UBt, ub_i, n, W, f"W_{nm}", ofs[nm], dk)
                    dst, dkey = dst_f(b)
                    if has_rope and grp == 0:
                        ps2, pk2 = fm_group(UBt, ub_i, n, W, f"W_{nm}p", ofs[nm + "p"], dk)
                        fi = 0 if nm == "q" else 2
                        rope_evac(dst, dkey, ps, pk, ps2, pk2, ROPE[:, fi, :n], ROPE[:, fi + 1, :n], "ROPE", n, TR, ti)
                        ti += 1
                    else:
                        sc = qscale if nm == "q" else 1.0
                        act(dst, ps[0:dk, :n], AF.Copy, [pk], [dkey], scale=sc)
                if has_g:
                    ps, pk = fm_group(UBt, ub_i, n, W, "W_g", ofs["g"], 128)
                    t1 = TR[ti % 2]
                    act(t1[:, :n], ps[:, :n], AF.Silu, [pk], [f"TR{ti % 2}"])
                    ts(SGN[:, t0:t0 + n], t1[:, :n], gn_ap, ALU.mult, [f"TR{ti % 2}", "GN"], [f"sgn_b{b}"])
                    ti += 1
                if LRT is not None:
                    for di in range(2):
                        ps, pk = fm_group(UBt, ub_i, n, W, "W_lr", ofs["lr"] + 16 * di, 16)
                        cp(LRT[di][0:16, :n], ps[0:16, :n], [pk], [f"lrt{di}"], eng="act")
                ps, pk = nb()
                ntile = n // 128
                for tI in range(ntile):
                    for k in range(8):
                        mm(ps[:, tI * 128:(tI + 1) * 128], UBt[:, k, tI * 128:(tI + 1) * 128],
                           W[:, k, ofs["v"]:ofs["v"] + 128], k == 0, k == 7, ["W_v", f"UB{ub_i}k{k}"], [pk])
                c0 = t0 // 128
                cp(V[:, c0:c0 + ntile, :], ps[:, :n].rearrange("p (c d) -> p c d", d=128), [pk], [f"vb{b}"], eng="act")
                if after_block is not None:
                    after_block(b)

        def head_norm(ps_o, pk_o, n, center, hs, out_ap, outkey, post_ap, postkeys, scale_ap=None, src_sb=None):
            OSB, SQh, RS = hs["OSB"], hs["SQh"], hs["RS"]
            M2 = SQh
            if src_sb is None:
                cp(OSB[:, :n], ps_o[:, :n], [pk_o], ["OSB"], eng="act")
                act(SQh[:, :n], ps_o[:, :n], AF.Square, [pk_o], ["SQh"])
            else:
                act(SQh[:, :n], OSB[:, :n], AF.Square, ["OSB"], ["SQh"])
            pss, pks = nb()
            mm(pss[:, :n], ONESH, SQh[:, :n], True, True, ["SQh", "CST"], [pks])
            if center:
                psm, pkm = nb()
                mm(psm[:, :n], ONESH, OSB[:, :n], True, True, ["OSB", "CST"], [pkm])
                act(M2[:, :n], psm[:, :n], AF.Square, [pkm, "SQh"], ["SQh"])
                tt(RS[:, :n], pss[:, :n], M2[:, :n], ALU.subtract, [pks, "SQh"], ["RS"])
                tt(OSB[:, :n], OSB[:, :n], psm[:, :n], ALU.subtract, ["OSB", pkm], ["OSB"])
                act(RS[:, :n], RS[:, :n], AF.Ln, ["RS"], ["RS"], bias=EPSC[:, 0:1])
            else:
                act(RS[:, :n], pss[:, :n], AF.Ln, [pks], ["RS"], bias=EPSC[:, 0:1])
            act(RS[:, :n], RS[:, :n], AF.Exp, ["RS"], ["RS"], scale=-0.5)
            if scale_ap is None:
                tt(OSB[:, :n], OSB[:, :n], RS[:, :n], ALU.mult, ["OSB", "RS"], ["OSB"])
                tt(out_ap, OSB[:, :n], post_ap, ALU.mult, ["OSB"] + postkeys, [outkey])
            else:
                stt(out_ap, OSB[:, :n], scale_ap, RS[:, :n], ALU.mult, ALU.mult, ["OSB", "RS"] + postkeys, [outkey])

        def mixer_ab(l, need_ctx):
            i = l // 2
            wsrc = ab_w_in_d[i]
            with ExitStack() as s2:
                hs = dict(
                    W=sb("Wh", [128, 8, 768], BF16, s2),
                    UB=[sb("UB0", [128, 8, 512], BF16, s2)],
                    TR=[sb(f"TR{j}", [128, 512], F32, s2) for j in range(2)],
                    OSB=sb("OSB", [128, 512], F32, s2), SQh=sb("SQh", [128, 512], F32, s2),
                    RS=sb("RS", [128, 512], F32, s2),
                )
                Qt = sb("Qt", [128, 512], BF16, s2)
                Kt = sb("Kt", [128, 512], BF16, s2)
                V = sb("V", [128, 18, 128], BF16, s2)
                SGN = sb("SGN", [128, NT], BF16, s2)
                QDF = sb("QDF", [128, NT], BF16, s2)
                QDB = sb("QDB", [128, NT], BF16, s2)
                ATT = sb("ATT", [128, NT], BF16, s2)
                SFb = sb("SFb", [128, 18, 128], BF16, s2)
                SBb = sb("SBb", [128, 18, 128], BF16, s2)
                CUR = [sb(f"CUR{j}", [128, 128], F32, s2) for j in range(4)]
                KD = [sb(f"KD{j}", [128, 512], BF16, s2) for j in range(4)]
                KTT = [sb(f"KTT{j}", [128, 4, 128], BF16, s2) for j in range(2)]

                def Qf(b):
                    return Qt[:, :BLOCKS[b][1]], "qt"

                def Kf(b):
                    return Kt[:, :BLOCKS[b][1]], "kt"

                def Qf64(b):
                    return Qt[0:64, :BLOCKS[b][1]], "qt"

                def Kf64(b):
                    return Kt[0:64, :BLOCKS[b][1]], "kt"

                def run_head(h, ph):
                    isA = h < 4
                    dk = 128 if isA else 64
                    hh = h if isA else h - 4
                    if isA:
                        RET = ph["RET"]
                        P.dma("sp", RET[:], ret_d[:, hh, :, :], [], ["RET"])
                    else:
                        LRT, WLRb, LS, LS2, EXS, TAB, TOT, DEC = (ph[k_] for k_ in ("LRT", "WLRb", "LS", "LS2", "EXS", "TAB", "TOT", "DEC"))
                    P.op("dve", lambda: nc.vector.memset(SFb[:, 16, :], 0.0), [], ["SFb"])
                    P.op("dve", lambda: nc.vector.memset(SBb[:, 17, :], 0.0), [], ["SBb"])

                    def pass1(b):
                        t0, n, grp = BLOCKS[b]
                        nch = n // 128
                        c0 = t0 // 128
                        if (not isA) and DBG_B < 1:
                            return
                        qv = Qt[0:dk, :n]
                        kv = Kt[0:dk, :n]
                        if isA:
                            tabs = [RET[:, j, :].unsqueeze(1).broadcast_to([128, nch, 128]) for j in range(6)]
                            tkeys = ["RET"]

                            def rr(ap):
                                return ap.rearrange("p (c d) -> p c d", d=128)
                        else:
                            psg, pkg = nb()
                            for c in range(nch):
                                for di in range(2):
                                    mm(psg[:, c * 128 + di * 64:c * 128 + di * 64 + 64],
                                       LRT[di][:, c * 128:(c + 1) * 128], WLRb[:, di, hh * 64:(hh + 1) * 64],
                                       True, True, [f"lrt{di}", "WLRb"], [pkg])
                            act(EXS[:, :n], psg[:, :n], AF.Exp, [pkg], ["EXS"], scale=-1.0)
                            ex4 = EXS[:, :n].rearrange("p (c a d) -> p c a d", a=2, d=64)
                            act(LS[:, 0:nch, :, :], ex4, AF.Ln, ["EXS"], ["LS"], bias=ONEC[:, 0:1])
                            act(LS2[:, 0:nch, 0, :], ex4[:, :, 1, :], AF.Ln, ["EXS"], ["LS2"], bias=ONEC[:, 0:1])
                            act(LS2[:, 0:nch, 1, :], ex4[:, :, 0, :], AF.Ln, ["EXS", "LS2"], ["LS2"], bias=ONEC[:, 0:1])
                            if DBG_C < "b":
                                return
                            psf, pkf = nb()
                            psb, pkb = nb()
                            psp, pkp = nb()
                            for c in range(nch):
                                mm(psf[:, c * 128:(c + 1) * 128], LS[:, c, :, :].rearrange("p a d -> p (a d)"), TRIF, True, True, ["LS", "CST"], [pkf])
                            for c in range(nch):
                                mm(psb[:, c * 128:(c + 1) * 128], LS2[:, c, :, :].rearrange("p a d -> p (a d)"), TRIB, True, True, ["LS2", "CST2"], [pkb])
                            for c in range(nch):
                                mm(psp[:, c * 128:(c + 1) * 128], LS2[:, c, :, :].rearrange("p a d -> p (a d)"), TRIBP, True, True, ["LS2", "CST2"], [pkp])
                            if DBG_C < "c":
                                return
                            f3 = psf[0:64, :n].rearrange("p (c d) -> p c d", d=128)
                            b3 = psb[0:64, :n].rearrange("p (c d) -> p c d", d=128)
                            cp(TOT[0:64, 0:nch, 0:1], f3[:, :, 127:128], [pkf], ["TOT"])
                            cp(TOT[0:64, 0:nch, 1:2], b3[:, :, 0:1], [pkb, "TOT"], ["TOT"])
                            if DBG_C < "d":
                                return
                            act(TAB[0][0:64, :n], psf[0:64, :n], AF.Exp, [pkf, "TOT"], ["TAB0"])
                            act(TAB[1][0:64, :n], psf[0:64, :n], AF.Exp, [pkf, "TOT"], ["TAB1"], scale=-1.0)
                            act(TAB[3][0:64, :n], psp[0:64, :n], AF.Exp, [pkp, "TOT"], ["TAB3"])
                            act(TAB[4][0:64, :n], psb[0:64, :n], AF.Exp, [pkb, "TOT"], ["TAB4"], scale=-1.0)
                            if DBG_C < "e":
                                return
                            for c in range(nch):
                                act(TAB[2][0:64, c * 128:(c + 1) * 128], psf[0:64, c * 128:(c + 1) * 128], AF.Exp,
                                    [pkf, "TOT"], ["TAB2"], scale=-1.0, bias=TOT[0:64, c, 0:1])
                                act(TAB[5][0:64, c * 128:(c + 1) * 128], psb[0:64, c * 128:(c + 1) * 128], AF.Exp,
                                    [pkb, "TOT"], ["TAB5"], scale=-1.0, bias=TOT[0:64, c, 1:2])
                            act(DEC[0:64, c0:c0 + nch, :], TOT[0:64, 0:nch, :], AF.Exp, ["TOT"], ["DEC"])
                            tabs = [TAB[j][0:64, :n] for j in range(6)]
                            tkeys = [f"TAB{j}" for j in range(6)]

                            def rr(ap):
                                return ap
                        if (not isA) and DBG_B < 2:
                            return
                        tt(rr(QDF[0:dk, t0:t0 + n]), rr(qv), tabs[0], ALU.mult, ["qt"] + tkeys, [f"qdf{b}"])
                        tt(rr(QDB[0:dk, t0:t0 + n]), rr(qv), tabs[3], ALU.mult, ["qt"] + tkeys, [f"qdb{b}"])
                        tt(rr(KD[0][0:dk, :n]), rr(kv), tabs[1], ALU.mult, ["kt"] + tkeys, ["KD0"])
                        tt(rr(KD[1][0:dk, :n]), rr(kv), tabs[4], ALU.mult, ["kt"] + tkeys, ["KD1"])
                        tt(rr(KD[2][0:dk, :n]), rr(kv), tabs[2], ALU.mult, ["kt"] + tkeys, ["KD2"])
                        tt(rr(KD[3][0:dk, :n]), rr(kv), tabs[5], ALU.mult, ["kt"] + tkeys, ["KD3"])
                        psa, pka = nb()
                        psb2, pkb2 = nb()
                        for c in range(nch):
                            sl = slice(c * 128, (c + 1) * 128)
                            gsl = slice(t0 + c * 128, t0 + (c + 1) * 128)
                            mm(psa[:, sl], KD[0][0:dk, sl], QDF[0:dk, gsl], True, True, ["KD0", f"qdf{b}"], [pka])
                            mm(psb2[:, sl], KD[1][0:dk, sl], QDB[0:dk, gsl], True, True, ["KD1", f"qdb{b}"], [pkb2])
                        cp(ATT[:, t0:t0 + n], psa[:, :n], [pka], [f"att{b}"], eng="act")
                        P.op("dve", lambda: nc.vector.copy_predicated(out=ATT[:, t0:t0 + n], mask=MASK[:, :n],
                                                                       data=psb2[:, :n]),
                             [pkb2, "MASK", f"att{b}"], [f"att{b}"])
                        if (not isA) and DBG_B < 3:
                            return
                        for di in range(2):
                            psk, pkk = nb()
                            for c in range(nch):
                                mm(psk[:, c * 128:c * 128 + dk], KD[2 + di][0:dk, c * 128:(c + 1) * 128], IDB[0:dk, 0:dk],
                                   True, True, [f"KD{2 + di}", "CSTB"], [pkk])
                            cp(KTT[di][:, 0:nch, 0:dk], psk[:, :n].rearrange("p (c d) -> p c d", d=128)[:, :, 0:dk],
                               [pkk], [f"KTT{di}"], eng="act")
                            psd, pkd = nb()
                            for c in range(nch):
                                mm(psd[0:dk, c * 128:(c + 1) * 128], KTT[di][:, c, 0:dk], V[:, c0 + c, :], True, True,
                                   [f"KTT{di}", f"vb{b}"], [pkd])
                            d3 = psd[0:dk, :n].rearrange("p (c d) -> p c d", d=128)
                            if di == 0:
                                if grp == 0:
                                    m = nch if c0 + nch < 16 else nch - 1
                                    cp(SFb[0:dk, c0 + 1:c0 + 1 + m, :], d3[:, 0:m, :], [pkd], ["SFb"])
                                else:
                                    cp(SFb[0:dk, 17, :], d3[:, 0, :], [pkd], ["SFb"])
                                    cp(SFb[0:dk, 0, :], d3[:, 1, :], [pkd], ["SFb"])
                            else:
                                if c0 == 0:
                                    cp(SBb[0:dk, 0:nch - 1, :], d3[:, 1:nch, :], [pkd], ["SBb"])
                                else:
                                    cp(SBb[0:dk, c0 - 1:c0 - 1 + nch, :], d3[:, :, :], [pkd], ["SBb"])

                    if isA:
                        cols = dict(q=hh * 128, k=512 + hh * 128, v=1024 + hh * 128, g=1536 + hh * 128)
                        hs["ROPE"] = ph["ROPE"]
                        in_proj_head(l, hs, "A", wsrc, cols, Qf, Kf, V, SGN, GN[:, i, h:h + 1], ropeA_d, dk ** -0.5,
                                     after_block=pass1)
                    else:
                        cols = dict(q=2048 + hh * 64, k=2304 + hh * 64, v=2560 + hh * 128, g=3072 + hh * 128)
                        hs["ROPE"] = None
                        in_proj_head(l, hs, "B", wsrc, cols, Qf64, Kf64, V, SGN, GN[:, i, h:h + 1], None, dk ** -0.5,
                                     LRT=LRT, lr_cols=3584, after_block=pass1)
                    if (not isA) and DBG_B < 4:
                        return
                    for di, (ST, order, skey) in enumerate(((SFb, ORDER_F, "SFb"), (SBb, ORDER_B, "SBb"))):
                        c_a, c_b = CUR[2 * di], CUR[2 * di + 1]
                        ka, kb_ = f"CUR{2 * di}", f"CUR{2 * di + 1}"
                        P.op("dve", lambda c_a=c_a: nc.vector.memset(c_a[:], 0.0), [], [ka])
                        for oi in range(17):
                            nn, nx = order[oi], order[oi + 1]
                            if isA:
                                g_ = 1.0 - 2.0 ** (-((5.0 if di == 0 else 5.5) + hh))
                                sc_ = float(np.float32(g_) ** 128)
                            else:
                                sc_ = DEC[0:64, nn, di:di + 1]
                            stt(c_b[0:dk, :], c_a[0:dk, :], sc_, ST[0:dk, nx, :], ALU.mult, ALU.add,
                                [ka, skey] + ([] if isA else ["DEC"]), [kb_])
                            cp(ST[0:dk, nx, :], c_b[0:dk, :], [kb_], [skey], eng="act")
                            c_a, c_b, ka, kb_ = c_b, c_a, kb_, ka
                    if (not isA) and DBG_B < 5:
                        return
                    for b in range(5 if need_ctx else 4):
                        t0, n, grp = BLOCKS[b]
                        nch = n // 128
                        c0 = t0 // 128
                        pso, pko = nb()
                        for c in range(nch):
                            sl = slice(c * 128, (c + 1) * 128)
                            gsl = slice(t0 + c * 128, t0 + (c + 1) * 128)
                            mm(pso[:, sl], V[:, c0 + c, :], ATT[:, gsl], True, False, [f"vb{b}", f"att{b}"], [pko])
                            mm(pso[:, sl], SFb[0:dk, c0 + c, :], QDF[0:dk, gsl], False, False, ["SFb", f"qdf{b}"], [pko])
                            mm(pso[:, sl], SBb[0:dk, c0 + c, :], QDB[0:dk, gsl], False, True, ["SBb", f"qdb{b}"], [pko])
                        head_norm(pso, pko, n, isA, hs, UY[:, h, t0:t0 + n], f"u{h}b{b}", SGN[:, t0:t0 + n], [f"sgn_b{b}"])

                with ExitStack() as s3:
                    ph = dict(ROPE=sb("ROPE", [128, 4, 512], F32, s3), RET=sb("RET", [128, 6, 128], F32, s3))
                    for h in range(min(4, DBG_HEADS)):
                        run_head(h, ph)
                P.barrier()
                with ExitStack() as s3:
                    WLR = sb("WLR", [32, 2, 256], F32, s3)
                    ph = dict(
                        LRT=[sb(f"LRT{j}", [32, 512], BF16, s3) for j in range(2)],
                        WLRb=sb("WLRb", [32, 2, 256], BF16, s3),
                        LS=sb("LS", [128, 4, 2, 64], F32, s3),
                        LS2=sb("LS2", [128, 4, 2, 64], F32, s3),
                        EXS=sb("EXS", [128, 512], F32, s3),
                        TAB=[sb(f"TAB{j}", [128, 512], BF16, s3) for j in range(6)],
                        TOT=sb("TOT", [128, 4, 2], F32, s3),
                        DEC=sb("DEC", [128, 18, 2], F32, s3),
                    )
                    P.dma("sp", WLR[:], wlr_d[:, i, :, :], [], ["WLR"])
                    cp(ph["WLRb"][:], WLR[:], ["WLR"], ["WLRb"])
                    for di in range(2):
                        P.op("dve", lambda di=di: nc.vector.memset(ph["LRT"][di][:], 1.0), [], [f"lrt{di}"])
                    for h in range(4, min(8, DBG_HEADS)):
                        run_head(h, ph)
            P.barrier()

        def mixer_c(l, need_ctx):
            i = l // 2
            wsrc = c_w_qkv_d[i]
            with ExitStack() as s2:
                hs = dict(
                    W=sb("Wh", [128, 8, 640], BF16, s2),
                    UB=[sb(f"UB{j}", [128, 8, 512], BF16, s2) for j in range(2)],
                    TR=[sb(f"TR{j}", [128, 512], F32, s2) for j in range(2)],
                    ROPE=sb("ROPE", [128, 4, 512], F32, s2),
                    OSB=sb("OSB", [128, 512], F32, s2), SQh=sb("SQh", [128, 512], F32, s2),
                    RS=sb("RS", [128, 512], F32, s2),
                )
                Q = sb("Q", [128, NT], BF16, s2)
                K = sb("K", [128, NT], BF16, s2)
                V = sb("V", [128, 18, 128], BF16, s2)
                E = [sb(f"E{j}", [128, 512], BF16, s2) for j in range(4)]
                R1 = sb("R1", [128, 512], F32, s2)
                R2 = sb("R2", [128, 512], F32, s2)
                A1 = sb("A1", [128, 512], F32, s2)
                for h in range(8):
                    cols = dict(q=h * 128, k=1024 + h * 128, v=2048 + h * 128)
                    in_proj_head(l, hs, "C", wsrc, cols,
                                 lambda b: (Q[:, BLOCKS[b][0]:BLOCKS[b][0] + BLOCKS[b][1]], f"qb{b}"),
                                 lambda b: (K[:, BLOCKS[b][0]:BLOCKS[b][0] + BLOCKS[b][1]], f"kb{b}"),
                                 V, None, None, ropeC_d, 0.125)
                    for b in range(5 if need_ctx else 4):
                        t0, n, grp = BLOCKS[b]
                        kchunks = list(range(18)) if grp == 0 else [16, 17]
                        acc = [(PS[j], f"ps{j}") for j in range(4)]
                        for ci, c in enumerate(kchunks):
                            first, last = ci == 0, ci == len(kchunks) - 1
                            kb_ = c // 4 if c < 16 else 4
                            ksl = slice(c * 128, (c + 1) * 128)
                            for comp in range(2):
                                sj = 4 + 2 * (ci % 2) + comp
                                pss, pks = PS[sj], f"ps{sj}"
                                rsl = slice(comp * 64, (comp + 1) * 64)
                                mm(pss[:, :n], K[rsl, ksl], Q[rsl, t0:t0 + n], True, True, [f"kb{kb_}", f"qb{b}"], [pks])
                                ej = 2 * (ci % 2) + comp
                                act(E[ej][:, :n], pss[:, :n], AF.Exp, [pks], [f"E{ej}"])
                                po, pko = acc[2 * comp]
                                pd, pkd = acc[2 * comp + 1]
                                mm(po[:, :n], V[:, c, :], E[ej][:, :n], first, last, [f"vb{kb_}", f"E{ej}"], [pko])
                                mm(pd[:, :n], ONESB, E[ej][:, :n], first, last, ["CSTB", f"E{ej}"], [pkd])
                        act(R1[:, :n], acc[1][0][:, :n], AF.Ln, [acc[1][1]], ["R1"])
                        act(R1[:, :n], R1[:, :n], AF.Exp, ["R1"], ["R1"], scale=-1.0)
                        act(R2[:, :n], acc[3][0][:, :n], AF.Ln, [acc[3][1]], ["R2"])
                        act(R2[:, :n], R2[:, :n], AF.Exp, ["R2"], ["R2"], scale=-1.0)
                        tt(A1[:, :n], acc[0][0][:, :n], R1[:, :n], ALU.mult, [acc[0][1], "R1"], ["A1"])
                        tt(R2[:, :n], acc[2][0][:, :n], R2[:, :n], ALU.mult, [acc[2][1], "R2"], ["R2"])
                        stt(hs["OSB"][:, :n], R2[:, :n], LAM[:, i, h:h + 1], A1[:, :n], ALU.mult, ALU.add,
                            ["R2", "A1", "LAM"], ["OSB"])
                        psc[0] = 4
                        head_norm(None, None, n, False, hs, UY[:, h, t0:t0 + n], f"u{h}b{b}", None, ["SUBG"],
                                  scale_ap=SUBG[:, i, h:h + 1], src_sb=True)
                        psc[0] = 4
            P.barrier()

        def moe(l, need_ctx):
            nblk = 5 if need_ctx else 4
            with ExitStack() as s2:
                WB = [sb(f"WB{j}", [128, 4096], BF16, s2) for j in range(4)]
                WCT = sb("WCT", [32, NT], BF16, s2)
                SELE = sb("SELE", [32, 32 * 128], BF16, s2)
                BC = [sb(f"BC{j}", [128, NT], BF16, s2) for j in range(2)]
                H = [sb(f"H{j}", [128, 4, 512], BF16, s2) for j in range(2)]
                SG = [sb(f"SG{j}", [128, 512], F32, s2) for j in range(2)]
                TF = [sb(f"TF{j}", [128, 512], F32, s2) for j in range(3)]
                SQ = [sb(f"SQ{j}", [128, 512], F32, s2) for j in range(2)]
                MEAN = sb("MEAN", [128, 512], F32, s2)
                RSTD = sb("RSTD", [128, 512], F32, s2)
                LG = sb("LG", [128, 36], F32, s2)
                SM = sb("SM", [128, 16], F32, s2)
                GOH = sb("GOH", [128, 4], F32, s2)
                GE = sb("GE", [128, 4], F32, s2)
                ET = sb("ET", [128, 32], F32, s2)
                ES = sb("ES", [128, 8], F32, s2)
                T8 = sb("T8", [128, 8], F32, s2)
                SEL = sb("SEL", [128, 8], F32, s2)
                EX = sb("EX", [128, 8], F32, s2)
                WC = sb("WC", [128, 32], F32, s2)
                P.dma("sp", SELE[:], sele_d, [], ["SELE"])
                WR = sb("WRl", [128, 8, 36], F32, s2)
                P.dma("sp", WR[:], wr_d[:, l, :, :], [], ["WR"])
                items = []
                for e in range(32):
                    items.append(("g", e, wg_d[l, e].rearrange("(k p) n -> p k n", p=128)))
                    items.append(("u", e, wu_d[l, e].rearrange("(k p) n -> p k n", p=128)))
                    items.append(("d", e, wd_d[l, e].rearrange("(k p) n -> p k n", p=128)))

                def load_item(j):
                    if j >= len(items):
                        return
                    kind, e, src = items[j]
                    if kind == "d":
                        dst = WB[j % 4][:, :].rearrange("p (k n) -> p k n", n=1024)
                    else:
                        dst = WB[j % 4][:, :].rearrange("p (k n) -> p k n", n=512)
                    P.dma("pool", dst, src, [], [f"WB{j % 4}"])

                for j in range(4):
                    load_item(j)
                ti = 0
                for b in range(nblk):
                    t0, n, grp = BLOCKS[b]
                    ntile = n // 128
                    pr = [nb() for _ in range(ntile)]
                    for k in range(8):
                        tf = TF[ti % 3]
                        tk = f"TF{ti % 3}"
                        ti += 1
                        act(tf[:, :n], XT[:, k, t0:t0 + n], AF.Identity, [f"x{k}b{b}", "MOD"], [tk],
                            scale=mod(l, 4, k, grp), bias=mod(l, 3, k, grp))
                        cp(UY[:, k, t0:t0 + n], tf[:, :n], [tk], [f"u{k}b{b}"])
                        for tI in range(ntile):
                            mm(pr[tI][0][:, 0:36], tf[:, tI * 128:(tI + 1) * 128], WR[:, k, :], k == 0, False,
                               [tk, "WR"], [pr[tI][1]])
                    for tI in range(ntile):
                        ps, pk = pr[tI]
                        mm(ps[:, 0:36], ONES1, RB[0:1, l, :], False, True, ["CST", "RB"], [pk])
                        cp(LG[:], ps[:, 0:36], [pk], ["LG"])
                        R_ = ["LG", "SM", "GOH", "GE", "ET", "ES", "T8", "SEL", "EX", "WC"]

                        def dv(fn):
                            P.op("dve", fn, R_, R_)
                        dv(lambda: nc.vector.tensor_reduce(out=SM[:, 0:1], in_=LG[:, 0:4], axis=AX.X, op=ALU.max))
                        dv(lambda: nc.vector.tensor_scalar(out=GOH[:], in0=LG[:, 0:4], scalar1=SM[:, 0:1], scalar2=None,
                                                           op0=ALU.is_equal))
                        dv(lambda: nc.vector.tensor_scalar(out=SM[:, 1:2], in0=SM[:, 0:1], scalar1=-1.0, scalar2=None,
                                                           op0=ALU.mult))
                        act(GE[:], LG[:, 0:4], AF.Exp, R_, R_, bias=SM[:, 1:2])
                        dv(lambda: nc.vector.tensor_reduce(out=SM[:, 2:3], in_=GE[:], axis=AX.X, op=ALU.add))
                        dv(lambda: nc.vector.reciprocal(out=SM[:, 3:4], in_=SM[:, 2:3]))
                        dv(lambda: nc.vector.tensor_tensor(
                            out=ET[:].rearrange("p (g e) -> p g e", e=8),
                            in0=LG[:, 4:36].rearrange("p (g e) -> p g e", e=8),
                            in1=GOH[:].unsqueeze(2).broadcast_to([128, 4, 8]), op=ALU.mult))
                        dv(lambda: nc.vector.tensor_reduce(out=ES[:], in_=ET[:].rearrange("p (g e) -> p e g", e=8),
                                                           axis=AX.X, op=ALU.add))
                        dv(lambda: nc.vector.max(out=T8[:], in_=ES[:]))
                        dv(lambda: nc.vector.tensor_scalar(out=SEL[:], in0=ES[:], scalar1=T8[:, 1:2], scalar2=None,
                                                           op0=ALU.is_ge))
                        dv(lambda: nc.vector.tensor_scalar(out=SM[:, 4:5], in0=T8[:, 0:1], scalar1=-1.0, scalar2=None,
                                                           op0=ALU.mult))
                        act(EX[:], ES[:], AF.Exp, R_, R_, bias=SM[:, 4:5])
                        dv(lambda: nc.vector.tensor_tensor(out=EX[:], in0=EX[:], in1=SEL[:], op=ALU.mult))
                        dv(lambda: nc.vector.tensor_reduce(out=SM[:, 5:6], in_=EX[:], axis=AX.X, op=ALU.add))
                        dv(lambda: nc.vector.reciprocal(out=SM[:, 6:7], in_=SM[:, 5:6]))
                        dv(lambda: nc.vector.tensor_tensor(out=SM[:, 7:8], in0=SM[:, 6:7], in1=SM[:, 3:4], op=ALU.mult))
                        dv(lambda: nc.vector.tensor_scalar(out=EX[:], in0=EX[:], scalar1=SM[:, 7:8], scalar2=None,
                                                           op0=ALU.mult))
                        dv(lambda: nc.vector.tensor_tensor(
                            out=WC[:].rearrange("p (g e) -> p g e", e=8),
                            in0=GOH[:].unsqueeze(2).broadcast_to([128, 4, 8]),
                            in1=EX[:].unsqueeze(1).broadcast_to([128, 4, 8]), op=ALU.mult))
                        pt, pkt = nb()
                        P.op("pe", lambda: nc.tensor.transpose(out=pt[0:32, 0:128], in_=WC[:], identity=IDF),
                             R_ + ["CST"], [pkt])
                        cp(WCT[:, t0 + tI * 128:t0 + (tI + 1) * 128], pt[0:32, 0:128], [pkt], [f"wct{b}"], eng="act")
                for b in range(nblk):
                    t0, n, grp = BLOCKS[b]
                    for k in range(8):
                        xk = XT[:, k, t0:t0 + n]
                        if k % 2 == 0:
                            act(xk, xk, AF.Copy, [f"x{k}b{b}"], [f"x{k}b{b}"], scale=ALPHA)
                        else:
                            ts(xk, xk, ALPHA, ALU.mult, [f"x{k}b{b}"], [f"x{k}b{b}"])
                hi = 0
                for e in range(32):
                    j0 = 3 * e
                    WG = WB[j0 % 4][:, :].rearrange("p (k n) -> p k n", n=512)
                    WU = WB[(j0 + 1) % 4][:, :].rearrange("p (k n) -> p k n", n=512)
                    WD = WB[(j0 + 2) % 4][:, :].rearrange("p (k n) -> p k n", n=1024)
                    kg, ku, kd = f"WB{j0 % 4}", f"WB{(j0 + 1) % 4}", f"WB{(j0 + 2) % 4}"
                    bc = BC[e % 2]
                    bck = f"BC{e % 2}"
                    for b in range(nblk):
                        t0, n, grp = BLOCKS[b]
                        ps, pk = nb()
                        mm(ps[:, :n], SELE[:, e * 128:(e + 1) * 128], WCT[:, t0:t0 + n], True, True, ["SELE", f"wct{b}"], [pk])
                        cp(bc[:, t0:t0 + n], ps[:, :n], [pk], [bck + f"b{b}"], eng="act")
                    for b in range(nblk):
                        t0, n, grp = BLOCKS[b]
                        Hb = H[hi % 2]
                        hk = f"H{hi % 2}"
                        hi += 1
                        for fc in range(4):
                            pg, pkg = nb()
                            pu, pku = nb()
                            for k in range(8):
                                mm(pg[:, :n], WG[:, k, fc * 128:(fc + 1) * 128], UY[:, k, t0:t0 + n], k == 0, k == 7,
                                   [kg, f"u{k}b{b}"], [pkg])
                            for k in range(8):
                                mm(pu[:, :n], WU[:, k, fc * 128:(fc + 1) * 128], UY[:, k, t0:t0 + n], k == 0, k == 7,
                                   [ku, f"u{k}b{b}"], [pku])
                            sg = SG[fc % 2]
                            act(sg[:, :n], pg[:, :n], AF.Silu, [pkg], [f"SG{fc % 2}"])
                            tt(sg[:, :n], sg[:, :n], pu[:, :n], ALU.mult, [f"SG{fc % 2}", pku], [f"SG{fc % 2}"])
                            tt(Hb[:, fc, :n], sg[:, :n], bc[:, t0:t0 + n], ALU.mult, [f"SG{fc % 2}", bck + f"b{b}"],
                               [hk + f"f{fc}"])
                        if b == nblk - 1:
                            load_item(j0 + 4)
                            load_item(j0 + 5)
                        for oc in range(8):
                            py, pky = nb()
                            for fc in range(4):
                                mm(py[:, :n], WD[:, fc, oc * 128:(oc + 1) * 128], Hb[:, fc, :n], fc == 0, fc == 3,
                                   [kd, hk + f"f{fc}"], [pky])
                            xk = XT[:, oc, t0:t0 + n]
                            stt(xk, py[:, :n], mod(l, 5, oc, grp), xk, ALU.mult, ALU.add, [pky, "MOD", f"x{oc}b{b}"],
                                [f"x{oc}b{b}"])
                    load_item(j0 + 6)
                for b in range(nblk):
                    ln_block(l, 1, b, (SQ, MEAN, RSTD))
            P.barrier()

        for l in range(n_layers):
            if stop == "ada":
                break
            lastl = l == DEPTH - 1
            need_ctx = not lastl
            is_dbg_last = (l == n_layers - 1)
            if l % 2 == 0:
                mixer_ab(l, need_ctx)
                wo = ab_w_out_d[l // 2]
            else:
                mixer_c(l, need_ctx)
                wo = c_w_out_d[l // 2]
            raw = is_dbg_last and stop == "mix"
            if is_dbg_last and stop == "y":
                for b in range(5):
                    t0, n, grp = BLOCKS[b]
                    for k in range(8):
                        cp(XT[:, k, t0:t0 + n], UY[:, k, t0:t0 + n], [f"u{k}b{b}"], [f"x{k}b{b}"])
                break
            proj_ln(l, wo, 5 if need_ctx else 4, raw)
            if is_dbg_last and stop in ("mix", "ln1"):
                break
            moe(l, need_ctx)

        for k in range(8):
            P.dma("sp", out_d[k], XT[:, k, :], xkeys(ks=[k]), [f"out{k}"])
        P.finish("sp", [f"out{k}" for k in range(8)])
        print("bass ops:", P.nops, P.cnt)
    return nc


def _fm(v):
    v = np.asarray(v, np.float32)
    lead = v.shape[:-1]
    r = v.reshape(lead + (8, 128))
    r = np.moveaxis(r, -1, 0)
    return np.ascontiguousarray(r)


def _rope_tables(head_dim, per, qscale):
    quarter = head_dim // 4
    row = np.repeat(np.arange(T // 64, dtype=np.float32), 64)
    col = np.tile(np.arange(64, dtype=np.float32), T // 64)
    inv = (np.float32(10000.0) ** (-np.arange(quarter, dtype=np.float32) / np.float32(quarter))).astype(np.float32)
    ang_r = row[:, None] * inv
    ang_c = col[:, None] * inv
    ang = np.concatenate([ang_r, ang_r, ang_c, ang_c], axis=-1)
    cos = np.cos(ang).astype(np.float32).T
    sin = np.sin(ang).astype(np.float32).T
    sign = np.ones((head_dim, 1), np.float32)
    sign[0:quarter] = -1.0
    sign[2 * quarter:3 * quarter] = -1.0
    sin = sin * sign
    rep = 128 // head_dim
    cos = np.tile(cos, (rep, 1))
    sin = np.tile(sin, (rep, 1))
    return np.ascontiguousarray(np.stack([cos * qscale, sin * qscale, cos, sin]).astype(np.float32))


_CONSTS = None


def _consts():
    global _CONSTS
    if _CONSTS is not None:
        return _CONSTS
    c = {}
    c["ropeA"] = _rope_tables(128, 128, np.float32(128 ** -0.5))
    c["ropeC"] = _rope_tables(64, 64, np.float32(0.125))
    ii = np.arange(128)
    cst = np.zeros((128, 1024), np.float32)
    cst[:, 0:128] = np.eye(128, dtype=np.float32)
    cst[:, 128:256] = 1.0 / 1024.0
    cst[:, 256:384] = 1.0 / 128.0
    cst[:, 384:512] = np.where(ii[:, None] <= ii[None, :], -1.0 / 16.0, 0.0)
    cst[:, 512:640] = 1.0
    c["cst"] = cst
    cst2 = np.zeros((128, 256), np.float32)
    cst2[:, 0:128] = np.where(ii[:, None] >= ii[None, :], -1.0 / 16.0, 0.0)
    cst2[:, 128:256] = np.where(ii[:, None] > ii[None, :], -1.0 / 16.0, 0.0)
    c["cst2"] = cst2
    cstb = np.zeros((128, 256), np.float32)
    cstb[:, 0:128] = np.eye(128)
    cstb[:, 128:256] = 1.0
    c["cstb"] = cstb.astype(ml_dtypes.bfloat16)
    m = (ii[:, None] > ii[None, :]).astype(np.uint8)
    c["mask"] = np.ascontiguousarray(np.tile(m, (1, 4)))
    sele = np.zeros((32, 32, 128), np.float32)
    for e in range(32):
        sele[e, e, :] = 1.0
    c["sele"] = sele.reshape(32, 32 * 128).astype(ml_dtypes.bfloat16)
    ret = np.zeros((128, 4, 6, 128), np.float32)
    pos = np.arange(128, dtype=np.float64)
    for h in range(4):
        lf = float(np.log1p(-np.exp2(-np.float32(5.0 + h)), dtype=np.float32))
        lb = float(np.log1p(-np.exp2(-np.float32(5.5 + h)), dtype=np.float32))
        ret[:, h, 0, :] = np.exp(lf * (pos + 1))
        ret[:, h, 1, :] = np.exp(-lf * (pos + 1))
        ret[:, h, 2, :] = np.exp(lf * (127 - pos))
        ret[:, h, 3, :] = np.exp(lb * (127 - pos))
        ret[:, h, 4, :] = np.exp(-lb * (128 - pos))
        ret[:, h, 5, :] = np.exp(lb * pos)
    c["ret"] = ret
    _CONSTS = c
    return c


def _prep_inputs(inputs):
    f = lambda a: np.ascontiguousarray(np.asarray(a, np.float32))
    shared = dict(_consts())
    shared["adab"] = np.ascontiguousarray(np.asarray(inputs["ada_b"], np.float32).reshape(4, 48, 128).transpose(2, 0, 1))
    lnp = np.stack([inputs["ln1_g"], inputs["ln1_b"], inputs["ln2_g"], inputs["ln2_b"]], axis=1)
    shared["lnp"] = np.ascontiguousarray(np.asarray(lnp, np.float32).reshape(4, 4, 8, 128).transpose(3, 0, 1, 2))
    gn = np.concatenate([inputs["ab_gn_a"], inputs["ab_gn_b"]], axis=1)
    shared["gn"] = np.ascontiguousarray(np.asarray(gn, np.float32).reshape(2, 8, 128).transpose(2, 0, 1))
    shared["subg"] = np.ascontiguousarray(np.asarray(inputs["c_subln_g"], np.float32).reshape(2, 8, 128).transpose(2, 0, 1))
    wlr = np.zeros((32, 2, 2, 256), np.float32)
    wlr[0:16, :, 0, :] = np.asarray(inputs["ab_w_lr_f"]).transpose(1, 0, 2)
    wlr[0:16, :, 1, :] = np.asarray(inputs["ab_w_lr_b"]).transpose(1, 0, 2)
    wlr[16, :, 0, :] = np.asarray(inputs["ab_b_lr_f"])
    wlr[16, :, 1, :] = np.asarray(inputs["ab_b_lr_b"])
    shared["wlr"] = wlr
    wr = np.concatenate([inputs["moe_w_grp"], inputs["moe_w_rexp"]], axis=2)
    shared["wr"] = np.ascontiguousarray(np.asarray(wr, np.float32).reshape(4, 8, 128, 36).transpose(2, 0, 1, 3))
    rb = np.concatenate([inputs["moe_b_grp"], inputs["moe_b_rexp"]], axis=1)
    shared["rb"] = np.ascontiguousarray(np.asarray(rb, np.float32)[None])
    lqk = np.stack([inputs["c_lq1"], inputs["c_lk1"], inputs["c_lq2"], inputs["c_lk2"]], axis=1)
    shared["lqk"] = np.ascontiguousarray(np.asarray(lqk, np.float32).transpose(3, 0, 1, 2))
    for nm in ("ada_w", "ab_w_in", "ab_w_out", "c_w_qkv", "c_w_out", "moe_w_gate", "moe_w_up", "moe_w_down"):
        shared[nm] = f(inputs[nm])
    x = np.asarray(inputs["x"], np.float32)
    ctx = np.asarray(inputs["ctx"], np.float32)
    c = np.asarray(inputs["c"], np.float32)
    c_ctx = np.asarray(inputs["c_ctx"], np.float32)
    in_maps = []
    for b in range(8):
        tok = np.concatenate([x[b], ctx[b]], axis=0)
        xT = np.ascontiguousarray(tok.T.reshape(8, 128, NT))
        cc = np.stack([c[b].reshape(8, 128).T, c_ctx.reshape(8, 128).T], axis=-1)
        m = dict(shared)
        m["xT"] = xT
        m["cc"] = np.ascontiguousarray(cc.astype(np.float32))
        in_maps.append(m)
    return in_maps


_NC_CACHE = {}


def run(inputs, n_layers=DEPTH, stop=None, ncores=8):
    key = (n_layers, stop)
    if key not in _NC_CACHE:
        _NC_CACHE[key] = build(n_layers, stop)
    nc = _NC_CACHE[key]
    in_maps = _prep_inputs(inputs)[:ncores]
    if stop in ("ada", "mix", "ln1", "y") and n_layers <= 1:
        for m in in_maps:
            for nm in ("moe_w_gate", "moe_w_up", "moe_w_down"):
                m.pop(nm)
    res = run_bass_kernel_spmd(nc, in_maps, core_ids=list(range(ncores)))
    outs = [np.asarray(r["outT"]).reshape(D, NT).T for r in res.results]
    return outs


def kernel(**inputs):
    outs = run(inputs)
    return np.ascontiguousarray(np.stack([o[:T] for o in outs], axis=0).astype(np.float32))
```

```python
import math
import numpy as np
import ml_dtypes
from contextlib import ExitStack
import concourse.bass as bass
import concourse.mybir as mybir
from concourse.bass_utils import run_bass_kernel_spmd

F32 = mybir.dt.float32
BF16 = mybir.dt.bfloat16
U8 = mybir.dt.uint8
U32 = mybir.dt.uint32
I32 = mybir.dt.int32
AF = mybir.ActivationFunctionType
ALU = mybir.AluOpType
AX = mybir.AxisListType

D = 1024
T = 2048
TC = 256
NT = T + TC
DEPTH = 4
ALPHA = (2 * DEPTH) ** 0.25
EPS = 1e-5
BLOCKS = [(0, 512, 0), (512, 512, 0), (1024, 512, 0), (1536, 512, 0), (2048, 256, 1)]
ORDER_F = [16, 17] + list(range(16))
ORDER_B = [17, 16] + list(range(15, -1, -1))
AB_IN = 3616
import os as _os
SPARSE = int(_os.environ.get("MOE_SPARSE", "1"))
DBG_HEADS = int(_os.environ.get("DBG_HEADS", "8"))
DBG_B = int(_os.environ.get("DBG_B", "9"))
DBG_C = _os.environ.get("DBG_C", "z")


class Prog:
    def __init__(self, nc, es, ndma=36):
        self.nc = nc
        self.e = dict(pe=nc.tensor, act=nc.scalar, dve=nc.vector, pool=nc.gpsimd, sp=nc.sync)
        self.sem = {k: es.enter_context(nc.semaphore("s_" + k)) for k in self.e}
        self.cnt = {k: 0 for k in self.e}
        self.seen = {k: {} for k in self.e}
        self.st = {}
        self.dq = {}
        for q in ("sp", "pool"):
            self.dq[q] = dict(sems=[es.enter_context(nc.semaphore(f"d_{q}{i}")) for i in range(ndma)],
                              vals=[0] * ndma, idx=0)
        self.nops = 0
        self.bar = es.enter_context(nc.sbuf_tensor("barrier_t", [128, 1], F32))

    def _wait(self, eng, ticks):
        need = {}
        for t in ticks:
            if t is None:
                continue
            key, sem, val = t
            if key == "pe" and eng == "pe":
                continue
            if need.get(key, (None, 0))[1] < val:
                need[key] = (sem, val)
        for key, (sem, val) in need.items():
            if self.seen[eng].get(key, 0) >= val:
                continue
            self.e[eng].wait_ge(sem, val)
            self.seen[eng][key] = val

    def _deps(self, R, W):
        ticks = []
        for k in R:
            s = self.st.get(k)
            if s is not None:
                ticks.append(s[0])
                if k.startswith("ps"):
                    ticks.extend(s[1].values())
        for k in W:
            s = self.st.get(k)
            if s is not None:
                ticks.append(s[0])
                ticks.extend(s[1].values())
        return ticks

    def _update(self, tick, R, W):
        for k in W:
            self.st[k] = [tick, {}]
        for k in R:
            s = self.st.get(k)
            if s is None:
                s = self.st[k] = [None, {}]
            s[1][tick[0]] = tick

    def op(self, eng, fn, R, W):
        self._wait(eng, self._deps(R, W))
        inst = fn()
        inst.then_inc(self.sem[eng], 1)
        self.cnt[eng] += 1
        self.nops += 1
        self._update((eng, self.sem[eng], self.cnt[eng]), R, W)

    def dma(self, q, out, in_, R, W):
        d = self.dq[q]
        i = d["idx"]
        d["idx"] = (i + 1) % len(d["sems"])
        key = ("d", q, i)
        ticks = self._deps(R, W)
        if d["vals"][i] > 0:
            ticks.append((key, d["sems"][i], d["vals"][i]))
        self._wait(q, ticks)
        self.e[q].dma_start(out=out, in_=in_).then_inc(d["sems"][i], 16)
        d["vals"][i] += 16
        self.nops += 1
        self._update((key, d["sems"][i], d["vals"][i]), R, W)

    def dma_fn(self, q, fn, R, W):
        d = self.dq[q]
        i = d["idx"]
        d["idx"] = (i + 1) % len(d["sems"])
        key = ("d", q, i)
        ticks = self._deps(R, W)
        if d["vals"][i] > 0:
            ticks.append((key, d["sems"][i], d["vals"][i]))
        self._wait(q, ticks)
        fn(d["sems"][i])
        d["vals"][i] += 16
        self.nops += 1
        self._update((key, d["sems"][i], d["vals"][i]), R, W)

    def barrier(self):
        ticks = []
        for e in self.e:
            if self.cnt[e] > 0:
                ticks.append((e, self.sem[e], self.cnt[e]))
        for q, d in self.dq.items():
            for i, v in enumerate(d["vals"]):
                if v > 0:
                    ticks.append((("d", q, i), d["sems"][i], v))
        self._wait("dve", ticks)
        inst = self.nc.vector.memset(self.bar[:], 0.0)
        inst.then_inc(self.sem["dve"], 1)
        self.cnt["dve"] += 1
        t = ("dve", self.sem["dve"], self.cnt["dve"])
        for e in ("pe", "act", "pool", "sp"):
            self._wait(e, [t])

    def finish(self, eng, keys):
        self._wait(eng, self._deps(keys, []))


def build(n_layers=DEPTH, stop=None):
    nc = bass.Bass("TRN2", target_bir_lowering=False)

    def din(name, shape, dt=F32):
        return nc.dram_tensor(name, list(shape), dt, kind="ExternalInput").ap()

    xT_d = din("xT", [8, 128, NT])
    cc_d = din("cc", [128, 8, 2])
    adab_d = din("adab", [128, 4, 48])
    lnp_d = din("lnp", [128, 4, 4, 8])
    gn_d = din("gn", [128, 2, 8])
    subg_d = din("subg", [128, 2, 8])
    wlr_d = din("wlr", [32, 2, 2, 256])
    wr_d = din("wr", [128, 4, 8, 36])
    rb_d = din("rb", [1, 4, 36])
    lqk_d = din("lqk", [64, 2, 4, 8])
    ret_d = din("ret", [128, 4, 6, 128])
    ropeA_d = din("ropeA", [4, 128, T])
    ropeC_d = din("ropeC", [4, 128, T])
    cst_d = din("cst", [128, 1024])
    cst2_d = din("cst2", [128, 256])
    cstb_d = din("cstb", [128, 256], BF16)
    mask_d = din("mask", [128, 512], U8)
    cst3_d = din("cst3", [128, 256])
    cstb2_d = din("cstb2", [128, 128], BF16)
    xs_d = nc.dram_tensor("xs_scr", [9216, 1024], BF16, kind="Internal").ap()
    ys_d = nc.dram_tensor("ys_scr", [9216, 1024], F32, kind="Internal").ap()
    sele_d = din("sele", [32, 32 * 128], BF16)
    ada_w_d = din("ada_w", [4, D, 6 * D])
    ab_w_in_d = din("ab_w_in", [2, D, AB_IN])
    ab_w_out_d = din("ab_w_out", [2, D, D])
    c_w_qkv_d = din("c_w_qkv", [2, D, 3 * D])
    c_w_out_d = din("c_w_out", [2, D, D])
    use_moe = not (stop in ("ada", "mix", "ln1", "y") and n_layers <= 1)
    if use_moe and SPARSE:
        wc_d = [din(f"wc{c}", [4 * 32 * 128, 2048]) for c in range(6)]
    elif use_moe:
        wg_d = din("moe_w_gate", [4, 32, D, 512])
        wu_d = din("moe_w_up", [4, 32, D, 512])
        wd_d = din("moe_w_down", [4, 32, 512, D])
    out_d = nc.dram_tensor("outT", [8, 128, NT], F32, kind="ExternalOutput").ap()

    es = ExitStack()
    with es:
        P = Prog(nc, es)

        uid = [0]

        def sb(name, shape, dt=F32, stack=es):
            uid[0] += 1
            return stack.enter_context(nc.sbuf_tensor(f"{name}_{uid[0]}", list(shape), dt))

        PS = [es.enter_context(nc.psum_tensor(f"ps{i}", [128, 512], F32)) for i in range(8)]
        psc = [0]

        def nb():
            i = psc[0]
            psc[0] = (i + 1) % 8
            return PS[i], f"ps{i}"

        def mm(out, lhsT, rhs, start, stop, R, W):
            P.op("pe", lambda: nc.tensor.matmul(out, lhsT=lhsT, rhs=rhs, start=start, stop=stop), R, W)

        def act(out, in_, func, R, W, scale=None, bias=None, accum_out=None):
            kw = {}
            if scale is not None:
                kw["scale"] = scale
            if bias is None and func != AF.Copy:
                sp_, np_ = in_.start_partition(), in_.partition_size()
                bias = ZEROC[sp_:sp_ + np_, 0:1]
                R = list(R) + ["ZEROC"]
            if bias is not None:
                kw["bias"] = bias
            if accum_out is not None:
                kw["accum_out"] = accum_out
            P.op("act", lambda: nc.scalar.activation(out=out, in_=in_, func=func, **kw), R, W)

        def tt(out, a, b, op, R, W, eng="dve"):
            e = nc.vector if eng == "dve" else nc.gpsimd
            P.op(eng, lambda: e.tensor_tensor(out=out, in0=a, in1=b, op=op), R, W)

        def ts(out, a, s1, op0, R, W, s2=None, op1=None, eng="dve"):
            e = nc.vector if eng == "dve" else nc.gpsimd
            if op1 is None:
                P.op(eng, lambda: e.tensor_scalar(out=out, in0=a, scalar1=s1, scalar2=None, op0=op0), R, W)
            else:
                P.op(eng, lambda: e.tensor_scalar(out=out, in0=a, scalar1=s1, scalar2=s2, op0=op0, op1=op1), R, W)

        def stt(out, a, s, b, op0, op1, R, W):
            P.op("dve", lambda: nc.vector.scalar_tensor_tensor(out=out, in0=a, scalar=s, in1=b, op0=op0, op1=op1), R, W)

        def cp(out, in_, R, W, eng="dve"):
            if eng == "act":
                act(out, in_, AF.Copy, R, W)
            else:
                e = nc.vector if eng == "dve" else nc.gpsimd
                P.op(eng, lambda: e.tensor_copy(out=out, in_=in_), R, W)

        XT = sb("XT", [128, 8, NT])
        UY = sb("UY", [128, 8, NT], BF16)
        MOD = sb("MOD", [128, 4, 6, 8, 2])
        LNP = sb("LNP", [128, 4, 4, 8])
        GN = sb("GN", [128, 2, 8])
        SUBG = sb("SUBG", [128, 2, 8])
        RB = sb("RB", [1, 4, 36])
        LAM = sb("LAM", [128, 2, 8])
        CST = sb("CST", [128, 1024])
        CST2 = sb("CST2", [128, 256])
        CSTB = sb("CSTB", [128, 256], BF16)
        MASK = sb("MASK", [128, 512], U8)
        CST3 = sb("CST3", [128, 256])
        CSTB2 = sb("CSTB2", [128, 128], BF16)
        TRIS = CSTB2[:, 0:128]
        EPSC = sb("EPSC", [128, 1])
        ONEC = sb("ONEC", [128, 1])
        ZEROC = sb("ZEROC", [128, 1])
        IDF = CST[:, 0:128]
        ONESD = CST[:, 128:256]
        ONESH = CST[:, 256:384]
        TRIF = CST[:, 384:512]
        ONES1 = CST[0:1, 512:640]
        TRIB = CST2[:, 0:128]
        TRIBP = CST2[:, 128:256]
        IDB = CSTB[:, 0:128]
        ONESB = CSTB[:, 128:256]

        def xkeys(blocks=range(5), ks=range(8)):
            return [f"x{k}b{b}" for k in ks for b in blocks]

        def ukeys(blocks=range(5), ks=range(8)):
            return [f"u{k}b{b}" for k in ks for b in blocks]

        for k in range(8):
            P.dma("sp", XT[:, k, :], xT_d[k], [], xkeys(ks=[k]))
        for (t_, d_, kname) in [(LNP, lnp_d, "LNP"), (GN, gn_d, "GN"), (SUBG, subg_d, "SUBG"),
                                (RB, rb_d, "RB"), (CST, cst_d, "CST"), (CST2, cst2_d, "CST2"), (CSTB, cstb_d, "CSTB"),
                                (MASK, mask_d, "MASK"), (CST3, cst3_d, "CST3"), (CSTB2, cstb2_d, "CSTB2")]:
            P.dma("sp", t_[:], d_, [], [kname])
        P.op("dve", lambda: nc.vector.memset(EPSC[:], EPS), [], ["EPSC"])
        P.op("dve", lambda: nc.vector.memset(ONEC[:], 1.0), [], ["ONEC"])
        P.op("dve", lambda: nc.vector.memset(ZEROC[:], 0.0), [], ["ZEROC"])

        with ExitStack() as s1:
            CC = sb("CC", [128, 8, 2], F32, s1)
            SCB = sb("SCB", [128, 8, 2], BF16, s1)
            ADAB = sb("ADAB", [128, 4, 48], F32, s1)
            WA = [sb(f"WA{i}", [128, 8, 1024], BF16, s1) for i in range(2)]
            LQK = sb("LQK", [64, 2, 4, 8], F32, s1)
            LQP = sb("LQP", [64, 2, 2, 8], F32, s1)
            P.dma("sp", CC[:], cc_d, [], ["CC"])
            P.dma("sp", ADAB[:], adab_d, [], ["ADAB"])
            P.dma("sp", LQK[:], lqk_d, [], ["LQK"])
            act(SCB[:], CC[:], AF.Silu, ["CC"], ["SCB"])
            it = 0
            for l in range(n_layers):
                for j6 in range(6):
                    w = WA[it % 2]
                    wk = f"WA{it % 2}"
                    it += 1
                    src = ada_w_d[l].rearrange("(k p) n -> p k n", p=128)[:, :, j6 * 1024:(j6 + 1) * 1024]
                    P.dma("pool", w[:], src, [], [wk])
                    ps, pk = nb()
                    for jj in range(8):
                        for k in range(8):
                            mm(ps[:, jj * 2:jj * 2 + 2], w[:, k, jj * 128:(jj + 1) * 128], SCB[:, k, :],
                               k == 0, k == 7, [wk, "SCB"], [pk])
                    tt(MOD[:, l, j6, :, :], ps[:, 0:16].rearrange("p (j g) -> p j g", g=2),
                       ADAB[:, l, j6 * 8:(j6 + 1) * 8].unsqueeze(2).broadcast_to([128, 8, 2]), ALU.add,
                       [pk, "ADAB"], ["MOD"])
                for j6 in (1, 4):
                    ts(MOD[:, l, j6, :, :], MOD[:, l, j6, :, :], 1.0, ALU.add, ["MOD"], ["MOD"])
            tt(LQP[:, :, 0, :], LQK[:, :, 0, :], LQK[:, :, 1, :], ALU.mult, ["LQK"], ["LQP"])
            tt(LQP[:, :, 1, :], LQK[:, :, 2, :], LQK[:, :, 3, :], ALU.mult, ["LQP", "LQK"], ["LQP"])
            ps, pk = nb()
            mm(ps[:, 0:32], CST[0:64, 512:640], LQP[:].rearrange("p a b c -> p (a b c)"), True, True,
               ["CST", "LQP"], [pk])
            LE = sb("LE", [128, 32], F32, s1)
            act(LE[:], ps[:, 0:32], AF.Exp, [pk], ["LE"])
            lev = LE[:].rearrange("p (a b c) -> p a b c", a=2, b=2)
            for i in range(2):
                lam_init = 0.8 - 0.6 * math.exp(-0.3 * (2 * i + 1))
                tt(LAM[:, i, :], lev[:, i, 1, :], lev[:, i, 0, :], ALU.subtract, ["LE", "LAM"], ["LAM"])
                ts(LAM[:, i, :], LAM[:, i, :], -lam_init, ALU.add, ["LAM"], ["LAM"])
                ts(SUBG[:, i, :], SUBG[:, i, :], 1.0 - lam_init, ALU.mult, ["SUBG"], ["SUBG"])

        P.barrier()

        def mod(l, which, k, grp):
            return MOD[:, l, which, k, grp:grp + 1]

        def ln_block(l, which_ln, b, scr):
            t0, n, grp = BLOCKS[b]
            SQ, MEAN, RSTD = scr
            psm, pkm = nb()
            pss, pks = nb()
            for k in range(8):
                xk = XT[:, k, t0:t0 + n]
                mm(psm[:, :n], ONESD, xk, k == 0, k == 7, [f"x{k}b{b}", "CST"], [pkm])
                sq = SQ[k % 2]
                act(sq[:, :n], xk, AF.Square, [f"x{k}b{b}"], [f"SQ{k % 2}"])
                mm(pss[:, :n], ONESD, sq[:, :n], k == 0, k == 7, [f"SQ{k % 2}", "CST"], [pks])
            cp(MEAN[:, :n], psm[:, :n], [pkm], ["MEAN"], eng="act")
            act(RSTD[:, :n], psm[:, :n], AF.Square, [pkm], ["RSTD"])
            tt(RSTD[:, :n], pss[:, :n], RSTD[:, :n], ALU.subtract, [pks, "RSTD"], ["RSTD"])
            act(RSTD[:, :n], RSTD[:, :n], AF.Ln, ["RSTD"], ["RSTD"], bias=EPSC[:, 0:1])
            act(RSTD[:, :n], RSTD[:, :n], AF.Exp, ["RSTD"], ["RSTD"], scale=-0.5)
            for k in range(8):
                xk = XT[:, k, t0:t0 + n]
                kk = [f"x{k}b{b}"]
                tt(xk, xk, MEAN[:, :n], ALU.subtract, kk + ["MEAN"], kk)
                tt(xk, xk, RSTD[:, :n], ALU.mult, kk + ["RSTD"], kk)
                act(xk, xk, AF.Identity, kk + ["LNP"], kk, scale=LNP[:, l, 2 * which_ln, k:k + 1],
                    bias=LNP[:, l, 2 * which_ln + 1, k:k + 1])

        def proj_ln(l, w_dram, nblocks, raw):
            with ExitStack() as s2:
                WO = sb("WO", [128, 8, 1024], BF16, s2)
                TMP = [sb(f"TMPO{i}", [128, 512], F32, s2) for i in range(2)]
                SQ = [sb(f"SQ{i}", [128, 512], F32, s2) for i in range(2)]
                MEAN = sb("MEAN", [128, 512], F32, s2)
                RSTD = sb("RSTD", [128, 512], F32, s2)
                P.dma("pool", WO[:], w_dram.rearrange("(k p) n -> p k n", p=128), [], ["WO"])
                for b in range(nblocks):
                    t0, n, grp = BLOCKS[b]
                    for c in range(8):
                        ps, pk = nb()
                        for h in range(8):
                            mm(ps[:, :n], WO[:, h, c * 128:(c + 1) * 128], UY[:, h, t0:t0 + n], h == 0, h == 7,
                               ["WO", f"u{h}b{b}"], [pk])
                        xk = XT[:, c, t0:t0 + n]
                        kk = [f"x{c}b{b}"]
                        if raw:
                            cp(xk, ps[:, :n], [pk], kk, eng="act")
                        else:
                            tmp = TMP[c % 2]
                            act(tmp[:, :n], ps[:, :n], AF.Identity, [pk, "MOD"], [f"TMPO{c % 2}"], scale=mod(l, 2, c, grp))
                            stt(xk, xk, ALPHA, tmp[:, :n], ALU.mult, ALU.add, kk + [f"TMPO{c % 2}"], kk)
                    if not raw:
                        ln_block(l, 0, b, (SQ, MEAN, RSTD))
            P.barrier()

        def make_u(l, b, UB, ub_i, which_s, which_sh):
            t0, n, grp = BLOCKS[b]
            for k in range(8):
                src = XT[:, k, t0:t0 + n]
                dst = UB[ub_i][:, k, :n]
                R = [f"x{k}b{b}", "MOD"]
                W = [f"UB{ub_i}k{k}"]
                if k % 2 == 0:
                    act(dst, src, AF.Identity, R, W, scale=mod(l, which_s, k, grp), bias=mod(l, which_sh, k, grp))
                else:
                    ts(dst, src, mod(l, which_s, k, grp), ALU.mult, R, W, s2=mod(l, which_sh, k, grp), op1=ALU.add)

        def fm_group(UBt, ub_i, n, Wt, wkey, c0, M):
            ps, pk = nb()
            for k in range(8):
                mm(ps[0:M, :n], Wt[:, k, c0:c0 + M], UBt[:, k, :n], k == 0, k == 7, [wkey, f"UB{ub_i}k{k}"], [pk])
            return ps, pk

        def rope_evac(dst, dkey, ps_a, pk_a, ps_b, pk_b, cos_t, sin_t, tkey, n, TR, ti):
            t1 = TR[0]
            t2 = TR[1]
            tt(t1[:, :n], ps_a[:, :n], cos_t, ALU.mult, [pk_a, tkey], ["TR0"])
            tt(t2[:, :n], ps_b[:, :n], sin_t, ALU.mult, [pk_b, tkey], ["TR1"])
            tt(dst, t1[:, :n], t2[:, :n], ALU.add, ["TR0", "TR1"], [dkey])

        def in_proj_head(l, hs, kind, wsrc, cols, Qf, Kf, V, SGN, gn_ap, rope_d, qscale, LRT=None, lr_cols=None,
                         after_block=None, nblocks=5):
            dk = 64 if kind == "B" else 128
            has_rope = kind in ("A", "C")
            has_g = kind in ("A", "B")
            wv = wsrc.rearrange("(k p) n -> p k n", p=128)
            W = hs["W"]
            ofs = {}
            o = 0
            names = ["q", "k", "v"] + (["g"] if has_g else [])
            for nm in names:
                w_ = dk if nm in ("q", "k") else 128
                P.dma("pool", W[:, :, o:o + w_], wv[:, :, cols[nm]:cols[nm] + w_], [], [f"W_{nm}"])
                ofs[nm] = o
                o += w_
            if LRT is not None:
                P.dma("pool", W[:, :, o:o + 32], wv[:, :, lr_cols:lr_cols + 32], [], ["W_lr"])
                ofs["lr"] = o
                o += 32
            if has_rope:
                blk = 32 if kind == "A" else 16
                for nm in ("q", "k"):
                    s_ = W[:, :, ofs[nm]:ofs[nm] + 128].rearrange("p k (a two b) -> p k a two b", two=2, b=blk)
                    d_ = W[:, :, o:o + 128].rearrange("p k (a two b) -> p k a two b", two=2, b=blk)
                    cp(d_[:, :, :, 0, :], s_[:, :, :, 1, :], [f"W_{nm}"], [f"W_{nm}p"])
                    cp(d_[:, :, :, 1, :], s_[:, :, :, 0, :], [f"W_{nm}", f"W_{nm}p"], [f"W_{nm}p"])
                    ofs[nm + "p"] = o
                    o += 128
            UB = hs["UB"]
            TR = hs["TR"]
            ROPE = hs["ROPE"]
            ti = 0
            for b in range(nblocks):
                t0, n, grp = BLOCKS[b]
                ub_i = b % len(UB)
                make_u(l, b, UB, ub_i, 1, 0)
                UBt = UB[ub_i]
                if has_rope and grp == 0:
                    P.dma("sp", ROPE[:, :, :], rope_d[:, :, t0:t0 + n].rearrange("f p t -> p f t"), [], ["ROPE"])
                for nm, dst_f in (("q", Qf), ("k", Kf)):
                    ps, pk = fm_group(UBt, ub_i, n, W, f"W_{nm}", ofs[nm], dk)
                    dst, dkey = dst_f(b)
                    if has_rope and grp == 0:
                        ps2, pk2 = fm_group(UBt, ub_i, n, W, f"W_{nm}p", ofs[nm + "p"], dk)
                        fi = 0 if nm == "q" else 2
                        rope_evac(dst, dkey, ps, pk, ps2, pk2, ROPE[:, fi, :n], ROPE[:, fi + 1, :n], "ROPE", n, TR, ti)
                        ti += 1
                    else:
                        sc = qscale if nm == "q" else 1.0
                        act(dst, ps[0:dk, :n], AF.Copy, [pk], [dkey], scale=sc)
                if has_g:
                    ps, pk = fm_group(UBt, ub_i, n, W, "W_g", ofs["g"], 128)
                    t1 = TR[ti % 2]
                    act(t1[:, :n], ps[:, :n], AF.Silu, [pk], [f"TR{ti % 2}"])
                    ts(SGN[:, t0:t0 + n], t1[:, :n], gn_ap, ALU.mult, [f"TR{ti % 2}", "GN"], [f"sgn_b{b}"])
                    ti += 1
                if LRT is not None:
                    for di in range(2):
                        ps, pk = fm_group(UBt, ub_i, n, W, "W_lr", ofs["lr"] + 16 * di, 16)
                        cp(LRT[di][0:16, :n], ps[0:16, :n], [pk], [f"lrt{di}"], eng="act")
                ps, pk = nb()
                ntile = n // 128
                for tI in range(ntile):
                    for k in range(8):
                        mm(ps[:, tI * 128:(tI + 1) * 128], UBt[:, k, tI * 128:(tI + 1) * 128],
                           W[:, k, ofs["v"]:ofs["v"] + 128], k == 0, k == 7, ["W_v", f"UB{ub_i}k{k}"], [pk])
                c0 = t0 // 128
                cp(V[:, c0:c0 + ntile, :], ps[:, :n].rearrange("p (c d) -> p c d", d=128), [pk], [f"vb{b}"], eng="act")
                if after_block is not None:
                    after_block(b)

        def head_norm(ps_o, pk_o, n, center, hs, out_ap, outkey, post_ap, postkeys, scale_ap=None, src_sb=None):
            OSB, SQh, RS = hs["OSB"], hs["SQh"], hs["RS"]
            M2 = SQh
            if src_sb is None:
                cp(OSB[:, :n], ps_o[:, :n], [pk_o], ["OSB"], eng="act")
                act(SQh[:, :n], ps_o[:, :n], AF.Square, [pk_o], ["SQh"])
            else:
                act(SQh[:, :n], OSB[:, :n], AF.Square, ["OSB"], ["SQh"])
            pss, pks = nb()
            mm(pss[:, :n], ONESH, SQh[:, :n], True, True, ["SQh", "CST"], [pks])
            if center:
                psm, pkm = nb()
                mm(psm[:, :n], ONESH, OSB[:, :n], True, True, ["OSB", "CST"], [pkm])
                act(M2[:, :n], psm[:, :n], AF.Square, [pkm, "SQh"], ["SQh"])
                tt(RS[:, :n], pss[:, :n], M2[:, :n], ALU.subtract, [pks, "SQh"], ["RS"])
                tt(OSB[:, :n], OSB[:, :n], psm[:, :n], ALU.subtract, ["OSB", pkm], ["OSB"])
                act(RS[:, :n], RS[:, :n], AF.Ln, ["RS"], ["RS"], bias=EPSC[:, 0:1])
            else:
                act(RS[:, :n], pss[:, :n], AF.Ln, [pks], ["RS"], bias=EPSC[:, 0:1])
            act(RS[:, :n], RS[:, :n], AF.Exp, ["RS"], ["RS"], scale=-0.5)
            if scale_ap is None:
                tt(OSB[:, :n], OSB[:, :n], RS[:, :n], ALU.mult, ["OSB", "RS"], ["OSB"])
                tt(out_ap, OSB[:, :n], post_ap, ALU.mult, ["OSB"] + postkeys, [outkey])
            else:
                stt(out_ap, OSB[:, :n], scale_ap, RS[:, :n], ALU.mult, ALU.mult, ["OSB", "RS"] + postkeys, [outkey])

        def mixer_ab(l, need_ctx):
            i = l // 2
            wsrc = ab_w_in_d[i]
            with ExitStack() as s2:
                hs = dict(
                    W=sb("Wh", [128, 8, 768], BF16, s2),
                    UB=[sb("UB0", [128, 8, 512], BF16, s2)],
                    TR=[sb(f"TR{j}", [128, 512], F32, s2) for j in range(2)],
                    OSB=sb("OSB", [128, 512], F32, s2), SQh=sb("SQh", [128, 512], F32, s2),
                    RS=sb("RS", [128, 512], F32, s2),
                )
                Qt = sb("Qt", [128, 512], BF16, s2)
                Kt = sb("Kt", [128, 512], BF16, s2)
                V = sb("V", [128, 18, 128], BF16, s2)
                SGN = sb("SGN", [128, NT], BF16, s2)
                QDF = sb("QDF", [128, NT], BF16, s2)
                QDB = sb("QDB", [128, NT], BF16, s2)
                ATT = sb("ATT", [128, NT], BF16, s2)
                SFb = sb("SFb", [128, 18, 128], BF16, s2)
                SBb = sb("SBb", [128, 18, 128], BF16, s2)
                CUR = [sb(f"CUR{j}", [128, 128], F32, s2) for j in range(4)]
                KD = [sb(f"KD{j}", [128, 512], BF16, s2) for j in range(4)]
                KTT = [sb(f"KTT{j}", [128, 4, 128], BF16, s2) for j in range(2)]

                def Qf(b):
                    return Qt[:, :BLOCKS[b][1]], "qt"

                def Kf(b):
                    return Kt[:, :BLOCKS[b][1]], "kt"

                def Qf64(b):
                    return Qt[0:64, :BLOCKS[b][1]], "qt"

                def Kf64(b):
                    return Kt[0:64, :BLOCKS[b][1]], "kt"

                def run_head(h, ph):
                    isA = h < 4
                    dk = 128 if isA else 64
                    hh = h if isA else h - 4
                    if isA:
                        RET = ph["RET"]
                        P.dma("sp", RET[:], ret_d[:, hh, :, :], [], ["RET"])
                    else:
                        LRT, WLRb, LS, LS2, EXS, TAB, TOT, DEC = (ph[k_] for k_ in ("LRT", "WLRb", "LS", "LS2", "EXS", "TAB", "TOT", "DEC"))
                    P.op("dve", lambda: nc.vector.memset(SFb[:, 16, :], 0.0), [], ["SFb"])
                    P.op("dve", lambda: nc.vector.memset(SBb[:, 17, :], 0.0), [], ["SBb"])

                    def pass1(b):
                        t0, n, grp = BLOCKS[b]
                        nch = n // 128
                        c0 = t0 // 128
                        if (not isA) and DBG_B < 1:
                            return
                        qv = Qt[0:dk, :n]
                        kv = Kt[0:dk, :n]
                        if isA:
                            tabs = [RET[:, j, :].unsqueeze(1).broadcast_to([128, nch, 128]) for j in range(6)]
                            tkeys = ["RET"]

                            def rr(ap):
                                return ap.rearrange("p (c d) -> p c d", d=128)
                        else:
                            psg, pkg = nb()
                            for c in range(nch):
                                for di in range(2):
                                    mm(psg[:, c * 128 + di * 64:c * 128 + di * 64 + 64],
                                       LRT[di][:, c * 128:(c + 1) * 128], WLRb[:, di, hh * 64:(hh + 1) * 64],
                                       True, True, [f"lrt{di}", "WLRb"], [pkg])
                            act(EXS[:, :n], psg[:, :n], AF.Exp, [pkg], ["EXS"], scale=-1.0)
                            ex4 = EXS[:, :n].rearrange("p (c a d) -> p c a d", a=2, d=64)
                            act(LS[:, 0:nch, :, :], ex4, AF.Ln, ["EXS"], ["LS"], bias=ONEC[:, 0:1])
                            act(LS2[:, 0:nch, 0, :], ex4[:, :, 1, :], AF.Ln, ["EXS"], ["LS2"], bias=ONEC[:, 0:1])
                            act(LS2[:, 0:nch, 1, :], ex4[:, :, 0, :], AF.Ln, ["EXS", "LS2"], ["LS2"], bias=ONEC[:, 0:1])
                            if DBG_C < "b":
                                return
                            psf, pkf = nb()
                            psb, pkb = nb()
                            psp, pkp = nb()
                            for c in range(nch):
                                mm(psf[:, c * 128:(c + 1) * 128], LS[:, c, :, :].rearrange("p a d -> p (a d)"), TRIF, True, True, ["LS", "CST"], [pkf])
                            for c in range(nch):
                                mm(psb[:, c * 128:(c + 1) * 128], LS2[:, c, :, :].rearrange("p a d -> p (a d)"), TRIB, True, True, ["LS2", "CST2"], [pkb])
                            for c in range(nch):
                                mm(psp[:, c * 128:(c + 1) * 128], LS2[:, c, :, :].rearrange("p a d -> p (a d)"), TRIBP, True, True, ["LS2", "CST2"], [pkp])
                            if DBG_C < "c":
                                return
                            f3 = psf[0:64, :n].rearrange("p (c d) -> p c d", d=128)
                            b3 = psb[0:64, :n].rearrange("p (c d) -> p c d", d=128)
                            cp(TOT[0:64, 0:nch, 0:1], f3[:, :, 127:128], [pkf], ["TOT"])
                            cp(TOT[0:64, 0:nch, 1:2], b3[:, :, 0:1], [pkb, "TOT"], ["TOT"])
                            if DBG_C < "d":
                                return
                            act(TAB[0][0:64, :n], psf[0:64, :n], AF.Exp, [pkf, "TOT"], ["TAB0"])
                            act(TAB[1][0:64, :n], psf[0:64, :n], AF.Exp, [pkf, "TOT"], ["TAB1"], scale=-1.0)
                            act(TAB[3][0:64, :n], psp[0:64, :n], AF.Exp, [pkp, "TOT"], ["TAB3"])
                            act(TAB[4][0:64, :n], psb[0:64, :n], AF.Exp, [pkb, "TOT"], ["TAB4"], scale=-1.0)
                            if DBG_C < "e":
                                return
                            for c in range(nch):
                                act(TAB[2][0:64, c * 128:(c + 1) * 128], psf[0:64, c * 128:(c + 1) * 128], AF.Exp,
                                    [pkf, "TOT"], ["TAB2"], scale=-1.0, bias=TOT[0:64, c, 0:1])
                                act(TAB[5][0:64, c * 128:(c + 1) * 128], psb[0:64, c * 128:(c + 1) * 128], AF.Exp,
                                    [pkb, "TOT"], ["TAB5"], scale=-1.0, bias=TOT[0:64, c, 1:2])
                            act(DEC[0:64, c0:c0 + nch, :], TOT[0:64, 0:nch, :], AF.Exp, ["TOT"], ["DEC"])
                            tabs = [TAB[j][0:64, :n] for j in range(6)]
                            tkeys = [f"TAB{j}" for j in range(6)]

                            def rr(ap):
                                return ap
                        if (not isA) and DBG_B < 2:
                            return
                        tt(rr(QDF[0:dk, t0:t0 + n]), rr(qv), tabs[0], ALU.mult, ["qt"] + tkeys, [f"qdf{b}"])
                        tt(rr(QDB[0:dk, t0:t0 + n]), rr(qv), tabs[3], ALU.mult, ["qt"] + tkeys, [f"qdb{b}"])
                        tt(rr(KD[0][0:dk, :n]), rr(kv), tabs[1], ALU.mult, ["kt"] + tkeys, ["KD0"])
                        tt(rr(KD[1][0:dk, :n]), rr(kv), tabs[4], ALU.mult, ["kt"] + tkeys, ["KD1"])
                        tt(rr(KD[2][0:dk, :n]), rr(kv), tabs[2], ALU.mult, ["kt"] + tkeys, ["KD2"])
                        tt(rr(KD[3][0:dk, :n]), rr(kv), tabs[5], ALU.mult, ["kt"] + tkeys, ["KD3"])
                        psa, pka = nb()
                        psb2, pkb2 = nb()
                        for c in range(nch):
                            sl = slice(c * 128, (c + 1) * 128)
                            gsl = slice(t0 + c * 128, t0 + (c + 1) * 128)
                            mm(psa[:, sl], KD[0][0:dk, sl], QDF[0:dk, gsl], True, True, ["KD0", f"qdf{b}"], [pka])
                            mm(psb2[:, sl], KD[1][0:dk, sl], QDB[0:dk, gsl], True, True, ["KD1", f"qdb{b}"], [pkb2])
                        cp(ATT[:, t0:t0 + n], psa[:, :n], [pka], [f"att{b}"], eng="act")
                        P.op("dve", lambda: nc.vector.copy_predicated(out=ATT[:, t0:t0 + n], mask=MASK[:, :n],
                                                                       data=psb2[:, :n]),
                             [pkb2, "MASK", f"att{b}"], [f"att{b}"])
                        if (not isA) and DBG_B < 3:
                            return
                        for di in range(2):
                            psk, pkk = nb()
                            for c in range(nch):
                                mm(psk[:, c * 128:c * 128 + dk], KD[2 + di][0:dk, c * 128:(c + 1) * 128], IDB[0:dk, 0:dk],
                                   True, True, [f"KD{2 + di}", "CSTB"], [pkk])
                            cp(KTT[di][:, 0:nch, 0:dk], psk[:, :n].rearrange("p (c d) -> p c d", d=128)[:, :, 0:dk],
                               [pkk], [f"KTT{di}"], eng="act")
                            psd, pkd = nb()
                            for c in range(nch):
                                mm(psd[0:dk, c * 128:(c + 1) * 128], KTT[di][:, c, 0:dk], V[:, c0 + c, :], True, True,
                                   [f"KTT{di}", f"vb{b}"], [pkd])
                            d3 = psd[0:dk, :n].rearrange("p (c d) -> p c d", d=128)
                            if di == 0:
                                if grp == 0:
                                    m = nch if c0 + nch < 16 else nch - 1
                                    cp(SFb[0:dk, c0 + 1:c0 + 1 + m, :], d3[:, 0:m, :], [pkd], ["SFb"])
                                else:
                                    cp(SFb[0:dk, 17, :], d3[:, 0, :], [pkd], ["SFb"])
                                    cp(SFb[0:dk, 0, :], d3[:, 1, :], [pkd], ["SFb"])
                            else:
                                if c0 == 0:
                                    cp(SBb[0:dk, 0:nch - 1, :], d3[:, 1:nch, :], [pkd], ["SBb"])
                                else:
                                    cp(SBb[0:dk, c0 - 1:c0 - 1 + nch, :], d3[:, :, :], [pkd], ["SBb"])

                    if isA:
                        cols = dict(q=hh * 128, k=512 + hh * 128, v=1024 + hh * 128, g=1536 + hh * 128)
                        hs["ROPE"] = ph["ROPE"]
                        in_proj_head(l, hs, "A", wsrc, cols, Qf, Kf, V, SGN, GN[:, i, h:h + 1], ropeA_d, dk ** -0.5,
                                     after_block=pass1)
                    else:
                        cols = dict(q=2048 + hh * 64, k=2304 + hh * 64, v=2560 + hh * 128, g=3072 + hh * 128)
                        hs["ROPE"] = None
                        in_proj_head(l, hs, "B", wsrc, cols, Qf64, Kf64, V, SGN, GN[:, i, h:h + 1], None, dk ** -0.5,
                                     LRT=LRT, lr_cols=3584, after_block=pass1)
                    if (not isA) and DBG_B < 4:
                        return
                    for di, (ST, order, skey) in enumerate(((SFb, ORDER_F, "SFb"), (SBb, ORDER_B, "SBb"))):
                        c_a, c_b = CUR[2 * di], CUR[2 * di + 1]
                        ka, kb_ = f"CUR{2 * di}", f"CUR{2 * di + 1}"
                        P.op("dve", lambda c_a=c_a: nc.vector.memset(c_a[:], 0.0), [], [ka])
                        for oi in range(17):
                            nn, nx = order[oi], order[oi + 1]
                            if isA:
                                g_ = 1.0 - 2.0 ** (-((5.0 if di == 0 else 5.5) + hh))
                                sc_ = float(np.float32(g_) ** 128)
                            else:
                                sc_ = DEC[0:64, nn, di:di + 1]
                            stt(c_b[0:dk, :], c_a[0:dk, :], sc_, ST[0:dk, nx, :], ALU.mult, ALU.add,
                                [ka, skey] + ([] if isA else ["DEC"]), [kb_])
                            cp(ST[0:dk, nx, :], c_b[0:dk, :], [kb_], [skey], eng="act")
                            c_a, c_b, ka, kb_ = c_b, c_a, kb_, ka
                    if (not isA) and DBG_B < 5:
                        return
                    for b in range(5 if need_ctx else 4):
                        t0, n, grp = BLOCKS[b]
                        nch = n // 128
                        c0 = t0 // 128
                        pso, pko = nb()
                        for c in range(nch):
                            sl = slice(c * 128, (c + 1) * 128)
                            gsl = slice(t0 + c * 128, t0 + (c + 1) * 128)
                            mm(pso[:, sl], V[:, c0 + c, :], ATT[:, gsl], True, False, [f"vb{b}", f"att{b}"], [pko])
                            mm(pso[:, sl], SFb[0:dk, c0 + c, :], QDF[0:dk, gsl], False, False, ["SFb", f"qdf{b}"], [pko])
                            mm(pso[:, sl], SBb[0:dk, c0 + c, :], QDB[0:dk, gsl], False, True, ["SBb", f"qdb{b}"], [pko])
                        head_norm(pso, pko, n, isA, hs, UY[:, h, t0:t0 + n], f"u{h}b{b}", SGN[:, t0:t0 + n], [f"sgn_b{b}"])

                with ExitStack() as s3:
                    ph = dict(ROPE=sb("ROPE", [128, 4, 512], F32, s3), RET=sb("RET", [128, 6, 128], F32, s3))
                    for h in range(min(4, DBG_HEADS)):
                        run_head(h, ph)
                P.barrier()
                with ExitStack() as s3:
                    WLR = sb("WLR", [32, 2, 256], F32, s3)
                    ph = dict(
                        LRT=[sb(f"LRT{j}", [32, 512], BF16, s3) for j in range(2)],
                        WLRb=sb("WLRb", [32, 2, 256], BF16, s3),
                        LS=sb("LS", [128, 4, 2, 64], F32, s3),
                        LS2=sb("LS2", [128, 4, 2, 64], F32, s3),
                        EXS=sb("EXS", [128, 512], F32, s3),
                        TAB=[sb(f"TAB{j}", [128, 512], BF16, s3) for j in range(6)],
                        TOT=sb("TOT", [128, 4, 2], F32, s3),
                        DEC=sb("DEC", [128, 18, 2], F32, s3),
                    )
                    P.dma("sp", WLR[:], wlr_d[:, i, :, :], [], ["WLR"])
                    cp(ph["WLRb"][:], WLR[:], ["WLR"], ["WLRb"])
                    for di in range(2):
                        P.op("dve", lambda di=di: nc.vector.memset(ph["LRT"][di][:], 1.0), [], [f"lrt{di}"])
                    for h in range(4, min(8, DBG_HEADS)):
                        run_head(h, ph)
            P.barrier()

        def mixer_c(l, need_ctx):
            i = l // 2
            wsrc = c_w_qkv_d[i]
            with ExitStack() as s2:
                hs = dict(
                    W=sb("Wh", [128, 8, 640], BF16, s2),
                    UB=[sb(f"UB{j}", [128, 8, 512], BF16, s2) for j in range(2)],
                    TR=[sb(f"TR{j}", [128, 512], F32, s2) for j in range(2)],
                    ROPE=sb("ROPE", [128, 4, 512], F32, s2),
                    OSB=sb("OSB", [128, 512], F32, s2), SQh=sb("SQh", [128, 512], F32, s2),
                    RS=sb("RS", [128, 512], F32, s2),
                )
                Q = sb("Q", [128, NT], BF16, s2)
                K = sb("K", [128, NT], BF16, s2)
                V = sb("V", [128, 18, 128], BF16, s2)
                E = [sb(f"E{j}", [128, 512], BF16, s2) for j in range(4)]
                R1 = sb("R1", [128, 512], F32, s2)
                R2 = sb("R2", [128, 512], F32, s2)
                A1 = sb("A1", [128, 512], F32, s2)
                for h in range(8):
                    cols = dict(q=h * 128, k=1024 + h * 128, v=2048 + h * 128)
                    in_proj_head(l, hs, "C", wsrc, cols,
                                 lambda b: (Q[:, BLOCKS[b][0]:BLOCKS[b][0] + BLOCKS[b][1]], f"qb{b}"),
                                 lambda b: (K[:, BLOCKS[b][0]:BLOCKS[b][0] + BLOCKS[b][1]], f"kb{b}"),
                                 V, None, None, ropeC_d, 0.125)
                    for b in range(5 if need_ctx else 4):
                        t0, n, grp = BLOCKS[b]
                        kchunks = list(range(18)) if grp == 0 else [16, 17]
                        acc = [(PS[j], f"ps{j}") for j in range(4)]
                        for ci, c in enumerate(kchunks):
                            first, last = ci == 0, ci == len(kchunks) - 1
                            kb_ = c // 4 if c < 16 else 4
                            ksl = slice(c * 128, (c + 1) * 128)
                            for comp in range(2):
                                sj = 4 + 2 * (ci % 2) + comp
                                pss, pks = PS[sj], f"ps{sj}"
                                rsl = slice(comp * 64, (comp + 1) * 64)
                                mm(pss[:, :n], K[rsl, ksl], Q[rsl, t0:t0 + n], True, True, [f"kb{kb_}", f"qb{b}"], [pks])
                                ej = 2 * (ci % 2) + comp
                                act(E[ej][:, :n], pss[:, :n], AF.Exp, [pks], [f"E{ej}"])
                                po, pko = acc[2 * comp]
                                pd, pkd = acc[2 * comp + 1]
                                mm(po[:, :n], V[:, c, :], E[ej][:, :n], first, last, [f"vb{kb_}", f"E{ej}"], [pko])
                                mm(pd[:, :n], ONESB, E[ej][:, :n], first, last, ["CSTB", f"E{ej}"], [pkd])
                        act(R1[:, :n], acc[1][0][:, :n], AF.Ln, [acc[1][1]], ["R1"])
                        act(R1[:, :n], R1[:, :n], AF.Exp, ["R1"], ["R1"], scale=-1.0)
                        act(R2[:, :n], acc[3][0][:, :n], AF.Ln, [acc[3][1]], ["R2"])
                        act(R2[:, :n], R2[:, :n], AF.Exp, ["R2"], ["R2"], scale=-1.0)
                        tt(A1[:, :n], acc[0][0][:, :n], R1[:, :n], ALU.mult, [acc[0][1], "R1"], ["A1"])
                        tt(R2[:, :n], acc[2][0][:, :n], R2[:, :n], ALU.mult, [acc[2][1], "R2"], ["R2"])
                        stt(hs["OSB"][:, :n], R2[:, :n], LAM[:, i, h:h + 1], A1[:, :n], ALU.mult, ALU.add,
                            ["R2", "A1", "LAM"], ["OSB"])
                        psc[0] = 4
                        head_norm(None, None, n, False, hs, UY[:, h, t0:t0 + n], f"u{h}b{b}", None, ["SUBG"],
                                  scale_ap=SUBG[:, i, h:h + 1], src_sb=True)
                        psc[0] = 4
            P.barrier()

        def moe(l, need_ctx):
            nblk = 5 if need_ctx else 4
            with ExitStack() as s2:
                WB = [sb(f"WB{j}", [128, 4096], BF16, s2) for j in range(4)]
                WCT = sb("WCT", [32, NT], BF16, s2)
                SELE = sb("SELE", [32, 32 * 128], BF16, s2)
                BC = [sb(f"BC{j}", [128, NT], BF16, s2) for j in range(2)]
                H = [sb(f"H{j}", [128, 4, 512], BF16, s2) for j in range(2)]
                SG = [sb(f"SG{j}", [128, 512], F32, s2) for j in range(2)]
                TF = [sb(f"TF{j}", [128, 512], F32, s2) for j in range(3)]
                SQ = [sb(f"SQ{j}", [128, 512], F32, s2) for j in range(2)]
                MEAN = sb("MEAN", [128, 512], F32, s2)
                RSTD = sb("RSTD", [128, 512], F32, s2)
                LG = sb("LG", [128, 36], F32, s2)
                SM = sb("SM", [128, 16], F32, s2)
                GOH = sb("GOH", [128, 4], F32, s2)
                GE = sb("GE", [128, 4], F32, s2)
                ET = sb("ET", [128, 32], F32, s2)
                ES = sb("ES", [128, 8], F32, s2)
                T8 = sb("T8", [128, 8], F32, s2)
                SEL = sb("SEL", [128, 8], F32, s2)
                EX = sb("EX", [128, 8], F32, s2)
                WC = sb("WC", [128, 32], F32, s2)
                P.dma("sp", SELE[:], sele_d, [], ["SELE"])
                WR = sb("WRl", [128, 8, 36], F32, s2)
                P.dma("sp", WR[:], wr_d[:, l, :, :], [], ["WR"])
                items = []
                for e in range(32):
                    items.append(("g", e, wg_d[l, e].rearrange("(k p) n -> p k n", p=128)))
                    items.append(("u", e, wu_d[l, e].rearrange("(k p) n -> p k n", p=128)))
                    items.append(("d", e, wd_d[l, e].rearrange("(k p) n -> p k n", p=128)))

                def load_item(j):
                    if j >= len(items):
                        return
                    kind, e, src = items[j]
                    if kind == "d":
                        dst = WB[j % 4][:, :].rearrange("p (k n) -> p k n", n=1024)
                    else:
                        dst = WB[j % 4][:, :].rearrange("p (k n) -> p k n", n=512)
                    P.dma("pool", dst, src, [], [f"WB{j % 4}"])

                for j in range(4):
                    load_item(j)
                ti = 0
                for b in range(nblk):
                    t0, n, grp = BLOCKS[b]
                    ntile = n // 128
                    pr = [nb() for _ in range(ntile)]
                    for k in range(8):
                        tf = TF[ti % 3]
                        tk = f"TF{ti % 3}"
                        ti += 1
                        act(tf[:, :n], XT[:, k, t0:t0 + n], AF.Identity, [f"x{k}b{b}", "MOD"], [tk],
                            scale=mod(l, 4, k, grp), bias=mod(l, 3, k, grp))
                        cp(UY[:, k, t0:t0 + n], tf[:, :n], [tk], [f"u{k}b{b}"])
                        for tI in range(ntile):
                            mm(pr[tI][0][:, 0:36], tf[:, tI * 128:(tI + 1) * 128], WR[:, k, :], k == 0, False,
                               [tk, "WR"], [pr[tI][1]])
                    for tI in range(ntile):
                        ps, pk = pr[tI]
                        mm(ps[:, 0:36], ONES1, RB[0:1, l, :], False, True, ["CST", "RB"], [pk])
                        cp(LG[:], ps[:, 0:36], [pk], ["LG"])
                        R_ = ["LG", "SM", "GOH", "GE", "ET", "ES", "T8", "SEL", "EX", "WC"]

                        def dv(fn):
                            P.op("dve", fn, R_, R_)
                        dv(lambda: nc.vector.tensor_reduce(out=SM[:, 0:1], in_=LG[:, 0:4], axis=AX.X, op=ALU.max))
                        dv(lambda: nc.vector.tensor_scalar(out=GOH[:], in0=LG[:, 0:4], scalar1=SM[:, 0:1], scalar2=None,
                                                           op0=ALU.is_equal))
                        dv(lambda: nc.vector.tensor_scalar(out=SM[:, 1:2], in0=SM[:, 0:1], scalar1=-1.0, scalar2=None,
                                                           op0=ALU.mult))
                        act(GE[:], LG[:, 0:4], AF.Exp, R_, R_, bias=SM[:, 1:2])
                        dv(lambda: nc.vector.tensor_reduce(out=SM[:, 2:3], in_=GE[:], axis=AX.X, op=ALU.add))
                        dv(lambda: nc.vector.reciprocal(out=SM[:, 3:4], in_=SM[:, 2:3]))
                        dv(lambda: nc.vector.tensor_tensor(
                            out=ET[:].rearrange("p (g e) -> p g e", e=8),
                            in0=LG[:, 4:36].rearrange("p (g e) -> p g e", e=8),
                            in1=GOH[:].unsqueeze(2).broadcast_to([128, 4, 8]), op=ALU.mult))
                        dv(lambda: nc.vector.tensor_reduce(out=ES[:], in_=ET[:].rearrange("p (g e) -> p e g", e=8),
                                                           axis=AX.X, op=ALU.add))
                        dv(lambda: nc.vector.max(out=T8[:], in_=ES[:]))
                        dv(lambda: nc.vector.tensor_scalar(out=SEL[:], in0=ES[:], scalar1=T8[:, 1:2], scalar2=None,
                                                           op0=ALU.is_ge))
                        dv(lambda: nc.vector.tensor_scalar(out=SM[:, 4:5], in0=T8[:, 0:1], scalar1=-1.0, scalar2=None,
                                                           op0=ALU.mult))
                        act(EX[:], ES[:], AF.Exp, R_, R_, bias=SM[:, 4:5])
                        dv(lambda: nc.vector.tensor_tensor(out=EX[:], in0=EX[:], in1=SEL[:], op=ALU.mult))
                        dv(lambda: nc.vector.tensor_reduce(out=SM[:, 5:6], in_=EX[:], axis=AX.X, op=ALU.add))
                        dv(lambda: nc.vector.reciprocal(out=SM[:, 6:7], in_=SM[:, 5:6]))
                        dv(lambda: nc.vector.tensor_tensor(out=SM[:, 7:8], in0=SM[:, 6:7], in1=SM[:, 3:4], op=ALU.mult))
                        dv(lambda: nc.vector.tensor_scalar(out=EX[:], in0=EX[:], scalar1=SM[:, 7:8], scalar2=None,
                                                           op0=ALU.mult))
                        dv(lambda: nc.vector.tensor_tensor(
                            out=WC[:].rearrange("p (g e) -> p g e", e=8),
                            in0=GOH[:].unsqueeze(2).broadcast_to([128, 4, 8]),
                            in1=EX[:].unsqueeze(1).broadcast_to([128, 4, 8]), op=ALU.mult))
                        pt, pkt = nb()
                        P.op("pe", lambda: nc.tensor.transpose(out=pt[0:32, 0:128], in_=WC[:], identity=IDF),
                             R_ + ["CST"], [pkt])
                        cp(WCT[:, t0 + tI * 128:t0 + (tI + 1) * 128], pt[0:32, 0:128], [pkt], [f"wct{b}"], eng="act")
                for b in range(nblk):
                    t0, n, grp = BLOCKS[b]
                    for k in range(8):
                        xk = XT[:, k, t0:t0 + n]
                        if k % 2 == 0:
                            act(xk, xk, AF.Copy, [f"x{k}b{b}"], [f"x{k}b{b}"], scale=ALPHA)
                        else:
                            ts(xk, xk, ALPHA, ALU.mult, [f"x{k}b{b}"], [f"x{k}b{b}"])
                hi = 0
                for e in range(32):
                    j0 = 3 * e
                    WG = WB[j0 % 4][:, :].rearrange("p (k n) -> p k n", n=512)
                    WU = WB[(j0 + 1) % 4][:, :].rearrange("p (k n) -> p k n", n=512)
                    WD = WB[(j0 + 2) % 4][:, :].rearrange("p (k n) -> p k n", n=1024)
                    kg, ku, kd = f"WB{j0 % 4}", f"WB{(j0 + 1) % 4}", f"WB{(j0 + 2) % 4}"
                    bc = BC[e % 2]
                    bck = f"BC{e % 2}"
                    for b in range(nblk):
                        t0, n, grp = BLOCKS[b]
                        ps, pk = nb()
                        mm(ps[:, :n], SELE[:, e * 128:(e + 1) * 128], WCT[:, t0:t0 + n], True, True, ["SELE", f"wct{b}"], [pk])
                        cp(bc[:, t0:t0 + n], ps[:, :n], [pk], [bck + f"b{b}"], eng="act")
                    for b in range(nblk):
                        t0, n, grp = BLOCKS[b]
                        Hb = H[hi % 2]
                        hk = f"H{hi % 2}"
                        hi += 1
                        for fc in range(4):
                            pg, pkg = nb()
                            pu, pku = nb()
                            for k in range(8):
                                mm(pg[:, :n], WG[:, k, fc * 128:(fc + 1) * 128], UY[:, k, t0:t0 + n], k == 0, k == 7,
                                   [kg, f"u{k}b{b}"], [pkg])
                            for k in range(8):
                                mm(pu[:, :n], WU[:, k, fc * 128:(fc + 1) * 128], UY[:, k, t0:t0 + n], k == 0, k == 7,
                                   [ku, f"u{k}b{b}"], [pku])
                            sg = SG[fc % 2]
                            act(sg[:, :n], pg[:, :n], AF.Silu, [pkg], [f"SG{fc % 2}"])
                            tt(sg[:, :n], sg[:, :n], pu[:, :n], ALU.mult, [f"SG{fc % 2}", pku], [f"SG{fc % 2}"])
                            tt(Hb[:, fc, :n], sg[:, :n], bc[:, t0:t0 + n], ALU.mult, [f"SG{fc % 2}", bck + f"b{b}"],
                               [hk + f"f{fc}"])
                        if b == nblk - 1:
                            load_item(j0 + 4)
                            load_item(j0 + 5)
                        for oc in range(8):
                            py, pky = nb()
                            for fc in range(4):
                                mm(py[:, :n], WD[:, fc, oc * 128:(oc + 1) * 128], Hb[:, fc, :n], fc == 0, fc == 3,
                                   [kd, hk + f"f{fc}"], [pky])
                            xk = XT[:, oc, t0:t0 + n]
                            stt(xk, py[:, :n], mod(l, 5, oc, grp), xk, ALU.mult, ALU.add, [pky, "MOD", f"x{oc}b{b}"],
                                [f"x{oc}b{b}"])
                    load_item(j0 + 6)
                for b in range(nblk):
                    ln_block(l, 1, b, (SQ, MEAN, RSTD))
            P.barrier()

        def moe_sparse(l, need_ctx):
            nblk = 5 if need_ctx else 4
            ntiles = 18 if need_ctx else 16
            NB_ = (ntiles * 128 * 2) // 128 + 32
            POOL = (mybir.EngineType.Pool,)
            with ExitStack() as s2:
                AST = sb("AST", [128, 18, 32], BF16, s2)
                WCS = sb("WCS", [128, 18, 32], F32, s2)
                CSS = sb("CSS", [128, 18, 32], F32, s2)
                PRE = sb("PRE", [128, 19, 32], F32, s2)
                PST = sb("PST", [128, 32], F32, s2)
                PEN = sb("PEN", [128, 32], F32, s2)
                DESTF = sb("DESTF", [128, 18, 2], F32, s2)
                DEST = sb("DEST", [128, 18, 2], U32, s2)
                WSEL = sb("WSEL", [128, 18, 2], F32, s2)
                IDXW = sb("IDXW", [128, 72], U32, s2)
                SQ = [sb(f"SQ{j}", [128, 512], F32, s2) for j in range(2)]
                MEAN = sb("MEAN", [128, 512], F32, s2)
                RSTD = sb("RSTD", [128, 512], F32, s2)
                with ExitStack() as s3:
                    TF = [sb(f"TF{j}", [128, 512], F32, s3) for j in range(3)]
                    WR = sb("WRl", [128, 8, 36], F32, s3)
                    LG = sb("LG", [128, 36], F32, s3)
                    SM = sb("SM", [128, 16], F32, s3)
                    GOH = sb("GOH", [128, 4], F32, s3)
                    GE = sb("GE", [128, 4], F32, s3)
                    ET = sb("ET", [128, 32], F32, s3)
                    ES = sb("ES", [128, 8], F32, s3)
                    T8 = sb("T8", [128, 8], F32, s3)
                    SEL = sb("SEL", [128, 8], F32, s3)
                    EX = sb("EX", [128, 8], F32, s3)
                    NBK = sb("NBK", [128, 32, 36], F32, s3)
                    CNT = sb("CNT", [128, 32], F32, s3)
                    PADB = sb("PADB", [32, 128], F32, s3)
                    DP1 = sb("DP1", [128, 32], F32, s3)
                    EQ = sb("EQ", [128, 32], F32, s3)
                    BLE = sb("BLE", [128, 72, 32], F32, s3)
                    BLKB = sb("BLKB", [128, 72], F32, s3)
                    P.dma("sp", WR[:], wr_d[:, l, :, :], [], ["WR"])
                    ti = 0
                    for b in range(nblk):
                        t0, n, grp = BLOCKS[b]
                        ntile = n // 128
                        pr = [nb() for _ in range(ntile)]
                        for k in range(8):
                            tf = TF[ti % 3]
                            tk = f"TF{ti % 3}"
                            ti += 1
                            act(tf[:, :n], XT[:, k, t0:t0 + n], AF.Identity, [f"x{k}b{b}", "MOD"], [tk],
                                scale=mod(l, 4, k, grp), bias=mod(l, 3, k, grp))
                            cp(UY[:, k, t0:t0 + n], tf[:, :n], [tk], [f"u{k}b{b}"])
                            for tI in range(ntile):
                                mm(pr[tI][0][:, 0:36], tf[:, tI * 128:(tI + 1) * 128], WR[:, k, :], k == 0, False,
                                   [tk, "WR"], [pr[tI][1]])
                        for tI in range(ntile):
                            gi = t0 // 128 + tI
                            ps, pk = pr[tI]
                            mm(ps[:, 0:36], ONES1, RB[0:1, l, :], False, True, ["CST", "RB"], [pk])
                            cp(LG[:], ps[:, 0:36], [pk], ["LG"])
                            R_ = ["LG", "SM", "GOH", "GE", "ET", "ES", "T8", "SEL", "EX"]

                            def dv(fn, extra_w=()):
                                P.op("dve", fn, R_, R_ + list(extra_w))
                            dv(lambda: nc.vector.tensor_reduce(out=SM[:, 0:1], in_=LG[:, 0:4], axis=AX.X, op=ALU.max))
                            dv(lambda: nc.vector.tensor_scalar(out=GOH[:], in0=LG[:, 0:4], scalar1=SM[:, 0:1], scalar2=None,
                                                               op0=ALU.is_equal))
                            dv(lambda: nc.vector.tensor_scalar(out=SM[:, 1:2], in0=SM[:, 0:1], scalar1=-1.0, scalar2=None,
                                                               op0=ALU.mult))
                            act(GE[:], LG[:, 0:4], AF.Exp, R_, R_, bias=SM[:, 1:2])
                            dv(lambda: nc.vector.tensor_reduce(out=SM[:, 2:3], in_=GE[:], axis=AX.X, op=ALU.add))
                            dv(lambda: nc.vector.reciprocal(out=SM[:, 3:4], in_=SM[:, 2:3]))
                            dv(lambda: nc.vector.tensor_tensor(
                                out=ET[:].rearrange("p (g e) -> p g e", e=8),
                                in0=LG[:, 4:36].rearrange("p (g e) -> p g e", e=8),
                                in1=GOH[:].unsqueeze(2).broadcast_to([128, 4, 8]), op=ALU.mult))
                            dv(lambda: nc.vector.tensor_reduce(out=ES[:], in_=ET[:].rearrange("p (g e) -> p e g", e=8),
                                                               axis=AX.X, op=ALU.add))
                            dv(lambda: nc.vector.max(out=T8[:], in_=ES[:]))
                            dv(lambda: nc.vector.tensor_scalar(out=SEL[:], in0=ES[:], scalar1=T8[:, 1:2], scalar2=None,
                                                               op0=ALU.is_ge))
                            dv(lambda: nc.vector.tensor_scalar(out=SM[:, 4:5], in0=T8[:, 0:1], scalar1=-1.0, scalar2=None,
                                                               op0=ALU.mult))
                            act(EX[:], ES[:], AF.Exp, R_, R_, bias=SM[:, 4:5])
                            dv(lambda: nc.vector.tensor_tensor(out=EX[:], in0=EX[:], in1=SEL[:], op=ALU.mult))
                            dv(lambda: nc.vector.tensor_reduce(out=SM[:, 5:6], in_=EX[:], axis=AX.X, op=ALU.add))
                            dv(lambda: nc.vector.reciprocal(out=SM[:, 6:7], in_=SM[:, 5:6]))
                            dv(lambda: nc.vector.tensor_tensor(out=SM[:, 7:8], in0=SM[:, 6:7], in1=SM[:, 3:4], op=ALU.mult))
                            dv(lambda: nc.vector.tensor_scalar(out=EX[:], in0=EX[:], scalar1=SM[:, 7:8], scalar2=None,
                                                               op0=ALU.mult))
                            dv(lambda: nc.vector.tensor_tensor(
                                out=WCS[:, gi, :].rearrange("p (g e) -> p g e", e=8),
                                in0=GOH[:].unsqueeze(2).broadcast_to([128, 4, 8]),
                                in1=EX[:].unsqueeze(1).broadcast_to([128, 4, 8]), op=ALU.mult), ["WCS"])
                            dv(lambda: nc.vector.tensor_tensor(
                                out=AST[:, gi, :].rearrange("p (g e) -> p g e", e=8),
                                in0=GOH[:].unsqueeze(2).broadcast_to([128, 4, 8]),
                                in1=SEL[:].unsqueeze(1).broadcast_to([128, 4, 8]), op=ALU.mult), ["AST"])
                    for g4 in range(0, ntiles, 4):
                        m4 = min(4, ntiles - g4)
                        ps, pk = nb()
                        for j in range(m4):
                            mm(ps[:, j * 32:(j + 1) * 32], ONESB, AST[:, g4 + j, :], True, True, ["CSTB", "AST"], [pk])
                        cp(CSS[:, g4:g4 + m4, :], ps[:, 0:m4 * 32].rearrange("p (j e) -> p j e", e=32), [pk], ["CSS"])
                    P.op("dve", lambda: nc.vector.memset(PRE[:, 0, :], 0.0), [], ["PRE"])
                    for j in range(ntiles):
                        tt(PRE[:, j + 1, :], PRE[:, j, :], CSS[:, j, :], ALU.add, ["PRE", "CSS"], ["PRE"])
                    ps, pk = nb()
                    for j in range(ntiles):
                        mm(ps[0:32, 0:2], AST[:, j, :], ONESB[:, 0:2], j == 0, j == ntiles - 1, ["AST", "CSTB"], [pk])
                    cp(CNT[0:32, 0:1], ps[0:32, 0:1], [pk], ["CNT"])
                    tt(NBK[0:32, 0, :], CNT[0:32, 0:1].broadcast_to([32, 36]), CST3[0:32, 0:36], ALU.is_gt,
                       ["CNT", "CST3"], ["NBK"])
                    P.op("dve", lambda: nc.vector.tensor_reduce(out=CNT[0:32, 1:2], in_=NBK[0:32, 0, :], axis=AX.X, op=ALU.add),
                         ["NBK", "CNT"], ["CNT"])
                    ts(CNT[0:32, 2:3], CNT[0:32, 1:2], 128.0, ALU.mult, ["CNT"], ["CNT"])
                    cp(PADB[:, :], CNT[0:32, 2:3].broadcast_to([32, 128]), ["CNT"], ["PADB"])
                    ps, pk = nb()
                    mm(ps[:, 0:32], PADB[:, :], CST3[0:32, 64:96], True, True, ["PADB", "CST3"], [pk])
                    mm(ps[:, 32:64], PADB[:, :], CST3[0:32, 96:128], True, True, ["PADB", "CST3"], [pk])
                    cp(PST[:], ps[:, 0:32], [pk], ["PST"])
                    cp(PEN[:], ps[:, 32:64], [pk], ["PEN"])
                    tt(BLE[:, 0:NB_, :], PEN[:].unsqueeze(1).broadcast_to([128, NB_, 32]),
                       CST3[:, 136:136 + NB_].unsqueeze(2).broadcast_to([128, NB_, 32]), ALU.is_le, ["PEN", "CST3"], ["BLE"])
                    P.op("dve", lambda: nc.vector.tensor_reduce(out=BLKB[:, 0:NB_], in_=BLE[:, 0:NB_, :], axis=AX.X, op=ALU.add),
                         ["BLE"], ["BLKB"])
                    ts(BLKB[:, 0:NB_], BLKB[:, 0:NB_], 31.0, ALU.min, ["BLKB"], ["BLKB"], s2=128.0, op1=ALU.mult)
                    ts(BLKB[:, 0:NB_], BLKB[:, 0:NB_], CST3[:, 129:130], ALU.add, ["BLKB", "CST3"], ["BLKB"],
                       s2=float(l * 4096), op1=ALU.add)
                    cp(IDXW[:, 0:NB_], BLKB[:, 0:NB_], ["BLKB"], ["IDXW"])
                    for g4 in range(0, ntiles, 4):
                        m4 = min(4, ntiles - g4)
                        ps, pk = nb()
                        for j in range(m4):
                            mm(ps[:, j * 32:(j + 1) * 32], TRIS, AST[:, g4 + j, :], True, True, ["CSTB2", "AST"], [pk])
                        for j in range(m4):
                            gi = g4 + j
                            R2 = ["DP1", "EQ", "T8"]
                            tt(DP1[:], ps[:, j * 32:(j + 1) * 32], PRE[:, gi, :], ALU.add, [pk, "PRE"] + R2, R2)
                            tt(DP1[:], DP1[:], PST[:], ALU.add, R2 + ["PST"], R2)
                            stt(DP1[:], DP1[:], 1.0, AST[:, gi, :], ALU.add, ALU.mult, R2 + ["AST"], R2)
                            P.op("dve", lambda: nc.vector.max(out=T8[:], in_=DP1[:]), R2 + ["LG"], R2 + ["LG"])
                            ts(DESTF[:, gi, :], T8[:, 0:2], -1.0, ALU.add, R2, ["DESTF"])
                            for kk in range(2):
                                ts(EQ[:], DP1[:], T8[:, kk:kk + 1], ALU.is_equal, R2, R2)
                                tt(EQ[:], EQ[:], WCS[:, gi, :], ALU.mult, R2 + ["WCS"], R2)
                                P.op("dve", lambda kk=kk, gi=gi: nc.vector.tensor_reduce(
                                    out=WSEL[:, gi, kk:kk + 1], in_=EQ[:], axis=AX.X, op=ALU.add), R2 + ["WSEL"], R2 + ["WSEL"])
                    cp(DEST[:], DESTF[:], ["DESTF"], ["DEST"])
                P.barrier()
                with ExitStack() as s3:
                    WB = [sb(f"WB{j}", [128, 4096], BF16, s3) for j in range(4)]
                    UT = [sb(f"UT{j}", [128, 1024], BF16, s3) for j in range(2)]
                    XS = [sb(f"XS{j}", [128, 1024], BF16, s3) for j in range(2)]
                    XST = [sb(f"XST{j}", [128, 8, 128], BF16, s3) for j in range(2)]
                    H = [sb(f"H{j}", [128, 4, 128], BF16, s3) for j in range(2)]
                    SG = [sb(f"SG{j}", [128, 512], F32, s3) for j in range(2)]
                    YS = [sb(f"YS{j}", [128, 1024], F32, s3) for j in range(2)]
                    ZT = sb("ZT", [128, 1024], BF16, s3)
                    P.op("dve", lambda: nc.vector.memset(ZT[:], 0.0), [], ["ZT"])
                    for bb in range(NB_):
                        P.dma("sp", xs_d[bb * 128:(bb + 1) * 128, :], ZT[:, :], ["ZT"], ["xsd"])
                    for gi in range(ntiles):
                        ut = UT[gi % 2]
                        uk = f"UT{gi % 2}"
                        b = gi // 4 if gi < 16 else 4
                        for half in range(2):
                            ps, pk = nb()
                            for kk in range(4):
                                k = half * 4 + kk
                                mm(ps[:, kk * 128:(kk + 1) * 128], UY[:, k, gi * 128:(gi + 1) * 128], IDB, True, True,
                                   [f"u{k}b{b}", "CSTB"], [pk])
                            cp(ut[:, half * 512:(half + 1) * 512], ps[:, :], [pk], [uk], eng="act" if half else "dve")
                        for kk in range(2):
                            P.dma_fn("pool", lambda sem, ut=ut, gi=gi, kk=kk: nc.gpsimd.indirect_dma_start(
                                out=xs_d, out_offset=bass.IndirectOffsetOnAxis(ap=DEST[:, gi, kk:kk + 1], axis=0),
                                in_=ut[:, :], in_offset=None).then_inc(sem, 16), [uk, "DEST"], ["xsd"])
                    items = []
                    for bb in range(NB_):
                        items += [("g", bb), ("u", bb), ("d", bb)]
                    def load_item(j):
                        if j >= len(items):
                            return
                        kind, bb = items[j]
                        c0_ = {"g": 0, "u": 2, "d": 4}[kind]
                        for hh_ in range(2):
                            P.dma_fn("pool", lambda sem, j=j, bb=bb, c=c0_ + hh_, hh_=hh_: nc.gpsimd.indirect_dma_start(
                                out=WB[j % 4][:, hh_ * 2048:(hh_ + 1) * 2048], out_offset=None, in_=wc_d[c],
                                in_offset=bass.IndirectOffsetOnAxis(ap=IDXW[:, bb:bb + 1], axis=0)).then_inc(sem, 16),
                                ["IDXW"], [f"WB{j % 4}"])

                    for j in range(4):
                        load_item(j)
                    for bb in range(NB_):
                        j0 = 3 * bb
                        WG = WB[j0 % 4][:, :].rearrange("p (k n) -> p k n", n=512)
                        WU = WB[(j0 + 1) % 4][:, :].rearrange("p (k n) -> p k n", n=512)
                        WD = WB[(j0 + 2) % 4][:, :].rearrange("p (k n) -> p k n", n=1024)
                        kg, ku, kd = f"WB{j0 % 4}", f"WB{(j0 + 1) % 4}", f"WB{(j0 + 2) % 4}"
                        xs, xk = XS[bb % 2], f"XS{bb % 2}"
                        xst, xtk = XST[bb % 2], f"XST{bb % 2}"
                        Hb, hk = H[bb % 2], f"H{bb % 2}"
                        ys, yk = YS[bb % 2], f"YS{bb % 2}"
                        P.dma("sp", xs[:, :], xs_d[bb * 128:(bb + 1) * 128, :], ["xsd"], [xk])
                        for half in range(2):
                            ps, pk = nb()
                            for kk in range(4):
                                k = half * 4 + kk
                                mm(ps[:, kk * 128:(kk + 1) * 128], xs[:, k * 128:(k + 1) * 128], IDB, True, True,
                                   [xk, "CSTB"], [pk])
                            cp(xst[:, half * 4:(half + 1) * 4, :], ps[:, :].rearrange("p (k r) -> p k r", r=128), [pk], [xtk],
                               eng="act" if half else "dve")
                        pg, pkg = nb()
                        pu, pku = nb()
                        for fc in range(4):
                            for k in range(8):
                                mm(pg[:, fc * 128:(fc + 1) * 128], WG[:, k, fc * 128:(fc + 1) * 128], xst[:, k, :], k == 0, k == 7,
                                   [kg, xtk], [pkg])
                        for fc in range(4):
                            for k in range(8):
                                mm(pu[:, fc * 128:(fc + 1) * 128], WU[:, k, fc * 128:(fc + 1) * 128], xst[:, k, :], k == 0, k == 7,
                                   [ku, xtk], [pku])
                        sg = SG[bb % 2]
                        act(sg[:, :], pg[:, :], AF.Silu, [pkg], [f"SG{bb % 2}"])
                        tt(Hb[:, :, :], sg[:, :].rearrange("p (f r) -> p f r", r=128), pu[:, :].rearrange("p (f r) -> p f r", r=128),
                           ALU.mult, [f"SG{bb % 2}", pku], [hk])
                        load_item(j0 + 4)
                        load_item(j0 + 5)
                        for half in range(2):
                            py, pky = nb()
                            for fc in range(4):
                                mm(py[:, :], Hb[:, fc, :], WD[:, fc, half * 512:(half + 1) * 512], fc == 0, fc == 3,
                                   [kd, hk], [pky])
                            cp(ys[:, half * 512:(half + 1) * 512], py[:, :], [pky], [yk], eng="act" if half else "dve")
                        load_item(j0 + 6)
                        P.dma("sp", ys_d[bb * 128:(bb + 1) * 128, :], ys[:, :], [yk], ["ysd"])
                P.barrier()
                with ExitStack() as s3:
                    G0 = sb("G0", [128, 1024], F32, s3)
                    G1 = sb("G1", [128, 1024], F32, s3)
                    Z = sb("Z", [128, 1024], F32, s3)
                    for b in range(nblk):
                        t0, n, grp = BLOCKS[b]
                        for k in range(8):
                            xk = XT[:, k, t0:t0 + n]
                            if k % 2 == 0:
                                act(xk, xk, AF.Copy, [f"x{k}b{b}"], [f"x{k}b{b}"], scale=ALPHA)
                            else:
                                ts(xk, xk, ALPHA, ALU.mult, [f"x{k}b{b}"], [f"x{k}b{b}"])
                    for gi in range(ntiles):
                        b = gi // 4 if gi < 16 else 4
                        grp = BLOCKS[b][2]
                        for kk, (G, gk) in enumerate(((G0, "G0"), (G1, "G1"))):
                            P.dma_fn("pool", lambda sem, G=G, gi=gi, kk=kk: nc.gpsimd.indirect_dma_start(
                                out=G[:, :], out_offset=None, in_=ys_d,
                                in_offset=bass.IndirectOffsetOnAxis(ap=DEST[:, gi, kk:kk + 1], axis=0)).then_inc(sem, 16),
                                ["ysd", "DEST"], [gk])
                        ts(Z[:, :], G0[:, :], WSEL[:, gi, 0:1], ALU.mult, ["G0", "WSEL"], ["Z"])
                        stt(Z[:, :], G1[:, :], WSEL[:, gi, 1:2], Z[:, :], ALU.mult, ALU.add, ["G1", "WSEL", "Z"], ["Z"])
                        for half in range(2):
                            ps, pk = nb()
                            for kk in range(4):
                                k = half * 4 + kk
                                P.op("pe", lambda k=k, kk=kk, ps=ps: nc.tensor.transpose(
                                    out=ps[:, kk * 128:(kk + 1) * 128], in_=Z[:, k * 128:(k + 1) * 128], identity=IDF),
                                    ["Z", "CST"], [pk])
                            for kk in range(4):
                                k = half * 4 + kk
                                xk = XT[:, k, gi * 128:(gi + 1) * 128]
                                stt(xk, ps[:, kk * 128:(kk + 1) * 128], mod(l, 5, k, grp), xk, ALU.mult, ALU.add,
                                    [pk, "MOD", f"x{k}b{b}"], [f"x{k}b{b}"])
                    for b in range(nblk):
                        ln_block(l, 1, b, (SQ, MEAN, RSTD))
            P.barrier()

        for l in range(n_layers):
            if stop == "ada":
                break
            lastl = l == DEPTH - 1
            need_ctx = not lastl
            is_dbg_last = (l == n_layers - 1)
            if l % 2 == 0:
                mixer_ab(l, need_ctx)
                wo = ab_w_out_d[l // 2]
            else:
                mixer_c(l, need_ctx)
                wo = c_w_out_d[l // 2]
            raw = is_dbg_last and stop == "mix"
            if is_dbg_last and stop == "y":
                for b in range(5):
                    t0, n, grp = BLOCKS[b]
                    for k in range(8):
                        cp(XT[:, k, t0:t0 + n], UY[:, k, t0:t0 + n], [f"u{k}b{b}"], [f"x{k}b{b}"])
                break
            proj_ln(l, wo, 5 if need_ctx else 4, raw)
            if is_dbg_last and stop in ("mix", "ln1"):
                break
            if SPARSE:
                moe_sparse(l, need_ctx)
            else:
                moe(l, need_ctx)

        for k in range(8):
            P.dma("sp", out_d[k], XT[:, k, :], xkeys(ks=[k]), [f"out{k}"])
        P.finish("sp", [f"out{k}" for k in range(8)])
        print("bass ops:", P.nops, P.cnt)
    return nc


def _fm(v):
    v = np.asarray(v, np.float32)
    lead = v.shape[:-1]
    r = v.reshape(lead + (8, 128))
    r = np.moveaxis(r, -1, 0)
    return np.ascontiguousarray(r)


def _rope_tables(head_dim, per, qscale):
    quarter = head_dim // 4
    row = np.repeat(np.arange(T // 64, dtype=np.float32), 64)
    col = np.tile(np.arange(64, dtype=np.float32), T // 64)
    inv = (np.float32(10000.0) ** (-np.arange(quarter, dtype=np.float32) / np.float32(quarter))).astype(np.float32)
    ang_r = row[:, None] * inv
    ang_c = col[:, None] * inv
    ang = np.concatenate([ang_r, ang_r, ang_c, ang_c], axis=-1)
    cos = np.cos(ang).astype(np.float32).T
    sin = np.sin(ang).astype(np.float32).T
    sign = np.ones((head_dim, 1), np.float32)
    sign[0:quarter] = -1.0
    sign[2 * quarter:3 * quarter] = -1.0
    sin = sin * sign
    rep = 128 // head_dim
    cos = np.tile(cos, (rep, 1))
    sin = np.tile(sin, (rep, 1))
    return np.ascontiguousarray(np.stack([cos * qscale, sin * qscale, cos, sin]).astype(np.float32))


_CONSTS = None


def _consts():
    global _CONSTS
    if _CONSTS is not None:
        return _CONSTS
    c = {}
    c["ropeA"] = _rope_tables(128, 128, np.float32(128 ** -0.5))
    c["ropeC"] = _rope_tables(64, 64, np.float32(0.125))
    ii = np.arange(128)
    cst = np.zeros((128, 1024), np.float32)
    cst[:, 0:128] = np.eye(128, dtype=np.float32)
    cst[:, 128:256] = 1.0 / 1024.0
    cst[:, 256:384] = 1.0 / 128.0
    cst[:, 384:512] = np.where(ii[:, None] <= ii[None, :], -1.0 / 16.0, 0.0)
    cst[:, 512:640] = 1.0
    c["cst"] = cst
    cst2 = np.zeros((128, 256), np.float32)
    cst2[:, 0:128] = np.where(ii[:, None] >= ii[None, :], -1.0 / 16.0, 0.0)
    cst2[:, 128:256] = np.where(ii[:, None] > ii[None, :], -1.0 / 16.0, 0.0)
    c["cst2"] = cst2
    cstb = np.zeros((128, 256), np.float32)
    cstb[:, 0:128] = np.eye(128)
    cstb[:, 128:256] = 1.0
    c["cstb"] = cstb.astype(ml_dtypes.bfloat16)
    m = (ii[:, None] > ii[None, :]).astype(np.uint8)
    c["mask"] = np.ascontiguousarray(np.tile(m, (1, 4)))
    cst3 = np.zeros((128, 256), np.float32)
    cst3[:, 0:36] = (np.arange(36, dtype=np.float32) * 128.0)[None, :]
    e32 = np.arange(32)
    cst3[0:32, 64:96] = (e32[:, None] < e32[None, :]).astype(np.float32)
    cst3[0:32, 96:128] = (e32[:, None] <= e32[None, :]).astype(np.float32)
    cst3[:, 128] = np.arange(128, dtype=np.float32) * 128.0
    cst3[:, 129] = np.arange(128, dtype=np.float32)
    cst3[:, 136:208] = (np.arange(72, dtype=np.float32) * 128.0)[None, :]
    c["cst3"] = cst3
    c["cstb2"] = (ii[:, None] < ii[None, :]).astype(np.float32).astype(ml_dtypes.bfloat16)
    sele = np.zeros((32, 32, 128), np.float32)
    for e in range(32):
        sele[e, e, :] = 1.0
    c["sele"] = sele.reshape(32, 32 * 128).astype(ml_dtypes.bfloat16)
    ret = np.zeros((128, 4, 6, 128), np.float32)
    pos = np.arange(128, dtype=np.float64)
    for h in range(4):
        lf = float(np.log1p(-np.exp2(-np.float32(5.0 + h)), dtype=np.float32))
        lb = float(np.log1p(-np.exp2(-np.float32(5.5 + h)), dtype=np.float32))
        ret[:, h, 0, :] = np.exp(lf * (pos + 1))
        ret[:, h, 1, :] = np.exp(-lf * (pos + 1))
        ret[:, h, 2, :] = np.exp(lf * (127 - pos))
        ret[:, h, 3, :] = np.exp(lb * (127 - pos))
        ret[:, h, 4, :] = np.exp(-lb * (128 - pos))
        ret[:, h, 5, :] = np.exp(lb * pos)
    c["ret"] = ret
    _CONSTS = c
    return c


def _prep_inputs(inputs):
    f = lambda a: np.ascontiguousarray(np.asarray(a, np.float32))
    shared = dict(_consts())
    shared["adab"] = np.ascontiguousarray(np.asarray(inputs["ada_b"], np.float32).reshape(4, 48, 128).transpose(2, 0, 1))
    lnp = np.stack([inputs["ln1_g"], inputs["ln1_b"], inputs["ln2_g"], inputs["ln2_b"]], axis=1)
    shared["lnp"] = np.ascontiguousarray(np.asarray(lnp, np.float32).reshape(4, 4, 8, 128).transpose(3, 0, 1, 2))
    gn = np.concatenate([inputs["ab_gn_a"], inputs["ab_gn_b"]], axis=1)
    shared["gn"] = np.ascontiguousarray(np.asarray(gn, np.float32).reshape(2, 8, 128).transpose(2, 0, 1))
    shared["subg"] = np.ascontiguousarray(np.asarray(inputs["c_subln_g"], np.float32).reshape(2, 8, 128).transpose(2, 0, 1))
    wlr = np.zeros((32, 2, 2, 256), np.float32)
    wlr[0:16, :, 0, :] = np.asarray(inputs["ab_w_lr_f"]).transpose(1, 0, 2)
    wlr[0:16, :, 1, :] = np.asarray(inputs["ab_w_lr_b"]).transpose(1, 0, 2)
    wlr[16, :, 0, :] = np.asarray(inputs["ab_b_lr_f"])
    wlr[16, :, 1, :] = np.asarray(inputs["ab_b_lr_b"])
    shared["wlr"] = wlr
    wr = np.concatenate([inputs["moe_w_grp"], inputs["moe_w_rexp"]], axis=2)
    shared["wr"] = np.ascontiguousarray(np.asarray(wr, np.float32).reshape(4, 8, 128, 36).transpose(2, 0, 1, 3))
    rb = np.concatenate([inputs["moe_b_grp"], inputs["moe_b_rexp"]], axis=1)
    shared["rb"] = np.ascontiguousarray(np.asarray(rb, np.float32)[None])
    lqk = np.stack([inputs["c_lq1"], inputs["c_lk1"], inputs["c_lq2"], inputs["c_lk2"]], axis=1)
    shared["lqk"] = np.ascontiguousarray(np.asarray(lqk, np.float32).transpose(3, 0, 1, 2))
    for nm in ("ada_w", "ab_w_in", "ab_w_out", "c_w_qkv", "c_w_out"):
        shared[nm] = f(inputs[nm])
    if SPARSE:
        for ci, nm in ((0, "moe_w_gate"), (2, "moe_w_up")):
            wp = np.asarray(inputs[nm], np.float32).reshape(4, 32, 8, 128, 512).transpose(0, 1, 3, 2, 4).reshape(4 * 32 * 128, 4096)
            shared[f"wc{ci}"] = np.ascontiguousarray(wp[:, 0:2048])
            shared[f"wc{ci + 1}"] = np.ascontiguousarray(wp[:, 2048:4096])
        wp = np.asarray(inputs["moe_w_down"], np.float32).reshape(4, 32, 4, 128, 1024).transpose(0, 1, 3, 2, 4).reshape(4 * 32 * 128, 4096)
        shared["wc4"] = np.ascontiguousarray(wp[:, 0:2048])
        shared["wc5"] = np.ascontiguousarray(wp[:, 2048:4096])
    else:
        for nm in ("moe_w_gate", "moe_w_up", "moe_w_down"):
            shared[nm] = f(inputs[nm])
    x = np.asarray(inputs["x"], np.float32)
    ctx = np.asarray(inputs["ctx"], np.float32)
    c = np.asarray(inputs["c"], np.float32)
    c_ctx = np.asarray(inputs["c_ctx"], np.float32)
    in_maps = []
    for b in range(8):
        tok = np.concatenate([x[b], ctx[b]], axis=0)
        xT = np.ascontiguousarray(tok.T.reshape(8, 128, NT))
        cc = np.stack([c[b].reshape(8, 128).T, c_ctx.reshape(8, 128).T], axis=-1)
        m = dict(shared)
        m["xT"] = xT
        m["cc"] = np.ascontiguousarray(cc.astype(np.float32))
        in_maps.append(m)
    return in_maps


_NC_CACHE = {}


def run(inputs, n_layers=DEPTH, stop=None, ncores=8):
    key = (n_layers, stop)
    if key not in _NC_CACHE:
        _NC_CACHE[key] = build(n_layers, stop)
    nc = _NC_CACHE[key]
    in_maps = _prep_inputs(inputs)[:ncores]
    if stop in ("ada", "mix", "ln1", "y") and n_layers <= 1:
        for m in in_maps:
            for nm in (["wc%d" % c for c in range(6)] if SPARSE else ["moe_w_gate", "moe_w_up", "moe_w_down"]):
                m.pop(nm)
    res = run_bass_kernel_spmd(nc, in_maps, core_ids=list(range(ncores)))
    outs = [np.asarray(r["outT"]).reshape(D, NT).T for r in res.results]
    return outs


def kernel(**inputs):
    outs = run(inputs)
    return np.ascontiguousarray(np.stack([o[:T] for o in outs], axis=0).astype(np.float32))
```

```python
import math
import numpy as np
import ml_dtypes
from contextlib import ExitStack
import concourse.bass as bass
import concourse.mybir as mybir
from concourse.bass_utils import run_bass_kernel_spmd

F32 = mybir.dt.float32
BF16 = mybir.dt.bfloat16
U8 = mybir.dt.uint8
U32 = mybir.dt.uint32
I32 = mybir.dt.int32
AF = mybir.ActivationFunctionType
ALU = mybir.AluOpType
AX = mybir.AxisListType

D = 1024
T = 2048
TC = 256
NT = T + TC
DEPTH = 4
ALPHA = (2 * DEPTH) ** 0.25
EPS = 1e-5
BLOCKS = [(0, 512, 0), (512, 512, 0), (1024, 512, 0), (1536, 512, 0), (2048, 256, 1)]
ORDER_F = [16, 17] + list(range(16))
ORDER_B = [17, 16] + list(range(15, -1, -1))
AB_IN = 3616
import os as _os
SPARSE = int(_os.environ.get("MOE_SPARSE", "1"))
DBG_HEADS = int(_os.environ.get("DBG_HEADS", "8"))
DBG_B = int(_os.environ.get("DBG_B", "9"))
DBG_C = _os.environ.get("DBG_C", "z")


class Prog:
    def __init__(self, nc, es, ndma=36):
        self.nc = nc
        self.e = dict(pe=nc.tensor, act=nc.scalar, dve=nc.vector, pool=nc.gpsimd, sp=nc.sync)
        self.sem = {k: es.enter_context(nc.semaphore("s_" + k)) for k in self.e}
        self.cnt = {k: 0 for k in self.e}
        self.seen = {k: {} for k in self.e}
        self.st = {}
        self.dq = {}
        for q in ("sp", "pool"):
            self.dq[q] = dict(sems=[es.enter_context(nc.semaphore(f"d_{q}{i}")) for i in range(ndma)],
                              vals=[0] * ndma, idx=0)
        self.nops = 0
        self.bar = es.enter_context(nc.sbuf_tensor("barrier_t", [128, 1], F32))

    def _wait(self, eng, ticks):
        need = {}
        for t in ticks:
            if t is None:
                continue
            key, sem, val = t
            if key == "pe" and eng == "pe":
                continue
            if need.get(key, (None, 0))[1] < val:
                need[key] = (sem, val)
        for key, (sem, val) in need.items():
            if self.seen[eng].get(key, 0) >= val:
                continue
            self.e[eng].wait_ge(sem, val)
            self.seen[eng][key] = val

    def _deps(self, R, W):
        ticks = []
        for k in R:
            s = self.st.get(k)
            if s is not None:
                ticks.append(s[0])
                if k.startswith("ps"):
                    ticks.extend(s[1].values())
        for k in W:
            s = self.st.get(k)
            if s is not None:
                ticks.append(s[0])
                ticks.extend(s[1].values())
        return ticks

    def _update(self, tick, R, W):
        for k in W:
            self.st[k] = [tick, {}]
        for k in R:
            s = self.st.get(k)
            if s is None:
                s = self.st[k] = [None, {}]
            s[1][tick[0]] = tick

    def op(self, eng, fn, R, W):
        self._wait(eng, self._deps(R, W))
        inst = fn()
        inst.then_inc(self.sem[eng], 1)
        self.cnt[eng] += 1
        self.nops += 1
        self._update((eng, self.sem[eng], self.cnt[eng]), R, W)

    def dma(self, q, out, in_, R, W):
        d = self.dq[q]
        i = d["idx"]
        d["idx"] = (i + 1) % len(d["sems"])
        key = ("d", q, i)
        ticks = self._deps(R, W)
        if d["vals"][i] > 0:
            ticks.append((key, d["sems"][i], d["vals"][i]))
        self._wait(q, ticks)
        self.e[q].dma_start(out=out, in_=in_).then_inc(d["sems"][i], 16)
        d["vals"][i] += 16
        self.nops += 1
        self._update((key, d["sems"][i], d["vals"][i]), R, W)

    def dma_fn(self, q, fn, R, W):
        d = self.dq[q]
        i = d["idx"]
        d["idx"] = (i + 1) % len(d["sems"])
        key = ("d", q, i)
        ticks = self._deps(R, W)
        if d["vals"][i] > 0:
            ticks.append((key, d["sems"][i], d["vals"][i]))
        self._wait(q, ticks)
        fn(d["sems"][i])
        d["vals"][i] += 16
        self.nops += 1
        self._update((key, d["sems"][i], d["vals"][i]), R, W)

    def barrier(self):
        ticks = []
        for e in self.e:
            if self.cnt[e] > 0:
                ticks.append((e, self.sem[e], self.cnt[e]))
        for q, d in self.dq.items():
            for i, v in enumerate(d["vals"]):
                if v > 0:
                    ticks.append((("d", q, i), d["sems"][i], v))
        self._wait("dve", ticks)
        inst = self.nc.vector.memset(self.bar[:], 0.0)
        inst.then_inc(self.sem["dve"], 1)
        self.cnt["dve"] += 1
        t = ("dve", self.sem["dve"], self.cnt["dve"])
        for e in ("pe", "act", "pool", "sp"):
            self._wait(e, [t])

    def finish(self, eng, keys):
        self._wait(eng, self._deps(keys, []))


def build(n_layers=DEPTH, stop=None):
    nc = bass.Bass("TRN2", target_bir_lowering=False)

    def din(name, shape, dt=F32):
        return nc.dram_tensor(name, list(shape), dt, kind="ExternalInput").ap()

    xT_d = din("xT", [8, 128, NT])
    cc_d = din("cc", [128, 8, 2])
    adab_d = din("adab", [128, 4, 48])
    lnp_d = din("lnp", [128, 4, 4, 8])
    gn_d = din("gn", [128, 2, 8])
    subg_d = din("subg", [128, 2, 8])
    wlr_d = din("wlr", [32, 2, 2, 256])
    wr_d = din("wr", [128, 4, 8, 36])
    rb_d = din("rb", [1, 4, 36])
    lqk_d = din("lqk", [64, 2, 4, 8])
    ret_d = din("ret", [128, 4, 6, 128])
    ropeA_d = din("ropeA", [4, 128, T])
    ropeC_d = din("ropeC", [4, 128, T])
    cst_d = din("cst", [128, 1024])
    cst2_d = din("cst2", [128, 256])
    cstb_d = din("cstb", [128, 256], BF16)
    mask_d = din("mask", [128, 512], U8)
    cst3_d = din("cst3", [128, 256])
    cstb2_d = din("cstb2", [128, 128], BF16)
    xs_d = nc.dram_tensor("xs_scr", [9216, 1024], BF16, kind="Internal").ap()
    ys_d = nc.dram_tensor("ys_scr", [9216, 1024], F32, kind="Internal").ap()
    sele_d = din("sele", [32, 32 * 128], BF16)
    ada_w_d = din("ada_w", [4, D, 6 * D])
    ab_w_in_d = din("ab_w_in", [2, D, AB_IN])
    ab_w_out_d = din("ab_w_out", [2, D, D])
    c_w_qkv_d = din("c_w_qkv", [2, D, 3 * D])
    c_w_out_d = din("c_w_out", [2, D, D])
    use_moe = not (stop in ("ada", "mix", "ln1", "y") and n_layers <= 1)
    if use_moe and SPARSE:
        wc_d = [din(f"wc{c}", [4 * 32 * 128, 2048]) for c in range(6)]
    elif use_moe:
        wg_d = din("moe_w_gate", [4, 32, D, 512])
        wu_d = din("moe_w_up", [4, 32, D, 512])
        wd_d = din("moe_w_down", [4, 32, 512, D])
    out_d = nc.dram_tensor("outT", [8, 128, NT], F32, kind="ExternalOutput").ap()

    es = ExitStack()
    with es:
        P = Prog(nc, es)

        uid = [0]

        def sb(name, shape, dt=F32, stack=es):
            uid[0] += 1
            return stack.enter_context(nc.sbuf_tensor(f"{name}_{uid[0]}", list(shape), dt))

        PS = [es.enter_context(nc.psum_tensor(f"ps{i}", [128, 512], F32)) for i in range(8)]
        psc = [0]

        def nb():
            i = psc[0]
            psc[0] = (i + 1) % 8
            return PS[i], f"ps{i}"

        def mm(out, lhsT, rhs, start, stop, R, W):
            P.op("pe", lambda: nc.tensor.matmul(out, lhsT=lhsT, rhs=rhs, start=start, stop=stop), R, W)

        def act(out, in_, func, R, W, scale=None, bias=None, accum_out=None):
            kw = {}
            if scale is not None:
                kw["scale"] = scale
            if bias is None and func != AF.Copy:
                sp_, np_ = in_.start_partition(), in_.partition_size()
                bias = ZEROC[sp_:sp_ + np_, 0:1]
                R = list(R) + ["ZEROC"]
            if bias is not None:
                kw["bias"] = bias
            if accum_out is not None:
                kw["accum_out"] = accum_out
            P.op("act", lambda: nc.scalar.activation(out=out, in_=in_, func=func, **kw), R, W)

        def tt(out, a, b, op, R, W, eng="dve"):
            e = nc.vector if eng == "dve" else nc.gpsimd
            P.op(eng, lambda: e.tensor_tensor(out=out, in0=a, in1=b, op=op), R, W)

        def ts(out, a, s1, op0, R, W, s2=None, op1=None, eng="dve"):
            e = nc.vector if eng == "dve" else nc.gpsimd
            if op1 is None:
                P.op(eng, lambda: e.tensor_scalar(out=out, in0=a, scalar1=s1, scalar2=None, op0=op0), R, W)
            else:
                P.op(eng, lambda: e.tensor_scalar(out=out, in0=a, scalar1=s1, scalar2=s2, op0=op0, op1=op1), R, W)

        def stt(out, a, s, b, op0, op1, R, W):
            P.op("dve", lambda: nc.vector.scalar_tensor_tensor(out=out, in0=a, scalar=s, in1=b, op0=op0, op1=op1), R, W)

        def cp(out, in_, R, W, eng="dve"):
            if eng == "act":
                act(out, in_, AF.Copy, R, W)
            else:
                e = nc.vector if eng == "dve" else nc.gpsimd
                P.op(eng, lambda: e.tensor_copy(out=out, in_=in_), R, W)

        XT = sb("XT", [128, 8, NT])
        UY = sb("UY", [128, 8, NT], BF16)
        MOD = sb("MOD", [128, 4, 6, 8, 2])
        LNP = sb("LNP", [128, 4, 4, 8])
        GN = sb("GN", [128, 2, 8])
        SUBG = sb("SUBG", [128, 2, 8])
        RB = sb("RB", [1, 4, 36])
        LAM = sb("LAM", [128, 2, 8])
        CST = sb("CST", [128, 1024])
        CST2 = sb("CST2", [128, 256])
        CSTB = sb("CSTB", [128, 256], BF16)
        MASK = sb("MASK", [128, 512], U8)
        CST3 = sb("CST3", [128, 256])
        CSTB2 = sb("CSTB2", [128, 128], BF16)
        TRIS = CSTB2[:, 0:128]
        EPSC = sb("EPSC", [128, 1])
        ONEC = sb("ONEC", [128, 1])
        ZEROC = sb("ZEROC", [128, 1])
        IDF = CST[:, 0:128]
        ONESD = CST[:, 128:256]
        ONESH = CST[:, 256:384]
        TRIF = CST[:, 384:512]
        ONES1 = CST[0:1, 512:640]
        TRIB = CST2[:, 0:128]
        TRIBP = CST2[:, 128:256]
        IDB = CSTB[:, 0:128]
        ONESB = CSTB[:, 128:256]

        def xkeys(blocks=range(5), ks=range(8)):
            return [f"x{k}b{b}" for k in ks for b in blocks]

        def ukeys(blocks=range(5), ks=range(8)):
            return [f"u{k}b{b}" for k in ks for b in blocks]

        for k in range(8):
            P.dma("sp", XT[:, k, :], xT_d[k], [], xkeys(ks=[k]))
        for (t_, d_, kname) in [(LNP, lnp_d, "LNP"), (GN, gn_d, "GN"), (SUBG, subg_d, "SUBG"),
                                (RB, rb_d, "RB"), (CST, cst_d, "CST"), (CST2, cst2_d, "CST2"), (CSTB, cstb_d, "CSTB"),
                                (MASK, mask_d, "MASK"), (CST3, cst3_d, "CST3"), (CSTB2, cstb2_d, "CSTB2")]:
            P.dma("sp", t_[:], d_, [], [kname])
        P.op("dve", lambda: nc.vector.memset(EPSC[:], EPS), [], ["EPSC"])
        P.op("dve", lambda: nc.vector.memset(ONEC[:], 1.0), [], ["ONEC"])
        P.op("dve", lambda: nc.vector.memset(ZEROC[:], 0.0), [], ["ZEROC"])

        with ExitStack() as s1:
            CC = sb("CC", [128, 8, 2], F32, s1)
            SCB = sb("SCB", [128, 8, 2], BF16, s1)
            ADAB = sb("ADAB", [128, 4, 48], F32, s1)
            WA = [sb(f"WA{i}", [128, 8, 1024], BF16, s1) for i in range(2)]
            LQK = sb("LQK", [64, 2, 4, 8], F32, s1)
            LQP = sb("LQP", [64, 2, 2, 8], F32, s1)
            P.dma("sp", CC[:], cc_d, [], ["CC"])
            P.dma("sp", ADAB[:], adab_d, [], ["ADAB"])
            P.dma("sp", LQK[:], lqk_d, [], ["LQK"])
            act(SCB[:], CC[:], AF.Silu, ["CC"], ["SCB"])
            it = 0
            for l in range(n_layers):
                for j6 in range(6):
                    w = WA[it % 2]
                    wk = f"WA{it % 2}"
                    it += 1
                    src = ada_w_d[l].rearrange("(k p) n -> p k n", p=128)[:, :, j6 * 1024:(j6 + 1) * 1024]
                    P.dma("pool", w[:], src, [], [wk])
                    ps, pk = nb()
                    for jj in range(8):
                        for k in range(8):
                            mm(ps[:, jj * 2:jj * 2 + 2], w[:, k, jj * 128:(jj + 1) * 128], SCB[:, k, :],
                               k == 0, k == 7, [wk, "SCB"], [pk])
                    tt(MOD[:, l, j6, :, :], ps[:, 0:16].rearrange("p (j g) -> p j g", g=2),
                       ADAB[:, l, j6 * 8:(j6 + 1) * 8].unsqueeze(2).broadcast_to([128, 8, 2]), ALU.add,
                       [pk, "ADAB"], ["MOD"])
                for j6 in (1, 4):
                    ts(MOD[:, l, j6, :, :], MOD[:, l, j6, :, :], 1.0, ALU.add, ["MOD"], ["MOD"])
            tt(LQP[:, :, 0, :], LQK[:, :, 0, :], LQK[:, :, 1, :], ALU.mult, ["LQK"], ["LQP"])
            tt(LQP[:, :, 1, :], LQK[:, :, 2, :], LQK[:, :, 3, :], ALU.mult, ["LQP", "LQK"], ["LQP"])
            ps, pk = nb()
            mm(ps[:, 0:32], CST[0:64, 512:640], LQP[:].rearrange("p a b c -> p (a b c)"), True, True,
               ["CST", "LQP"], [pk])
            LE = sb("LE", [128, 32], F32, s1)
            act(LE[:], ps[:, 0:32], AF.Exp, [pk], ["LE"])
            lev = LE[:].rearrange("p (a b c) -> p a b c", a=2, b=2)
            for i in range(2):
                lam_init = 0.8 - 0.6 * math.exp(-0.3 * (2 * i + 1))
                tt(LAM[:, i, :], lev[:, i, 1, :], lev[:, i, 0, :], ALU.subtract, ["LE", "LAM"], ["LAM"])
                ts(LAM[:, i, :], LAM[:, i, :], -lam_init, ALU.add, ["LAM"], ["LAM"])
                ts(SUBG[:, i, :], SUBG[:, i, :], 1.0 - lam_init, ALU.mult, ["SUBG"], ["SUBG"])

        P.barrier()

        def mod(l, which, k, grp):
            return MOD[:, l, which, k, grp:grp + 1]

        def ln_block(l, which_ln, b, scr):
            t0, n, grp = BLOCKS[b]
            SQ, MEAN, RSTD = scr
            psm, pkm = nb()
            pss, pks = nb()
            for k in range(8):
                xk = XT[:, k, t0:t0 + n]
                mm(psm[:, :n], ONESD, xk, k == 0, k == 7, [f"x{k}b{b}", "CST"], [pkm])
                sq = SQ[k % 2]
                act(sq[:, :n], xk, AF.Square, [f"x{k}b{b}"], [f"SQ{k % 2}"])
                mm(pss[:, :n], ONESD, sq[:, :n], k == 0, k == 7, [f"SQ{k % 2}", "CST"], [pks])
            cp(MEAN[:, :n], psm[:, :n], [pkm], ["MEAN"], eng="act")
            act(RSTD[:, :n], psm[:, :n], AF.Square, [pkm], ["RSTD"])
            tt(RSTD[:, :n], pss[:, :n], RSTD[:, :n], ALU.subtract, [pks, "RSTD"], ["RSTD"])
            act(RSTD[:, :n], RSTD[:, :n], AF.Ln, ["RSTD"], ["RSTD"], bias=EPSC[:, 0:1])
            act(RSTD[:, :n], RSTD[:, :n], AF.Exp, ["RSTD"], ["RSTD"], scale=-0.5)
            for k in range(8):
                xk = XT[:, k, t0:t0 + n]
                kk = [f"x{k}b{b}"]
                tt(xk, xk, MEAN[:, :n], ALU.subtract, kk + ["MEAN"], kk)
                tt(xk, xk, RSTD[:, :n], ALU.mult, kk + ["RSTD"], kk)
                act(xk, xk, AF.Identity, kk + ["LNP"], kk, scale=LNP[:, l, 2 * which_ln, k:k + 1],
                    bias=LNP[:, l, 2 * which_ln + 1, k:k + 1])

        def proj_ln(l, w_dram, nblocks, raw):
            with ExitStack() as s2:
                WO = sb("WO", [128, 8, 1024], BF16, s2)
                TMP = [sb(f"TMPO{i}", [128, 512], F32, s2) for i in range(2)]
                SQ = [sb(f"SQ{i}", [128, 512], F32, s2) for i in range(2)]
                MEAN = sb("MEAN", [128, 512], F32, s2)
                RSTD = sb("RSTD", [128, 512], F32, s2)
                P.dma("pool", WO[:], w_dram.rearrange("(k p) n -> p k n", p=128), [], ["WO"])
                for b in range(nblocks):
                    t0, n, grp = BLOCKS[b]
                    for c in range(8):
                        ps, pk = nb()
                        for h in range(8):
                            mm(ps[:, :n], WO[:, h, c * 128:(c + 1) * 128], UY[:, h, t0:t0 + n], h == 0, h == 7,
                               ["WO", f"u{h}b{b}"], [pk])
                        xk = XT[:, c, t0:t0 + n]
                        kk = [f"x{c}b{b}"]
                        if raw:
                            cp(xk, ps[:, :n], [pk], kk, eng="act")
                        else:
                            tmp = TMP[c % 2]
                            act(tmp[:, :n], ps[:, :n], AF.Identity, [pk, "MOD"], [f"TMPO{c % 2}"], scale=mod(l, 2, c, grp))
                            stt(xk, xk, ALPHA, tmp[:, :n], ALU.mult, ALU.add, kk + [f"TMPO{c % 2}"], kk)
                    if not raw:
                        ln_block(l, 0, b, (SQ, MEAN, RSTD))
            P.barrier()

        def make_u(l, b, UB, ub_i, which_s, which_sh):
            t0, n, grp = BLOCKS[b]
            for k in range(8):
                src = XT[:, k, t0:t0 + n]
                dst = UB[ub_i][:, k, :n]
                R = [f"x{k}b{b}", "MOD"]
                W = [f"UB{ub_i}k{k}"]
                if k % 2 == 0:
                    act(dst, src, AF.Identity, R, W, scale=mod(l, which_s, k, grp), bias=mod(l, which_sh, k, grp))
                else:
                    ts(dst, src, mod(l, which_s, k, grp), ALU.mult, R, W, s2=mod(l, which_sh, k, grp), op1=ALU.add)

        def fm_group(UBt, ub_i, n, Wt, wkey, c0, M):
            ps, pk = nb()
            for k in range(8):
                mm(ps[0:M, :n], Wt[:, k, c0:c0 + M], UBt[:, k, :n], k == 0, k == 7, [wkey, f"UB{ub_i}k{k}"], [pk])
            return ps, pk

        def rope_evac(dst, dkey, ps_a, pk_a, ps_b, pk_b, cos_t, sin_t, tkey, n, TR, ti):
            t1 = TR[0]
            t2 = TR[1]
            tt(t1[:, :n], ps_a[:, :n], cos_t, ALU.mult, [pk_a, tkey], ["TR0"])
            tt(t2[:, :n], ps_b[:, :n], sin_t, ALU.mult, [pk_b, tkey], ["TR1"])
            tt(dst, t1[:, :n], t2[:, :n], ALU.add, ["TR0", "TR1"], [dkey])

        def in_proj_head(l, hs, kind, wsrc, cols, Qf, Kf, V, SGN, gn_ap, rope_d, qscale, LRT=None, lr_cols=None,
                         after_block=None, nblocks=5):
            dk = 64 if kind == "B" else 128
            has_rope = kind in ("A", "C")
            has_g = kind in ("A", "B")
            wv = wsrc.rearrange("(k p) n -> p k n", p=128)
            W = hs["W"]
            ofs = {}
            o = 0
            names = ["q", "k", "v"] + (["g"] if has_g else [])
            for nm in names:
                w_ = dk if nm in ("q", "k") else 128
                P.dma("pool", W[:, :, o:o + w_], wv[:, :, cols[nm]:cols[nm] + w_], [], [f"W_{nm}"])
                ofs[nm] = o
                o += w_
            if LRT is not None:
                P.dma("pool", W[:, :, o:o + 32], wv[:, :, lr_cols:lr_cols + 32], [], ["W_lr"])
                ofs["lr"] = o
                o += 32
            if has_rope:
                blk = 32 if kind == "A" else 16
                for nm in ("q", "k"):
                    s_ = W[:, :, ofs[nm]:ofs[nm] + 128].rearrange("p k (a two b) -> p k a two b", two=2, b=blk)
                    d_ = W[:, :, o:o + 128].rearrange("p k (a two b) -> p k a two b", two=2, b=blk)
                    cp(d_[:, :, :, 0, :], s_[:, :, :, 1, :], [f"W_{nm}"], [f"W_{nm}p"])
                    cp(d_[:, :, :, 1, :], s_[:, :, :, 0, :], [f"W_{nm}", f"W_{nm}p"], [f"W_{nm}p"])
                    ofs[nm + "p"] = o
                    o += 128
            UB = hs["UB"]
            TR = hs["TR"]
            ROPE = hs["ROPE"]
            ti = 0
            for b in range(nblocks):
                t0, n, grp = BLOCKS[b]
                ub_i = b % len(UB)
                make_u(l, b, UB, ub_i, 1, 0)
                UBt = UB[ub_i]
                if has_rope and grp == 0:
                    P.dma("sp", ROPE[:, :, :], rope_d[:, :, t0:t0 + n].rearrange("f p t -> p f t"), [], ["ROPE"])
                for nm, dst_f in (("q", Qf), ("k", Kf)):
                    ps, pk = fm_group(UBt, ub_i, n, W, f"W_{nm}", ofs[nm], dk)
                    dst, dkey = dst_f(b)
                    if has_rope and grp == 0:
                        ps2, pk2 = fm_group(UBt, ub_i, n, W, f"W_{nm}p", ofs[nm + "p"], dk)
                        fi = 0 if nm == "q" else 2
                        rope_evac(dst, dkey, ps, pk, ps2, pk2, ROPE[:, fi, :n], ROPE[:, fi + 1, :n], "ROPE", n, TR, ti)
                        ti += 1
                    else:
                        sc = qscale if nm == "q" else 1.0
                        act(dst, ps[0:dk, :n], AF.Copy, [pk], [dkey], scale=sc)
                if has_g:
                    ps, pk = fm_group(UBt, ub_i, n, W, "W_g", ofs["g"], 128)
                    t1 = TR[ti % 2]
                    act(t1[:, :n], ps[:, :n], AF.Silu, [pk], [f"TR{ti % 2}"])
                    ts(SGN[:, t0:t0 + n], t1[:, :n], gn_ap, ALU.mult, [f"TR{ti % 2}", "GN"], [f"sgn_b{b}"])
                    ti += 1
                if LRT is not None:
                    for di in range(2):
                        ps, pk = fm_group(UBt, ub_i, n, W, "W_lr", ofs["lr"] + 16 * di, 16)
                        cp(LRT[di][0:16, :n], ps[0:16, :n], [pk], [f"lrt{di}"], eng="act")
                ps, pk = nb()
                ntile = n // 128
                for tI in range(ntile):
                    for k in range(8):
                        mm(ps[:, tI * 128:(tI + 1) * 128], UBt[:, k, tI * 128:(tI + 1) * 128],
                           W[:, k, ofs["v"]:ofs["v"] + 128], k == 0, k == 7, ["W_v", f"UB{ub_i}k{k}"], [pk])
                c0 = t0 // 128
                cp(V[:, c0:c0 + ntile, :], ps[:, :n].rearrange("p (c d) -> p c d", d=128), [pk], [f"vb{b}"], eng="act")
                if after_block is not None:
                    after_block(b)

        def head_norm(ps_o, pk_o, n, center, hs, out_ap, outkey, post_ap, postkeys, scale_ap=None, src_sb=None):
            OSB, SQh, RS = hs["OSB"], hs["SQh"], hs["RS"]
            M2 = SQh
            if src_sb is None:
                cp(OSB[:, :n], ps_o[:, :n], [pk_o], ["OSB"], eng="act")
                act(SQh[:, :n], ps_o[:, :n], AF.Square, [pk_o], ["SQh"])
            else:
                act(SQh[:, :n], OSB[:, :n], AF.Square, ["OSB"], ["SQh"])
            pss, pks = nb()
            mm(pss[:, :n], ONESH, SQh[:, :n], True, True, ["SQh", "CST"], [pks])
            if center:
                psm, pkm = nb()
                mm(psm[:, :n], ONESH, OSB[:, :n], True, True, ["OSB", "CST"], [pkm])
                act(M2[:, :n], psm[:, :n], AF.Square, [pkm, "SQh"], ["SQh"])
                tt(RS[:, :n], pss[:, :n], M2[:, :n], ALU.subtract, [pks, "SQh"], ["RS"])
                tt(OSB[:, :n], OSB[:, :n], psm[:, :n], ALU.subtract, ["OSB", pkm], ["OSB"])
                act(RS[:, :n], RS[:, :n], AF.Ln, ["RS"], ["RS"], bias=EPSC[:, 0:1])
            else:
                act(RS[:, :n], pss[:, :n], AF.Ln, [pks], ["RS"], bias=EPSC[:, 0:1])
            act(RS[:, :n], RS[:, :n], AF.Exp, ["RS"], ["RS"], scale=-0.5)
            if scale_ap is None:
                tt(OSB[:, :n], OSB[:, :n], RS[:, :n], ALU.mult, ["OSB", "RS"], ["OSB"])
                tt(out_ap, OSB[:, :n], post_ap, ALU.mult, ["OSB"] + postkeys, [outkey])
            else:
                stt(out_ap, OSB[:, :n], scale_ap, RS[:, :n], ALU.mult, ALU.mult, ["OSB", "RS"] + postkeys, [outkey])

        def mixer_ab(l, need_ctx):
            i = l // 2
            wsrc = ab_w_in_d[i]
            with ExitStack() as s2:
                hs = dict(
                    W=sb("Wh", [128, 8, 768], BF16, s2),
                    UB=[sb("UB0", [128, 8, 512], BF16, s2)],
                    TR=[sb(f"TR{j}", [128, 512], F32, s2) for j in range(2)],
                    OSB=sb("OSB", [128, 512], F32, s2), SQh=sb("SQh", [128, 512], F32, s2),
                    RS=sb("RS", [128, 512], F32, s2),
                )
                Qt = sb("Qt", [128, 512], BF16, s2)
                Kt = sb("Kt", [128, 512], BF16, s2)
                V = sb("V", [128, 18, 128], BF16, s2)
                SGN = sb("SGN", [128, NT], BF16, s2)
                QDF = sb("QDF", [128, NT], BF16, s2)
                QDB = sb("QDB", [128, NT], BF16, s2)
                ATT = sb("ATT", [128, NT], BF16, s2)
                SFb = sb("SFb", [128, 18, 128], BF16, s2)
                SBb = sb("SBb", [128, 18, 128], BF16, s2)
                CUR = [sb(f"CUR{j}", [128, 128], F32, s2) for j in range(4)]
                KD = [sb(f"KD{j}", [128, 512], BF16, s2) for j in range(4)]
                KTT = [sb(f"KTT{j}", [128, 4, 128], BF16, s2) for j in range(2)]

                def Qf(b):
                    return Qt[:, :BLOCKS[b][1]], "qt"

                def Kf(b):
                    return Kt[:, :BLOCKS[b][1]], "kt"

                def Qf64(b):
                    return Qt[0:64, :BLOCKS[b][1]], "qt"

                def Kf64(b):
                    return Kt[0:64, :BLOCKS[b][1]], "kt"

                def run_head(h, ph):
                    isA = h < 4
                    dk = 128 if isA else 64
                    hh = h if isA else h - 4
                    if isA:
                        RET = ph["RET"]
                        P.dma("sp", RET[:], ret_d[:, hh, :, :], [], ["RET"])
                    else:
                        LRT, WLRb, LS, LS2, EXS, TAB, TOT, DEC = (ph[k_] for k_ in ("LRT", "WLRb", "LS", "LS2", "EXS", "TAB", "TOT", "DEC"))
                    P.op("dve", lambda: nc.vector.memset(SFb[:, 16, :], 0.0), [], ["SFb"])
                    P.op("dve", lambda: nc.vector.memset(SBb[:, 17, :], 0.0), [], ["SBb"])

                    def pass1(b):
                        t0, n, grp = BLOCKS[b]
                        nch = n // 128
                        c0 = t0 // 128
                        if (not isA) and DBG_B < 1:
                            return
                        qv = Qt[0:dk, :n]
                        kv = Kt[0:dk, :n]
                        if isA:
                            tabs = [RET[:, j, :].unsqueeze(1).broadcast_to([128, nch, 128]) for j in range(6)]
                            tkeys = ["RET"]

                            def rr(ap):
                                return ap.rearrange("p (c d) -> p c d", d=128)
                        else:
                            psg, pkg = nb()
                            for c in range(nch):
                                for di in range(2):
                                    mm(psg[:, c * 128 + di * 64:c * 128 + di * 64 + 64],
                                       LRT[di][:, c * 128:(c + 1) * 128], WLRb[:, di, hh * 64:(hh + 1) * 64],
                                       True, True, [f"lrt{di}", "WLRb"], [pkg])
                            act(EXS[:, :n], psg[:, :n], AF.Exp, [pkg], ["EXS"], scale=-1.0)
                            ex4 = EXS[:, :n].rearrange("p (c a d) -> p c a d", a=2, d=64)
                            act(LS[:, 0:nch, :, :], ex4, AF.Ln, ["EXS"], ["LS"], bias=ONEC[:, 0:1])
                            act(LS2[:, 0:nch, 0, :], ex4[:, :, 1, :], AF.Ln, ["EXS"], ["LS2"], bias=ONEC[:, 0:1])
                            act(LS2[:, 0:nch, 1, :], ex4[:, :, 0, :], AF.Ln, ["EXS", "LS2"], ["LS2"], bias=ONEC[:, 0:1])
                            if DBG_C < "b":
                                return
                            psf, pkf = nb()
                            psb, pkb = nb()
                            psp, pkp = nb()
                            for c in range(nch):
                                mm(psf[:, c * 128:(c + 1) * 128], LS[:, c, :, :].rearrange("p a d -> p (a d)"), TRIF, True, True, ["LS", "CST"], [pkf])
                            for c in range(nch):
                                mm(psb[:, c * 128:(c + 1) * 128], LS2[:, c, :, :].rearrange("p a d -> p (a d)"), TRIB, True, True, ["LS2", "CST2"], [pkb])
                            for c in range(nch):
                                mm(psp[:, c * 128:(c + 1) * 128], LS2[:, c, :, :].rearrange("p a d -> p (a d)"), TRIBP, True, True, ["LS2", "CST2"], [pkp])
                            if DBG_C < "c":
                                return
                            f3 = psf[0:64, :n].rearrange("p (c d) -> p c d", d=128)
                            b3 = psb[0:64, :n].rearrange("p (c d) -> p c d", d=128)
                            cp(TOT[0:64, 0:nch, 0:1], f3[:, :, 127:128], [pkf], ["TOT"])
                            cp(TOT[0:64, 0:nch, 1:2], b3[:, :, 0:1], [pkb, "TOT"], ["TOT"])
                            if DBG_C < "d":
                                return
                            act(TAB[0][0:64, :n], psf[0:64, :n], AF.Exp, [pkf, "TOT"], ["TAB0"])
                            act(TAB[1][0:64, :n], psf[0:64, :n], AF.Exp, [pkf, "TOT"], ["TAB1"], scale=-1.0)
                            act(TAB[3][0:64, :n], psp[0:64, :n], AF.Exp, [pkp, "TOT"], ["TAB3"])
                            act(TAB[4][0:64, :n], psb[0:64, :n], AF.Exp, [pkb, "TOT"], ["TAB4"], scale=-1.0)
                            if DBG_C < "e":
                                return
                            for c in range(nch):
                                act(TAB[2][0:64, c * 128:(c + 1) * 128], psf[0:64, c * 128:(c + 1) * 128], AF.Exp,
                                    [pkf, "TOT"], ["TAB2"], scale=-1.0, bias=TOT[0:64, c, 0:1])
                                act(TAB[5][0:64, c * 128:(c + 1) * 128], psb[0:64, c * 128:(c + 1) * 128], AF.Exp,
                                    [pkb, "TOT"], ["TAB5"], scale=-1.0, bias=TOT[0:64, c, 1:2])
                            act(DEC[0:64, c0:c0 + nch, :], TOT[0:64, 0:nch, :], AF.Exp, ["TOT"], ["DEC"])
                            tabs = [TAB[j][0:64, :n] for j in range(6)]
                            tkeys = [f"TAB{j}" for j in range(6)]

                            def rr(ap):
                                return ap
                        if (not isA) and DBG_B < 2:
                            return
                        tt(rr(QDF[0:dk, t0:t0 + n]), rr(qv), tabs[0], ALU.mult, ["qt"] + tkeys, [f"qdf{b}"])
                        tt(rr(QDB[0:dk, t0:t0 + n]), rr(qv), tabs[3], ALU.mult, ["qt"] + tkeys, [f"qdb{b}"])
                        tt(rr(KD[0][0:dk, :n]), rr(kv), tabs[1], ALU.mult, ["kt"] + tkeys, ["KD0"])
                        tt(rr(KD[1][0:dk, :n]), rr(kv), tabs[4], ALU.mult, ["kt"] + tkeys, ["KD1"])
                        tt(rr(KD[2][0:dk, :n]), rr(kv), tabs[2], ALU.mult, ["kt"] + tkeys, ["KD2"])
                        tt(rr(KD[3][0:dk, :n]), rr(kv), tabs[5], ALU.mult, ["kt"] + tkeys, ["KD3"])
                        psa, pka = nb()
                        psb2, pkb2 = nb()
                        for c in range(nch):
                            sl = slice(c * 128, (c + 1) * 128)
                            gsl = slice(t0 + c * 128, t0 + (c + 1) * 128)
                            mm(psa[:, sl], KD[0][0:dk, sl], QDF[0:dk, gsl], True, True, ["KD0", f"qdf{b}"], [pka])
                            mm(psb2[:, sl], KD[1][0:dk, sl], QDB[0:dk, gsl], True, True, ["KD1", f"qdb{b}"], [pkb2])
                        cp(ATT[:, t0:t0 + n], psa[:, :n], [pka], [f"att{b}"], eng="act")
                        P.op("dve", lambda: nc.vector.copy_predicated(out=ATT[:, t0:t0 + n], mask=MASK[:, :n],
                                                                       data=psb2[:, :n]),
                             [pkb2, "MASK", f"att{b}"], [f"att{b}"])
                        if (not isA) and DBG_B < 3:
                            return
                        for di in range(2):
                            psk, pkk = nb()
                            for c in range(nch):
                                mm(psk[:, c * 128:c * 128 + dk], KD[2 + di][0:dk, c * 128:(c + 1) * 128], IDB[0:dk, 0:dk],
                                   True, True, [f"KD{2 + di}", "CSTB"], [pkk])
                            cp(KTT[di][:, 0:nch, 0:dk], psk[:, :n].rearrange("p (c d) -> p c d", d=128)[:, :, 0:dk],
                               [pkk], [f"KTT{di}"], eng="act")
                            psd, pkd = nb()
                            for c in range(nch):
                                mm(psd[0:dk, c * 128:(c + 1) * 128], KTT[di][:, c, 0:dk], V[:, c0 + c, :], True, True,
                                   [f"KTT{di}", f"vb{b}"], [pkd])
                            d3 = psd[0:dk, :n].rearrange("p (c d) -> p c d", d=128)
                            if di == 0:
                                if grp == 0:
                                    m = nch if c0 + nch < 16 else nch - 1
                                    cp(SFb[0:dk, c0 + 1:c0 + 1 + m, :], d3[:, 0:m, :], [pkd], ["SFb"])
                                else:
                                    cp(SFb[0:dk, 17, :], d3[:, 0, :], [pkd], ["SFb"])
                                    cp(SFb[0:dk, 0, :], d3[:, 1, :], [pkd], ["SFb"])
                            else:
                                if c0 == 0:
                                    cp(SBb[0:dk, 0:nch - 1, :], d3[:, 1:nch, :], [pkd], ["SBb"])
                                else:
                                    cp(SBb[0:dk, c0 - 1:c0 - 1 + nch, :], d3[:, :, :], [pkd], ["SBb"])

                    if isA:
                        cols = dict(q=hh * 128, k=512 + hh * 128, v=1024 + hh * 128, g=1536 + hh * 128)
                        hs["ROPE"] = ph["ROPE"]
                        in_proj_head(l, hs, "A", wsrc, cols, Qf, Kf, V, SGN, GN[:, i, h:h + 1], ropeA_d, dk ** -0.5,
                                     after_block=pass1)
                    else:
                        cols = dict(q=2048 + hh * 64, k=2304 + hh * 64, v=2560 + hh * 128, g=3072 + hh * 128)
                        hs["ROPE"] = None
                        in_proj_head(l, hs, "B", wsrc, cols, Qf64, Kf64, V, SGN, GN[:, i, h:h + 1], None, dk ** -0.5,
                                     LRT=LRT, lr_cols=3584, after_block=pass1)
                    if (not isA) and DBG_B < 4:
                        return
                    for di, (ST, order, skey) in enumerate(((SFb, ORDER_F, "SFb"), (SBb, ORDER_B, "SBb"))):
                        c_a, c_b = CUR[2 * di], CUR[2 * di + 1]
                        ka, kb_ = f"CUR{2 * di}", f"CUR{2 * di + 1}"
                        P.op("dve", lambda c_a=c_a: nc.vector.memset(c_a[:], 0.0), [], [ka])
                        for oi in range(17):
                            nn, nx = order[oi], order[oi + 1]
                            if isA:
                                g_ = 1.0 - 2.0 ** (-((5.0 if di == 0 else 5.5) + hh))
                                sc_ = float(np.float32(g_) ** 128)
                            else:
                                sc_ = DEC[0:64, nn, di:di + 1]
                            stt(c_b[0:dk, :], c_a[0:dk, :], sc_, ST[0:dk, nx, :], ALU.mult, ALU.add,
                                [ka, skey] + ([] if isA else ["DEC"]), [kb_])
                            cp(ST[0:dk, nx, :], c_b[0:dk, :], [kb_], [skey], eng="act")
                            c_a, c_b, ka, kb_ = c_b, c_a, kb_, ka
                    if (not isA) and DBG_B < 5:
                        return
                    for b in range(5 if need_ctx else 4):
                        t0, n, grp = BLOCKS[b]
                        nch = n // 128
                        c0 = t0 // 128
                        pso, pko = nb()
                        for c in range(nch):
                            sl = slice(c * 128, (c + 1) * 128)
                            gsl = slice(t0 + c * 128, t0 + (c + 1) * 128)
                            mm(pso[:, sl], V[:, c0 + c, :], ATT[:, gsl], True, False, [f"vb{b}", f"att{b}"], [pko])
                            mm(pso[:, sl], SFb[0:dk, c0 + c, :], QDF[0:dk, gsl], False, False, ["SFb", f"qdf{b}"], [pko])
                            mm(pso[:, sl], SBb[0:dk, c0 + c, :], QDB[0:dk, gsl], False, True, ["SBb", f"qdb{b}"], [pko])
                        head_norm(pso, pko, n, isA, hs, UY[:, h, t0:t0 + n], f"u{h}b{b}", SGN[:, t0:t0 + n], [f"sgn_b{b}"])

                with ExitStack() as s3:
                    ph = dict(ROPE=sb("ROPE", [128, 4, 512], F32, s3), RET=sb("RET", [128, 6, 128], F32, s3))
                    for h in range(min(4, DBG_HEADS)):
                        run_head(h, ph)
                P.barrier()
                with ExitStack() as s3:
                    WLR = sb("WLR", [32, 2, 256], F32, s3)
                    ph = dict(
                        LRT=[sb(f"LRT{j}", [32, 512], BF16, s3) for j in range(2)],
                        WLRb=sb("WLRb", [32, 2, 256], BF16, s3),
                        LS=sb("LS", [128, 4, 2, 64], F32, s3),
                        LS2=sb("LS2", [128, 4, 2, 64], F32, s3),
                        EXS=sb("EXS", [128, 512], F32, s3),
                        TAB=[sb(f"TAB{j}", [128, 512], BF16, s3) for j in range(6)],
                        TOT=sb("TOT", [128, 4, 2], F32, s3),
                        DEC=sb("DEC", [128, 18, 2], F32, s3),
                    )
                    P.dma("sp", WLR[:], wlr_d[:, i, :, :], [], ["WLR"])
                    cp(ph["WLRb"][:], WLR[:], ["WLR"], ["WLRb"])
                    for di in range(2):
                        P.op("dve", lambda di=di: nc.vector.memset(ph["LRT"][di][:], 1.0), [], [f"lrt{di}"])
                    for h in range(4, min(8, DBG_HEADS)):
                        run_head(h, ph)
            P.barrier()

        def mixer_c(l, need_ctx):
            i = l // 2
            wsrc = c_w_qkv_d[i]
            with ExitStack() as s2:
                hs = dict(
                    W=sb("Wh", [128, 8, 640], BF16, s2),
                    UB=[sb(f"UB{j}", [128, 8, 512], BF16, s2) for j in range(2)],
                    TR=[sb(f"TR{j}", [128, 512], F32, s2) for j in range(2)],
                    ROPE=sb("ROPE", [128, 4, 512], F32, s2),
                    OSB=sb("OSB", [128, 512], F32, s2), SQh=sb("SQh", [128, 512], F32, s2),
                    RS=sb("RS", [128, 512], F32, s2),
                )
                Q = sb("Q", [128, NT], BF16, s2)
                K = sb("K", [128, NT], BF16, s2)
                V = sb("V", [128, 18, 128], BF16, s2)
                E = [sb(f"E{j}", [128, 512], BF16, s2) for j in range(4)]
                R1 = sb("R1", [128, 512], F32, s2)
                R2 = sb("R2", [128, 512], F32, s2)
                A1 = sb("A1", [128, 512], F32, s2)
                for h in range(8):
                    cols = dict(q=h * 128, k=1024 + h * 128, v=2048 + h * 128)
                    in_proj_head(l, hs, "C", wsrc, cols,
                                 lambda b: (Q[:, BLOCKS[b][0]:BLOCKS[b][0] + BLOCKS[b][1]], f"qb{b}"),
                                 lambda b: (K[:, BLOCKS[b][0]:BLOCKS[b][0] + BLOCKS[b][1]], f"kb{b}"),
                                 V, None, None, ropeC_d, 0.125)
                    for b in range(5 if need_ctx else 4):
                        t0, n, grp = BLOCKS[b]
                        kchunks = list(range(18)) if grp == 0 else [16, 17]
                        acc = [(PS[j], f"ps{j}") for j in range(4)]
                        for ci, c in enumerate(kchunks):
                            first, last = ci == 0, ci == len(kchunks) - 1
                            kb_ = c // 4 if c < 16 else 4
                            ksl = slice(c * 128, (c + 1) * 128)
                            for comp in range(2):
                                sj = 4 + 2 * (ci % 2) + comp
                                pss, pks = PS[sj], f"ps{sj}"
                                rsl = slice(comp * 64, (comp + 1) * 64)
                                mm(pss[:, :n], K[rsl, ksl], Q[rsl, t0:t0 + n], True, True, [f"kb{kb_}", f"qb{b}"], [pks])
                                ej = 2 * (ci % 2) + comp
                                act(E[ej][:, :n], pss[:, :n], AF.Exp, [pks], [f"E{ej}"])
                                po, pko = acc[2 * comp]
                                pd, pkd = acc[2 * comp + 1]
                                mm(po[:, :n], V[:, c, :], E[ej][:, :n], first, last, [f"vb{kb_}", f"E{ej}"], [pko])
                                mm(pd[:, :n], ONESB, E[ej][:, :n], first, last, ["CSTB", f"E{ej}"], [pkd])
                        act(R1[:, :n], acc[1][0][:, :n], AF.Ln, [acc[1][1]], ["R1"])
                        act(R1[:, :n], R1[:, :n], AF.Exp, ["R1"], ["R1"], scale=-1.0)
                        act(R2[:, :n], acc[3][0][:, :n], AF.Ln, [acc[3][1]], ["R2"])
                        act(R2[:, :n], R2[:, :n], AF.Exp, ["R2"], ["R2"], scale=-1.0)
                        tt(A1[:, :n], acc[0][0][:, :n], R1[:, :n], ALU.mult, [acc[0][1], "R1"], ["A1"])
                        tt(R2[:, :n], acc[2][0][:, :n], R2[:, :n], ALU.mult, [acc[2][1], "R2"], ["R2"])
                        stt(hs["OSB"][:, :n], R2[:, :n], LAM[:, i, h:h + 1], A1[:, :n], ALU.mult, ALU.add,
                            ["R2", "A1", "LAM"], ["OSB"])
                        psc[0] = 4
                        head_norm(None, None, n, False, hs, UY[:, h, t0:t0 + n], f"u{h}b{b}", None, ["SUBG"],
                                  scale_ap=SUBG[:, i, h:h + 1], src_sb=True)
                        psc[0] = 4
            P.barrier()

        def moe(l, need_ctx):
            nblk = 5 if need_ctx else 4
            with ExitStack() as s2:
                WB = [sb(f"WB{j}", [128, 4096], BF16, s2) for j in range(4)]
                WCT = sb("WCT", [32, NT], BF16, s2)
                SELE = sb("SELE", [32, 32 * 128], BF16, s2)
                BC = [sb(f"BC{j}", [128, NT], BF16, s2) for j in range(2)]
                H = [sb(f"H{j}", [128, 4, 512], BF16, s2) for j in range(2)]
                SG = [sb(f"SG{j}", [128, 512], F32, s2) for j in range(2)]
                TF = [sb(f"TF{j}", [128, 512], F32, s2) for j in range(3)]
                SQ = [sb(f"SQ{j}", [128, 512], F32, s2) for j in range(2)]
                MEAN = sb("MEAN", [128, 512], F32, s2)
                RSTD = sb("RSTD", [128, 512], F32, s2)
                LG = sb("LG", [128, 36], F32, s2)
                SM = sb("SM", [128, 16], F32, s2)
                GOH = sb("GOH", [128, 4], F32, s2)
                GE = sb("GE", [128, 4], F32, s2)
                ET = sb("ET", [128, 32], F32, s2)
                ES = sb("ES", [128, 8], F32, s2)
                T8 = sb("T8", [128, 8], F32, s2)
                SEL = sb("SEL", [128, 8], F32, s2)
                EX = sb("EX", [128, 8], F32, s2)
                WC = sb("WC", [128, 32], F32, s2)
                P.dma("sp", SELE[:], sele_d, [], ["SELE"])
                WR = sb("WRl", [128, 8, 36], F32, s2)
                P.dma("sp", WR[:], wr_d[:, l, :, :], [], ["WR"])
                items = []
                for e in range(32):
                    items.append(("g", e, wg_d[l, e].rearrange("(k p) n -> p k n", p=128)))
                    items.append(("u", e, wu_d[l, e].rearrange("(k p) n -> p k n", p=128)))
                    items.append(("d", e, wd_d[l, e].rearrange("(k p) n -> p k n", p=128)))

                def load_item(j):
                    if j >= len(items):
                        return
                    kind, e, src = items[j]
                    if kind == "d":
                        dst = WB[j % 4][:, :].rearrange("p (k n) -> p k n", n=1024)
                    else:
                        dst = WB[j % 4][:, :].rearrange("p (k n) -> p k n", n=512)
                    P.dma("pool", dst, src, [], [f"WB{j % 4}"])

                for j in range(4):
                    load_item(j)
                ti = 0
                for b in range(nblk):
                    t0, n, grp = BLOCKS[b]
                    ntile = n // 128
                    pr = [nb() for _ in range(ntile)]
                    for k in range(8):
                        tf = TF[ti % 3]
                        tk = f"TF{ti % 3}"
                        ti += 1
                        act(tf[:, :n], XT[:, k, t0:t0 + n], AF.Identity, [f"x{k}b{b}", "MOD"], [tk],
                            scale=mod(l, 4, k, grp), bias=mod(l, 3, k, grp))
                        cp(UY[:, k, t0:t0 + n], tf[:, :n], [tk], [f"u{k}b{b}"])
                        for tI in range(ntile):
                            mm(pr[tI][0][:, 0:36], tf[:, tI * 128:(tI + 1) * 128], WR[:, k, :], k == 0, False,
                               [tk, "WR"], [pr[tI][1]])
                    for tI in range(ntile):
                        ps, pk = pr[tI]
                        mm(ps[:, 0:36], ONES1, RB[0:1, l, :], False, True, ["CST", "RB"], [pk])
                        cp(LG[:], ps[:, 0:36], [pk], ["LG"])
                        R_ = ["LG", "SM", "GOH", "GE", "ET", "ES", "T8", "SEL", "EX", "WC"]

                        def dv(fn):
                            P.op("dve", fn, R_, R_)
                        dv(lambda: nc.vector.tensor_reduce(out=SM[:, 0:1], in_=LG[:, 0:4], axis=AX.X, op=ALU.max))
                        dv(lambda: nc.vector.tensor_scalar(out=GOH[:], in0=LG[:, 0:4], scalar1=SM[:, 0:1], scalar2=None,
                                                           op0=ALU.is_equal))
                        dv(lambda: nc.vector.tensor_scalar(out=SM[:, 1:2], in0=SM[:, 0:1], scalar1=-1.0, scalar2=None,
                                                           op0=ALU.mult))
                        act(GE[:], LG[:, 0:4], AF.Exp, R_, R_, bias=SM[:, 1:2])
                        dv(lambda: nc.vector.tensor_reduce(out=SM[:, 2:3], in_=GE[:], axis=AX.X, op=ALU.add))
                        dv(lambda: nc.vector.reciprocal(out=SM[:, 3:4], in_=SM[:, 2:3]))
                        dv(lambda: nc.vector.tensor_tensor(
                            out=ET[:].rearrange("p (g e) -> p g e", e=8),
                            in0=LG[:, 4:36].rearrange("p (g e) -> p g e", e=8),
                            in1=GOH[:].unsqueeze(2).broadcast_to([128, 4, 8]), op=ALU.mult))
                        dv(lambda: nc.vector.tensor_reduce(out=ES[:], in_=ET[:].rearrange("p (g e) -> p e g", e=8),
                                                           axis=AX.X, op=ALU.add))
                        dv(lambda: nc.vector.max(out=T8[:], in_=ES[:]))
                        dv(lambda: nc.vector.tensor_scalar(out=SEL[:], in0=ES[:], scalar1=T8[:, 1:2], scalar2=None,
                                                           op0=ALU.is_ge))
                        dv(lambda: nc.vector.tensor_scalar(out=SM[:, 4:5], in0=T8[:, 0:1], scalar1=-1.0, scalar2=None,
                                                           op0=ALU.mult))
                        act(EX[:], ES[:], AF.Exp, R_, R_, bias=SM[:, 4:5])
                        dv(lambda: nc.vector.tensor_tensor(out=EX[:], in0=EX[:], in1=SEL[:], op=ALU.mult))
                        dv(lambda: nc.vector.tensor_reduce(out=SM[:, 5:6], in_=EX[:], axis=AX.X, op=ALU.add))
                        dv(lambda: nc.vector.reciprocal(out=SM[:, 6:7], in_=SM[:, 5:6]))
                        dv(lambda: nc.vector.tensor_tensor(out=SM[:, 7:8], in0=SM[:, 6:7], in1=SM[:, 3:4], op=ALU.mult))
                        dv(lambda: nc.vector.tensor_scalar(out=EX[:], in0=EX[:], scalar1=SM[:, 7:8], scalar2=None,
                                                           op0=ALU.mult))
                        dv(lambda: nc.vector.tensor_tensor(
                            out=WC[:].rearrange("p (g e) -> p g e", e=8),
                            in0=GOH[:].unsqueeze(2).broadcast_to([128, 4, 8]),
                            in1=EX[:].unsqueeze(1).broadcast_to([128, 4, 8]), op=ALU.mult))
                        pt, pkt = nb()
                        P.op("pe", lambda: nc.tensor.transpose(out=pt[0:32, 0:128], in_=WC[:], identity=IDF),
                             R_ + ["CST"], [pkt])
                        cp(WCT[:, t0 + tI * 128:t0 + (tI + 1) * 128], pt[0:32, 0:128], [pkt], [f"wct{b}"], eng="act")
                for b in range(nblk):
                    t0, n, grp = BLOCKS[b]
                    for k in range(8):
                        xk = XT[:, k, t0:t0 + n]
                        if k % 2 == 0:
                            act(xk, xk, AF.Copy, [f"x{k}b{b}"], [f"x{k}b{b}"], scale=ALPHA)
                        else:
                            ts(xk, xk, ALPHA, ALU.mult, [f"x{k}b{b}"], [f"x{k}b{b}"])
                hi = 0
                for e in range(32):
                    j0 = 3 * e
                    WG = WB[j0 % 4][:, :].rearrange("p (k n) -> p k n", n=512)
                    WU = WB[(j0 + 1) % 4][:, :].rearrange("p (k n) -> p k n", n=512)
                    WD = WB[(j0 + 2) % 4][:, :].rearrange("p (k n) -> p k n", n=1024)
                    kg, ku, kd = f"WB{j0 % 4}", f"WB{(j0 + 1) % 4}", f"WB{(j0 + 2) % 4}"
                    bc = BC[e % 2]
                    bck = f"BC{e % 2}"
                    for b in range(nblk):
                        t0, n, grp = BLOCKS[b]
                        ps, pk = nb()
                        mm(ps[:, :n], SELE[:, e * 128:(e + 1) * 128], WCT[:, t0:t0 + n], True, True, ["SELE", f"wct{b}"], [pk])
                        cp(bc[:, t0:t0 + n], ps[:, :n], [pk], [bck + f"b{b}"], eng="act")
                    for b in range(nblk):
                        t0, n, grp = BLOCKS[b]
                        Hb = H[hi % 2]
                        hk = f"H{hi % 2}"
                        hi += 1
                        for fc in range(4):
                            pg, pkg = nb()
                            pu, pku = nb()
                            for k in range(8):
                                mm(pg[:, :n], WG[:, k, fc * 128:(fc + 1) * 128], UY[:, k, t0:t0 + n], k == 0, k == 7,
                                   [kg, f"u{k}b{b}"], [pkg])
                            for k in range(8):
                                mm(pu[:, :n], WU[:, k, fc * 128:(fc + 1) * 128], UY[:, k, t0:t0 + n], k == 0, k == 7,
                                   [ku, f"u{k}b{b}"], [pku])
                            sg = SG[fc % 2]
                            act(sg[:, :n], pg[:, :n], AF.Silu, [pkg], [f"SG{fc % 2}"])
                            tt(sg[:, :n], sg[:, :n], pu[:, :n], ALU.mult, [f"SG{fc % 2}", pku], [f"SG{fc % 2}"])
                            tt(Hb[:, fc, :n], sg[:, :n], bc[:, t0:t0 + n], ALU.mult, [f"SG{fc % 2}", bck + f"b{b}"],
                               [hk + f"f{fc}"])
                        if b == nblk - 1:
                            load_item(j0 + 4)
                            load_item(j0 + 5)
                        for oc in range(8):
                            py, pky = nb()
                            for fc in range(4):
                                mm(py[:, :n], WD[:, fc, oc * 128:(oc + 1) * 128], Hb[:, fc, :n], fc == 0, fc == 3,
                                   [kd, hk + f"f{fc}"], [pky])
                            xk = XT[:, oc, t0:t0 + n]
                            stt(xk, py[:, :n], mod(l, 5, oc, grp), xk, ALU.mult, ALU.add, [pky, "MOD", f"x{oc}b{b}"],
                                [f"x{oc}b{b}"])
                    load_item(j0 + 6)
                for b in range(nblk):
                    ln_block(l, 1, b, (SQ, MEAN, RSTD))
            P.barrier()

        BREG = []

        def moe_sparse(l, need_ctx):
            if not BREG:
                BREG.append(nc.gpsimd.to_reg(4 * 32 * 128 - 1))
            nblk = 5 if need_ctx else 4
            ntiles = 18 if need_ctx else 16
            NB_ = (ntiles * 128 * 2) // 128 + 32
            POOL = (mybir.EngineType.Pool,)
            with ExitStack() as s2:
                AST = sb("AST", [128, 18, 32], BF16, s2)
                WCS = sb("WCS", [128, 18, 32], F32, s2)
                CSS = sb("CSS", [128, 18, 32], F32, s2)
                PRE = sb("PRE", [128, 19, 32], F32, s2)
                PST = sb("PST", [128, 32], F32, s2)
                PEN = sb("PEN", [128, 32], F32, s2)
                DESTF = sb("DESTF", [128, 18, 2], F32, s2)
                DEST = sb("DEST", [128, 18, 2], U32, s2)
                WSEL = sb("WSEL", [128, 18, 2], F32, s2)
                IDXW = sb("IDXW", [128, 72], U32, s2)
                SQ = [sb(f"SQ{j}", [128, 512], F32, s2) for j in range(2)]
                MEAN = sb("MEAN", [128, 512], F32, s2)
                RSTD = sb("RSTD", [128, 512], F32, s2)
                with ExitStack() as s3:
                    TF = [sb(f"TF{j}", [128, 512], F32, s3) for j in range(3)]
                    WR = sb("WRl", [128, 8, 36], F32, s3)
                    LG = sb("LG", [128, 36], F32, s3)
                    SM = sb("SM", [128, 16], F32, s3)
                    GOH = sb("GOH", [128, 4], F32, s3)
                    GE = sb("GE", [128, 4], F32, s3)
                    ET = sb("ET", [128, 32], F32, s3)
                    ES = sb("ES", [128, 8], F32, s3)
                    T8 = sb("T8", [128, 8], F32, s3)
                    SEL = sb("SEL", [128, 8], F32, s3)
                    EX = sb("EX", [128, 8], F32, s3)
                    NBK = sb("NBK", [128, 32, 36], F32, s3)
                    CNT = sb("CNT", [128, 32], F32, s3)
                    PADB = sb("PADB", [32, 128], F32, s3)
                    DP1 = sb("DP1", [128, 32], F32, s3)
                    EQ = sb("EQ", [128, 32], F32, s3)
                    BLE = sb("BLE", [128, 72, 32], F32, s3)
                    BLKB = sb("BLKB", [128, 72], F32, s3)
                    P.dma("sp", WR[:], wr_d[:, l, :, :], [], ["WR"])
                    ti = 0
                    for b in range(nblk):
                        t0, n, grp = BLOCKS[b]
                        ntile = n // 128
                        pr = [nb() for _ in range(ntile)]
                        for k in range(8):
                            tf = TF[ti % 3]
                            tk = f"TF{ti % 3}"
                            ti += 1
                            act(tf[:, :n], XT[:, k, t0:t0 + n], AF.Identity, [f"x{k}b{b}", "MOD"], [tk],
                                scale=mod(l, 4, k, grp), bias=mod(l, 3, k, grp))
                            cp(UY[:, k, t0:t0 + n], tf[:, :n], [tk], [f"u{k}b{b}"])
                            for tI in range(ntile):
                                mm(pr[tI][0][:, 0:36], tf[:, tI * 128:(tI + 1) * 128], WR[:, k, :], k == 0, False,
                                   [tk, "WR"], [pr[tI][1]])
                        for tI in range(ntile):
                            gi = t0 // 128 + tI
                            ps, pk = pr[tI]
                            mm(ps[:, 0:36], ONES1, RB[0:1, l, :], False, True, ["CST", "RB"], [pk])
                            cp(LG[:], ps[:, 0:36], [pk], ["LG"])
                            R_ = ["LG", "SM", "GOH", "GE", "ET", "ES", "T8", "SEL", "EX"]

                            def dv(fn, extra_w=()):
                                P.op("dve", fn, R_, R_ + list(extra_w))
                            dv(lambda: nc.vector.tensor_reduce(out=SM[:, 0:1], in_=LG[:, 0:4], axis=AX.X, op=ALU.max))
                            dv(lambda: nc.vector.tensor_scalar(out=GOH[:], in0=LG[:, 0:4], scalar1=SM[:, 0:1], scalar2=None,
                                                               op0=ALU.is_equal))
                            dv(lambda: nc.vector.tensor_scalar(out=SM[:, 1:2], in0=SM[:, 0:1], scalar1=-1.0, scalar2=None,
                                                               op0=ALU.mult))
                            act(GE[:], LG[:, 0:4], AF.Exp, R_, R_, bias=SM[:, 1:2])
                            dv(lambda: nc.vector.tensor_reduce(out=SM[:, 2:3], in_=GE[:], axis=AX.X, op=ALU.add))
                            dv(lambda: nc.vector.reciprocal(out=SM[:, 3:4], in_=SM[:, 2:3]))
                            dv(lambda: nc.vector.tensor_tensor(
                                out=ET[:].rearrange("p (g e) -> p g e", e=8),
                                in0=LG[:, 4:36].rearrange("p (g e) -> p g e", e=8),
                                in1=GOH[:].unsqueeze(2).broadcast_to([128, 4, 8]), op=ALU.mult))
                            dv(lambda: nc.vector.tensor_reduce(out=ES[:], in_=ET[:].rearrange("p (g e) -> p e g", e=8),
                                                               axis=AX.X, op=ALU.add))
                            dv(lambda: nc.vector.max(out=T8[:], in_=ES[:]))
                            dv(lambda: nc.vector.tensor_scalar(out=SEL[:], in0=ES[:], scalar1=T8[:, 1:2], scalar2=None,
                                                               op0=ALU.is_ge))
                            dv(lambda: nc.vector.tensor_scalar(out=SM[:, 4:5], in0=T8[:, 0:1], scalar1=-1.0, scalar2=None,
                                                               op0=ALU.mult))
                            act(EX[:], ES[:], AF.Exp, R_, R_, bias=SM[:, 4:5])
                            dv(lambda: nc.vector.tensor_tensor(out=EX[:], in0=EX[:], in1=SEL[:], op=ALU.mult))
                            dv(lambda: nc.vector.tensor_reduce(out=SM[:, 5:6], in_=EX[:], axis=AX.X, op=ALU.add))
                            dv(lambda: nc.vector.reciprocal(out=SM[:, 6:7], in_=SM[:, 5:6]))
                            dv(lambda: nc.vector.tensor_tensor(out=SM[:, 7:8], in0=SM[:, 6:7], in1=SM[:, 3:4], op=ALU.mult))
                            dv(lambda: nc.vector.tensor_scalar(out=EX[:], in0=EX[:], scalar1=SM[:, 7:8], scalar2=None,
                                                               op0=ALU.mult))
                            dv(lambda: nc.vector.tensor_tensor(
                                out=WCS[:, gi, :].rearrange("p (g e) -> p g e", e=8),
                                in0=GOH[:].unsqueeze(2).broadcast_to([128, 4, 8]),
                                in1=EX[:].unsqueeze(1).broadcast_to([128, 4, 8]), op=ALU.mult), ["WCS"])
                            dv(lambda: nc.vector.tensor_tensor(
                                out=AST[:, gi, :].rearrange("p (g e) -> p g e", e=8),
                                in0=GOH[:].unsqueeze(2).broadcast_to([128, 4, 8]),
                                in1=SEL[:].unsqueeze(1).broadcast_to([128, 4, 8]), op=ALU.mult), ["AST"])
                    for g4 in range(0, ntiles, 4):
                        m4 = min(4, ntiles - g4)
                        ps, pk = nb()
                        for j in range(m4):
                            mm(ps[:, j * 32:(j + 1) * 32], ONESB, AST[:, g4 + j, :], True, True, ["CSTB", "AST"], [pk])
                        cp(CSS[:, g4:g4 + m4, :], ps[:, 0:m4 * 32].rearrange("p (j e) -> p j e", e=32), [pk], ["CSS"])
                    P.op("dve", lambda: nc.vector.memset(PRE[:, 0, :], 0.0), [], ["PRE"])
                    for j in range(ntiles):
                        tt(PRE[:, j + 1, :], PRE[:, j, :], CSS[:, j, :], ALU.add, ["PRE", "CSS"], ["PRE"])
                    ps, pk = nb()
                    for j in range(ntiles):
                        mm(ps[0:32, 0:2], AST[:, j, :], ONESB[:, 0:2], j == 0, j == ntiles - 1, ["AST", "CSTB"], [pk])
                    cp(CNT[0:32, 0:1], ps[0:32, 0:1], [pk], ["CNT"])
                    tt(NBK[0:32, 0, :], CNT[0:32, 0:1].broadcast_to([32, 36]), CST3[0:32, 0:36], ALU.is_gt,
                       ["CNT", "CST3"], ["NBK"])
                    P.op("dve", lambda: nc.vector.tensor_reduce(out=CNT[0:32, 1:2], in_=NBK[0:32, 0, :], axis=AX.X, op=ALU.add),
                         ["NBK", "CNT"], ["CNT"])
                    ts(CNT[0:32, 2:3], CNT[0:32, 1:2], 128.0, ALU.mult, ["CNT"], ["CNT"])
                    cp(PADB[:, :], CNT[0:32, 2:3].broadcast_to([32, 128]), ["CNT"], ["PADB"])
                    ps, pk = nb()
                    mm(ps[:, 0:32], PADB[:, :], CST3[0:32, 64:96], True, True, ["PADB", "CST3"], [pk])
                    mm(ps[:, 32:64], PADB[:, :], CST3[0:32, 96:128], True, True, ["PADB", "CST3"], [pk])
                    cp(PST[:], ps[:, 0:32], [pk], ["PST"])
                    cp(PEN[:], ps[:, 32:64], [pk], ["PEN"])
                    tt(BLE[:, 0:NB_, :], PEN[:].unsqueeze(1).broadcast_to([128, NB_, 32]),
                       CST3[:, 136:136 + NB_].unsqueeze(2).broadcast_to([128, NB_, 32]), ALU.is_le, ["PEN", "CST3"], ["BLE"])
                    P.op("dve", lambda: nc.vector.tensor_reduce(out=BLKB[:, 0:NB_], in_=BLE[:, 0:NB_, :], axis=AX.X, op=ALU.add),
                         ["BLE"], ["BLKB"])
                    ts(BLKB[:, 0:NB_], BLKB[:, 0:NB_], 31.0, ALU.min, ["BLKB"], ["BLKB"], s2=128.0, op1=ALU.mult)
                    ts(BLKB[:, 0:NB_], BLKB[:, 0:NB_], CST3[:, 129:130], ALU.add, ["BLKB", "CST3"], ["BLKB"],
                       s2=float(l * 4096), op1=ALU.add)
                    blef = BLE[:, 0:3, :].rearrange("p a b -> p (a b)")[:, 0:NB_]
                    ts(blef, CST3[:, 136:136 + NB_], PEN[:, 31:32], ALU.is_ge, ["CST3", "PEN", "BLE"], ["BLE"])
                    stt(BLKB[:, 0:NB_], blef, 1.0e6, BLKB[:, 0:NB_], ALU.mult, ALU.add, ["BLE", "BLKB"], ["BLKB"])
                    cp(IDXW[:, 0:NB_], BLKB[:, 0:NB_], ["BLKB"], ["IDXW"])
                    for g4 in range(0, ntiles, 4):
                        m4 = min(4, ntiles - g4)
                        ps, pk = nb()
                        for j in range(m4):
                            mm(ps[:, j * 32:(j + 1) * 32], TRIS, AST[:, g4 + j, :], True, True, ["CSTB2", "AST"], [pk])
                        for j in range(m4):
                            gi = g4 + j
                            R2 = ["DP1", "EQ", "T8"]
                            tt(DP1[:], ps[:, j * 32:(j + 1) * 32], PRE[:, gi, :], ALU.add, [pk, "PRE"] + R2, R2)
                            tt(DP1[:], DP1[:], PST[:], ALU.add, R2 + ["PST"], R2)
                            stt(DP1[:], DP1[:], 1.0, AST[:, gi, :], ALU.add, ALU.mult, R2 + ["AST"], R2)
                            P.op("dve", lambda: nc.vector.max(out=T8[:], in_=DP1[:]), R2 + ["LG"], R2 + ["LG"])
                            ts(DESTF[:, gi, :], T8[:, 0:2], -1.0, ALU.add, R2, ["DESTF"])
                            for kk in range(2):
                                ts(EQ[:], DP1[:], T8[:, kk:kk + 1], ALU.is_equal, R2, R2)
                                tt(EQ[:], EQ[:], WCS[:, gi, :], ALU.mult, R2 + ["WCS"], R2)
                                P.op("dve", lambda kk=kk, gi=gi: nc.vector.tensor_reduce(
                                    out=WSEL[:, gi, kk:kk + 1], in_=EQ[:], axis=AX.X, op=ALU.add), R2 + ["WSEL"], R2 + ["WSEL"])
                    cp(DEST[:], DESTF[:], ["DESTF"], ["DEST"])
                P.barrier()
                with ExitStack() as s3:
                    WB = [sb(f"WB{j}", [128, 4096], BF16, s3) for j in range(4)]
                    UT = [sb(f"UT{j}", [128, 1024], BF16, s3) for j in range(2)]
                    XS = [sb(f"XS{j}", [128, 1024], BF16, s3) for j in range(2)]
                    XST = [sb(f"XST{j}", [128, 8, 128], BF16, s3) for j in range(2)]
                    H = [sb(f"H{j}", [128, 4, 128], BF16, s3) for j in range(2)]
                    SG = [sb(f"SG{j}", [128, 512], F32, s3) for j in range(2)]
                    YS = [sb(f"YS{j}", [128, 1024], F32, s3) for j in range(2)]
                    ZT = sb("ZT", [128, 1024], BF16, s3)
                    P.op("dve", lambda: nc.vector.memset(ZT[:], 0.0), [], ["ZT"])
                    for bb in range(NB_):
                        P.dma("sp", xs_d[bb * 128:(bb + 1) * 128, :], ZT[:, :], ["ZT"], ["xsd"])
                    for gi in range(ntiles):
                        ut = UT[gi % 2]
                        uk = f"UT{gi % 2}"
                        b = gi // 4 if gi < 16 else 4
                        for half in range(2):
                            ps, pk = nb()
                            for kk in range(4):
                                k = half * 4 + kk
                                mm(ps[:, kk * 128:(kk + 1) * 128], UY[:, k, gi * 128:(gi + 1) * 128], IDB, True, True,
                                   [f"u{k}b{b}", "CSTB"], [pk])
                            cp(ut[:, half * 512:(half + 1) * 512], ps[:, :], [pk], [uk], eng="act" if half else "dve")
                        for kk in range(2):
                            P.dma_fn("pool", lambda sem, ut=ut, gi=gi, kk=kk: nc.gpsimd.indirect_dma_start(
                                out=xs_d, out_offset=bass.IndirectOffsetOnAxis(ap=DEST[:, gi, kk:kk + 1], axis=0),
                                in_=ut[:, :], in_offset=None).then_inc(sem, 16), [uk, "DEST"], ["xsd"])
                    items = []
                    for bb in range(NB_):
                        items += [("g", bb), ("u", bb), ("d", bb)]
                    def load_item(j):
                        if j >= len(items):
                            return
                        kind, bb = items[j]
                        c0_ = {"g": 0, "u": 2, "d": 4}[kind]
                        for hh_ in range(2):
                            P.dma_fn("pool", lambda sem, j=j, bb=bb, c=c0_ + hh_, hh_=hh_: nc.gpsimd.indirect_dma_start(
                                out=WB[j % 4][:, hh_ * 2048:(hh_ + 1) * 2048], out_offset=None, in_=wc_d[c],
                                in_offset=bass.IndirectOffsetOnAxis(ap=IDXW[:, bb:bb + 1], axis=0),
                                bounds_check=BREG[0], oob_is_err=False).then_inc(sem, 16),
                                ["IDXW"], [f"WB{j % 4}"])

                    for j in range(4):
                        load_item(j)
                    for bb in range(NB_):
                        j0 = 3 * bb
                        WG = WB[j0 % 4][:, :].rearrange("p (k n) -> p k n", n=512)
                        WU = WB[(j0 + 1) % 4][:, :].rearrange("p (k n) -> p k n", n=512)
                        WD = WB[(j0 + 2) % 4][:, :].rearrange("p (k n) -> p k n", n=1024)
                        kg, ku, kd = f"WB{j0 % 4}", f"WB{(j0 + 1) % 4}", f"WB{(j0 + 2) % 4}"
                        xs, xk = XS[bb % 2], f"XS{bb % 2}"
                        xst, xtk = XST[bb % 2], f"XST{bb % 2}"
                        Hb, hk = H[bb % 2], f"H{bb % 2}"
                        ys, yk = YS[bb % 2], f"YS{bb % 2}"
                        P.dma("sp", xs[:, :], xs_d[bb * 128:(bb + 1) * 128, :], ["xsd"], [xk])
                        for half in range(2):
                            ps, pk = nb()
                            for kk in range(4):
                                k = half * 4 + kk
                                mm(ps[:, kk * 128:(kk + 1) * 128], xs[:, k * 128:(k + 1) * 128], IDB, True, True,
                                   [xk, "CSTB"], [pk])
                            cp(xst[:, half * 4:(half + 1) * 4, :], ps[:, :].rearrange("p (k r) -> p k r", r=128), [pk], [xtk],
                               eng="act" if half else "dve")
                        pg, pkg = nb()
                        pu, pku = nb()
                        for fc in range(4):
                            for k in range(8):
                                mm(pg[:, fc * 128:(fc + 1) * 128], WG[:, k, fc * 128:(fc + 1) * 128], xst[:, k, :], k == 0, k == 7,
                                   [kg, xtk], [pkg])
                        for fc in range(4):
                            for k in range(8):
                                mm(pu[:, fc * 128:(fc + 1) * 128], WU[:, k, fc * 128:(fc + 1) * 128], xst[:, k, :], k == 0, k == 7,
                                   [ku, xtk], [pku])
                        sg = SG[bb % 2]
                        act(sg[:, :], pg[:, :], AF.Silu, [pkg], [f"SG{bb % 2}"])
                        tt(Hb[:, :, :], sg[:, :].rearrange("p (f r) -> p f r", r=128), pu[:, :].rearrange("p (f r) -> p f r", r=128),
                           ALU.mult, [f"SG{bb % 2}", pku], [hk])
                        load_item(j0 + 4)
                        load_item(j0 + 5)
                        for half in range(2):
                            py, pky = nb()
                            for fc in range(4):
                                mm(py[:, :], Hb[:, fc, :], WD[:, fc, half * 512:(half + 1) * 512], fc == 0, fc == 3,
                                   [kd, hk], [pky])
                            cp(ys[:, half * 512:(half + 1) * 512], py[:, :], [pky], [yk], eng="act" if half else "dve")
                        load_item(j0 + 6)
                        P.dma("sp", ys_d[bb * 128:(bb + 1) * 128, :], ys[:, :], [yk], ["ysd"])
                P.barrier()
                with ExitStack() as s3:
                    G0 = sb("G0", [128, 1024], F32, s3)
                    G1 = sb("G1", [128, 1024], F32, s3)
                    Z = sb("Z", [128, 1024], F32, s3)
                    for b in range(nblk):
                        t0, n, grp = BLOCKS[b]
                        for k in range(8):
                            xk = XT[:, k, t0:t0 + n]
                            if k % 2 == 0:
                                act(xk, xk, AF.Copy, [f"x{k}b{b}"], [f"x{k}b{b}"], scale=ALPHA)
                            else:
                                ts(xk, xk, ALPHA, ALU.mult, [f"x{k}b{b}"], [f"x{k}b{b}"])
                    for gi in range(ntiles):
                        b = gi // 4 if gi < 16 else 4
                        grp = BLOCKS[b][2]
                        for kk, (G, gk) in enumerate(((G0, "G0"), (G1, "G1"))):
                            P.dma_fn("pool", lambda sem, G=G, gi=gi, kk=kk: nc.gpsimd.indirect_dma_start(
                                out=G[:, :], out_offset=None, in_=ys_d,
                                in_offset=bass.IndirectOffsetOnAxis(ap=DEST[:, gi, kk:kk + 1], axis=0)).then_inc(sem, 16),
                                ["ysd", "DEST"], [gk])
                        ts(Z[:, :], G0[:, :], WSEL[:, gi, 0:1], ALU.mult, ["G0", "WSEL"], ["Z"])
                        stt(Z[:, :], G1[:, :], WSEL[:, gi, 1:2], Z[:, :], ALU.mult, ALU.add, ["G1", "WSEL", "Z"], ["Z"])
                        for half in range(2):
                            ps, pk = nb()
                            for kk in range(4):
                                k = half * 4 + kk
                                P.op("pe", lambda k=k, kk=kk, ps=ps: nc.tensor.transpose(
                                    out=ps[:, kk * 128:(kk + 1) * 128], in_=Z[:, k * 128:(k + 1) * 128], identity=IDF),
                                    ["Z", "CST"], [pk])
                            for kk in range(4):
                                k = half * 4 + kk
                                xk = XT[:, k, gi * 128:(gi + 1) * 128]
                                stt(xk, ps[:, kk * 128:(kk + 1) * 128], mod(l, 5, k, grp), xk, ALU.mult, ALU.add,
                                    [pk, "MOD", f"x{k}b{b}"], [f"x{k}b{b}"])
                    for b in range(nblk):
                        ln_block(l, 1, b, (SQ, MEAN, RSTD))
            P.barrier()

        for l in range(n_layers):
            if stop == "ada":
                break
            lastl = l == DEPTH - 1
            need_ctx = not lastl
            is_dbg_last = (l == n_layers - 1)
            if l % 2 == 0:
                mixer_ab(l, need_ctx)
                wo = ab_w_out_d[l // 2]
            else:
                mixer_c(l, need_ctx)
                wo = c_w_out_d[l // 2]
            raw = is_dbg_last and stop == "mix"
            if is_dbg_last and stop == "y":
                for b in range(5):
                    t0, n, grp = BLOCKS[b]
                    for k in range(8):
                        cp(XT[:, k, t0:t0 + n], UY[:, k, t0:t0 + n], [f"u{k}b{b}"], [f"x{k}b{b}"])
                break
            proj_ln(l, wo, 5 if need_ctx else 4, raw)
            if is_dbg_last and stop in ("mix", "ln1"):
                break
            if SPARSE:
                moe_sparse(l, need_ctx)
            else:
                moe(l, need_ctx)

        for k in range(8):
            P.dma("sp", out_d[k], XT[:, k, :], xkeys(ks=[k]), [f"out{k}"])
        P.finish("sp", [f"out{k}" for k in range(8)])
        print("bass ops:", P.nops, P.cnt)
    return nc


def _fm(v):
    v = np.asarray(v, np.float32)
    lead = v.shape[:-1]
    r = v.reshape(lead + (8, 128))
    r = np.moveaxis(r, -1, 0)
    return np.ascontiguousarray(r)


def _rope_tables(head_dim, per, qscale):
    quarter = head_dim // 4
    row = np.repeat(np.arange(T // 64, dtype=np.float32), 64)
    col = np.tile(np.arange(64, dtype=np.float32), T // 64)
    inv = (np.float32(10000.0) ** (-np.arange(quarter, dtype=np.float32) / np.float32(quarter))).astype(np.float32)
    ang_r = row[:, None] * inv
    ang_c = col[:, None] * inv
    ang = np.concatenate([ang_r, ang_r, ang_c, ang_c], axis=-1)
    cos = np.cos(ang).astype(np.float32).T
    sin = np.sin(ang).astype(np.float32).T
    sign = np.ones((head_dim, 1), np.float32)
    sign[0:quarter] = -1.0
    sign[2 * quarter:3 * quarter] = -1.0
    sin = sin * sign
    rep = 128 // head_dim
    cos = np.tile(cos, (rep, 1))
    sin = np.tile(sin, (rep, 1))
    return np.ascontiguousarray(np.stack([cos * qscale, sin * qscale, cos, sin]).astype(np.float32))


_CONSTS = None


def _consts():
    global _CONSTS
    if _CONSTS is not None:
        return _CONSTS
    c = {}
    c["ropeA"] = _rope_tables(128, 128, np.float32(128 ** -0.5))
    c["ropeC"] = _rope_tables(64, 64, np.float32(0.125))
    ii = np.arange(128)
    cst = np.zeros((128, 1024), np.float32)
    cst[:, 0:128] = np.eye(128, dtype=np.float32)
    cst[:, 128:256] = 1.0 / 1024.0
    cst[:, 256:384] = 1.0 / 128.0
    cst[:, 384:512] = np.where(ii[:, None] <= ii[None, :], -1.0 / 16.0, 0.0)
    cst[:, 512:640] = 1.0
    c["cst"] = cst
    cst2 = np.zeros((128, 256), np.float32)
    cst2[:, 0:128] = np.where(ii[:, None] >= ii[None, :], -1.0 / 16.0, 0.0)
    cst2[:, 128:256] = np.where(ii[:, None] > ii[None, :], -1.0 / 16.0, 0.0)
    c["cst2"] = cst2
    cstb = np.zeros((128, 256), np.float32)
    cstb[:, 0:128] = np.eye(128)
    cstb[:, 128:256] = 1.0
    c["cstb"] = cstb.astype(ml_dtypes.bfloat16)
    m = (ii[:, None] > ii[None, :]).astype(np.uint8)
    c["mask"] = np.ascontiguousarray(np.tile(m, (1, 4)))
    cst3 = np.zeros((128, 256), np.float32)
    cst3[:, 0:36] = (np.arange(36, dtype=np.float32) * 128.0)[None, :]
    e32 = np.arange(32)
    cst3[0:32, 64:96] = (e32[:, None] < e32[None, :]).astype(np.float32)
    cst3[0:32, 96:128] = (e32[:, None] <= e32[None, :]).astype(np.float32)
    cst3[:, 128] = np.arange(128, dtype=np.float32) * 128.0
    cst3[:, 129] = np.arange(128, dtype=np.float32)
    cst3[:, 136:208] = (np.arange(72, dtype=np.float32) * 128.0)[None, :]
    c["cst3"] = cst3
    c["cstb2"] = (ii[:, None] < ii[None, :]).astype(np.float32).astype(ml_dtypes.bfloat16)
    sele = np.zeros((32, 32, 128), np.float32)
    for e in range(32):
        sele[e, e, :] = 1.0
    c["sele"] = sele.reshape(32, 32 * 128).astype(ml_dtypes.bfloat16)
    ret = np.zeros((128, 4, 6, 128), np.float32)
    pos = np.arange(128, dtype=np.float64)
    for h in range(4):
        lf = float(np.log1p(-np.exp2(-np.float32(5.0 + h)), dtype=np.float32))
        lb = float(np.log1p(-np.exp2(-np.float32(5.5 + h)), dtype=np.float32))
        ret[:, h, 0, :] = np.exp(lf * (pos + 1))
        ret[:, h, 1, :] = np.exp(-lf * (pos + 1))
        ret[:, h, 2, :] = np.exp(lf * (127 - pos))
        ret[:, h, 3, :] = np.exp(lb * (127 - pos))
        ret[:, h, 4, :] = np.exp(-lb * (128 - pos))
        ret[:, h, 5, :] = np.exp(lb * pos)
    c["ret"] = ret
    _CONSTS = c
    return c


def _prep_inputs(inputs):
    f = lambda a: np.ascontiguousarray(np.asarray(a, np.float32))
    shared = dict(_consts())
    shared["adab"] = np.ascontiguousarray(np.asarray(inputs["ada_b"], np.float32).reshape(4, 48, 128).transpose(2, 0, 1))
    lnp = np.stack([inputs["ln1_g"], inputs["ln1_b"], inputs["ln2_g"], inputs["ln2_b"]], axis=1)
    shared["lnp"] = np.ascontiguousarray(np.asarray(lnp, np.float32).reshape(4, 4, 8, 128).transpose(3, 0, 1, 2))
    gn = np.concatenate([inputs["ab_gn_a"], inputs["ab_gn_b"]], axis=1)
    shared["gn"] = np.ascontiguousarray(np.asarray(gn, np.float32).reshape(2, 8, 128).transpose(2, 0, 1))
    shared["subg"] = np.ascontiguousarray(np.asarray(inputs["c_subln_g"], np.float32).reshape(2, 8, 128).transpose(2, 0, 1))
    wlr = np.zeros((32, 2, 2, 256), np.float32)
    wlr[0:16, :, 0, :] = np.asarray(inputs["ab_w_lr_f"]).transpose(1, 0, 2)
    wlr[0:16, :, 1, :] = np.asarray(inputs["ab_w_lr_b"]).transpose(1, 0, 2)
    wlr[16, :, 0, :] = np.asarray(inputs["ab_b_lr_f"])
    wlr[16, :, 1, :] = np.asarray(inputs["ab_b_lr_b"])
    shared["wlr"] = wlr
    wr = np.concatenate([inputs["moe_w_grp"], inputs["moe_w_rexp"]], axis=2)
    shared["wr"] = np.ascontiguousarray(np.asarray(wr, np.float32).reshape(4, 8, 128, 36).transpose(2, 0, 1, 3))
    rb = np.concatenate([inputs["moe_b_grp"], inputs["moe_b_rexp"]], axis=1)
    shared["rb"] = np.ascontiguousarray(np.asarray(rb, np.float32)[None])
    lqk = np.stack([inputs["c_lq1"], inputs["c_lk1"], inputs["c_lq2"], inputs["c_lk2"]], axis=1)
    shared["lqk"] = np.ascontiguousarray(np.asarray(lqk, np.float32).transpose(3, 0, 1, 2))
    for nm in ("ada_w", "ab_w_in", "ab_w_out", "c_w_qkv", "c_w_out"):
        shared[nm] = f(inputs[nm])
    if SPARSE:
        for ci, nm in ((0, "moe_w_gate"), (2, "moe_w_up")):
            wp = np.asarray(inputs[nm], np.float32).reshape(4, 32, 8, 128, 512).transpose(0, 1, 3, 2, 4).reshape(4 * 32 * 128, 4096)
            shared[f"wc{ci}"] = np.ascontiguousarray(wp[:, 0:2048])
            shared[f"wc{ci + 1}"] = np.ascontiguousarray(wp[:, 2048:4096])
        wp = np.asarray(inputs["moe_w_down"], np.float32).reshape(4, 32, 4, 128, 1024).transpose(0, 1, 3, 2, 4).reshape(4 * 32 * 128, 4096)
        shared["wc4"] = np.ascontiguousarray(wp[:, 0:2048])
        shared["wc5"] = np.ascontiguousarray(wp[:, 2048:4096])
    else:
        for nm in ("moe_w_gate", "moe_w_up", "moe_w_down"):
            shared[nm] = f(inputs[nm])
    x = np.asarray(inputs["x"], np.float32)
    ctx = np.asarray(inputs["ctx"], np.float32)
    c = np.asarray(inputs["c"], np.float32)
    c_ctx = np.asarray(inputs["c_ctx"], np.float32)
    in_maps = []
    for b in range(8):
        tok = np.concatenate([x[b], ctx[b]], axis=0)
        xT = np.ascontiguousarray(tok.T.reshape(8, 128, NT))
        cc = np.stack([c[b].reshape(8, 128).T, c_ctx.reshape(8, 128).T], axis=-1)
        m = dict(shared)
        m["xT"] = xT
        m["cc"] = np.ascontiguousarray(cc.astype(np.float32))
        in_maps.append(m)
    return in_maps


_NC_CACHE = {}


def run(inputs, n_layers=DEPTH, stop=None, ncores=8):
    key = (n_layers, stop)
    if key not in _NC_CACHE:
        _NC_CACHE[key] = build(n_layers, stop)
    nc = _NC_CACHE[key]
    in_maps = _prep_inputs(inputs)[:ncores]
    if stop in ("ada", "mix", "ln1", "y") and n_layers <= 1:
        for m in in_maps:
            for nm in (["wc%d" % c for c in range(6)] if SPARSE else ["moe_w_gate", "moe_w_up", "moe_w_down"]):
                m.pop(nm)
    res = run_bass_kernel_spmd(nc, in_maps, core_ids=list(range(ncores)))
    outs = [np.asarray(r["outT"]).reshape(D, NT).T for r in res.results]
    return outs


def kernel(**inputs):
    outs = run(inputs)
    return np.ascontiguousarray(np.stack([o[:T] for o in outs], axis=0).astype(np.float32))
```

```python
import math
import numpy as np
import ml_dtypes
from contextlib import ExitStack
import concourse.bass as bass
import concourse.mybir as mybir
from concourse.bass_utils import run_bass_kernel_spmd

F32 = mybir.dt.float32
BF16 = mybir.dt.bfloat16
U8 = mybir.dt.uint8
U32 = mybir.dt.uint32
I32 = mybir.dt.int32
AF = mybir.ActivationFunctionType
ALU = mybir.AluOpType
AX = mybir.AxisListType

D = 1024
T = 2048
TC = 256
NT = T + TC
DEPTH = 4
ALPHA = (2 * DEPTH) ** 0.25
EPS = 1e-5
BLOCKS = [(0, 512, 0), (512, 512, 0), (1024, 512, 0), (1536, 512, 0), (2048, 256, 1)]
ORDER_F = [16, 17] + list(range(16))
ORDER_B = [17, 16] + list(range(15, -1, -1))
AB_IN = 3616
import os as _os
SPARSE = int(_os.environ.get("MOE_SPARSE", "1"))
DBG_HEADS = int(_os.environ.get("DBG_HEADS", "8"))
DBG_B = int(_os.environ.get("DBG_B", "9"))
DBG_C = _os.environ.get("DBG_C", "z")


class Prog:
    def __init__(self, nc, es, ndma=28):
        self.nc = nc
        self.e = dict(pe=nc.tensor, act=nc.scalar, dve=nc.vector, pool=nc.gpsimd, sp=nc.sync)
        self.sem = {k: es.enter_context(nc.semaphore("s_" + k)) for k in self.e}
        self.cnt = {k: 0 for k in self.e}
        self.seen = {k: {} for k in self.e}
        self.st = {}
        self.dq = {}
        for q in ("sp", "pool", "act"):
            self.dq[q] = dict(sems=[es.enter_context(nc.semaphore(f"d_{q}{i}")) for i in range(ndma)],
                              vals=[0] * ndma, idx=0)
        self.nops = 0
        self.bar = es.enter_context(nc.sbuf_tensor("barrier_t", [128, 1], F32))

    def _wait(self, eng, ticks):
        need = {}
        for t in ticks:
            if t is None:
                continue
            key, sem, val = t
            if key == "pe" and eng == "pe":
                continue
            if need.get(key, (None, 0))[1] < val:
                need[key] = (sem, val)
        for key, (sem, val) in need.items():
            if self.seen[eng].get(key, 0) >= val:
                continue
            self.e[eng].wait_ge(sem, val)
            self.seen[eng][key] = val

    def _deps(self, R, W):
        ticks = []
        for k in R:
            s = self.st.get(k)
            if s is not None:
                ticks.append(s[0])
                if k.startswith("ps"):
                    ticks.extend(s[1].values())
        for k in W:
            s = self.st.get(k)
            if s is not None:
                ticks.append(s[0])
                ticks.extend(s[1].values())
        return ticks

    def _update(self, tick, R, W):
        for k in W:
            self.st[k] = [tick, {}]
        for k in R:
            s = self.st.get(k)
            if s is None:
                s = self.st[k] = [None, {}]
            s[1][tick[0]] = tick

    def op(self, eng, fn, R, W):
        self._wait(eng, self._deps(R, W))
        inst = fn()
        inst.then_inc(self.sem[eng], 1)
        self.cnt[eng] += 1
        self.nops += 1
        self._update((eng, self.sem[eng], self.cnt[eng]), R, W)

    def dma(self, q, out, in_, R, W):
        d = self.dq[q]
        i = d["idx"]
        d["idx"] = (i + 1) % len(d["sems"])
        key = ("d", q, i)
        ticks = self._deps(R, W)
        if d["vals"][i] > 0:
            ticks.append((key, d["sems"][i], d["vals"][i]))
        self._wait(q, ticks)
        self.e[q].dma_start(out=out, in_=in_).then_inc(d["sems"][i], 16)
        d["vals"][i] += 16
        self.nops += 1
        self._update((key, d["sems"][i], d["vals"][i]), R, W)

    def dma_fn(self, q, fn, R, W):
        d = self.dq[q]
        i = d["idx"]
        d["idx"] = (i + 1) % len(d["sems"])
        key = ("d", q, i)
        ticks = self._deps(R, W)
        if d["vals"][i] > 0:
            ticks.append((key, d["sems"][i], d["vals"][i]))
        self._wait(q, ticks)
        fn(d["sems"][i])
        d["vals"][i] += 16
        self.nops += 1
        self._update((key, d["sems"][i], d["vals"][i]), R, W)

    def barrier(self):
        ticks = []
        for e in self.e:
            if self.cnt[e] > 0:
                ticks.append((e, self.sem[e], self.cnt[e]))
        for q, d in self.dq.items():
            for i, v in enumerate(d["vals"]):
                if v > 0:
                    ticks.append((("d", q, i), d["sems"][i], v))
        self._wait("dve", ticks)
        inst = self.nc.vector.memset(self.bar[:], 0.0)
        inst.then_inc(self.sem["dve"], 1)
        self.cnt["dve"] += 1
        t = ("dve", self.sem["dve"], self.cnt["dve"])
        for e in ("pe", "act", "pool", "sp"):
            self._wait(e, [t])

    def finish(self, eng, keys):
        self._wait(eng, self._deps(keys, []))


def build(n_layers=DEPTH, stop=None):
    nc = bass.Bass("TRN2", target_bir_lowering=False)

    def din(name, shape, dt=F32):
        return nc.dram_tensor(name, list(shape), dt, kind="ExternalInput").ap()

    xT_d = din("xT", [8, 128, NT])
    cc_d = din("cc", [128, 8, 2])
    adab_d = din("adab", [128, 4, 48])
    lnp_d = din("lnp", [128, 4, 4, 8])
    gn_d = din("gn", [128, 2, 8])
    subg_d = din("subg", [128, 2, 8])
    wlr_d = din("wlr", [32, 2, 2, 256])
    wr_d = din("wr", [128, 4, 8, 36])
    rb_d = din("rb", [1, 4, 36])
    lqk_d = din("lqk", [64, 2, 4, 8])
    ret_d = din("ret", [128, 4, 6, 128])
    ropeA_d = din("ropeA", [4, 128, T])
    ropeC_d = din("ropeC", [4, 128, T])
    cst_d = din("cst", [128, 1024])
    cst2_d = din("cst2", [128, 256])
    cstb_d = din("cstb", [128, 256], BF16)
    mask_d = din("mask", [128, 512], U8)
    cst3_d = din("cst3", [128, 256])
    cstb2_d = din("cstb2", [128, 128], BF16)
    xs_d = nc.dram_tensor("xs_scr", [9216, 1024], BF16, kind="Internal").ap()
    ys_d = nc.dram_tensor("ys_scr", [9216, 1024], F32, kind="Internal").ap()
    sele_d = din("sele", [32, 32 * 128], BF16)
    ada_w_d = din("ada_w", [4, D, 6 * D])
    ab_w_in_d = din("ab_w_in", [2, D, AB_IN])
    ab_w_out_d = din("ab_w_out", [2, D, D])
    c_w_qkv_d = din("c_w_qkv", [2, D, 3 * D])
    c_w_out_d = din("c_w_out", [2, D, D])
    use_moe = not (stop in ("ada", "mix", "ln1", "y") and n_layers <= 1)
    if use_moe and SPARSE:
        wc_d = [din(f"wc{c}", [4 * 32 * 128, 2048]) for c in range(6)]
    elif use_moe:
        wg_d = din("moe_w_gate", [4, 32, D, 512])
        wu_d = din("moe_w_up", [4, 32, D, 512])
        wd_d = din("moe_w_down", [4, 32, 512, D])
    out_d = nc.dram_tensor("outT", [8, 128, NT], F32, kind="ExternalOutput").ap()

    es = ExitStack()
    with es:
        P = Prog(nc, es)

        uid = [0]

        def sb(name, shape, dt=F32, stack=es):
            uid[0] += 1
            return stack.enter_context(nc.sbuf_tensor(f"{name}_{uid[0]}", list(shape), dt))

        PS = [es.enter_context(nc.psum_tensor(f"ps{i}", [128, 512], F32)) for i in range(8)]
        psc = [0]

        def nb():
            i = psc[0]
            psc[0] = (i + 1) % 8
            return PS[i], f"ps{i}"

        def mm(out, lhsT, rhs, start, stop, R, W):
            P.op("pe", lambda: nc.tensor.matmul(out, lhsT=lhsT, rhs=rhs, start=start, stop=stop), R, W)

        def act(out, in_, func, R, W, scale=None, bias=None, accum_out=None):
            kw = {}
            if scale is not None:
                kw["scale"] = scale
            if bias is None and func != AF.Copy:
                sp_, np_ = in_.start_partition(), in_.partition_size()
                bias = ZEROC[sp_:sp_ + np_, 0:1]
                R = list(R) + ["ZEROC"]
            if bias is not None:
                kw["bias"] = bias
            if accum_out is not None:
                kw["accum_out"] = accum_out
            P.op("act", lambda: nc.scalar.activation(out=out, in_=in_, func=func, **kw), R, W)

        def tt(out, a, b, op, R, W, eng="dve"):
            e = nc.vector if eng == "dve" else nc.gpsimd
            P.op(eng, lambda: e.tensor_tensor(out=out, in0=a, in1=b, op=op), R, W)

        def ts(out, a, s1, op0, R, W, s2=None, op1=None, eng="dve"):
            e = nc.vector if eng == "dve" else nc.gpsimd
            if op1 is None:
                P.op(eng, lambda: e.tensor_scalar(out=out, in0=a, scalar1=s1, scalar2=None, op0=op0), R, W)
            else:
                P.op(eng, lambda: e.tensor_scalar(out=out, in0=a, scalar1=s1, scalar2=s2, op0=op0, op1=op1), R, W)

        def stt(out, a, s, b, op0, op1, R, W):
            P.op("dve", lambda: nc.vector.scalar_tensor_tensor(out=out, in0=a, scalar=s, in1=b, op0=op0, op1=op1), R, W)

        def cp(out, in_, R, W, eng="dve"):
            if eng == "act":
                act(out, in_, AF.Copy, R, W)
            else:
                e = nc.vector if eng == "dve" else nc.gpsimd
                P.op(eng, lambda: e.tensor_copy(out=out, in_=in_), R, W)

        XT = sb("XT", [128, 8, NT])
        UY = sb("UY", [128, 8, NT], BF16)
        MOD = sb("MOD", [128, 4, 6, 8, 2])
        LNP = sb("LNP", [128, 4, 4, 8])
        GN = sb("GN", [128, 2, 8])
        SUBG = sb("SUBG", [128, 2, 8])
        RB = sb("RB", [1, 4, 36])
        LAM = sb("LAM", [128, 2, 8])
        CST = sb("CST", [128, 1024])
        CST2 = sb("CST2", [128, 256])
        CSTB = sb("CSTB", [128, 256], BF16)
        MASK = sb("MASK", [128, 512], U8)
        CST3 = sb("CST3", [128, 256])
        CSTB2 = sb("CSTB2", [128, 128], BF16)
        TRIS = CSTB2[:, 0:128]
        EPSC = sb("EPSC", [128, 1])
        ONEC = sb("ONEC", [128, 1])
        ZEROC = sb("ZEROC", [128, 1])
        IDF = CST[:, 0:128]
        ONESD = CST[:, 128:256]
        ONESH = CST[:, 256:384]
        TRIF = CST[:, 384:512]
        ONES1 = CST[0:1, 512:640]
        TRIB = CST2[:, 0:128]
        TRIBP = CST2[:, 128:256]
        IDB = CSTB[:, 0:128]
        ONESB = CSTB[:, 128:256]

        def xkeys(blocks=range(5), ks=range(8)):
            return [f"x{k}b{b}" for k in ks for b in blocks]

        def ukeys(blocks=range(5), ks=range(8)):
            return [f"u{k}b{b}" for k in ks for b in blocks]

        for k in range(8):
            P.dma("sp", XT[:, k, :], xT_d[k], [], xkeys(ks=[k]))
        for (t_, d_, kname) in [(LNP, lnp_d, "LNP"), (GN, gn_d, "GN"), (SUBG, subg_d, "SUBG"),
                                (RB, rb_d, "RB"), (CST, cst_d, "CST"), (CST2, cst2_d, "CST2"), (CSTB, cstb_d, "CSTB"),
                                (MASK, mask_d, "MASK"), (CST3, cst3_d, "CST3"), (CSTB2, cstb2_d, "CSTB2")]:
            P.dma("sp", t_[:], d_, [], [kname])
        P.op("dve", lambda: nc.vector.memset(EPSC[:], EPS), [], ["EPSC"])
        P.op("dve", lambda: nc.vector.memset(ONEC[:], 1.0), [], ["ONEC"])
        P.op("dve", lambda: nc.vector.memset(ZEROC[:], 0.0), [], ["ZEROC"])

        with ExitStack() as s1:
            CC = sb("CC", [128, 8, 2], F32, s1)
            SCB = sb("SCB", [128, 8, 2], BF16, s1)
            ADAB = sb("ADAB", [128, 4, 48], F32, s1)
            WA = [sb(f"WA{i}", [128, 8, 1024], BF16, s1) for i in range(2)]
            LQK = sb("LQK", [64, 2, 4, 8], F32, s1)
            LQP = sb("LQP", [64, 2, 2, 8], F32, s1)
            P.dma("sp", CC[:], cc_d, [], ["CC"])
            P.dma("sp", ADAB[:], adab_d, [], ["ADAB"])
            P.dma("sp", LQK[:], lqk_d, [], ["LQK"])
            act(SCB[:], CC[:], AF.Silu, ["CC"], ["SCB"])
            it = 0
            for l in range(n_layers):
                for j6 in range(6):
                    w = WA[it % 2]
                    wk = f"WA{it % 2}"
                    it += 1
                    src = ada_w_d[l].rearrange("(k p) n -> p k n", p=128)[:, :, j6 * 1024:(j6 + 1) * 1024]
                    P.dma("pool", w[:], src, [], [wk])
                    ps, pk = nb()
                    for jj in range(8):
                        for k in range(8):
                            mm(ps[:, jj * 2:jj * 2 + 2], w[:, k, jj * 128:(jj + 1) * 128], SCB[:, k, :],
                               k == 0, k == 7, [wk, "SCB"], [pk])
                    tt(MOD[:, l, j6, :, :], ps[:, 0:16].rearrange("p (j g) -> p j g", g=2),
                       ADAB[:, l, j6 * 8:(j6 + 1) * 8].unsqueeze(2).broadcast_to([128, 8, 2]), ALU.add,
                       [pk, "ADAB"], ["MOD"])
                for j6 in (1, 4):
                    ts(MOD[:, l, j6, :, :], MOD[:, l, j6, :, :], 1.0, ALU.add, ["MOD"], ["MOD"])
            tt(LQP[:, :, 0, :], LQK[:, :, 0, :], LQK[:, :, 1, :], ALU.mult, ["LQK"], ["LQP"])
            tt(LQP[:, :, 1, :], LQK[:, :, 2, :], LQK[:, :, 3, :], ALU.mult, ["LQP", "LQK"], ["LQP"])
            ps, pk = nb()
            mm(ps[:, 0:32], CST[0:64, 512:640], LQP[:].rearrange("p a b c -> p (a b c)"), True, True,
               ["CST", "LQP"], [pk])
            LE = sb("LE", [128, 32], F32, s1)
            act(LE[:], ps[:, 0:32], AF.Exp, [pk], ["LE"])
            lev = LE[:].rearrange("p (a b c) -> p a b c", a=2, b=2)
            for i in range(2):
                lam_init = 0.8 - 0.6 * math.exp(-0.3 * (2 * i + 1))
                tt(LAM[:, i, :], lev[:, i, 1, :], lev[:, i, 0, :], ALU.subtract, ["LE", "LAM"], ["LAM"])
                ts(LAM[:, i, :], LAM[:, i, :], -lam_init, ALU.add, ["LAM"], ["LAM"])
                ts(SUBG[:, i, :], SUBG[:, i, :], 1.0 - lam_init, ALU.mult, ["SUBG"], ["SUBG"])

        P.barrier()

        def mod(l, which, k, grp):
            return MOD[:, l, which, k, grp:grp + 1]

        def ln_block(l, which_ln, b, scr):
            t0, n, grp = BLOCKS[b]
            SQ, MEAN, RSTD = scr
            psm, pkm = nb()
            pss, pks = nb()
            for k in range(8):
                xk = XT[:, k, t0:t0 + n]
                mm(psm[:, :n], ONESD, xk, k == 0, k == 7, [f"x{k}b{b}", "CST"], [pkm])
                sq = SQ[k % 2]
                act(sq[:, :n], xk, AF.Square, [f"x{k}b{b}"], [f"SQ{k % 2}"])
                mm(pss[:, :n], ONESD, sq[:, :n], k == 0, k == 7, [f"SQ{k % 2}", "CST"], [pks])
            cp(MEAN[:, :n], psm[:, :n], [pkm], ["MEAN"], eng="act")
            act(RSTD[:, :n], psm[:, :n], AF.Square, [pkm], ["RSTD"])
            tt(RSTD[:, :n], pss[:, :n], RSTD[:, :n], ALU.subtract, [pks, "RSTD"], ["RSTD"])
            act(RSTD[:, :n], RSTD[:, :n], AF.Ln, ["RSTD"], ["RSTD"], bias=EPSC[:, 0:1])
            act(RSTD[:, :n], RSTD[:, :n], AF.Exp, ["RSTD"], ["RSTD"], scale=-0.5)
            for k in range(8):
                xk = XT[:, k, t0:t0 + n]
                kk = [f"x{k}b{b}"]
                tt(xk, xk, MEAN[:, :n], ALU.subtract, kk + ["MEAN"], kk)
                tt(xk, xk, RSTD[:, :n], ALU.mult, kk + ["RSTD"], kk)
                act(xk, xk, AF.Identity, kk + ["LNP"], kk, scale=LNP[:, l, 2 * which_ln, k:k + 1],
                    bias=LNP[:, l, 2 * which_ln + 1, k:k + 1])

        def proj_ln(l, w_dram, nblocks, raw):
            with ExitStack() as s2:
                WO = sb("WO", [128, 8, 1024], BF16, s2)
                TMP = [sb(f"TMPO{i}", [128, 512], F32, s2) for i in range(2)]
                SQ = [sb(f"SQ{i}", [128, 512], F32, s2) for i in range(2)]
                MEAN = sb("MEAN", [128, 512], F32, s2)
                RSTD = sb("RSTD", [128, 512], F32, s2)
                P.dma("pool", WO[:], w_dram.rearrange("(k p) n -> p k n", p=128), [], ["WO"])
                for b in range(nblocks):
                    t0, n, grp = BLOCKS[b]
                    for c in range(8):
                        ps, pk = nb()
                        for h in range(8):
                            mm(ps[:, :n], WO[:, h, c * 128:(c + 1) * 128], UY[:, h, t0:t0 + n], h == 0, h == 7,
                               ["WO", f"u{h}b{b}"], [pk])
                        xk = XT[:, c, t0:t0 + n]
                        kk = [f"x{c}b{b}"]
                        if raw:
                            cp(xk, ps[:, :n], [pk], kk, eng="act")
                        else:
                            tmp = TMP[c % 2]
                            act(tmp[:, :n], ps[:, :n], AF.Identity, [pk, "MOD"], [f"TMPO{c % 2}"], scale=mod(l, 2, c, grp))
                            stt(xk, xk, ALPHA, tmp[:, :n], ALU.mult, ALU.add, kk + [f"TMPO{c % 2}"], kk)
                    if not raw:
                        ln_block(l, 0, b, (SQ, MEAN, RSTD))
            P.barrier()

        def make_u(l, b, UB, ub_i, which_s, which_sh):
            t0, n, grp = BLOCKS[b]
            for k in range(8):
                src = XT[:, k, t0:t0 + n]
                dst = UB[ub_i][:, k, :n]
                R = [f"x{k}b{b}", "MOD"]
                W = [f"UB{ub_i}k{k}"]
                if k % 2 == 0:
                    act(dst, src, AF.Identity, R, W, scale=mod(l, which_s, k, grp), bias=mod(l, which_sh, k, grp))
                else:
                    ts(dst, src, mod(l, which_s, k, grp), ALU.mult, R, W, s2=mod(l, which_sh, k, grp), op1=ALU.add)

        def fm_group(UBt, ub_i, n, Wt, wkey, c0, M):
            ps, pk = nb()
            for k in range(8):
                mm(ps[0:M, :n], Wt[:, k, c0:c0 + M], UBt[:, k, :n], k == 0, k == 7, [wkey, f"UB{ub_i}k{k}"], [pk])
            return ps, pk

        def rope_evac(dst, dkey, ps_a, pk_a, ps_b, pk_b, cos_t, sin_t, tkey, n, TR, ti):
            t1 = TR[0]
            t2 = TR[1]
            tt(t1[:, :n], ps_a[:, :n], cos_t, ALU.mult, [pk_a, tkey], ["TR0"])
            tt(t2[:, :n], ps_b[:, :n], sin_t, ALU.mult, [pk_b, tkey], ["TR1"])
            tt(dst, t1[:, :n], t2[:, :n], ALU.add, ["TR0", "TR1"], [dkey])

        def in_proj_head(l, hs, kind, wsrc, cols, Qf, Kf, V, SGN, gn_ap, rope_d, qscale, LRT=None, lr_cols=None,
                         after_block=None, nblocks=5):
            dk = 64 if kind == "B" else 128
            has_rope = kind in ("A", "C")
            has_g = kind in ("A", "B")
            wv = wsrc.rearrange("(k p) n -> p k n", p=128)
            W = hs["W"]
            ofs = {}
            o = 0
            names = ["q", "k", "v"] + (["g"] if has_g else [])
            for nm in names:
                w_ = dk if nm in ("q", "k") else 128
                P.dma("pool", W[:, :, o:o + w_], wv[:, :, cols[nm]:cols[nm] + w_], [], [f"W_{nm}"])
                ofs[nm] = o
                o += w_
            if LRT is not None:
                P.dma("pool", W[:, :, o:o + 32], wv[:, :, lr_cols:lr_cols + 32], [], ["W_lr"])
                ofs["lr"] = o
                o += 32
            if has_rope:
                blk = 32 if kind == "A" else 16
                for nm in ("q", "k"):
                    s_ = W[:, :, ofs[nm]:ofs[nm] + 128].rearrange("p k (a two b) -> p k a two b", two=2, b=blk)
                    d_ = W[:, :, o:o + 128].rearrange("p k (a two b) -> p k a two b", two=2, b=blk)
                    cp(d_[:, :, :, 0, :], s_[:, :, :, 1, :], [f"W_{nm}"], [f"W_{nm}p"])
                    cp(d_[:, :, :, 1, :], s_[:, :, :, 0, :], [f"W_{nm}", f"W_{nm}p"], [f"W_{nm}p"])
                    ofs[nm + "p"] = o
                    o += 128
            UB = hs["UB"]
            TR = hs["TR"]
            ROPE = hs["ROPE"]
            ti = 0
            for b in range(nblocks):
                t0, n, grp = BLOCKS[b]
                ub_i = b % len(UB)
                make_u(l, b, UB, ub_i, 1, 0)
                UBt = UB[ub_i]
                if has_rope and grp == 0:
                    P.dma("sp", ROPE[:, :, :], rope_d[:, :, t0:t0 + n].rearrange("f p t -> p f t"), [], ["ROPE"])
                for nm, dst_f in (("q", Qf), ("k", Kf)):
                    ps, pk = fm_group(UBt, ub_i, n, W, f"W_{nm}", ofs[nm], dk)
                    dst, dkey = dst_f(b)
                    if has_rope and grp == 0:
                        ps2, pk2 = fm_group(UBt, ub_i, n, W, f"W_{nm}p", ofs[nm + "p"], dk)
                        fi = 0 if nm == "q" else 2
                        rope_evac(dst, dkey, ps, pk, ps2, pk2, ROPE[:, fi, :n], ROPE[:, fi + 1, :n], "ROPE", n, TR, ti)
                        ti += 1
                    else:
                        sc = qscale if nm == "q" else 1.0
                        act(dst, ps[0:dk, :n], AF.Copy, [pk], [dkey], scale=sc)
                if has_g:
                    ps, pk = fm_group(UBt, ub_i, n, W, "W_g", ofs["g"], 128)
                    t1 = TR[ti % 2]
                    act(t1[:, :n], ps[:, :n], AF.Silu, [pk], [f"TR{ti % 2}"])
                    ts(SGN[:, t0:t0 + n], t1[:, :n], gn_ap, ALU.mult, [f"TR{ti % 2}", "GN"], [f"sgn_b{b}"])
                    ti += 1
                if LRT is not None:
                    for di in range(2):
                        ps, pk = fm_group(UBt, ub_i, n, W, "W_lr", ofs["lr"] + 16 * di, 16)
                        cp(LRT[di][0:16, :n], ps[0:16, :n], [pk], [f"lrt{di}"], eng="act")
                ps, pk = nb()
                ntile = n // 128
                for tI in range(ntile):
                    for k in range(8):
                        mm(ps[:, tI * 128:(tI + 1) * 128], UBt[:, k, tI * 128:(tI + 1) * 128],
                           W[:, k, ofs["v"]:ofs["v"] + 128], k == 0, k == 7, ["W_v", f"UB{ub_i}k{k}"], [pk])
                c0 = t0 // 128
                cp(V[:, c0:c0 + ntile, :], ps[:, :n].rearrange("p (c d) -> p c d", d=128), [pk], [f"vb{b}"], eng="act")
                if after_block is not None:
                    after_block(b)

        def head_norm(ps_o, pk_o, n, center, hs, out_ap, outkey, post_ap, postkeys, scale_ap=None, src_sb=None):
            OSB, SQh, RS = hs["OSB"], hs["SQh"], hs["RS"]
            M2 = SQh
            if src_sb is None:
                cp(OSB[:, :n], ps_o[:, :n], [pk_o], ["OSB"], eng="act")
                act(SQh[:, :n], ps_o[:, :n], AF.Square, [pk_o], ["SQh"])
            else:
                act(SQh[:, :n], OSB[:, :n], AF.Square, ["OSB"], ["SQh"])
            pss, pks = nb()
            mm(pss[:, :n], ONESH, SQh[:, :n], True, True, ["SQh", "CST"], [pks])
            if center:
                psm, pkm = nb()
                mm(psm[:, :n], ONESH, OSB[:, :n], True, True, ["OSB", "CST"], [pkm])
                act(M2[:, :n], psm[:, :n], AF.Square, [pkm, "SQh"], ["SQh"])
                tt(RS[:, :n], pss[:, :n], M2[:, :n], ALU.subtract, [pks, "SQh"], ["RS"])
                tt(OSB[:, :n], OSB[:, :n], psm[:, :n], ALU.subtract, ["OSB", pkm], ["OSB"])
                act(RS[:, :n], RS[:, :n], AF.Ln, ["RS"], ["RS"], bias=EPSC[:, 0:1])
            else:
                act(RS[:, :n], pss[:, :n], AF.Ln, [pks], ["RS"], bias=EPSC[:, 0:1])
            act(RS[:, :n], RS[:, :n], AF.Exp, ["RS"], ["RS"], scale=-0.5)
            if scale_ap is None:
                tt(OSB[:, :n], OSB[:, :n], RS[:, :n], ALU.mult, ["OSB", "RS"], ["OSB"])
                tt(out_ap, OSB[:, :n], post_ap, ALU.mult, ["OSB"] + postkeys, [outkey])
            else:
                stt(out_ap, OSB[:, :n], scale_ap, RS[:, :n], ALU.mult, ALU.mult, ["OSB", "RS"] + postkeys, [outkey])

        def mixer_ab(l, need_ctx):
            i = l // 2
            wsrc = ab_w_in_d[i]
            with ExitStack() as s2:
                hs = dict(
                    W=sb("Wh", [128, 8, 768], BF16, s2),
                    UB=[sb("UB0", [128, 8, 512], BF16, s2)],
                    TR=[sb(f"TR{j}", [128, 512], F32, s2) for j in range(2)],
                    OSB=sb("OSB", [128, 512], F32, s2), SQh=sb("SQh", [128, 512], F32, s2),
                    RS=sb("RS", [128, 512], F32, s2),
                )
                Qt = sb("Qt", [128, 512], BF16, s2)
                Kt = sb("Kt", [128, 512], BF16, s2)
                V = sb("V", [128, 18, 128], BF16, s2)
                SGN = sb("SGN", [128, NT], BF16, s2)
                QDF = sb("QDF", [128, NT], BF16, s2)
                QDB = sb("QDB", [128, NT], BF16, s2)
                ATT = sb("ATT", [128, NT], BF16, s2)
                SFb = sb("SFb", [128, 18, 128], BF16, s2)
                SBb = sb("SBb", [128, 18, 128], BF16, s2)
                CUR = [sb(f"CUR{j}", [128, 128], F32, s2) for j in range(4)]
                KD = [sb(f"KD{j}", [128, 512], BF16, s2) for j in range(4)]
                KTT = [sb(f"KTT{j}", [128, 4, 128], BF16, s2) for j in range(2)]

                def Qf(b):
                    return Qt[:, :BLOCKS[b][1]], "qt"

                def Kf(b):
                    return Kt[:, :BLOCKS[b][1]], "kt"

                def Qf64(b):
                    return Qt[0:64, :BLOCKS[b][1]], "qt"

                def Kf64(b):
                    return Kt[0:64, :BLOCKS[b][1]], "kt"

                def run_head(h, ph):
                    isA = h < 4
                    dk = 128 if isA else 64
                    hh = h if isA else h - 4
                    if isA:
                        RET = ph["RET"]
                        P.dma("sp", RET[:], ret_d[:, hh, :, :], [], ["RET"])
                    else:
                        LRT, WLRb, LS, LS2, EXS, TAB, TOT, DEC = (ph[k_] for k_ in ("LRT", "WLRb", "LS", "LS2", "EXS", "TAB", "TOT", "DEC"))
                    P.op("dve", lambda: nc.vector.memset(SFb[:, 16, :], 0.0), [], ["SFb"])
                    P.op("dve", lambda: nc.vector.memset(SBb[:, 17, :], 0.0), [], ["SBb"])

                    def pass1(b):
                        t0, n, grp = BLOCKS[b]
                        nch = n // 128
                        c0 = t0 // 128
                        if (not isA) and DBG_B < 1:
                            return
                        qv = Qt[0:dk, :n]
                        kv = Kt[0:dk, :n]
                        if isA:
                            tabs = [RET[:, j, :].unsqueeze(1).broadcast_to([128, nch, 128]) for j in range(6)]
                            tkeys = ["RET"]

                            def rr(ap):
                                return ap.rearrange("p (c d) -> p c d", d=128)
                        else:
                            psg, pkg = nb()
                            for c in range(nch):
                                for di in range(2):
                                    mm(psg[:, c * 128 + di * 64:c * 128 + di * 64 + 64],
                                       LRT[di][:, c * 128:(c + 1) * 128], WLRb[:, di, hh * 64:(hh + 1) * 64],
                                       True, True, [f"lrt{di}", "WLRb"], [pkg])
                            act(EXS[:, :n], psg[:, :n], AF.Exp, [pkg], ["EXS"], scale=-1.0)
                            ex4 = EXS[:, :n].rearrange("p (c a d) -> p c a d", a=2, d=64)
                            act(LS[:, 0:nch, :, :], ex4, AF.Ln, ["EXS"], ["LS"], bias=ONEC[:, 0:1])
                            act(LS2[:, 0:nch, 0, :], ex4[:, :, 1, :], AF.Ln, ["EXS"], ["LS2"], bias=ONEC[:, 0:1])
                            act(LS2[:, 0:nch, 1, :], ex4[:, :, 0, :], AF.Ln, ["EXS", "LS2"], ["LS2"], bias=ONEC[:, 0:1])
                            if DBG_C < "b":
                                return
                            psf, pkf = nb()
                            psb, pkb = nb()
                            psp, pkp = nb()
                            for c in range(nch):
                                mm(psf[:, c * 128:(c + 1) * 128], LS[:, c, :, :].rearrange("p a d -> p (a d)"), TRIF, True, True, ["LS", "CST"], [pkf])
                            for c in range(nch):
                                mm(psb[:, c * 128:(c + 1) * 128], LS2[:, c, :, :].rearrange("p a d -> p (a d)"), TRIB, True, True, ["LS2", "CST2"], [pkb])
                            for c in range(nch):
                                mm(psp[:, c * 128:(c + 1) * 128], LS2[:, c, :, :].rearrange("p a d -> p (a d)"), TRIBP, True, True, ["LS2", "CST2"], [pkp])
                            if DBG_C < "c":
                                return
                            f3 = psf[0:64, :n].rearrange("p (c d) -> p c d", d=128)
                            b3 = psb[0:64, :n].rearrange("p (c d) -> p c d", d=128)
                            cp(TOT[0:64, 0:nch, 0:1], f3[:, :, 127:128], [pkf], ["TOT"])
                            cp(TOT[0:64, 0:nch, 1:2], b3[:, :, 0:1], [pkb, "TOT"], ["TOT"])
                            if DBG_C < "d":
                                return
                            act(TAB[0][0:64, :n], psf[0:64, :n], AF.Exp, [pkf, "TOT"], ["TAB0"])
                            act(TAB[1][0:64, :n], psf[0:64, :n], AF.Exp, [pkf, "TOT"], ["TAB1"], scale=-1.0)
                            act(TAB[3][0:64, :n], psp[0:64, :n], AF.Exp, [pkp, "TOT"], ["TAB3"])
                            act(TAB[4][0:64, :n], psb[0:64, :n], AF.Exp, [pkb, "TOT"], ["TAB4"], scale=-1.0)
                            if DBG_C < "e":
                                return
                            for c in range(nch):
                                act(TAB[2][0:64, c * 128:(c + 1) * 128], psf[0:64, c * 128:(c + 1) * 128], AF.Exp,
                                    [pkf, "TOT"], ["TAB2"], scale=-1.0, bias=TOT[0:64, c, 0:1])
                                act(TAB[5][0:64, c * 128:(c + 1) * 128], psb[0:64, c * 128:(c + 1) * 128], AF.Exp,
                                    [pkb, "TOT"], ["TAB5"], scale=-1.0, bias=TOT[0:64, c, 1:2])
                            act(DEC[0:64, c0:c0 + nch, :], TOT[0:64, 0:nch, :], AF.Exp, ["TOT"], ["DEC"])
                            tabs = [TAB[j][0:64, :n] for j in range(6)]
                            tkeys = [f"TAB{j}" for j in range(6)]

                            def rr(ap):
                                return ap
                        if (not isA) and DBG_B < 2:
                            return
                        tt(rr(QDF[0:dk, t0:t0 + n]), rr(qv), tabs[0], ALU.mult, ["qt"] + tkeys, [f"qdf{b}"])
                        tt(rr(QDB[0:dk, t0:t0 + n]), rr(qv), tabs[3], ALU.mult, ["qt"] + tkeys, [f"qdb{b}"])
                        tt(rr(KD[0][0:dk, :n]), rr(kv), tabs[1], ALU.mult, ["kt"] + tkeys, ["KD0"])
                        tt(rr(KD[1][0:dk, :n]), rr(kv), tabs[4], ALU.mult, ["kt"] + tkeys, ["KD1"])
                        tt(rr(KD[2][0:dk, :n]), rr(kv), tabs[2], ALU.mult, ["kt"] + tkeys, ["KD2"])
                        tt(rr(KD[3][0:dk, :n]), rr(kv), tabs[5], ALU.mult, ["kt"] + tkeys, ["KD3"])
                        psa, pka = nb()
                        psb2, pkb2 = nb()
                        for c in range(nch):
                            sl = slice(c * 128, (c + 1) * 128)
                            gsl = slice(t0 + c * 128, t0 + (c + 1) * 128)
                            mm(psa[:, sl], KD[0][0:dk, sl], QDF[0:dk, gsl], True, True, ["KD0", f"qdf{b}"], [pka])
                            mm(psb2[:, sl], KD[1][0:dk, sl], QDB[0:dk, gsl], True, True, ["KD1", f"qdb{b}"], [pkb2])
                        cp(ATT[:, t0:t0 + n], psa[:, :n], [pka], [f"att{b}"], eng="act")
                        P.op("dve", lambda: nc.vector.copy_predicated(out=ATT[:, t0:t0 + n], mask=MASK[:, :n],
                                                                       data=psb2[:, :n]),
                             [pkb2, "MASK", f"att{b}"], [f"att{b}"])
                        if (not isA) and DBG_B < 3:
                            return
                        for di in range(2):
                            psk, pkk = nb()
                            for c in range(nch):
                                mm(psk[:, c * 128:c * 128 + dk], KD[2 + di][0:dk, c * 128:(c + 1) * 128], IDB[0:dk, 0:dk],
                                   True, True, [f"KD{2 + di}", "CSTB"], [pkk])
                            cp(KTT[di][:, 0:nch, 0:dk], psk[:, :n].rearrange("p (c d) -> p c d", d=128)[:, :, 0:dk],
                               [pkk], [f"KTT{di}"], eng="act")
                            psd, pkd = nb()
                            for c in range(nch):
                                mm(psd[0:dk, c * 128:(c + 1) * 128], KTT[di][:, c, 0:dk], V[:, c0 + c, :], True, True,
                                   [f"KTT{di}", f"vb{b}"], [pkd])
                            d3 = psd[0:dk, :n].rearrange("p (c d) -> p c d", d=128)
                            if di == 0:
                                if grp == 0:
                                    m = nch if c0 + nch < 16 else nch - 1
                                    cp(SFb[0:dk, c0 + 1:c0 + 1 + m, :], d3[:, 0:m, :], [pkd], ["SFb"])
                                else:
                                    cp(SFb[0:dk, 17, :], d3[:, 0, :], [pkd], ["SFb"])
                                    cp(SFb[0:dk, 0, :], d3[:, 1, :], [pkd], ["SFb"])
                            else:
                                if c0 == 0:
                                    cp(SBb[0:dk, 0:nch - 1, :], d3[:, 1:nch, :], [pkd], ["SBb"])
                                else:
                                    cp(SBb[0:dk, c0 - 1:c0 - 1 + nch, :], d3[:, :, :], [pkd], ["SBb"])

                    if isA:
                        cols = dict(q=hh * 128, k=512 + hh * 128, v=1024 + hh * 128, g=1536 + hh * 128)
                        hs["ROPE"] = ph["ROPE"]
                        in_proj_head(l, hs, "A", wsrc, cols, Qf, Kf, V, SGN, GN[:, i, h:h + 1], ropeA_d, dk ** -0.5,
                                     after_block=pass1)
                    else:
                        cols = dict(q=2048 + hh * 64, k=2304 + hh * 64, v=2560 + hh * 128, g=3072 + hh * 128)
                        hs["ROPE"] = None
                        in_proj_head(l, hs, "B", wsrc, cols, Qf64, Kf64, V, SGN, GN[:, i, h:h + 1], None, dk ** -0.5,
                                     LRT=LRT, lr_cols=3584, after_block=pass1)
                    if (not isA) and DBG_B < 4:
                        return
                    for di, (ST, order, skey) in enumerate(((SFb, ORDER_F, "SFb"), (SBb, ORDER_B, "SBb"))):
                        c_a, c_b = CUR[2 * di], CUR[2 * di + 1]
                        ka, kb_ = f"CUR{2 * di}", f"CUR{2 * di + 1}"
                        P.op("dve", lambda c_a=c_a: nc.vector.memset(c_a[:], 0.0), [], [ka])
                        for oi in range(17):
                            nn, nx = order[oi], order[oi + 1]
                            if isA:
                                g_ = 1.0 - 2.0 ** (-((5.0 if di == 0 else 5.5) + hh))
                                sc_ = float(np.float32(g_) ** 128)
                            else:
                                sc_ = DEC[0:64, nn, di:di + 1]
                            stt(c_b[0:dk, :], c_a[0:dk, :], sc_, ST[0:dk, nx, :], ALU.mult, ALU.add,
                                [ka, skey] + ([] if isA else ["DEC"]), [kb_])
                            cp(ST[0:dk, nx, :], c_b[0:dk, :], [kb_], [skey], eng="act")
                            c_a, c_b, ka, kb_ = c_b, c_a, kb_, ka
                    if (not isA) and DBG_B < 5:
                        return
                    for b in range(5 if need_ctx else 4):
                        t0, n, grp = BLOCKS[b]
                        nch = n // 128
                        c0 = t0 // 128
                        pso, pko = nb()
                        for c in range(nch):
                            sl = slice(c * 128, (c + 1) * 128)
                            gsl = slice(t0 + c * 128, t0 + (c + 1) * 128)
                            mm(pso[:, sl], V[:, c0 + c, :], ATT[:, gsl], True, False, [f"vb{b}", f"att{b}"], [pko])
                            mm(pso[:, sl], SFb[0:dk, c0 + c, :], QDF[0:dk, gsl], False, False, ["SFb", f"qdf{b}"], [pko])
                            mm(pso[:, sl], SBb[0:dk, c0 + c, :], QDB[0:dk, gsl], False, True, ["SBb", f"qdb{b}"], [pko])
                        head_norm(pso, pko, n, isA, hs, UY[:, h, t0:t0 + n], f"u{h}b{b}", SGN[:, t0:t0 + n], [f"sgn_b{b}"])

                with ExitStack() as s3:
                    ph = dict(ROPE=sb("ROPE", [128, 4, 512], F32, s3), RET=sb("RET", [128, 6, 128], F32, s3))
                    for h in range(min(4, DBG_HEADS)):
                        run_head(h, ph)
                P.barrier()
                with ExitStack() as s3:
                    WLR = sb("WLR", [32, 2, 256], F32, s3)
                    ph = dict(
                        LRT=[sb(f"LRT{j}", [32, 512], BF16, s3) for j in range(2)],
                        WLRb=sb("WLRb", [32, 2, 256], BF16, s3),
                        LS=sb("LS", [128, 4, 2, 64], F32, s3),
                        LS2=sb("LS2", [128, 4, 2, 64], F32, s3),
                        EXS=sb("EXS", [128, 512], F32, s3),
                        TAB=[sb(f"TAB{j}", [128, 512], BF16, s3) for j in range(6)],
                        TOT=sb("TOT", [128, 4, 2], F32, s3),
                        DEC=sb("DEC", [128, 18, 2], F32, s3),
                    )
                    P.dma("sp", WLR[:], wlr_d[:, i, :, :], [], ["WLR"])
                    cp(ph["WLRb"][:], WLR[:], ["WLR"], ["WLRb"])
                    for di in range(2):
                        P.op("dve", lambda di=di: nc.vector.memset(ph["LRT"][di][:], 1.0), [], [f"lrt{di}"])
                    for h in range(4, min(8, DBG_HEADS)):
                        run_head(h, ph)
            P.barrier()

        def mixer_c(l, need_ctx):
            i = l // 2
            wsrc = c_w_qkv_d[i]
            with ExitStack() as s2:
                hs = dict(
                    W=sb("Wh", [128, 8, 640], BF16, s2),
                    UB=[sb(f"UB{j}", [128, 8, 512], BF16, s2) for j in range(2)],
                    TR=[sb(f"TR{j}", [128, 512], F32, s2) for j in range(2)],
                    ROPE=sb("ROPE", [128, 4, 512], F32, s2),
                    OSB=sb("OSB", [128, 512], F32, s2), SQh=sb("SQh", [128, 512], F32, s2),
                    RS=sb("RS", [128, 512], F32, s2),
                )
                Q = sb("Q", [128, NT], BF16, s2)
                K = sb("K", [128, NT], BF16, s2)
                V = sb("V", [128, 18, 128], BF16, s2)
                E = [sb(f"E{j}", [128, 512], BF16, s2) for j in range(4)]
                R1 = sb("R1", [128, 512], F32, s2)
                R2 = sb("R2", [128, 512], F32, s2)
                A1 = sb("A1", [128, 512], F32, s2)
                for h in range(8):
                    cols = dict(q=h * 128, k=1024 + h * 128, v=2048 + h * 128)
                    in_proj_head(l, hs, "C", wsrc, cols,
                                 lambda b: (Q[:, BLOCKS[b][0]:BLOCKS[b][0] + BLOCKS[b][1]], f"qb{b}"),
                                 lambda b: (K[:, BLOCKS[b][0]:BLOCKS[b][0] + BLOCKS[b][1]], f"kb{b}"),
                                 V, None, None, ropeC_d, 0.125)
                    for b in range(5 if need_ctx else 4):
                        t0, n, grp = BLOCKS[b]
                        kchunks = list(range(18)) if grp == 0 else [16, 17]
                        acc = [(PS[j], f"ps{j}") for j in range(4)]
                        def issue_S(ci):
                            c = kchunks[ci]
                            kb_ = c // 4 if c < 16 else 4
                            ksl = slice(c * 128, (c + 1) * 128)
                            for comp in range(2):
                                sj = 4 + 2 * (ci % 2) + comp
                                rsl = slice(comp * 64, (comp + 1) * 64)
                                mm(PS[sj][:, :n], K[rsl, ksl], Q[rsl, t0:t0 + n], True, True, [f"kb{kb_}", f"qb{b}"], [f"ps{sj}"])

                        def issue_rest(ci):
                            c = kchunks[ci]
                            first, last = ci == 0, ci == len(kchunks) - 1
                            kb_ = c // 4 if c < 16 else 4
                            for comp in range(2):
                                sj = 4 + 2 * (ci % 2) + comp
                                ej = 2 * (ci % 2) + comp
                                act(E[ej][:, :n], PS[sj][:, :n], AF.Exp, [f"ps{sj}"], [f"E{ej}"])
                            for comp in range(2):
                                ej = 2 * (ci % 2) + comp
                                po, pko = acc[2 * comp]
                                pd, pkd = acc[2 * comp + 1]
                                mm(po[:, :n], V[:, c, :], E[ej][:, :n], first, last, [f"vb{kb_}", f"E{ej}"], [pko])
                                mm(pd[:, :n], ONESB, E[ej][:, :n], first, last, ["CSTB", f"E{ej}"], [pkd])

                        issue_S(0)
                        for ci in range(len(kchunks)):
                            if ci + 1 < len(kchunks):
                                issue_S(ci + 1)
                            issue_rest(ci)
                        act(R1[:, :n], acc[1][0][:, :n], AF.Ln, [acc[1][1]], ["R1"])
                        act(R1[:, :n], R1[:, :n], AF.Exp, ["R1"], ["R1"], scale=-1.0)
                        act(R2[:, :n], acc[3][0][:, :n], AF.Ln, [acc[3][1]], ["R2"])
                        act(R2[:, :n], R2[:, :n], AF.Exp, ["R2"], ["R2"], scale=-1.0)
                        tt(A1[:, :n], acc[0][0][:, :n], R1[:, :n], ALU.mult, [acc[0][1], "R1"], ["A1"])
                        tt(R2[:, :n], acc[2][0][:, :n], R2[:, :n], ALU.mult, [acc[2][1], "R2"], ["R2"])
                        stt(hs["OSB"][:, :n], R2[:, :n], LAM[:, i, h:h + 1], A1[:, :n], ALU.mult, ALU.add,
                            ["R2", "A1", "LAM"], ["OSB"])
                        psc[0] = 4
                        head_norm(None, None, n, False, hs, UY[:, h, t0:t0 + n], f"u{h}b{b}", None, ["SUBG"],
                                  scale_ap=SUBG[:, i, h:h + 1], src_sb=True)
                        psc[0] = 4
            P.barrier()

        def moe(l, need_ctx):
            nblk = 5 if need_ctx else 4
            with ExitStack() as s2:
                WB = [sb(f"WB{j}", [128, 4096], BF16, s2) for j in range(4)]
                WCT = sb("WCT", [32, NT], BF16, s2)
                SELE = sb("SELE", [32, 32 * 128], BF16, s2)
                BC = [sb(f"BC{j}", [128, NT], BF16, s2) for j in range(2)]
                H = [sb(f"H{j}", [128, 4, 512], BF16, s2) for j in range(2)]
                SG = [sb(f"SG{j}", [128, 512], F32, s2) for j in range(2)]
                TF = [sb(f"TF{j}", [128, 512], F32, s2) for j in range(3)]
                SQ = [sb(f"SQ{j}", [128, 512], F32, s2) for j in range(2)]
                MEAN = sb("MEAN", [128, 512], F32, s2)
                RSTD = sb("RSTD", [128, 512], F32, s2)
                LG = sb("LG", [128, 36], F32, s2)
                SM = sb("SM", [128, 16], F32, s2)
                GOH = sb("GOH", [128, 4], F32, s2)
                GE = sb("GE", [128, 4], F32, s2)
                ET = sb("ET", [128, 32], F32, s2)
                ES = sb("ES", [128, 8], F32, s2)
                T8 = sb("T8", [128, 8], F32, s2)
                SEL = sb("SEL", [128, 8], F32, s2)
                EX = sb("EX", [128, 8], F32, s2)
                WC = sb("WC", [128, 32], F32, s2)
                P.dma("sp", SELE[:], sele_d, [], ["SELE"])
                WR = sb("WRl", [128, 8, 36], F32, s2)
                P.dma("sp", WR[:], wr_d[:, l, :, :], [], ["WR"])
                items = []
                for e in range(32):
                    items.append(("g", e, wg_d[l, e].rearrange("(k p) n -> p k n", p=128)))
                    items.append(("u", e, wu_d[l, e].rearrange("(k p) n -> p k n", p=128)))
                    items.append(("d", e, wd_d[l, e].rearrange("(k p) n -> p k n", p=128)))

                def load_item(j):
                    if j >= len(items):
                        return
                    kind, e, src = items[j]
                    if kind == "d":
                        dst = WB[j % 4][:, :].rearrange("p (k n) -> p k n", n=1024)
                    else:
                        dst = WB[j % 4][:, :].rearrange("p (k n) -> p k n", n=512)
                    P.dma("pool", dst, src, [], [f"WB{j % 4}"])

                for j in range(4):
                    load_item(j)
                ti = 0
                for b in range(nblk):
                    t0, n, grp = BLOCKS[b]
                    ntile = n // 128
                    pr = [nb() for _ in range(ntile)]
                    for k in range(8):
                        tf = TF[ti % 3]
                        tk = f"TF{ti % 3}"
                        ti += 1
                        act(tf[:, :n], XT[:, k, t0:t0 + n], AF.Identity, [f"x{k}b{b}", "MOD"], [tk],
                            scale=mod(l, 4, k, grp), bias=mod(l, 3, k, grp))
                        cp(UY[:, k, t0:t0 + n], tf[:, :n], [tk], [f"u{k}b{b}"])
                        for tI in range(ntile):
                            mm(pr[tI][0][:, 0:36], tf[:, tI * 128:(tI + 1) * 128], WR[:, k, :], k == 0, False,
                               [tk, "WR"], [pr[tI][1]])
                    for tI in range(ntile):
                        ps, pk = pr[tI]
                        mm(ps[:, 0:36], ONES1, RB[0:1, l, :], False, True, ["CST", "RB"], [pk])
                        cp(LG[:], ps[:, 0:36], [pk], ["LG"])
                        R_ = ["LG", "SM", "GOH", "GE", "ET", "ES", "T8", "SEL", "EX", "WC"]

                        def dv(fn):
                            P.op("dve", fn, R_, R_)
                        dv(lambda: nc.vector.tensor_reduce(out=SM[:, 0:1], in_=LG[:, 0:4], axis=AX.X, op=ALU.max))
                        dv(lambda: nc.vector.tensor_scalar(out=GOH[:], in0=LG[:, 0:4], scalar1=SM[:, 0:1], scalar2=None,
                                                           op0=ALU.is_equal))
                        dv(lambda: nc.vector.tensor_scalar(out=SM[:, 1:2], in0=SM[:, 0:1], scalar1=-1.0, scalar2=None,
                                                           op0=ALU.mult))
                        act(GE[:], LG[:, 0:4], AF.Exp, R_, R_, bias=SM[:, 1:2])
                        dv(lambda: nc.vector.tensor_reduce(out=SM[:, 2:3], in_=GE[:], axis=AX.X, op=ALU.add))
                        dv(lambda: nc.vector.reciprocal(out=SM[:, 3:4], in_=SM[:, 2:3]))
                        dv(lambda: nc.vector.tensor_tensor(
                            out=ET[:].rearrange("p (g e) -> p g e", e=8),
                            in0=LG[:, 4:36].rearrange("p (g e) -> p g e", e=8),
                            in1=GOH[:].unsqueeze(2).broadcast_to([128, 4, 8]), op=ALU.mult))
                        dv(lambda: nc.vector.tensor_reduce(out=ES[:], in_=ET[:].rearrange("p (g e) -> p e g", e=8),
                                                           axis=AX.X, op=ALU.add))
                        dv(lambda: nc.vector.max(out=T8[:], in_=ES[:]))
                        dv(lambda: nc.vector.tensor_scalar(out=SEL[:], in0=ES[:], scalar1=T8[:, 1:2], scalar2=None,
                                                           op0=ALU.is_ge))
                        dv(lambda: nc.vector.tensor_scalar(out=SM[:, 4:5], in0=T8[:, 0:1], scalar1=-1.0, scalar2=None,
                                                           op0=ALU.mult))
                        act(EX[:], ES[:], AF.Exp, R_, R_, bias=SM[:, 4:5])
                        dv(lambda: nc.vector.tensor_tensor(out=EX[:], in0=EX[:], in1=SEL[:], op=ALU.mult))
                        dv(lambda: nc.vector.tensor_reduce(out=SM[:, 5:6], in_=EX[:], axis=AX.X, op=ALU.add))
                        dv(lambda: nc.vector.reciprocal(out=SM[:, 6:7], in_=SM[:, 5:6]))
                        dv(lambda: nc.vector.tensor_tensor(out=SM[:, 7:8], in0=SM[:, 6:7], in1=SM[:, 3:4], op=ALU.mult))
                        dv(lambda: nc.vector.tensor_scalar(out=EX[:], in0=EX[:], scalar1=SM[:, 7:8], scalar2=None,
                                                           op0=ALU.mult))
                        dv(lambda: nc.vector.tensor_tensor(
                            out=WC[:].rearrange("p (g e) -> p g e", e=8),
                            in0=GOH[:].unsqueeze(2).broadcast_to([128, 4, 8]),
                            in1=EX[:].unsqueeze(1).broadcast_to([128, 4, 8]), op=ALU.mult))
                        pt, pkt = nb()
                        P.op("pe", lambda: nc.tensor.transpose(out=pt[0:32, 0:128], in_=WC[:], identity=IDF),
                             R_ + ["CST"], [pkt])
                        cp(WCT[:, t0 + tI * 128:t0 + (tI + 1) * 128], pt[0:32, 0:128], [pkt], [f"wct{b}"], eng="act")
                for b in range(nblk):
                    t0, n, grp = BLOCKS[b]
                    for k in range(8):
                        xk = XT[:, k, t0:t0 + n]
                        if k % 2 == 0:
                            act(xk, xk, AF.Copy, [f"x{k}b{b}"], [f"x{k}b{b}"], scale=ALPHA)
                        else:
                            ts(xk, xk, ALPHA, ALU.mult, [f"x{k}b{b}"], [f"x{k}b{b}"])
                hi = 0
                for e in range(32):
                    j0 = 3 * e
                    WG = WB[j0 % 4][:, :].rearrange("p (k n) -> p k n", n=512)
                    WU = WB[(j0 + 1) % 4][:, :].rearrange("p (k n) -> p k n", n=512)
                    WD = WB[(j0 + 2) % 4][:, :].rearrange("p (k n) -> p k n", n=1024)
                    kg, ku, kd = f"WB{j0 % 4}", f"WB{(j0 + 1) % 4}", f"WB{(j0 + 2) % 4}"
                    bc = BC[e % 2]
                    bck = f"BC{e % 2}"
                    for b in range(nblk):
                        t0, n, grp = BLOCKS[b]
                        ps, pk = nb()
                        mm(ps[:, :n], SELE[:, e * 128:(e + 1) * 128], WCT[:, t0:t0 + n], True, True, ["SELE", f"wct{b}"], [pk])
                        cp(bc[:, t0:t0 + n], ps[:, :n], [pk], [bck + f"b{b}"], eng="act")
                    for b in range(nblk):
                        t0, n, grp = BLOCKS[b]
                        Hb = H[hi % 2]
                        hk = f"H{hi % 2}"
                        hi += 1
                        for fc in range(4):
                            pg, pkg = nb()
                            pu, pku = nb()
                            for k in range(8):
                                mm(pg[:, :n], WG[:, k, fc * 128:(fc + 1) * 128], UY[:, k, t0:t0 + n], k == 0, k == 7,
                                   [kg, f"u{k}b{b}"], [pkg])
                            for k in range(8):
                                mm(pu[:, :n], WU[:, k, fc * 128:(fc + 1) * 128], UY[:, k, t0:t0 + n], k == 0, k == 7,
                                   [ku, f"u{k}b{b}"], [pku])
                            sg = SG[fc % 2]
                            act(sg[:, :n], pg[:, :n], AF.Silu, [pkg], [f"SG{fc % 2}"])
                            tt(sg[:, :n], sg[:, :n], pu[:, :n], ALU.mult, [f"SG{fc % 2}", pku], [f"SG{fc % 2}"])
                            tt(Hb[:, fc, :n], sg[:, :n], bc[:, t0:t0 + n], ALU.mult, [f"SG{fc % 2}", bck + f"b{b}"],
                               [hk + f"f{fc}"])
                        if b == nblk - 1:
                            load_item(j0 + 4)
                            load_item(j0 + 5)
                        for oc in range(8):
                            py, pky = nb()
                            for fc in range(4):
                                mm(py[:, :n], WD[:, fc, oc * 128:(oc + 1) * 128], Hb[:, fc, :n], fc == 0, fc == 3,
                                   [kd, hk + f"f{fc}"], [pky])
                            xk = XT[:, oc, t0:t0 + n]
                            stt(xk, py[:, :n], mod(l, 5, oc, grp), xk, ALU.mult, ALU.add, [pky, "MOD", f"x{oc}b{b}"],
                                [f"x{oc}b{b}"])
                    load_item(j0 + 6)
                for b in range(nblk):
                    ln_block(l, 1, b, (SQ, MEAN, RSTD))
            P.barrier()

        BREG = []

        def moe_sparse(l, need_ctx):
            if not BREG:
                BREG.append(nc.gpsimd.to_reg(4 * 32 * 128 - 1))
            nblk = 5 if need_ctx else 4
            ntiles = 18 if need_ctx else 16
            NB_ = (ntiles * 128 * 2) // 128 + 32
            POOL = (mybir.EngineType.Pool,)
            with ExitStack() as s2:
                AST = sb("AST", [128, 18, 32], BF16, s2)
                WCS = sb("WCS", [128, 18, 32], F32, s2)
                CSS = sb("CSS", [128, 18, 32], F32, s2)
                PRE = sb("PRE", [128, 19, 32], F32, s2)
                PST = sb("PST", [128, 32], F32, s2)
                PEN = sb("PEN", [128, 32], F32, s2)
                DESTF = sb("DESTF", [128, 18, 2], F32, s2)
                DEST = sb("DEST", [128, 18, 2], U32, s2)
                WSEL = sb("WSEL", [128, 18, 2], F32, s2)
                IDXW = sb("IDXW", [128, 72], U32, s2)
                SQ = [sb(f"SQ{j}", [128, 512], F32, s2) for j in range(2)]
                MEAN = sb("MEAN", [128, 512], F32, s2)
                RSTD = sb("RSTD", [128, 512], F32, s2)
                with ExitStack() as s3:
                    TF = [sb(f"TF{j}", [128, 512], F32, s3) for j in range(3)]
                    WR = sb("WRl", [128, 8, 36], F32, s3)
                    LG = sb("LG", [128, 36], F32, s3)
                    SM = sb("SM", [128, 16], F32, s3)
                    GOH = sb("GOH", [128, 4], F32, s3)
                    GE = sb("GE", [128, 4], F32, s3)
                    ET = sb("ET", [128, 32], F32, s3)
                    ES = sb("ES", [128, 8], F32, s3)
                    T8 = sb("T8", [128, 8], F32, s3)
                    SEL = sb("SEL", [128, 8], F32, s3)
                    EX = sb("EX", [128, 8], F32, s3)
                    NBK = sb("NBK", [128, 32, 36], F32, s3)
                    CNT = sb("CNT", [128, 32], F32, s3)
                    PADB = sb("PADB", [32, 128], F32, s3)
                    DP1 = sb("DP1", [128, 32], F32, s3)
                    EQ = sb("EQ", [128, 32], F32, s3)
                    BLE = sb("BLE", [128, 72, 32], F32, s3)
                    BLKB = sb("BLKB", [128, 72], F32, s3)
                    P.dma("sp", WR[:], wr_d[:, l, :, :], [], ["WR"])
                    ti = 0
                    for b in range(nblk):
                        t0, n, grp = BLOCKS[b]
                        ntile = n // 128
                        pr = [nb() for _ in range(ntile)]
                        for k in range(8):
                            tf = TF[ti % 3]
                            tk = f"TF{ti % 3}"
                            ti += 1
                            act(tf[:, :n], XT[:, k, t0:t0 + n], AF.Identity, [f"x{k}b{b}", "MOD"], [tk],
                                scale=mod(l, 4, k, grp), bias=mod(l, 3, k, grp))
                            cp(UY[:, k, t0:t0 + n], tf[:, :n], [tk], [f"u{k}b{b}"])
                            for tI in range(ntile):
                                mm(pr[tI][0][:, 0:36], tf[:, tI * 128:(tI + 1) * 128], WR[:, k, :], k == 0, False,
                                   [tk, "WR"], [pr[tI][1]])
                        for tI in range(ntile):
                            gi = t0 // 128 + tI
                            ps, pk = pr[tI]
                            mm(ps[:, 0:36], ONES1, RB[0:1, l, :], False, True, ["CST", "RB"], [pk])
                            cp(LG[:], ps[:, 0:36], [pk], ["LG"])
                            R_ = ["LG", "SM", "GOH", "GE", "ET", "ES", "T8", "SEL", "EX"]

                            def dv(fn, extra_w=()):
                                P.op("dve", fn, R_, R_ + list(extra_w))
                            dv(lambda: nc.vector.tensor_reduce(out=SM[:, 0:1], in_=LG[:, 0:4], axis=AX.X, op=ALU.max))
                            dv(lambda: nc.vector.tensor_scalar(out=GOH[:], in0=LG[:, 0:4], scalar1=SM[:, 0:1], scalar2=None,
                                                               op0=ALU.is_equal))
                            dv(lambda: nc.vector.tensor_scalar(out=SM[:, 1:2], in0=SM[:, 0:1], scalar1=-1.0, scalar2=None,
                                                               op0=ALU.mult))
                            act(GE[:], LG[:, 0:4], AF.Exp, R_, R_, bias=SM[:, 1:2])
                            dv(lambda: nc.vector.tensor_reduce(out=SM[:, 2:3], in_=GE[:], axis=AX.X, op=ALU.add))
                            dv(lambda: nc.vector.reciprocal(out=SM[:, 3:4], in_=SM[:, 2:3]))
                            dv(lambda: nc.vector.tensor_tensor(
                                out=ET[:].rearrange("p (g e) -> p g e", e=8),
                                in0=LG[:, 4:36].rearrange("p (g e) -> p g e", e=8),
                                in1=GOH[:].unsqueeze(2).broadcast_to([128, 4, 8]), op=ALU.mult))
                            dv(lambda: nc.vector.tensor_reduce(out=ES[:], in_=ET[:].rearrange("p (g e) -> p e g", e=8),
                                                               axis=AX.X, op=ALU.add))
                            dv(lambda: nc.vector.max(out=T8[:], in_=ES[:]))
                            dv(lambda: nc.vector.tensor_scalar(out=SEL[:], in0=ES[:], scalar1=T8[:, 1:2], scalar2=None,
                                                               op0=ALU.is_ge))
                            dv(lambda: nc.vector.tensor_scalar(out=SM[:, 4:5], in0=T8[:, 0:1], scalar1=-1.0, scalar2=None,
                                                               op0=ALU.mult))
                            act(EX[:], ES[:], AF.Exp, R_, R_, bias=SM[:, 4:5])
                            dv(lambda: nc.vector.tensor_tensor(out=EX[:], in0=EX[:], in1=SEL[:], op=ALU.mult))
                            dv(lambda: nc.vector.tensor_reduce(out=SM[:, 5:6], in_=EX[:], axis=AX.X, op=ALU.add))
                            dv(lambda: nc.vector.reciprocal(out=SM[:, 6:7], in_=SM[:, 5:6]))
                            dv(lambda: nc.vector.tensor_tensor(out=SM[:, 7:8], in0=SM[:, 6:7], in1=SM[:, 3:4], op=ALU.mult))
                            dv(lambda: nc.vector.tensor_scalar(out=EX[:], in0=EX[:], scalar1=SM[:, 7:8], scalar2=None,
                                                               op0=ALU.mult))
                            dv(lambda: nc.vector.tensor_tensor(
                                out=WCS[:, gi, :].rearrange("p (g e) -> p g e", e=8),
                                in0=GOH[:].unsqueeze(2).broadcast_to([128, 4, 8]),
                                in1=EX[:].unsqueeze(1).broadcast_to([128, 4, 8]), op=ALU.mult), ["WCS"])
                            dv(lambda: nc.vector.tensor_tensor(
                                out=AST[:, gi, :].rearrange("p (g e) -> p g e", e=8),
                                in0=GOH[:].unsqueeze(2).broadcast_to([128, 4, 8]),
                                in1=SEL[:].unsqueeze(1).broadcast_to([128, 4, 8]), op=ALU.mult), ["AST"])
                    for g4 in range(0, ntiles, 4):
                        m4 = min(4, ntiles - g4)
                        ps, pk = nb()
                        for j in range(m4):
                            mm(ps[:, j * 32:(j + 1) * 32], ONESB, AST[:, g4 + j, :], True, True, ["CSTB", "AST"], [pk])
                        cp(CSS[:, g4:g4 + m4, :], ps[:, 0:m4 * 32].rearrange("p (j e) -> p j e", e=32), [pk], ["CSS"])
                    P.op("dve", lambda: nc.vector.memset(PRE[:, 0, :], 0.0), [], ["PRE"])
                    for j in range(ntiles):
                        tt(PRE[:, j + 1, :], PRE[:, j, :], CSS[:, j, :], ALU.add, ["PRE", "CSS"], ["PRE"])
                    ps, pk = nb()
                    for j in range(ntiles):
                        mm(ps[0:32, 0:2], AST[:, j, :], ONESB[:, 0:2], j == 0, j == ntiles - 1, ["AST", "CSTB"], [pk])
                    cp(CNT[0:32, 0:1], ps[0:32, 0:1], [pk], ["CNT"])
                    tt(NBK[0:32, 0, :], CNT[0:32, 0:1].broadcast_to([32, 36]), CST3[0:32, 0:36], ALU.is_gt,
                       ["CNT", "CST3"], ["NBK"])
                    P.op("dve", lambda: nc.vector.tensor_reduce(out=CNT[0:32, 1:2], in_=NBK[0:32, 0, :], axis=AX.X, op=ALU.add),
                         ["NBK", "CNT"], ["CNT"])
                    ts(CNT[0:32, 2:3], CNT[0:32, 1:2], 128.0, ALU.mult, ["CNT"], ["CNT"])
                    cp(PADB[:, :], CNT[0:32, 2:3].broadcast_to([32, 128]), ["CNT"], ["PADB"])
                    ps, pk = nb()
                    mm(ps[:, 0:32], PADB[:, :], CST3[0:32, 64:96], True, True, ["PADB", "CST3"], [pk])
                    mm(ps[:, 32:64], PADB[:, :], CST3[0:32, 96:128], True, True, ["PADB", "CST3"], [pk])
                    cp(PST[:], ps[:, 0:32], [pk], ["PST"])
                    cp(PEN[:], ps[:, 32:64], [pk], ["PEN"])
                    tt(BLE[:, 0:NB_, :], PEN[:].unsqueeze(1).broadcast_to([128, NB_, 32]),
                       CST3[:, 136:136 + NB_].unsqueeze(2).broadcast_to([128, NB_, 32]), ALU.is_le, ["PEN", "CST3"], ["BLE"])
                    P.op("dve", lambda: nc.vector.tensor_reduce(out=BLKB[:, 0:NB_], in_=BLE[:, 0:NB_, :], axis=AX.X, op=ALU.add),
                         ["BLE"], ["BLKB"])
                    ts(BLKB[:, 0:NB_], BLKB[:, 0:NB_], 31.0, ALU.min, ["BLKB"], ["BLKB"], s2=128.0, op1=ALU.mult)
                    ts(BLKB[:, 0:NB_], BLKB[:, 0:NB_], CST3[:, 129:130], ALU.add, ["BLKB", "CST3"], ["BLKB"],
                       s2=float(l * 4096), op1=ALU.add)
                    blef = BLE[:, 0:3, :].rearrange("p a b -> p (a b)")[:, 0:NB_]
                    ts(blef, CST3[:, 136:136 + NB_], PEN[:, 31:32], ALU.is_ge, ["CST3", "PEN", "BLE"], ["BLE"])
                    stt(BLKB[:, 0:NB_], blef, 1.0e6, BLKB[:, 0:NB_], ALU.mult, ALU.add, ["BLE", "BLKB"], ["BLKB"])
                    cp(IDXW[:, 0:NB_], BLKB[:, 0:NB_], ["BLKB"], ["IDXW"])
                    for g4 in range(0, ntiles, 4):
                        m4 = min(4, ntiles - g4)
                        ps, pk = nb()
                        for j in range(m4):
                            mm(ps[:, j * 32:(j + 1) * 32], TRIS, AST[:, g4 + j, :], True, True, ["CSTB2", "AST"], [pk])
                        for j in range(m4):
                            gi = g4 + j
                            R2 = ["DP1", "EQ", "T8"]
                            tt(DP1[:], ps[:, j * 32:(j + 1) * 32], PRE[:, gi, :], ALU.add, [pk, "PRE"] + R2, R2)
                            tt(DP1[:], DP1[:], PST[:], ALU.add, R2 + ["PST"], R2)
                            stt(DP1[:], DP1[:], 1.0, AST[:, gi, :], ALU.add, ALU.mult, R2 + ["AST"], R2)
                            P.op("dve", lambda: nc.vector.max(out=T8[:], in_=DP1[:]), R2 + ["LG"], R2 + ["LG"])
                            ts(DESTF[:, gi, :], T8[:, 0:2], -1.0, ALU.add, R2, ["DESTF"])
                            for kk in range(2):
                                ts(EQ[:], DP1[:], T8[:, kk:kk + 1], ALU.is_equal, R2, R2)
                                tt(EQ[:], EQ[:], WCS[:, gi, :], ALU.mult, R2 + ["WCS"], R2)
                                P.op("dve", lambda kk=kk, gi=gi: nc.vector.tensor_reduce(
                                    out=WSEL[:, gi, kk:kk + 1], in_=EQ[:], axis=AX.X, op=ALU.add), R2 + ["WSEL"], R2 + ["WSEL"])
                    cp(DEST[:], DESTF[:], ["DESTF"], ["DEST"])
                P.barrier()
                with ExitStack() as s3:
                    WB = [sb(f"WB{j}", [128, 4096], BF16, s3) for j in range(4)]
                    UT = [sb(f"UT{j}", [128, 1024], BF16, s3) for j in range(2)]
                    XS = [sb(f"XS{j}", [128, 1024], BF16, s3) for j in range(4)]
                    XST = [sb(f"XST{j}", [128, 8, 128], BF16, s3) for j in range(2)]
                    H = [sb(f"H{j}", [128, 4, 128], BF16, s3) for j in range(2)]
                    SG = [sb(f"SG{j}", [128, 512], F32, s3) for j in range(2)]
                    YS = [sb(f"YS{j}", [128, 1024], F32, s3) for j in range(2)]
                    ZT = sb("ZT", [128, 1024], BF16, s3)
                    P.op("dve", lambda: nc.vector.memset(ZT[:], 0.0), [], ["ZT"])
                    for bb in range(NB_):
                        P.dma("sp", xs_d[bb * 128:(bb + 1) * 128, :], ZT[:, :], ["ZT"], [f"xsz{bb}"])
                    for gi in range(ntiles):
                        ut = UT[gi % 2]
                        uk = f"UT{gi % 2}"
                        b = gi // 4 if gi < 16 else 4
                        for half in range(2):
                            ps, pk = nb()
                            for kk in range(4):
                                k = half * 4 + kk
                                mm(ps[:, kk * 128:(kk + 1) * 128], UY[:, k, gi * 128:(gi + 1) * 128], IDB, True, True,
                                   [f"u{k}b{b}", "CSTB"], [pk])
                            cp(ut[:, half * 512:(half + 1) * 512], ps[:, :], [pk], [uk], eng="act" if half else "dve")
                        for kk in range(2):
                            P.dma_fn("pool", lambda sem, ut=ut, gi=gi, kk=kk: nc.gpsimd.indirect_dma_start(
                                out=xs_d, out_offset=bass.IndirectOffsetOnAxis(ap=DEST[:, gi, kk:kk + 1], axis=0),
                                in_=ut[:, :], in_offset=None).then_inc(sem, 16),
                                [uk, "DEST"] + [f"xsz{b_}" for b_ in range(NB_)], [f"xss{gi}_{kk}"])
                    items = []
                    for bb in range(NB_):
                        items += [("g", bb), ("u", bb), ("d", bb)]
                    def load_item(j):
                        if j >= len(items):
                            return
                        kind, bb = items[j]
                        c0_ = {"g": 0, "u": 2, "d": 4}[kind]
                        for hh_ in range(2):
                            P.dma_fn("pool", lambda sem, j=j, bb=bb, c=c0_ + hh_, hh_=hh_: nc.gpsimd.indirect_dma_start(
                                out=WB[j % 4][:, hh_ * 2048:(hh_ + 1) * 2048], out_offset=None, in_=wc_d[c],
                                in_offset=bass.IndirectOffsetOnAxis(ap=IDXW[:, bb:bb + 1], axis=0),
                                bounds_check=BREG[0], oob_is_err=False).then_inc(sem, 16),
                                ["IDXW"], [f"WB{j % 4}"])

                    for j in range(4):
                        load_item(j)
                    for bb in range(NB_):
                        j0 = 3 * bb
                        WG = WB[j0 % 4][:, :].rearrange("p (k n) -> p k n", n=512)
                        WU = WB[(j0 + 1) % 4][:, :].rearrange("p (k n) -> p k n", n=512)
                        WD = WB[(j0 + 2) % 4][:, :].rearrange("p (k n) -> p k n", n=1024)
                        kg, ku, kd = f"WB{j0 % 4}", f"WB{(j0 + 1) % 4}", f"WB{(j0 + 2) % 4}"
                        xs, xk = XS[bb % 4], f"XS{bb % 4}"
                        xst, xtk = XST[bb % 2], f"XST{bb % 2}"
                        Hb, hk = H[bb % 2], f"H{bb % 2}"
                        ys, yk = YS[bb % 2], f"YS{bb % 2}"
                        P.dma("sp", xs[:, :], xs_d[bb * 128:(bb + 1) * 128, :],
                              [f"xsz{bb}"] + [f"xss{g_}_{k_}" for g_ in range(ntiles) for k_ in range(2)], [xk])
                        for half in range(2):
                            ps, pk = nb()
                            for kk in range(4):
                                k = half * 4 + kk
                                mm(ps[:, kk * 128:(kk + 1) * 128], xs[:, k * 128:(k + 1) * 128], IDB, True, True,
                                   [xk, "CSTB"], [pk])
                            cp(xst[:, half * 4:(half + 1) * 4, :], ps[:, :].rearrange("p (k r) -> p k r", r=128), [pk], [xtk],
                               eng="act" if half else "dve")
                        pg, pkg = nb()
                        pu, pku = nb()
                        for fc in range(4):
                            for k in range(8):
                                mm(pg[:, fc * 128:(fc + 1) * 128], WG[:, k, fc * 128:(fc + 1) * 128], xst[:, k, :], k == 0, k == 7,
                                   [kg, xtk], [pkg])
                        for fc in range(4):
                            for k in range(8):
                                mm(pu[:, fc * 128:(fc + 1) * 128], WU[:, k, fc * 128:(fc + 1) * 128], xst[:, k, :], k == 0, k == 7,
                                   [ku, xtk], [pku])
                        sg = SG[bb % 2]
                        act(sg[:, :], pg[:, :], AF.Silu, [pkg], [f"SG{bb % 2}"])
                        tt(Hb[:, :, :], sg[:, :].rearrange("p (f r) -> p f r", r=128), pu[:, :].rearrange("p (f r) -> p f r", r=128),
                           ALU.mult, [f"SG{bb % 2}", pku], [hk])
                        load_item(j0 + 4)
                        load_item(j0 + 5)
                        for half in range(2):
                            py, pky = nb()
                            for fc in range(4):
                                mm(py[:, :], Hb[:, fc, :], WD[:, fc, half * 512:(half + 1) * 512], fc == 0, fc == 3,
                                   [kd, hk], [pky])
                            cp(ys[:, half * 512:(half + 1) * 512], py[:, :], [pky], [yk], eng="act" if half else "dve")
                        load_item(j0 + 6)
                        P.dma("act", ys_d[bb * 128:(bb + 1) * 128, :], ys[:, :], [yk], [f"ysd{bb}"])
                P.barrier()
                with ExitStack() as s3:
                    GG = [[sb(f"G{j}_{i_}", [128, 1024], F32, s3) for j in range(2)] for i_ in range(2)]
                    ZZ = [sb(f"Z{i_}", [128, 1024], F32, s3) for i_ in range(2)]
                    for b in range(nblk):
                        t0, n, grp = BLOCKS[b]
                        for k in range(8):
                            xk = XT[:, k, t0:t0 + n]
                            if k % 2 == 0:
                                act(xk, xk, AF.Copy, [f"x{k}b{b}"], [f"x{k}b{b}"], scale=ALPHA)
                            else:
                                ts(xk, xk, ALPHA, ALU.mult, [f"x{k}b{b}"], [f"x{k}b{b}"])
                    ysk = [f"ysd{b_}" for b_ in range(NB_)]
                    for gi in range(ntiles):
                        b = gi // 4 if gi < 16 else 4
                        grp = BLOCKS[b][2]
                        G0, G1 = GG[gi % 2]
                        Z = ZZ[gi % 2]
                        g0k, g1k, zk = f"G0_{gi % 2}", f"G1_{gi % 2}", f"Z{gi % 2}"
                        for kk, (G, gk) in enumerate(((G0, g0k), (G1, g1k))):
                            P.dma_fn("pool", lambda sem, G=G, gi=gi, kk=kk: nc.gpsimd.indirect_dma_start(
                                out=G[:, :], out_offset=None, in_=ys_d,
                                in_offset=bass.IndirectOffsetOnAxis(ap=DEST[:, gi, kk:kk + 1], axis=0)).then_inc(sem, 16),
                                ysk + ["DEST"], [gk])
                        ts(Z[:, :], G0[:, :], WSEL[:, gi, 0:1], ALU.mult, [g0k, "WSEL"], [zk])
                        stt(Z[:, :], G1[:, :], WSEL[:, gi, 1:2], Z[:, :], ALU.mult, ALU.add, [g1k, "WSEL", zk], [zk])
                        for half in range(2):
                            ps, pk = nb()
                            for kk in range(4):
                                k = half * 4 + kk
                                P.op("pe", lambda k=k, kk=kk, ps=ps, Z=Z: nc.tensor.transpose(
                                    out=ps[:, kk * 128:(kk + 1) * 128], in_=Z[:, k * 128:(k + 1) * 128], identity=IDF),
                                    [zk, "CST"], [pk])
                            for kk in range(4):
                                k = half * 4 + kk
                                xk = XT[:, k, gi * 128:(gi + 1) * 128]
                                stt(xk, ps[:, kk * 128:(kk + 1) * 128], mod(l, 5, k, grp), xk, ALU.mult, ALU.add,
                                    [pk, "MOD", f"x{k}b{b}"], [f"x{k}b{b}"])
                    for b in range(nblk):
                        ln_block(l, 1, b, (SQ, MEAN, RSTD))
            P.barrier()

        for l in range(n_layers):
            if stop == "ada":
                break
            lastl = l == DEPTH - 1
            need_ctx = not lastl
            is_dbg_last = (l == n_layers - 1)
            if l % 2 == 0:
                mixer_ab(l, need_ctx)
                wo = ab_w_out_d[l // 2]
            else:
                mixer_c(l, need_ctx)
                wo = c_w_out_d[l // 2]
            raw = is_dbg_last and stop == "mix"
            if is_dbg_last and stop == "y":
                for b in range(5):
                    t0, n, grp = BLOCKS[b]
                    for k in range(8):
                        cp(XT[:, k, t0:t0 + n], UY[:, k, t0:t0 + n], [f"u{k}b{b}"], [f"x{k}b{b}"])
                break
            proj_ln(l, wo, 5 if need_ctx else 4, raw)
            if is_dbg_last and stop in ("mix", "ln1"):
                break
            if SPARSE:
                moe_sparse(l, need_ctx)
            else:
                moe(l, need_ctx)

        for k in range(8):
            P.dma("sp", out_d[k], XT[:, k, :], xkeys(ks=[k]), [f"out{k}"])
        P.finish("sp", [f"out{k}" for k in range(8)])
        print("bass ops:", P.nops, P.cnt)
    return nc


def _fm(v):
    v = np.asarray(v, np.float32)
    lead = v.shape[:-1]
    r = v.reshape(lead + (8, 128))
    r = np.moveaxis(r, -1, 0)
    return np.ascontiguousarray(r)


def _rope_tables(head_dim, per, qscale):
    quarter = head_dim // 4
    row = np.repeat(np.arange(T // 64, dtype=np.float32), 64)
    col = np.tile(np.arange(64, dtype=np.float32), T // 64)
    inv = (np.float32(10000.0) ** (-np.arange(quarter, dtype=np.float32) / np.float32(quarter))).astype(np.float32)
    ang_r = row[:, None] * inv
    ang_c = col[:, None] * inv
    ang = np.concatenate([ang_r, ang_r, ang_c, ang_c], axis=-1)
    cos = np.cos(ang).astype(np.float32).T
    sin = np.sin(ang).astype(np.float32).T
    sign = np.ones((head_dim, 1), np.float32)
    sign[0:quarter] = -1.0
    sign[2 * quarter:3 * quarter] = -1.0
    sin = sin * sign
    rep = 128 // head_dim
    cos = np.tile(cos, (rep, 1))
    sin = np.tile(sin, (rep, 1))
    return np.ascontiguousarray(np.stack([cos * qscale, sin * qscale, cos, sin]).astype(np.float32))


_CONSTS = None


def _consts():
    global _CONSTS
    if _CONSTS is not None:
        return _CONSTS
    c = {}
    c["ropeA"] = _rope_tables(128, 128, np.float32(128 ** -0.5))
    c["ropeC"] = _rope_tables(64, 64, np.float32(0.125))
    ii = np.arange(128)
    cst = np.zeros((128, 1024), np.float32)
    cst[:, 0:128] = np.eye(128, dtype=np.float32)
    cst[:, 128:256] = 1.0 / 1024.0
    cst[:, 256:384] = 1.0 / 128.0
    cst[:, 384:512] = np.where(ii[:, None] <= ii[None, :], -1.0 / 16.0, 0.0)
    cst[:, 512:640] = 1.0
    c["cst"] = cst
    cst2 = np.zeros((128, 256), np.float32)
    cst2[:, 0:128] = np.where(ii[:, None] >= ii[None, :], -1.0 / 16.0, 0.0)
    cst2[:, 128:256] = np.where(ii[:, None] > ii[None, :], -1.0 / 16.0, 0.0)
    c["cst2"] = cst2
    cstb = np.zeros((128, 256), np.float32)
    cstb[:, 0:128] = np.eye(128)
    cstb[:, 128:256] = 1.0
    c["cstb"] = cstb.astype(ml_dtypes.bfloat16)
    m = (ii[:, None] > ii[None, :]).astype(np.uint8)
    c["mask"] = np.ascontiguousarray(np.tile(m, (1, 4)))
    cst3 = np.zeros((128, 256), np.float32)
    cst3[:, 0:36] = (np.arange(36, dtype=np.float32) * 128.0)[None, :]
    e32 = np.arange(32)
    cst3[0:32, 64:96] = (e32[:, None] < e32[None, :]).astype(np.float32)
    cst3[0:32, 96:128] = (e32[:, None] <= e32[None, :]).astype(np.float32)
    cst3[:, 128] = np.arange(128, dtype=np.float32) * 128.0
    cst3[:, 129] = np.arange(128, dtype=np.float32)
    cst3[:, 136:208] = (np.arange(72, dtype=np.float32) * 128.0)[None, :]
    c["cst3"] = cst3
    c["cstb2"] = (ii[:, None] < ii[None, :]).astype(np.float32).astype(ml_dtypes.bfloat16)
    sele = np.zeros((32, 32, 128), np.float32)
    for e in range(32):
        sele[e, e, :] = 1.0
    c["sele"] = sele.reshape(32, 32 * 128).astype(ml_dtypes.bfloat16)
    ret = np.zeros((128, 4, 6, 128), np.float32)
    pos = np.arange(128, dtype=np.float64)
    for h in range(4):
        lf = float(np.log1p(-np.exp2(-np.float32(5.0 + h)), dtype=np.float32))
        lb = float(np.log1p(-np.exp2(-np.float32(5.5 + h)), dtype=np.float32))
        ret[:, h, 0, :] = np.exp(lf * (pos + 1))
        ret[:, h, 1, :] = np.exp(-lf * (pos + 1))
        ret[:, h, 2, :] = np.exp(lf * (127 - pos))
        ret[:, h, 3, :] = np.exp(lb * (127 - pos))
        ret[:, h, 4, :] = np.exp(-lb * (128 - pos))
        ret[:, h, 5, :] = np.exp(lb * pos)
    c["ret"] = ret
    _CONSTS = c
    return c


def _prep_inputs(inputs):
    f = lambda a: np.ascontiguousarray(np.asarray(a, np.float32))
    shared = dict(_consts())
    shared["adab"] = np.ascontiguousarray(np.asarray(inputs["ada_b"], np.float32).reshape(4, 48, 128).transpose(2, 0, 1))
    lnp = np.stack([inputs["ln1_g"], inputs["ln1_b"], inputs["ln2_g"], inputs["ln2_b"]], axis=1)
    shared["lnp"] = np.ascontiguousarray(np.asarray(lnp, np.float32).reshape(4, 4, 8, 128).transpose(3, 0, 1, 2))
    gn = np.concatenate([inputs["ab_gn_a"], inputs["ab_gn_b"]], axis=1)
    shared["gn"] = np.ascontiguousarray(np.asarray(gn, np.float32).reshape(2, 8, 128).transpose(2, 0, 1))
    shared["subg"] = np.ascontiguousarray(np.asarray(inputs["c_subln_g"], np.float32).reshape(2, 8, 128).transpose(2, 0, 1))
    wlr = np.zeros((32, 2, 2, 256), np.float32)
    wlr[0:16, :, 0, :] = np.asarray(inputs["ab_w_lr_f"]).transpose(1, 0, 2)
    wlr[0:16, :, 1, :] = np.asarray(inputs["ab_w_lr_b"]).transpose(1, 0, 2)
    wlr[16, :, 0, :] = np.asarray(inputs["ab_b_lr_f"])
    wlr[16, :, 1, :] = np.asarray(inputs["ab_b_lr_b"])
    shared["wlr"] = wlr
    wr = np.concatenate([inputs["moe_w_grp"], inputs["moe_w_rexp"]], axis=2)
    shared["wr"] = np.ascontiguousarray(np.asarray(wr, np.float32).reshape(4, 8, 128, 36).transpose(2, 0, 1, 3))
    rb = np.concatenate([inputs["moe_b_grp"], inputs["moe_b_rexp"]], axis=1)
    shared["rb"] = np.ascontiguousarray(np.asarray(rb, np.float32)[None])
    lqk = np.stack([inputs["c_lq1"], inputs["c_lk1"], inputs["c_lq2"], inputs["c_lk2"]], axis=1)
    shared["lqk"] = np.ascontiguousarray(np.asarray(lqk, np.float32).transpose(3, 0, 1, 2))
    for nm in ("ada_w", "ab_w_in", "ab_w_out", "c_w_qkv", "c_w_out"):
        shared[nm] = f(inputs[nm])
    if SPARSE:
        for ci, nm in ((0, "moe_w_gate"), (2, "moe_w_up")):
            wp = np.asarray(inputs[nm], np.float32).reshape(4, 32, 8, 128, 512).transpose(0, 1, 3, 2, 4).reshape(4 * 32 * 128, 4096)
            shared[f"wc{ci}"] = np.ascontiguousarray(wp[:, 0:2048])
            shared[f"wc{ci + 1}"] = np.ascontiguousarray(wp[:, 2048:4096])
        wp = np.asarray(inputs["moe_w_down"], np.float32).reshape(4, 32, 4, 128, 1024).transpose(0, 1, 3, 2, 4).reshape(4 * 32 * 128, 4096)
        shared["wc4"] = np.ascontiguousarray(wp[:, 0:2048])
        shared["wc5"] = np.ascontiguousarray(wp[:, 2048:4096])
    else:
        for nm in ("moe_w_gate", "moe_w_up", "moe_w_down"):
            shared[nm] = f(inputs[nm])
    x = np.asarray(inputs["x"], np.float32)
    ctx = np.asarray(inputs["ctx"], np.float32)
    c = np.asarray(inputs["c"], np.float32)
    c_ctx = np.asarray(inputs["c_ctx"], np.float32)
    in_maps = []
    for b in range(8):
        tok = np.concatenate([x[b], ctx[b]], axis=0)
        xT = np.ascontiguousarray(tok.T.reshape(8, 128, NT))
        cc = np.stack([c[b].reshape(8, 128).T, c_ctx.reshape(8, 128).T], axis=-1)
        m = dict(shared)
        m["xT"] = xT
        m["cc"] = np.ascontiguousarray(cc.astype(np.float32))
        in_maps.append(m)
    return in_maps


_NC_CACHE = {}


def run(inputs, n_layers=DEPTH, stop=None, ncores=8):
    key = (n_layers, stop)
    if key not in _NC_CACHE:
        _NC_CACHE[key] = build(n_layers, stop)
    nc = _NC_CACHE[key]
    in_maps = _prep_inputs(inputs)[:ncores]
    if stop in ("ada", "mix", "ln1", "y") and n_layers <= 1:
        for m in in_maps:
            for nm in (["wc%d" % c for c in range(6)] if SPARSE else ["moe_w_gate", "moe_w_up", "moe_w_down"]):
                m.pop(nm)
    res = run_bass_kernel_spmd(nc, in_maps, core_ids=list(range(ncores)))
    outs = [np.asarray(r["outT"]).reshape(D, NT).T for r in res.results]
    return outs


def kernel(**inputs):
    outs = run(inputs)
    return np.ascontiguousarray(np.stack([o[:T] for o in outs], axis=0).astype(np.float32))
```

```python
import math
import numpy as np
import ml_dtypes
from contextlib import ExitStack
import concourse.bass as bass
import concourse.mybir as mybir
from concourse.bass_utils import run_bass_kernel_spmd

F32 = mybir.dt.float32
BF16 = mybir.dt.bfloat16
U8 = mybir.dt.uint8
U32 = mybir.dt.uint32
I32 = mybir.dt.int32
AF = mybir.ActivationFunctionType
ALU = mybir.AluOpType
AX = mybir.AxisListType

D = 1024
T = 2048
TC = 256
NT = T + TC
DEPTH = 4
ALPHA = (2 * DEPTH) ** 0.25
EPS = 1e-5
BLOCKS = [(0, 512, 0), (512, 512, 0), (1024, 512, 0), (1536, 512, 0), (2048, 256, 1)]
ORDER_F = [16, 17] + list(range(16))
ORDER_B = [17, 16] + list(range(15, -1, -1))
AB_IN = 3616
import os as _os
SPARSE = int(_os.environ.get("MOE_SPARSE", "1"))
MOE_STOP = int(_os.environ.get("MOE_STOP", "9"))
DBG_HEADS = int(_os.environ.get("DBG_HEADS", "8"))
DBG_B = int(_os.environ.get("DBG_B", "9"))
DBG_C = _os.environ.get("DBG_C", "z")


class Prog:
    def __init__(self, nc, es, ndma=28):
        self.nc = nc
        self.e = dict(pe=nc.tensor, act=nc.scalar, dve=nc.vector, pool=nc.gpsimd, sp=nc.sync)
        self.sem = {k: es.enter_context(nc.semaphore("s_" + k)) for k in self.e}
        self.cnt = {k: 0 for k in self.e}
        self.seen = {k: {} for k in self.e}
        self.st = {}
        self.dq = {}
        for q in ("sp", "pool", "act"):
            self.dq[q] = dict(sems=[es.enter_context(nc.semaphore(f"d_{q}{i}")) for i in range(ndma)],
                              vals=[0] * ndma, idx=0)
        self.nops = 0
        self.bar = es.enter_context(nc.sbuf_tensor("barrier_t", [128, 1], F32))

    def _wait(self, eng, ticks):
        need = {}
        for t in ticks:
            if t is None:
                continue
            key, sem, val = t
            if key == "pe" and eng == "pe":
                continue
            if need.get(key, (None, 0))[1] < val:
                need[key] = (sem, val)
        for key, (sem, val) in need.items():
            if self.seen[eng].get(key, 0) >= val:
                continue
            self.e[eng].wait_ge(sem, val)
            self.seen[eng][key] = val

    def _deps(self, R, W):
        ticks = []
        for k in R:
            s = self.st.get(k)
            if s is not None:
                ticks.append(s[0])
                if k.startswith("ps"):
                    ticks.extend(s[1].values())
        for k in W:
            s = self.st.get(k)
            if s is not None:
                ticks.append(s[0])
                ticks.extend(s[1].values())
        return ticks

    def _update(self, tick, R, W):
        for k in W:
            self.st[k] = [tick, {}]
        for k in R:
            s = self.st.get(k)
            if s is None:
                s = self.st[k] = [None, {}]
            s[1][tick[0]] = tick

    def op(self, eng, fn, R, W):
        self._wait(eng, self._deps(R, W))
        inst = fn()
        inst.then_inc(self.sem[eng], 1)
        self.cnt[eng] += 1
        self.nops += 1
        self._update((eng, self.sem[eng], self.cnt[eng]), R, W)

    def dma(self, q, out, in_, R, W):
        d = self.dq[q]
        i = d["idx"]
        d["idx"] = (i + 1) % len(d["sems"])
        key = ("d", q, i)
        ticks = self._deps(R, W)
        if d["vals"][i] > 0:
            ticks.append((key, d["sems"][i], d["vals"][i]))
        self._wait(q, ticks)
        self.e[q].dma_start(out=out, in_=in_).then_inc(d["sems"][i], 16)
        d["vals"][i] += 16
        self.nops += 1
        self._update((key, d["sems"][i], d["vals"][i]), R, W)

    def dma_fn(self, q, fn, R, W):
        d = self.dq[q]
        i = d["idx"]
        d["idx"] = (i + 1) % len(d["sems"])
        key = ("d", q, i)
        ticks = self._deps(R, W)
        if d["vals"][i] > 0:
            ticks.append((key, d["sems"][i], d["vals"][i]))
        self._wait(q, ticks)
        fn(d["sems"][i])
        d["vals"][i] += 16
        self.nops += 1
        self._update((key, d["sems"][i], d["vals"][i]), R, W)

    def barrier(self):
        ticks = []
        for e in self.e:
            if self.cnt[e] > 0:
                ticks.append((e, self.sem[e], self.cnt[e]))
        for q, d in self.dq.items():
            for i, v in enumerate(d["vals"]):
                if v > 0:
                    ticks.append((("d", q, i), d["sems"][i], v))
        self._wait("dve", ticks)
        inst = self.nc.vector.memset(self.bar[:], 0.0)
        inst.then_inc(self.sem["dve"], 1)
        self.cnt["dve"] += 1
        t = ("dve", self.sem["dve"], self.cnt["dve"])
        for e in ("pe", "act", "pool", "sp"):
            self._wait(e, [t])

    def finish(self, eng, keys):
        self._wait(eng, self._deps(keys, []))


def build(n_layers=DEPTH, stop=None):
    nc = bass.Bass("TRN2", target_bir_lowering=False)

    def din(name, shape, dt=F32):
        return nc.dram_tensor(name, list(shape), dt, kind="ExternalInput").ap()

    xT_d = din("xT", [8, 128, NT])
    cc_d = din("cc", [128, 8, 2])
    adab_d = din("adab", [128, 4, 48])
    lnp_d = din("lnp", [128, 4, 4, 8])
    gn_d = din("gn", [128, 2, 8])
    subg_d = din("subg", [128, 2, 8])
    wlr_d = din("wlr", [32, 2, 2, 256])
    wr_d = din("wr", [128, 4, 8, 36])
    rb_d = din("rb", [1, 4, 36])
    lqk_d = din("lqk", [64, 2, 4, 8])
    ret_d = din("ret", [128, 4, 6, 128])
    ropeA_d = din("ropeA", [4, 128, T])
    ropeC_d = din("ropeC", [4, 128, T])
    cst_d = din("cst", [128, 1024])
    cst2_d = din("cst2", [128, 256])
    cstb_d = din("cstb", [128, 256], BF16)
    mask_d = din("mask", [128, 512], U8)
    cst3_d = din("cst3", [128, 256])
    cstb2_d = din("cstb2", [128, 128], BF16)
    xs_d = nc.dram_tensor("xs_scr", [12800, 1024], BF16, kind="Internal").ap()
    ys_d = nc.dram_tensor("ys_scr", [12800, 1024], F32, kind="Internal").ap()
    sele_d = din("sele", [32, 32 * 128], BF16)
    ada_w_d = din("ada_w", [4, D, 6 * D])
    ab_w_in_d = din("ab_w_in", [2, D, AB_IN])
    ab_w_out_d = din("ab_w_out", [2, D, D])
    c_w_qkv_d = din("c_w_qkv", [2, D, 3 * D])
    c_w_out_d = din("c_w_out", [2, D, D])
    use_moe = not (stop in ("ada", "mix", "ln1", "y") and n_layers <= 1)
    if use_moe and SPARSE:
        wc_d = [din(f"wc{c}", [4 * 32 * 128, 2048]) for c in range(6)]
    elif use_moe:
        wg_d = din("moe_w_gate", [4, 32, D, 512])
        wu_d = din("moe_w_up", [4, 32, D, 512])
        wd_d = din("moe_w_down", [4, 32, 512, D])
    out_d = nc.dram_tensor("outT", [8, 128, NT], F32, kind="ExternalOutput").ap()

    es = ExitStack()
    with es:
        P = Prog(nc, es)

        uid = [0]

        def sb(name, shape, dt=F32, stack=es):
            uid[0] += 1
            return stack.enter_context(nc.sbuf_tensor(f"{name}_{uid[0]}", list(shape), dt))

        PS = [es.enter_context(nc.psum_tensor(f"ps{i}", [128, 512], F32)) for i in range(8)]
        psc = [0]

        def nb():
            i = psc[0]
            psc[0] = (i + 1) % 8
            return PS[i], f"ps{i}"

        def mm(out, lhsT, rhs, start, stop, R, W):
            P.op("pe", lambda: nc.tensor.matmul(out, lhsT=lhsT, rhs=rhs, start=start, stop=stop), R, W)

        def act(out, in_, func, R, W, scale=None, bias=None, accum_out=None):
            kw = {}
            if scale is not None:
                kw["scale"] = scale
            if bias is None and func != AF.Copy:
                sp_, np_ = in_.start_partition(), in_.partition_size()
                bias = ZEROC[sp_:sp_ + np_, 0:1]
                R = list(R) + ["ZEROC"]
            if bias is not None:
                kw["bias"] = bias
            if accum_out is not None:
                kw["accum_out"] = accum_out
            P.op("act", lambda: nc.scalar.activation(out=out, in_=in_, func=func, **kw), R, W)

        def tt(out, a, b, op, R, W, eng="dve"):
            e = nc.vector if eng == "dve" else nc.gpsimd
            P.op(eng, lambda: e.tensor_tensor(out=out, in0=a, in1=b, op=op), R, W)

        def ts(out, a, s1, op0, R, W, s2=None, op1=None, eng="dve"):
            e = nc.vector if eng == "dve" else nc.gpsimd
            if op1 is None:
                P.op(eng, lambda: e.tensor_scalar(out=out, in0=a, scalar1=s1, scalar2=None, op0=op0), R, W)
            else:
                P.op(eng, lambda: e.tensor_scalar(out=out, in0=a, scalar1=s1, scalar2=s2, op0=op0, op1=op1), R, W)

        def stt(out, a, s, b, op0, op1, R, W):
            P.op("dve", lambda: nc.vector.scalar_tensor_tensor(out=out, in0=a, scalar=s, in1=b, op0=op0, op1=op1), R, W)

        def cp(out, in_, R, W, eng="dve"):
            if eng == "act":
                act(out, in_, AF.Copy, R, W)
            else:
                e = nc.vector if eng == "dve" else nc.gpsimd
                P.op(eng, lambda: e.tensor_copy(out=out, in_=in_), R, W)

        XT = sb("XT", [128, 8, NT])
        UY = sb("UY", [128, 8, NT], BF16)
        MOD = sb("MOD", [128, 4, 6, 8, 2])
        LNP = sb("LNP", [128, 4, 4, 8])
        GN = sb("GN", [128, 2, 8])
        SUBG = sb("SUBG", [128, 2, 8])
        RB = sb("RB", [1, 4, 36])
        LAM = sb("LAM", [128, 2, 8])
        CST = sb("CST", [128, 1024])
        CST2 = sb("CST2", [128, 256])
        CSTB = sb("CSTB", [128, 256], BF16)
        MASK = sb("MASK", [128, 512], U8)
        CST3 = sb("CST3", [128, 256])
        CSTB2 = sb("CSTB2", [128, 128], BF16)
        TRIS = CSTB2[:, 0:128]
        EPSC = sb("EPSC", [128, 1])
        ONEC = sb("ONEC", [128, 1])
        ZEROC = sb("ZEROC", [128, 1])
        IDF = CST[:, 0:128]
        ONESD = CST[:, 128:256]
        ONESH = CST[:, 256:384]
        TRIF = CST[:, 384:512]
        ONES1 = CST[0:1, 512:640]
        TRIB = CST2[:, 0:128]
        TRIBP = CST2[:, 128:256]
        IDB = CSTB[:, 0:128]
        ONESB = CSTB[:, 128:256]

        def xkeys(blocks=range(5), ks=range(8)):
            return [f"x{k}b{b}" for k in ks for b in blocks]

        def ukeys(blocks=range(5), ks=range(8)):
            return [f"u{k}b{b}" for k in ks for b in blocks]

        for k in range(8):
            P.dma("sp", XT[:, k, :], xT_d[k], [], xkeys(ks=[k]))
        for (t_, d_, kname) in [(LNP, lnp_d, "LNP"), (GN, gn_d, "GN"), (SUBG, subg_d, "SUBG"),
                                (RB, rb_d, "RB"), (CST, cst_d, "CST"), (CST2, cst2_d, "CST2"), (CSTB, cstb_d, "CSTB"),
                                (MASK, mask_d, "MASK"), (CST3, cst3_d, "CST3"), (CSTB2, cstb2_d, "CSTB2")]:
            P.dma("sp", t_[:], d_, [], [kname])
        P.op("dve", lambda: nc.vector.memset(EPSC[:], EPS), [], ["EPSC"])
        P.op("dve", lambda: nc.vector.memset(ONEC[:], 1.0), [], ["ONEC"])
        P.op("dve", lambda: nc.vector.memset(ZEROC[:], 0.0), [], ["ZEROC"])

        with ExitStack() as s1:
            CC = sb("CC", [128, 8, 2], F32, s1)
            SCB = sb("SCB", [128, 8, 2], BF16, s1)
            ADAB = sb("ADAB", [128, 4, 48], F32, s1)
            WA = [sb(f"WA{i}", [128, 8, 1024], BF16, s1) for i in range(2)]
            LQK = sb("LQK", [64, 2, 4, 8], F32, s1)
            LQP = sb("LQP", [64, 2, 2, 8], F32, s1)
            P.dma("sp", CC[:], cc_d, [], ["CC"])
            P.dma("sp", ADAB[:], adab_d, [], ["ADAB"])
            P.dma("sp", LQK[:], lqk_d, [], ["LQK"])
            act(SCB[:], CC[:], AF.Silu, ["CC"], ["SCB"])
            it = 0
            for l in range(n_layers):
                for j6 in range(6):
                    w = WA[it % 2]
                    wk = f"WA{it % 2}"
                    it += 1
                    src = ada_w_d[l].rearrange("(k p) n -> p k n", p=128)[:, :, j6 * 1024:(j6 + 1) * 1024]
                    P.dma("pool", w[:], src, [], [wk])
                    ps, pk = nb()
                    for jj in range(8):
                        for k in range(8):
                            mm(ps[:, jj * 2:jj * 2 + 2], w[:, k, jj * 128:(jj + 1) * 128], SCB[:, k, :],
                               k == 0, k == 7, [wk, "SCB"], [pk])
                    tt(MOD[:, l, j6, :, :], ps[:, 0:16].rearrange("p (j g) -> p j g", g=2),
                       ADAB[:, l, j6 * 8:(j6 + 1) * 8].unsqueeze(2).broadcast_to([128, 8, 2]), ALU.add,
                       [pk, "ADAB"], ["MOD"])
                for j6 in (1, 4):
                    ts(MOD[:, l, j6, :, :], MOD[:, l, j6, :, :], 1.0, ALU.add, ["MOD"], ["MOD"])
            tt(LQP[:, :, 0, :], LQK[:, :, 0, :], LQK[:, :, 1, :], ALU.mult, ["LQK"], ["LQP"])
            tt(LQP[:, :, 1, :], LQK[:, :, 2, :], LQK[:, :, 3, :], ALU.mult, ["LQP", "LQK"], ["LQP"])
            ps, pk = nb()
            mm(ps[:, 0:32], CST[0:64, 512:640], LQP[:].rearrange("p a b c -> p (a b c)"), True, True,
               ["CST", "LQP"], [pk])
            LE = sb("LE", [128, 32], F32, s1)
            act(LE[:], ps[:, 0:32], AF.Exp, [pk], ["LE"])
            lev = LE[:].rearrange("p (a b c) -> p a b c", a=2, b=2)
            for i in range(2):
                lam_init = 0.8 - 0.6 * math.exp(-0.3 * (2 * i + 1))
                tt(LAM[:, i, :], lev[:, i, 1, :], lev[:, i, 0, :], ALU.subtract, ["LE", "LAM"], ["LAM"])
                ts(LAM[:, i, :], LAM[:, i, :], -lam_init, ALU.add, ["LAM"], ["LAM"])
                ts(SUBG[:, i, :], SUBG[:, i, :], 1.0 - lam_init, ALU.mult, ["SUBG"], ["SUBG"])

        P.barrier()

        def mod(l, which, k, grp):
            return MOD[:, l, which, k, grp:grp + 1]

        def ln_block(l, which_ln, b, scr):
            t0, n, grp = BLOCKS[b]
            SQ, MEAN, RSTD = scr
            psm, pkm = nb()
            pss, pks = nb()
            for k in range(8):
                xk = XT[:, k, t0:t0 + n]
                mm(psm[:, :n], ONESD, xk, k == 0, k == 7, [f"x{k}b{b}", "CST"], [pkm])
                sq = SQ[k % 2]
                act(sq[:, :n], xk, AF.Square, [f"x{k}b{b}"], [f"SQ{k % 2}"])
                mm(pss[:, :n], ONESD, sq[:, :n], k == 0, k == 7, [f"SQ{k % 2}", "CST"], [pks])
            cp(MEAN[:, :n], psm[:, :n], [pkm], ["MEAN"], eng="act")
            act(RSTD[:, :n], psm[:, :n], AF.Square, [pkm], ["RSTD"])
            tt(RSTD[:, :n], pss[:, :n], RSTD[:, :n], ALU.subtract, [pks, "RSTD"], ["RSTD"])
            act(RSTD[:, :n], RSTD[:, :n], AF.Ln, ["RSTD"], ["RSTD"], bias=EPSC[:, 0:1])
            act(RSTD[:, :n], RSTD[:, :n], AF.Exp, ["RSTD"], ["RSTD"], scale=-0.5)
            for k in range(8):
                xk = XT[:, k, t0:t0 + n]
                kk = [f"x{k}b{b}"]
                tt(xk, xk, MEAN[:, :n], ALU.subtract, kk + ["MEAN"], kk)
                tt(xk, xk, RSTD[:, :n], ALU.mult, kk + ["RSTD"], kk)
                act(xk, xk, AF.Identity, kk + ["LNP"], kk, scale=LNP[:, l, 2 * which_ln, k:k + 1],
                    bias=LNP[:, l, 2 * which_ln + 1, k:k + 1])

        def proj_ln(l, w_dram, nblocks, raw):
            with ExitStack() as s2:
                WO = sb("WO", [128, 8, 1024], BF16, s2)
                TMP = [sb(f"TMPO{i}", [128, 512], F32, s2) for i in range(2)]
                SQ = [sb(f"SQ{i}", [128, 512], F32, s2) for i in range(2)]
                MEAN = sb("MEAN", [128, 512], F32, s2)
                RSTD = sb("RSTD", [128, 512], F32, s2)
                P.dma("pool", WO[:], w_dram.rearrange("(k p) n -> p k n", p=128), [], ["WO"])
                for b in range(nblocks):
                    t0, n, grp = BLOCKS[b]
                    for c in range(8):
                        ps, pk = nb()
                        for h in range(8):
                            mm(ps[:, :n], WO[:, h, c * 128:(c + 1) * 128], UY[:, h, t0:t0 + n], h == 0, h == 7,
                               ["WO", f"u{h}b{b}"], [pk])
                        xk = XT[:, c, t0:t0 + n]
                        kk = [f"x{c}b{b}"]
                        if raw:
                            cp(xk, ps[:, :n], [pk], kk, eng="act")
                        else:
                            tmp = TMP[c % 2]
                            act(tmp[:, :n], ps[:, :n], AF.Identity, [pk, "MOD"], [f"TMPO{c % 2}"], scale=mod(l, 2, c, grp))
                            stt(xk, xk, ALPHA, tmp[:, :n], ALU.mult, ALU.add, kk + [f"TMPO{c % 2}"], kk)
                    if not raw:
                        ln_block(l, 0, b, (SQ, MEAN, RSTD))
            P.barrier()

        def make_u(l, b, UB, ub_i, which_s, which_sh):
            t0, n, grp = BLOCKS[b]
            for k in range(8):
                src = XT[:, k, t0:t0 + n]
                dst = UB[ub_i][:, k, :n]
                R = [f"x{k}b{b}", "MOD"]
                W = [f"UB{ub_i}k{k}"]
                if k % 2 == 0:
                    act(dst, src, AF.Identity, R, W, scale=mod(l, which_s, k, grp), bias=mod(l, which_sh, k, grp))
                else:
                    ts(dst, src, mod(l, which_s, k, grp), ALU.mult, R, W, s2=mod(l, which_sh, k, grp), op1=ALU.add)

        def fm_group(UBt, ub_i, n, Wt, wkey, c0, M):
            ps, pk = nb()
            for k in range(8):
                mm(ps[0:M, :n], Wt[:, k, c0:c0 + M], UBt[:, k, :n], k == 0, k == 7, [wkey, f"UB{ub_i}k{k}"], [pk])
            return ps, pk

        def rope_evac(dst, dkey, ps_a, pk_a, ps_b, pk_b, cos_t, sin_t, tkey, n, TR, ti):
            t1 = TR[0]
            t2 = TR[1]
            tt(t1[:, :n], ps_a[:, :n], cos_t, ALU.mult, [pk_a, tkey], ["TR0"])
            tt(t2[:, :n], ps_b[:, :n], sin_t, ALU.mult, [pk_b, tkey], ["TR1"])
            tt(dst, t1[:, :n], t2[:, :n], ALU.add, ["TR0", "TR1"], [dkey])

        def in_proj_head(l, hs, kind, wsrc, cols, Qf, Kf, V, SGN, gn_ap, rope_d, qscale, LRT=None, lr_cols=None,
                         after_block=None, nblocks=5):
            dk = 64 if kind == "B" else 128
            has_rope = kind in ("A", "C")
            has_g = kind in ("A", "B")
            wv = wsrc.rearrange("(k p) n -> p k n", p=128)
            W = hs["W"]
            ofs = {}
            o = 0
            names = ["q", "k", "v"] + (["g"] if has_g else [])
            for nm in names:
                w_ = dk if nm in ("q", "k") else 128
                P.dma("pool", W[:, :, o:o + w_], wv[:, :, cols[nm]:cols[nm] + w_], [], [f"W_{nm}"])
                ofs[nm] = o
                o += w_
            if LRT is not None:
                P.dma("pool", W[:, :, o:o + 32], wv[:, :, lr_cols:lr_cols + 32], [], ["W_lr"])
                ofs["lr"] = o
                o += 32
            if has_rope:
                blk = 32 if kind == "A" else 16
                for nm in ("q", "k"):
                    s_ = W[:, :, ofs[nm]:ofs[nm] + 128].rearrange("p k (a two b) -> p k a two b", two=2, b=blk)
                    d_ = W[:, :, o:o + 128].rearrange("p k (a two b) -> p k a two b", two=2, b=blk)
                    cp(d_[:, :, :, 0, :], s_[:, :, :, 1, :], [f"W_{nm}"], [f"W_{nm}p"])
                    cp(d_[:, :, :, 1, :], s_[:, :, :, 0, :], [f"W_{nm}", f"W_{nm}p"], [f"W_{nm}p"])
                    ofs[nm + "p"] = o
                    o += 128
            UB = hs["UB"]
            TR = hs["TR"]
            ROPE = hs["ROPE"]
            ti = 0
            for b in range(nblocks):
                t0, n, grp = BLOCKS[b]
                ub_i = b % len(UB)
                make_u(l, b, UB, ub_i, 1, 0)
                UBt = UB[ub_i]
                if has_rope and grp == 0:
                    P.dma("sp", ROPE[:, :, :], rope_d[:, :, t0:t0 + n].rearrange("f p t -> p f t"), [], ["ROPE"])
                for nm, dst_f in (("q", Qf), ("k", Kf)):
                    ps, pk = fm_group(UBt, ub_i, n, W, f"W_{nm}", ofs[nm], dk)
                    dst, dkey = dst_f(b)
                    if has_rope and grp == 0:
                        ps2, pk2 = fm_group(UBt, ub_i, n, W, f"W_{nm}p", ofs[nm + "p"], dk)
                        fi = 0 if nm == "q" else 2
                        rope_evac(dst, dkey, ps, pk, ps2, pk2, ROPE[:, fi, :n], ROPE[:, fi + 1, :n], "ROPE", n, TR, ti)
                        ti += 1
                    else:
                        sc = qscale if nm == "q" else 1.0
                        act(dst, ps[0:dk, :n], AF.Copy, [pk], [dkey], scale=sc)
                if has_g:
                    ps, pk = fm_group(UBt, ub_i, n, W, "W_g", ofs["g"], 128)
                    t1 = TR[ti % 2]
                    act(t1[:, :n], ps[:, :n], AF.Silu, [pk], [f"TR{ti % 2}"])
                    ts(SGN[:, t0:t0 + n], t1[:, :n], gn_ap, ALU.mult, [f"TR{ti % 2}", "GN"], [f"sgn_b{b}"])
                    ti += 1
                if LRT is not None:
                    for di in range(2):
                        ps, pk = fm_group(UBt, ub_i, n, W, "W_lr", ofs["lr"] + 16 * di, 16)
                        cp(LRT[di][0:16, :n], ps[0:16, :n], [pk], [f"lrt{di}"], eng="act")
                ps, pk = nb()
                ntile = n // 128
                for tI in range(ntile):
                    for k in range(8):
                        mm(ps[:, tI * 128:(tI + 1) * 128], UBt[:, k, tI * 128:(tI + 1) * 128],
                           W[:, k, ofs["v"]:ofs["v"] + 128], k == 0, k == 7, ["W_v", f"UB{ub_i}k{k}"], [pk])
                c0 = t0 // 128
                cp(V[:, c0:c0 + ntile, :], ps[:, :n].rearrange("p (c d) -> p c d", d=128), [pk], [f"vb{b}"], eng="act")
                if after_block is not None:
                    after_block(b)

        def head_norm(ps_o, pk_o, n, center, hs, out_ap, outkey, post_ap, postkeys, scale_ap=None, src_sb=None):
            OSB, SQh, RS = hs["OSB"], hs["SQh"], hs["RS"]
            M2 = SQh
            if src_sb is None:
                cp(OSB[:, :n], ps_o[:, :n], [pk_o], ["OSB"], eng="act")
                act(SQh[:, :n], ps_o[:, :n], AF.Square, [pk_o], ["SQh"])
            else:
                act(SQh[:, :n], OSB[:, :n], AF.Square, ["OSB"], ["SQh"])
            pss, pks = nb()
            mm(pss[:, :n], ONESH, SQh[:, :n], True, True, ["SQh", "CST"], [pks])
            if center:
                psm, pkm = nb()
                mm(psm[:, :n], ONESH, OSB[:, :n], True, True, ["OSB", "CST"], [pkm])
                act(M2[:, :n], psm[:, :n], AF.Square, [pkm, "SQh"], ["SQh"])
                tt(RS[:, :n], pss[:, :n], M2[:, :n], ALU.subtract, [pks, "SQh"], ["RS"])
                tt(OSB[:, :n], OSB[:, :n], psm[:, :n], ALU.subtract, ["OSB", pkm], ["OSB"])
                act(RS[:, :n], RS[:, :n], AF.Ln, ["RS"], ["RS"], bias=EPSC[:, 0:1])
            else:
                act(RS[:, :n], pss[:, :n], AF.Ln, [pks], ["RS"], bias=EPSC[:, 0:1])
            act(RS[:, :n], RS[:, :n], AF.Exp, ["RS"], ["RS"], scale=-0.5)
            if scale_ap is None:
                tt(OSB[:, :n], OSB[:, :n], RS[:, :n], ALU.mult, ["OSB", "RS"], ["OSB"])
                tt(out_ap, OSB[:, :n], post_ap, ALU.mult, ["OSB"] + postkeys, [outkey])
            else:
                stt(out_ap, OSB[:, :n], scale_ap, RS[:, :n], ALU.mult, ALU.mult, ["OSB", "RS"] + postkeys, [outkey])

        def mixer_ab(l, need_ctx):
            i = l // 2
            wsrc = ab_w_in_d[i]
            with ExitStack() as s2:
                hs = dict(
                    W=sb("Wh", [128, 8, 768], BF16, s2),
                    UB=[sb("UB0", [128, 8, 512], BF16, s2)],
                    TR=[sb(f"TR{j}", [128, 512], F32, s2) for j in range(2)],
                    OSB=sb("OSB", [128, 512], F32, s2), SQh=sb("SQh", [128, 512], F32, s2),
                    RS=sb("RS", [128, 512], F32, s2),
                )
                Qt = sb("Qt", [128, 512], BF16, s2)
                Kt = sb("Kt", [128, 512], BF16, s2)
                V = sb("V", [128, 18, 128], BF16, s2)
                SGN = sb("SGN", [128, NT], BF16, s2)
                QDF = sb("QDF", [128, NT], BF16, s2)
                QDB = sb("QDB", [128, NT], BF16, s2)
                ATT = sb("ATT", [128, NT], BF16, s2)
                SFb = sb("SFb", [128, 18, 128], BF16, s2)
                SBb = sb("SBb", [128, 18, 128], BF16, s2)
                CUR = [sb(f"CUR{j}", [128, 128], F32, s2) for j in range(4)]
                KD = [sb(f"KD{j}", [128, 512], BF16, s2) for j in range(4)]
                KTT = [sb(f"KTT{j}", [128, 4, 128], BF16, s2) for j in range(2)]

                def Qf(b):
                    return Qt[:, :BLOCKS[b][1]], "qt"

                def Kf(b):
                    return Kt[:, :BLOCKS[b][1]], "kt"

                def Qf64(b):
                    return Qt[0:64, :BLOCKS[b][1]], "qt"

                def Kf64(b):
                    return Kt[0:64, :BLOCKS[b][1]], "kt"

                def run_head(h, ph):
                    isA = h < 4
                    dk = 128 if isA else 64
                    hh = h if isA else h - 4
                    if isA:
                        RET = ph["RET"]
                        P.dma("sp", RET[:], ret_d[:, hh, :, :], [], ["RET"])
                    else:
                        LRT, WLRb, LS, LS2, EXS, TAB, TOT, DEC = (ph[k_] for k_ in ("LRT", "WLRb", "LS", "LS2", "EXS", "TAB", "TOT", "DEC"))
                    P.op("dve", lambda: nc.vector.memset(SFb[:, 16, :], 0.0), [], ["SFb"])
                    P.op("dve", lambda: nc.vector.memset(SBb[:, 17, :], 0.0), [], ["SBb"])

                    def pass1(b):
                        t0, n, grp = BLOCKS[b]
                        nch = n // 128
                        c0 = t0 // 128
                        if (not isA) and DBG_B < 1:
                            return
                        qv = Qt[0:dk, :n]
                        kv = Kt[0:dk, :n]
                        if isA:
                            tabs = [RET[:, j, :].unsqueeze(1).broadcast_to([128, nch, 128]) for j in range(6)]
                            tkeys = ["RET"]

                            def rr(ap):
                                return ap.rearrange("p (c d) -> p c d", d=128)
                        else:
                            psg, pkg = nb()
                            for c in range(nch):
                                for di in range(2):
                                    mm(psg[:, c * 128 + di * 64:c * 128 + di * 64 + 64],
                                       LRT[di][:, c * 128:(c + 1) * 128], WLRb[:, di, hh * 64:(hh + 1) * 64],
                                       True, True, [f"lrt{di}", "WLRb"], [pkg])
                            act(EXS[:, :n], psg[:, :n], AF.Exp, [pkg], ["EXS"], scale=-1.0)
                            ex4 = EXS[:, :n].rearrange("p (c a d) -> p c a d", a=2, d=64)
                            act(LS[:, 0:nch, :, :], ex4, AF.Ln, ["EXS"], ["LS"], bias=ONEC[:, 0:1])
                            act(LS2[:, 0:nch, 0, :], ex4[:, :, 1, :], AF.Ln, ["EXS"], ["LS2"], bias=ONEC[:, 0:1])
                            act(LS2[:, 0:nch, 1, :], ex4[:, :, 0, :], AF.Ln, ["EXS", "LS2"], ["LS2"], bias=ONEC[:, 0:1])
                            if DBG_C < "b":
                                return
                            psf, pkf = nb()
                            psb, pkb = nb()
                            psp, pkp = nb()
                            for c in range(nch):
                                mm(psf[:, c * 128:(c + 1) * 128], LS[:, c, :, :].rearrange("p a d -> p (a d)"), TRIF, True, True, ["LS", "CST"], [pkf])
                            for c in range(nch):
                                mm(psb[:, c * 128:(c + 1) * 128], LS2[:, c, :, :].rearrange("p a d -> p (a d)"), TRIB, True, True, ["LS2", "CST2"], [pkb])
                            for c in range(nch):
                                mm(psp[:, c * 128:(c + 1) * 128], LS2[:, c, :, :].rearrange("p a d -> p (a d)"), TRIBP, True, True, ["LS2", "CST2"], [pkp])
                            if DBG_C < "c":
                                return
                            f3 = psf[0:64, :n].rearrange("p (c d) -> p c d", d=128)
                            b3 = psb[0:64, :n].rearrange("p (c d) -> p c d", d=128)
                            cp(TOT[0:64, 0:nch, 0:1], f3[:, :, 127:128], [pkf], ["TOT"])
                            cp(TOT[0:64, 0:nch, 1:2], b3[:, :, 0:1], [pkb, "TOT"], ["TOT"])
                            if DBG_C < "d":
                                return
                            act(TAB[0][0:64, :n], psf[0:64, :n], AF.Exp, [pkf, "TOT"], ["TAB0"])
                            act(TAB[1][0:64, :n], psf[0:64, :n], AF.Exp, [pkf, "TOT"], ["TAB1"], scale=-1.0)
                            act(TAB[3][0:64, :n], psp[0:64, :n], AF.Exp, [pkp, "TOT"], ["TAB3"])
                            act(TAB[4][0:64, :n], psb[0:64, :n], AF.Exp, [pkb, "TOT"], ["TAB4"], scale=-1.0)
                            if DBG_C < "e":
                                return
                            for c in range(nch):
                                act(TAB[2][0:64, c * 128:(c + 1) * 128], psf[0:64, c * 128:(c + 1) * 128], AF.Exp,
                                    [pkf, "TOT"], ["TAB2"], scale=-1.0, bias=TOT[0:64, c, 0:1])
                                act(TAB[5][0:64, c * 128:(c + 1) * 128], psb[0:64, c * 128:(c + 1) * 128], AF.Exp,
                                    [pkb, "TOT"], ["TAB5"], scale=-1.0, bias=TOT[0:64, c, 1:2])
                            act(DEC[0:64, c0:c0 + nch, :], TOT[0:64, 0:nch, :], AF.Exp, ["TOT"], ["DEC"])
                            tabs = [TAB[j][0:64, :n] for j in range(6)]
                            tkeys = [f"TAB{j}" for j in range(6)]

                            def rr(ap):
                                return ap
                        if (not isA) and DBG_B < 2:
                            return
                        tt(rr(QDF[0:dk, t0:t0 + n]), rr(qv), tabs[0], ALU.mult, ["qt"] + tkeys, [f"qdf{b}"])
                        tt(rr(QDB[0:dk, t0:t0 + n]), rr(qv), tabs[3], ALU.mult, ["qt"] + tkeys, [f"qdb{b}"])
                        tt(rr(KD[0][0:dk, :n]), rr(kv), tabs[1], ALU.mult, ["kt"] + tkeys, ["KD0"])
                        tt(rr(KD[1][0:dk, :n]), rr(kv), tabs[4], ALU.mult, ["kt"] + tkeys, ["KD1"])
                        tt(rr(KD[2][0:dk, :n]), rr(kv), tabs[2], ALU.mult, ["kt"] + tkeys, ["KD2"])
                        tt(rr(KD[3][0:dk, :n]), rr(kv), tabs[5], ALU.mult, ["kt"] + tkeys, ["KD3"])
                        psa, pka = nb()
                        psb2, pkb2 = nb()
                        for c in range(nch):
                            sl = slice(c * 128, (c + 1) * 128)
                            gsl = slice(t0 + c * 128, t0 + (c + 1) * 128)
                            mm(psa[:, sl], KD[0][0:dk, sl], QDF[0:dk, gsl], True, True, ["KD0", f"qdf{b}"], [pka])
                            mm(psb2[:, sl], KD[1][0:dk, sl], QDB[0:dk, gsl], True, True, ["KD1", f"qdb{b}"], [pkb2])
                        cp(ATT[:, t0:t0 + n], psa[:, :n], [pka], [f"att{b}"], eng="act")
                        P.op("dve", lambda: nc.vector.copy_predicated(out=ATT[:, t0:t0 + n], mask=MASK[:, :n],
                                                                       data=psb2[:, :n]),
                             [pkb2, "MASK", f"att{b}"], [f"att{b}"])
                        if (not isA) and DBG_B < 3:
                            return
                        for di in range(2):
                            psk, pkk = nb()
                            for c in range(nch):
                                mm(psk[:, c * 128:c * 128 + dk], KD[2 + di][0:dk, c * 128:(c + 1) * 128], IDB[0:dk, 0:dk],
                                   True, True, [f"KD{2 + di}", "CSTB"], [pkk])
                            cp(KTT[di][:, 0:nch, 0:dk], psk[:, :n].rearrange("p (c d) -> p c d", d=128)[:, :, 0:dk],
                               [pkk], [f"KTT{di}"], eng="act")
                            psd, pkd = nb()
                            for c in range(nch):
                                mm(psd[0:dk, c * 128:(c + 1) * 128], KTT[di][:, c, 0:dk], V[:, c0 + c, :], True, True,
                                   [f"KTT{di}", f"vb{b}"], [pkd])
                            d3 = psd[0:dk, :n].rearrange("p (c d) -> p c d", d=128)
                            if di == 0:
                                if grp == 0:
                                    m = nch if c0 + nch < 16 else nch - 1
                                    cp(SFb[0:dk, c0 + 1:c0 + 1 + m, :], d3[:, 0:m, :], [pkd], ["SFb"])
                                else:
                                    cp(SFb[0:dk, 17, :], d3[:, 0, :], [pkd], ["SFb"])
                                    cp(SFb[0:dk, 0, :], d3[:, 1, :], [pkd], ["SFb"])
                            else:
                                if c0 == 0:
                                    cp(SBb[0:dk, 0:nch - 1, :], d3[:, 1:nch, :], [pkd], ["SBb"])
                                else:
                                    cp(SBb[0:dk, c0 - 1:c0 - 1 + nch, :], d3[:, :, :], [pkd], ["SBb"])

                    if isA:
                        cols = dict(q=hh * 128, k=512 + hh * 128, v=1024 + hh * 128, g=1536 + hh * 128)
                        hs["ROPE"] = ph["ROPE"]
                        in_proj_head(l, hs, "A", wsrc, cols, Qf, Kf, V, SGN, GN[:, i, h:h + 1], ropeA_d, dk ** -0.5,
                                     after_block=pass1)
                    else:
                        cols = dict(q=2048 + hh * 64, k=2304 + hh * 64, v=2560 + hh * 128, g=3072 + hh * 128)
                        hs["ROPE"] = None
                        in_proj_head(l, hs, "B", wsrc, cols, Qf64, Kf64, V, SGN, GN[:, i, h:h + 1], None, dk ** -0.5,
                                     LRT=LRT, lr_cols=3584, after_block=pass1)
                    if (not isA) and DBG_B < 4:
                        return
                    for di, (ST, order, skey) in enumerate(((SFb, ORDER_F, "SFb"), (SBb, ORDER_B, "SBb"))):
                        c_a, c_b = CUR[2 * di], CUR[2 * di + 1]
                        ka, kb_ = f"CUR{2 * di}", f"CUR{2 * di + 1}"
                        P.op("dve", lambda c_a=c_a: nc.vector.memset(c_a[:], 0.0), [], [ka])
                        for oi in range(17):
                            nn, nx = order[oi], order[oi + 1]
                            if isA:
                                g_ = 1.0 - 2.0 ** (-((5.0 if di == 0 else 5.5) + hh))
                                sc_ = float(np.float32(g_) ** 128)
                            else:
                                sc_ = DEC[0:64, nn, di:di + 1]
                            stt(c_b[0:dk, :], c_a[0:dk, :], sc_, ST[0:dk, nx, :], ALU.mult, ALU.add,
                                [ka, skey] + ([] if isA else ["DEC"]), [kb_])
                            cp(ST[0:dk, nx, :], c_b[0:dk, :], [kb_], [skey], eng="act")
                            c_a, c_b, ka, kb_ = c_b, c_a, kb_, ka
                    if (not isA) and DBG_B < 5:
                        return
                    for b in range(5 if need_ctx else 4):
                        t0, n, grp = BLOCKS[b]
                        nch = n // 128
                        c0 = t0 // 128
                        pso, pko = nb()
                        for c in range(nch):
                            sl = slice(c * 128, (c + 1) * 128)
                            gsl = slice(t0 + c * 128, t0 + (c + 1) * 128)
                            mm(pso[:, sl], V[:, c0 + c, :], ATT[:, gsl], True, False, [f"vb{b}", f"att{b}"], [pko])
                            mm(pso[:, sl], SFb[0:dk, c0 + c, :], QDF[0:dk, gsl], False, False, ["SFb", f"qdf{b}"], [pko])
                            mm(pso[:, sl], SBb[0:dk, c0 + c, :], QDB[0:dk, gsl], False, True, ["SBb", f"qdb{b}"], [pko])
                        head_norm(pso, pko, n, isA, hs, UY[:, h, t0:t0 + n], f"u{h}b{b}", SGN[:, t0:t0 + n], [f"sgn_b{b}"])

                with ExitStack() as s3:
                    ph = dict(ROPE=sb("ROPE", [128, 4, 512], F32, s3), RET=sb("RET", [128, 6, 128], F32, s3))
                    for h in range(min(4, DBG_HEADS)):
                        run_head(h, ph)
                P.barrier()
                with ExitStack() as s3:
                    WLR = sb("WLR", [32, 2, 256], F32, s3)
                    ph = dict(
                        LRT=[sb(f"LRT{j}", [32, 512], BF16, s3) for j in range(2)],
                        WLRb=sb("WLRb", [32, 2, 256], BF16, s3),
                        LS=sb("LS", [128, 4, 2, 64], F32, s3),
                        LS2=sb("LS2", [128, 4, 2, 64], F32, s3),
                        EXS=sb("EXS", [128, 512], F32, s3),
                        TAB=[sb(f"TAB{j}", [128, 512], BF16, s3) for j in range(6)],
                        TOT=sb("TOT", [128, 4, 2], F32, s3),
                        DEC=sb("DEC", [128, 18, 2], F32, s3),
                    )
                    P.dma("sp", WLR[:], wlr_d[:, i, :, :], [], ["WLR"])
                    cp(ph["WLRb"][:], WLR[:], ["WLR"], ["WLRb"])
                    for di in range(2):
                        P.op("dve", lambda di=di: nc.vector.memset(ph["LRT"][di][:], 1.0), [], [f"lrt{di}"])
                    for h in range(4, min(8, DBG_HEADS)):
                        run_head(h, ph)
            P.barrier()

        def mixer_c(l, need_ctx):
            i = l // 2
            wsrc = c_w_qkv_d[i]
            with ExitStack() as s2:
                hs = dict(
                    W=sb("Wh", [128, 8, 640], BF16, s2),
                    UB=[sb(f"UB{j}", [128, 8, 512], BF16, s2) for j in range(2)],
                    TR=[sb(f"TR{j}", [128, 512], F32, s2) for j in range(2)],
                    ROPE=sb("ROPE", [128, 4, 512], F32, s2),
                    OSB=sb("OSB", [128, 512], F32, s2), SQh=sb("SQh", [128, 512], F32, s2),
                    RS=sb("RS", [128, 512], F32, s2),
                )
                Q = sb("Q", [128, NT], BF16, s2)
                K = sb("K", [128, NT], BF16, s2)
                V = sb("V", [128, 18, 128], BF16, s2)
                E = [sb(f"E{j}", [128, 512], BF16, s2) for j in range(4)]
                R1 = sb("R1", [128, 512], F32, s2)
                R2 = sb("R2", [128, 512], F32, s2)
                A1 = sb("A1", [128, 512], F32, s2)
                for h in range(8):
                    cols = dict(q=h * 128, k=1024 + h * 128, v=2048 + h * 128)
                    in_proj_head(l, hs, "C", wsrc, cols,
                                 lambda b: (Q[:, BLOCKS[b][0]:BLOCKS[b][0] + BLOCKS[b][1]], f"qb{b}"),
                                 lambda b: (K[:, BLOCKS[b][0]:BLOCKS[b][0] + BLOCKS[b][1]], f"kb{b}"),
                                 V, None, None, ropeC_d, 0.125)
                    for b in range(5 if need_ctx else 4):
                        t0, n, grp = BLOCKS[b]
                        kchunks = list(range(18)) if grp == 0 else [16, 17]
                        acc = [(PS[j], f"ps{j}") for j in range(4)]
                        def issue_S(ci):
                            c = kchunks[ci]
                            kb_ = c // 4 if c < 16 else 4
                            ksl = slice(c * 128, (c + 1) * 128)
                            for comp in range(2):
                                sj = 4 + 2 * (ci % 2) + comp
                                rsl = slice(comp * 64, (comp + 1) * 64)
                                mm(PS[sj][:, :n], K[rsl, ksl], Q[rsl, t0:t0 + n], True, True, [f"kb{kb_}", f"qb{b}"], [f"ps{sj}"])

                        def issue_rest(ci):
                            c = kchunks[ci]
                            first, last = ci == 0, ci == len(kchunks) - 1
                            kb_ = c // 4 if c < 16 else 4
                            for comp in range(2):
                                sj = 4 + 2 * (ci % 2) + comp
                                ej = 2 * (ci % 2) + comp
                                act(E[ej][:, :n], PS[sj][:, :n], AF.Exp, [f"ps{sj}"], [f"E{ej}"])
                            for comp in range(2):
                                ej = 2 * (ci % 2) + comp
                                po, pko = acc[2 * comp]
                                pd, pkd = acc[2 * comp + 1]
                                mm(po[:, :n], V[:, c, :], E[ej][:, :n], first, last, [f"vb{kb_}", f"E{ej}"], [pko])
                                mm(pd[:, :n], ONESB, E[ej][:, :n], first, last, ["CSTB", f"E{ej}"], [pkd])

                        issue_S(0)
                        for ci in range(len(kchunks)):
                            if ci + 1 < len(kchunks):
                                issue_S(ci + 1)
                            issue_rest(ci)
                        act(R1[:, :n], acc[1][0][:, :n], AF.Ln, [acc[1][1]], ["R1"])
                        act(R1[:, :n], R1[:, :n], AF.Exp, ["R1"], ["R1"], scale=-1.0)
                        act(R2[:, :n], acc[3][0][:, :n], AF.Ln, [acc[3][1]], ["R2"])
                        act(R2[:, :n], R2[:, :n], AF.Exp, ["R2"], ["R2"], scale=-1.0)
                        tt(A1[:, :n], acc[0][0][:, :n], R1[:, :n], ALU.mult, [acc[0][1], "R1"], ["A1"])
                        tt(R2[:, :n], acc[2][0][:, :n], R2[:, :n], ALU.mult, [acc[2][1], "R2"], ["R2"])
                        stt(hs["OSB"][:, :n], R2[:, :n], LAM[:, i, h:h + 1], A1[:, :n], ALU.mult, ALU.add,
                            ["R2", "A1", "LAM"], ["OSB"])
                        psc[0] = 4
                        head_norm(None, None, n, False, hs, UY[:, h, t0:t0 + n], f"u{h}b{b}", None, ["SUBG"],
                                  scale_ap=SUBG[:, i, h:h + 1], src_sb=True)
                        psc[0] = 4
            P.barrier()

        def moe(l, need_ctx):
            nblk = 5 if need_ctx else 4
            with ExitStack() as s2:
                WB = [sb(f"WB{j}", [128, 4096], BF16, s2) for j in range(4)]
                WCT = sb("WCT", [32, NT], BF16, s2)
                SELE = sb("SELE", [32, 32 * 128], BF16, s2)
                BC = [sb(f"BC{j}", [128, NT], BF16, s2) for j in range(2)]
                H = [sb(f"H{j}", [128, 4, 512], BF16, s2) for j in range(2)]
                SG = [sb(f"SG{j}", [128, 512], F32, s2) for j in range(2)]
                TF = [sb(f"TF{j}", [128, 512], F32, s2) for j in range(3)]
                SQ = [sb(f"SQ{j}", [128, 512], F32, s2) for j in range(2)]
                MEAN = sb("MEAN", [128, 512], F32, s2)
                RSTD = sb("RSTD", [128, 512], F32, s2)
                LG = sb("LG", [128, 36], F32, s2)
                SM = sb("SM", [128, 16], F32, s2)
                GOH = sb("GOH", [128, 4], F32, s2)
                GE = sb("GE", [128, 4], F32, s2)
                ET = sb("ET", [128, 32], F32, s2)
                ES = sb("ES", [128, 8], F32, s2)
                T8 = sb("T8", [128, 8], F32, s2)
                SEL = sb("SEL", [128, 8], F32, s2)
                EX = sb("EX", [128, 8], F32, s2)
                WC = sb("WC", [128, 32], F32, s2)
                P.dma("sp", SELE[:], sele_d, [], ["SELE"])
                WR = sb("WRl", [128, 8, 36], F32, s2)
                P.dma("sp", WR[:], wr_d[:, l, :, :], [], ["WR"])
                items = []
                for e in range(32):
                    items.append(("g", e, wg_d[l, e].rearrange("(k p) n -> p k n", p=128)))
                    items.append(("u", e, wu_d[l, e].rearrange("(k p) n -> p k n", p=128)))
                    items.append(("d", e, wd_d[l, e].rearrange("(k p) n -> p k n", p=128)))

                def load_item(j):
                    if j >= len(items):
                        return
                    kind, e, src = items[j]
                    if kind == "d":
                        dst = WB[j % 4][:, :].rearrange("p (k n) -> p k n", n=1024)
                    else:
                        dst = WB[j % 4][:, :].rearrange("p (k n) -> p k n", n=512)
                    P.dma("pool", dst, src, [], [f"WB{j % 4}"])

                for j in range(4):
                    load_item(j)
                ti = 0
                for b in range(nblk):
                    t0, n, grp = BLOCKS[b]
                    ntile = n // 128
                    pr = [nb() for _ in range(ntile)]
                    for k in range(8):
                        tf = TF[ti % 3]
                        tk = f"TF{ti % 3}"
                        ti += 1
                        act(tf[:, :n], XT[:, k, t0:t0 + n], AF.Identity, [f"x{k}b{b}", "MOD"], [tk],
                            scale=mod(l, 4, k, grp), bias=mod(l, 3, k, grp))
                        cp(UY[:, k, t0:t0 + n], tf[:, :n], [tk], [f"u{k}b{b}"])
                        for tI in range(ntile):
                            mm(pr[tI][0][:, 0:36], tf[:, tI * 128:(tI + 1) * 128], WR[:, k, :], k == 0, False,
                               [tk, "WR"], [pr[tI][1]])
                    for tI in range(ntile):
                        ps, pk = pr[tI]
                        mm(ps[:, 0:36], ONES1, RB[0:1, l, :], False, True, ["CST", "RB"], [pk])
                        cp(LG[:], ps[:, 0:36], [pk], ["LG"])
                        R_ = ["LG", "SM", "GOH", "GE", "ET", "ES", "T8", "SEL", "EX", "WC"]

                        def dv(fn):
                            P.op("dve", fn, R_, R_)
                        dv(lambda: nc.vector.tensor_reduce(out=SM[:, 0:1], in_=LG[:, 0:4], axis=AX.X, op=ALU.max))
                        dv(lambda: nc.vector.tensor_scalar(out=GOH[:], in0=LG[:, 0:4], scalar1=SM[:, 0:1], scalar2=None,
                                                           op0=ALU.is_equal))
                        dv(lambda: nc.vector.tensor_scalar(out=SM[:, 1:2], in0=SM[:, 0:1], scalar1=-1.0, scalar2=None,
                                                           op0=ALU.mult))
                        act(GE[:], LG[:, 0:4], AF.Exp, R_, R_, bias=SM[:, 1:2])
                        dv(lambda: nc.vector.tensor_reduce(out=SM[:, 2:3], in_=GE[:], axis=AX.X, op=ALU.add))
                        dv(lambda: nc.vector.reciprocal(out=SM[:, 3:4], in_=SM[:, 2:3]))
                        dv(lambda: nc.vector.tensor_tensor(
                            out=ET[:].rearrange("p (g e) -> p g e", e=8),
                            in0=LG[:, 4:36].rearrange("p (g e) -> p g e", e=8),
                            in1=GOH[:].unsqueeze(2).broadcast_to([128, 4, 8]), op=ALU.mult))
                        dv(lambda: nc.vector.tensor_reduce(out=ES[:], in_=ET[:].rearrange("p (g e) -> p e g", e=8),
                                                           axis=AX.X, op=ALU.add))
                        dv(lambda: nc.vector.max(out=T8[:], in_=ES[:]))
                        dv(lambda: nc.vector.tensor_scalar(out=SEL[:], in0=ES[:], scalar1=T8[:, 1:2], scalar2=None,
                                                           op0=ALU.is_ge))
                        dv(lambda: nc.vector.tensor_scalar(out=SM[:, 4:5], in0=T8[:, 0:1], scalar1=-1.0, scalar2=None,
                                                           op0=ALU.mult))
                        act(EX[:], ES[:], AF.Exp, R_, R_, bias=SM[:, 4:5])
                        dv(lambda: nc.vector.tensor_tensor(out=EX[:], in0=EX[:], in1=SEL[:], op=ALU.mult))
                        dv(lambda: nc.vector.tensor_reduce(out=SM[:, 5:6], in_=EX[:], axis=AX.X, op=ALU.add))
                        dv(lambda: nc.vector.reciprocal(out=SM[:, 6:7], in_=SM[:, 5:6]))
                        dv(lambda: nc.vector.tensor_tensor(out=SM[:, 7:8], in0=SM[:, 6:7], in1=SM[:, 3:4], op=ALU.mult))
                        dv(lambda: nc.vector.tensor_scalar(out=EX[:], in0=EX[:], scalar1=SM[:, 7:8], scalar2=None,
                                                           op0=ALU.mult))
                        dv(lambda: nc.vector.tensor_tensor(
                            out=WC[:].rearrange("p (g e) -> p g e", e=8),
                            in0=GOH[:].unsqueeze(2).broadcast_to([128, 4, 8]),
                            in1=EX[:].unsqueeze(1).broadcast_to([128, 4, 8]), op=ALU.mult))
                        pt, pkt = nb()
                        P.op("pe", lambda: nc.tensor.transpose(out=pt[0:32, 0:128], in_=WC[:], identity=IDF),
                             R_ + ["CST"], [pkt])
                        cp(WCT[:, t0 + tI * 128:t0 + (tI + 1) * 128], pt[0:32, 0:128], [pkt], [f"wct{b}"], eng="act")
                for b in range(nblk):
                    t0, n, grp = BLOCKS[b]
                    for k in range(8):
                        xk = XT[:, k, t0:t0 + n]
                        if k % 2 == 0:
                            act(xk, xk, AF.Copy, [f"x{k}b{b}"], [f"x{k}b{b}"], scale=ALPHA)
                        else:
                            ts(xk, xk, ALPHA, ALU.mult, [f"x{k}b{b}"], [f"x{k}b{b}"])
                hi = 0
                for e in range(32):
                    j0 = 3 * e
                    WG = WB[j0 % 4][:, :].rearrange("p (k n) -> p k n", n=512)
                    WU = WB[(j0 + 1) % 4][:, :].rearrange("p (k n) -> p k n", n=512)
                    WD = WB[(j0 + 2) % 4][:, :].rearrange("p (k n) -> p k n", n=1024)
                    kg, ku, kd = f"WB{j0 % 4}", f"WB{(j0 + 1) % 4}", f"WB{(j0 + 2) % 4}"
                    bc = BC[e % 2]
                    bck = f"BC{e % 2}"
                    for b in range(nblk):
                        t0, n, grp = BLOCKS[b]
                        ps, pk = nb()
                        mm(ps[:, :n], SELE[:, e * 128:(e + 1) * 128], WCT[:, t0:t0 + n], True, True, ["SELE", f"wct{b}"], [pk])
                        cp(bc[:, t0:t0 + n], ps[:, :n], [pk], [bck + f"b{b}"], eng="act")
                    for b in range(nblk):
                        t0, n, grp = BLOCKS[b]
                        Hb = H[hi % 2]
                        hk = f"H{hi % 2}"
                        hi += 1
                        for fc in range(4):
                            pg, pkg = nb()
                            pu, pku = nb()
                            for k in range(8):
                                mm(pg[:, :n], WG[:, k, fc * 128:(fc + 1) * 128], UY[:, k, t0:t0 + n], k == 0, k == 7,
                                   [kg, f"u{k}b{b}"], [pkg])
                            for k in range(8):
                                mm(pu[:, :n], WU[:, k, fc * 128:(fc + 1) * 128], UY[:, k, t0:t0 + n], k == 0, k == 7,
                                   [ku, f"u{k}b{b}"], [pku])
                            sg = SG[fc % 2]
                            act(sg[:, :n], pg[:, :n], AF.Silu, [pkg], [f"SG{fc % 2}"])
                            tt(sg[:, :n], sg[:, :n], pu[:, :n], ALU.mult, [f"SG{fc % 2}", pku], [f"SG{fc % 2}"])
                            tt(Hb[:, fc, :n], sg[:, :n], bc[:, t0:t0 + n], ALU.mult, [f"SG{fc % 2}", bck + f"b{b}"],
                               [hk + f"f{fc}"])
                        if b == nblk - 1:
                            load_item(j0 + 4)
                            load_item(j0 + 5)
                        for oc in range(8):
                            py, pky = nb()
                            for fc in range(4):
                                mm(py[:, :n], WD[:, fc, oc * 128:(oc + 1) * 128], Hb[:, fc, :n], fc == 0, fc == 3,
                                   [kd, hk + f"f{fc}"], [pky])
                            xk = XT[:, oc, t0:t0 + n]
                            stt(xk, py[:, :n], mod(l, 5, oc, grp), xk, ALU.mult, ALU.add, [pky, "MOD", f"x{oc}b{b}"],
                                [f"x{oc}b{b}"])
                    load_item(j0 + 6)
                for b in range(nblk):
                    ln_block(l, 1, b, (SQ, MEAN, RSTD))
            P.barrier()

        BREG = []

        def moe_sparse(l, need_ctx):
            if not BREG:
                BREG.append(nc.gpsimd.to_reg(4 * 32 * 128 - 1))
            nblk = 5 if need_ctx else 4
            ntiles = 18 if need_ctx else 16
            RB_ = 256
            RT = RB_ // 128
            NB_ = (ntiles * 128 * 2) // RB_ + 32
            NZ = NB_ * RT
            POOL = (mybir.EngineType.Pool,)
            with ExitStack() as s2:
                AST = sb("AST", [128, 18, 32], BF16, s2)
                WCS = sb("WCS", [128, 18, 32], F32, s2)
                CSS = sb("CSS", [128, 18, 32], F32, s2)
                PRE = sb("PRE", [128, 19, 32], F32, s2)
                PST = sb("PST", [128, 32], F32, s2)
                PEN = sb("PEN", [128, 32], F32, s2)
                DESTF = sb("DESTF", [128, 18, 2], F32, s2)
                DEST = sb("DEST", [128, 18, 2], U32, s2)
                WSEL = sb("WSEL", [128, 18, 2], F32, s2)
                IDXW = sb("IDXW", [128, 72], U32, s2)
                with ExitStack() as s3:
                    TF = [sb(f"TF{j}", [128, 512], F32, s3) for j in range(3)]
                    WR = sb("WRl", [128, 8, 36], F32, s3)
                    LG = sb("LG", [128, 36], F32, s3)
                    SM = sb("SM", [128, 16], F32, s3)
                    GOH = sb("GOH", [128, 4], F32, s3)
                    GE = sb("GE", [128, 4], F32, s3)
                    ET = sb("ET", [128, 32], F32, s3)
                    ES = sb("ES", [128, 8], F32, s3)
                    T8 = sb("T8", [128, 8], F32, s3)
                    SEL = sb("SEL", [128, 8], F32, s3)
                    EX = sb("EX", [128, 8], F32, s3)
                    NBK = sb("NBK", [128, 32, 36], F32, s3)
                    CNT = sb("CNT", [128, 32], F32, s3)
                    PADB = sb("PADB", [32, 128], F32, s3)
                    DP1 = sb("DP1", [128, 32], F32, s3)
                    EQ = sb("EQ", [128, 32], F32, s3)
                    BLE = sb("BLE", [128, 72, 32], F32, s3)
                    BLKB = sb("BLKB", [128, 72], F32, s3)
                    P.dma("sp", WR[:], wr_d[:, l, :, :], [], ["WR"])
                    ti = 0
                    for b in range(nblk):
                        t0, n, grp = BLOCKS[b]
                        ntile = n // 128
                        pr = [nb() for _ in range(ntile)]
                        for k in range(8):
                            tf = TF[ti % 3]
                            tk = f"TF{ti % 3}"
                            ti += 1
                            act(tf[:, :n], XT[:, k, t0:t0 + n], AF.Identity, [f"x{k}b{b}", "MOD"], [tk],
                                scale=mod(l, 4, k, grp), bias=mod(l, 3, k, grp))
                            cp(UY[:, k, t0:t0 + n], tf[:, :n], [tk], [f"u{k}b{b}"])
                            for tI in range(ntile):
                                mm(pr[tI][0][:, 0:36], tf[:, tI * 128:(tI + 1) * 128], WR[:, k, :], k == 0, False,
                                   [tk, "WR"], [pr[tI][1]])
                        for tI in range(ntile):
                            gi = t0 // 128 + tI
                            ps, pk = pr[tI]
                            mm(ps[:, 0:36], ONES1, RB[0:1, l, :], False, True, ["CST", "RB"], [pk])
                            cp(LG[:], ps[:, 0:36], [pk], ["LG"])
                            R_ = ["LG", "SM", "GOH", "GE", "ET", "ES", "T8", "SEL", "EX"]

                            def dv(fn, extra_w=()):
                                P.op("dve", fn, R_, R_ + list(extra_w))
                            dv(lambda: nc.vector.tensor_reduce(out=SM[:, 0:1], in_=LG[:, 0:4], axis=AX.X, op=ALU.max))
                            dv(lambda: nc.vector.tensor_scalar(out=GOH[:], in0=LG[:, 0:4], scalar1=SM[:, 0:1], scalar2=None,
                                                               op0=ALU.is_equal))
                            dv(lambda: nc.vector.tensor_scalar(out=SM[:, 1:2], in0=SM[:, 0:1], scalar1=-1.0, scalar2=None,
                                                               op0=ALU.mult))
                            act(GE[:], LG[:, 0:4], AF.Exp, R_, R_, bias=SM[:, 1:2])
                            dv(lambda: nc.vector.tensor_reduce(out=SM[:, 2:3], in_=GE[:], axis=AX.X, op=ALU.add))
                            dv(lambda: nc.vector.reciprocal(out=SM[:, 3:4], in_=SM[:, 2:3]))
                            dv(lambda: nc.vector.tensor_tensor(
                                out=ET[:].rearrange("p (g e) -> p g e", e=8),
                                in0=LG[:, 4:36].rearrange("p (g e) -> p g e", e=8),
                                in1=GOH[:].unsqueeze(2).broadcast_to([128, 4, 8]), op=ALU.mult))
                            dv(lambda: nc.vector.tensor_reduce(out=ES[:], in_=ET[:].rearrange("p (g e) -> p e g", e=8),
                                                               axis=AX.X, op=ALU.add))
                            dv(lambda: nc.vector.max(out=T8[:], in_=ES[:]))
                            dv(lambda: nc.vector.tensor_scalar(out=SEL[:], in0=ES[:], scalar1=T8[:, 1:2], scalar2=None,
                                                               op0=ALU.is_ge))
                            dv(lambda: nc.vector.tensor_scalar(out=SM[:, 4:5], in0=T8[:, 0:1], scalar1=-1.0, scalar2=None,
                                                               op0=ALU.mult))
                            act(EX[:], ES[:], AF.Exp, R_, R_, bias=SM[:, 4:5])
                            dv(lambda: nc.vector.tensor_tensor(out=EX[:], in0=EX[:], in1=SEL[:], op=ALU.mult))
                            dv(lambda: nc.vector.tensor_reduce(out=SM[:, 5:6], in_=EX[:], axis=AX.X, op=ALU.add))
                            dv(lambda: nc.vector.reciprocal(out=SM[:, 6:7], in_=SM[:, 5:6]))
                            dv(lambda: nc.vector.tensor_tensor(out=SM[:, 7:8], in0=SM[:, 6:7], in1=SM[:, 3:4], op=ALU.mult))
                            dv(lambda: nc.vector.tensor_scalar(out=EX[:], in0=EX[:], scalar1=SM[:, 7:8], scalar2=None,
                                                               op0=ALU.mult))
                            dv(lambda: nc.vector.tensor_tensor(
                                out=WCS[:, gi, :].rearrange("p (g e) -> p g e", e=8),
                                in0=GOH[:].unsqueeze(2).broadcast_to([128, 4, 8]),
                                in1=EX[:].unsqueeze(1).broadcast_to([128, 4, 8]), op=ALU.mult), ["WCS"])
                            dv(lambda: nc.vector.tensor_tensor(
                                out=AST[:, gi, :].rearrange("p (g e) -> p g e", e=8),
                                in0=GOH[:].unsqueeze(2).broadcast_to([128, 4, 8]),
                                in1=SEL[:].unsqueeze(1).broadcast_to([128, 4, 8]), op=ALU.mult), ["AST"])
                    for g4 in range(0, ntiles, 4):
                        m4 = min(4, ntiles - g4)
                        ps, pk = nb()
                        for j in range(m4):
                            mm(ps[:, j * 32:(j + 1) * 32], ONESB, AST[:, g4 + j, :], True, True, ["CSTB", "AST"], [pk])
                        cp(CSS[:, g4:g4 + m4, :], ps[:, 0:m4 * 32].rearrange("p (j e) -> p j e", e=32), [pk], ["CSS"])
                    P.op("dve", lambda: nc.vector.memset(PRE[:, 0, :], 0.0), [], ["PRE"])
                    for j in range(ntiles):
                        tt(PRE[:, j + 1, :], PRE[:, j, :], CSS[:, j, :], ALU.add, ["PRE", "CSS"], ["PRE"])
                    ps, pk = nb()
                    for j in range(ntiles):
                        mm(ps[0:32, 0:2], AST[:, j, :], ONESB[:, 0:2], j == 0, j == ntiles - 1, ["AST", "CSTB"], [pk])
                    cp(CNT[0:32, 0:1], ps[0:32, 0:1], [pk], ["CNT"])
                    ts(CNT[0:32, 3:4], CNT[0:32, 0:1], 128.0 / RB_, ALU.mult, ["CNT"], ["CNT"])
                    tt(NBK[0:32, 0, :], CNT[0:32, 3:4].broadcast_to([32, 36]), CST3[0:32, 0:36], ALU.is_gt,
                       ["CNT", "CST3"], ["NBK"])
                    P.op("dve", lambda: nc.vector.tensor_reduce(out=CNT[0:32, 1:2], in_=NBK[0:32, 0, :], axis=AX.X, op=ALU.add),
                         ["NBK", "CNT"], ["CNT"])
                    ts(CNT[0:32, 2:3], CNT[0:32, 1:2], float(RB_), ALU.mult, ["CNT"], ["CNT"])
                    cp(PADB[:, :], CNT[0:32, 2:3].broadcast_to([32, 128]), ["CNT"], ["PADB"])
                    ps, pk = nb()
                    mm(ps[:, 0:32], PADB[:, :], CST3[0:32, 64:96], True, True, ["PADB", "CST3"], [pk])
                    mm(ps[:, 32:64], PADB[:, :], CST3[0:32, 96:128], True, True, ["PADB", "CST3"], [pk])
                    cp(PST[:], ps[:, 0:32], [pk], ["PST"])
                    cp(PEN[:], ps[:, 32:64], [pk], ["PEN"])
                    ts(EQ[:], PEN[:], 128.0 / RB_, ALU.mult, ["PEN", "EQ"], ["EQ"])
                    tt(BLE[:, 0:NB_, :], EQ[:].unsqueeze(1).broadcast_to([128, NB_, 32]),
                       CST3[:, 136:136 + NB_].unsqueeze(2).broadcast_to([128, NB_, 32]), ALU.is_le, ["EQ", "CST3"], ["BLE"])
                    P.op("dve", lambda: nc.vector.tensor_reduce(out=BLKB[:, 0:NB_], in_=BLE[:, 0:NB_, :], axis=AX.X, op=ALU.add),
                         ["BLE"], ["BLKB"])
                    ts(BLKB[:, 0:NB_], BLKB[:, 0:NB_], 31.0, ALU.min, ["BLKB"], ["BLKB"], s2=128.0, op1=ALU.mult)
                    ts(BLKB[:, 0:NB_], BLKB[:, 0:NB_], CST3[:, 129:130], ALU.add, ["BLKB", "CST3"], ["BLKB"],
                       s2=float(l * 4096), op1=ALU.add)
                    blef = BLE[:, 0:3, :].rearrange("p a b -> p (a b)")[:, 0:NB_]
                    ts(blef, CST3[:, 136:136 + NB_], EQ[:, 31:32], ALU.is_ge, ["CST3", "EQ", "BLE"], ["BLE"])
                    stt(BLKB[:, 0:NB_], blef, 1.0e6, BLKB[:, 0:NB_], ALU.mult, ALU.add, ["BLE", "BLKB"], ["BLKB"])
                    cp(IDXW[:, 0:NB_], BLKB[:, 0:NB_], ["BLKB"], ["IDXW"])
                    for g4 in range(0, ntiles, 4):
                        m4 = min(4, ntiles - g4)
                        ps, pk = nb()
                        for j in range(m4):
                            mm(ps[:, j * 32:(j + 1) * 32], TRIS, AST[:, g4 + j, :], True, True, ["CSTB2", "AST"], [pk])
                        for j in range(m4):
                            gi = g4 + j
                            R2 = ["DP1", "EQ", "T8"]
                            tt(DP1[:], ps[:, j * 32:(j + 1) * 32], PRE[:, gi, :], ALU.add, [pk, "PRE"] + R2, R2)
                            tt(DP1[:], DP1[:], PST[:], ALU.add, R2 + ["PST"], R2)
                            stt(DP1[:], DP1[:], 1.0, AST[:, gi, :], ALU.add, ALU.mult, R2 + ["AST"], R2)
                            P.op("dve", lambda: nc.vector.max(out=T8[:], in_=DP1[:]), R2 + ["LG"], R2 + ["LG"])
                            ts(DESTF[:, gi, :], T8[:, 0:2], -1.0, ALU.add, R2, ["DESTF"])
                            for kk in range(2):
                                ts(EQ[:], DP1[:], T8[:, kk:kk + 1], ALU.is_equal, R2, R2)
                                tt(EQ[:], EQ[:], WCS[:, gi, :], ALU.mult, R2 + ["WCS"], R2)
                                P.op("dve", lambda kk=kk, gi=gi: nc.vector.tensor_reduce(
                                    out=WSEL[:, gi, kk:kk + 1], in_=EQ[:], axis=AX.X, op=ALU.add), R2 + ["WSEL"], R2 + ["WSEL"])
                    cp(DEST[:], DESTF[:], ["DESTF"], ["DEST"])
                P.barrier()
                if MOE_STOP <= 1:
                    return
                with ExitStack() as s3:
                    UT = [sb(f"UT{j}", [128, 1024], BF16, s3) for j in range(2)]
                    ZT = sb("ZT", [128, 1024], BF16, s3)
                    P.op("dve", lambda: nc.vector.memset(ZT[:], 0.0), [], ["ZT"])
                    for bb in range(NZ):
                        P.dma("sp", xs_d[bb * 128:(bb + 1) * 128, :], ZT[:, :], ["ZT"], [f"xsz{bb}"])
                    for gi in range(ntiles):
                        ut = UT[gi % 2]
                        uk = f"UT{gi % 2}"
                        b = gi // 4 if gi < 16 else 4
                        for half in range(2):
                            ps, pk = nb()
                            for kk in range(4):
                                k = half * 4 + kk
                                mm(ps[:, kk * 128:(kk + 1) * 128], UY[:, k, gi * 128:(gi + 1) * 128], IDB, True, True,
                                   [f"u{k}b{b}", "CSTB"], [pk])
                            cp(ut[:, half * 512:(half + 1) * 512], ps[:, :], [pk], [uk], eng="act" if half else "dve")
                        for kk in range(2):
                            P.dma_fn("pool", lambda sem, ut=ut, gi=gi, kk=kk: nc.gpsimd.indirect_dma_start(
                                out=xs_d, out_offset=bass.IndirectOffsetOnAxis(ap=DEST[:, gi, kk:kk + 1], axis=0),
                                in_=ut[:, :], in_offset=None).then_inc(sem, 16),
                                [uk, "DEST"] + [f"xsz{b_}" for b_ in range(NZ)], [f"xss{gi}_{kk}"])
                P.barrier()
                with ExitStack() as s3:
                    NWB = 5
                    NXS = 2
                    WB = [sb(f"WB{j}", [128, 4096], BF16, s3) for j in range(NWB)]
                    XS = [sb(f"XS{j}", [128, RT, 1024], BF16, s3) for j in range(NXS)]
                    XST = [sb(f"XST{j}", [128, 8, RB_], BF16, s3) for j in range(2)]
                    H = [sb(f"H{j}", [128, 4, RB_], BF16, s3) for j in range(2)]
                    SG = [sb(f"SG{j}", [128, 512], F32, s3) for j in range(2)]
                    YS = [sb(f"YS{j}", [128, 1024], F32, s3) for j in range(2 * RT)]
                    items = []
                    for bb in range(NB_ if MOE_STOP > 2 else 0):
                        items += [("g", bb), ("u", bb), ("d", bb)]
                    def load_item(j):
                        if j >= len(items):
                            return
                        kind, bb = items[j]
                        c0_ = {"g": 0, "u": 2, "d": 4}[kind]
                        for hh_ in range(2):
                            P.dma_fn("pool", lambda sem, j=j, bb=bb, c=c0_ + hh_, hh_=hh_: nc.gpsimd.indirect_dma_start(
                                out=WB[j % NWB][:, hh_ * 2048:(hh_ + 1) * 2048], out_offset=None, in_=wc_d[c],
                                in_offset=bass.IndirectOffsetOnAxis(ap=IDXW[:, bb:bb + 1], axis=0),
                                bounds_check=BREG[0], oob_is_err=False).then_inc(sem, 16),
                                ["IDXW"], [f"WB{j % NWB}"])

                    for j in range(NWB):
                        load_item(j)
                    for bb in range(NB_ if MOE_STOP > 2 else 0):
                        j0 = 3 * bb
                        WG = WB[j0 % NWB][:, :].rearrange("p (k n) -> p k n", n=512)
                        WU = WB[(j0 + 1) % NWB][:, :].rearrange("p (k n) -> p k n", n=512)
                        WD = WB[(j0 + 2) % NWB][:, :].rearrange("p (k n) -> p k n", n=1024)
                        kg, ku, kd = f"WB{j0 % NWB}", f"WB{(j0 + 1) % NWB}", f"WB{(j0 + 2) % NWB}"
                        xs, xk = XS[bb % NXS], f"XS{bb % NXS}"
                        xst, xtk = XST[bb % 2], f"XST{bb % 2}"
                        Hb, hk = H[bb % 2], f"H{bb % 2}"
                        P.dma("sp", xs[:, :, :], xs_d[bb * RB_:(bb + 1) * RB_, :].rearrange("(t p) n -> p t n", p=128),
                              [f"xsz{bb * RT + r_}" for r_ in range(RT)]
                              + [f"xss{g_}_{k_}" for g_ in range(ntiles) for k_ in range(2)], [xk])
                        for rt in range(RT):
                            for half in range(2):
                                ps, pk = nb()
                                for kk in range(4):
                                    k = half * 4 + kk
                                    mm(ps[:, kk * 128:(kk + 1) * 128], xs[:, rt, k * 128:(k + 1) * 128], IDB, True, True,
                                       [xk, "CSTB"], [pk])
                                cp(xst[:, half * 4:(half + 1) * 4, rt * 128:(rt + 1) * 128],
                                   ps[:, :].rearrange("p (k r) -> p k r", r=128), [pk], [xtk], eng="act" if half else "dve")
                        pgs = [nb() for _ in range(RT)]
                        pus = [nb() for _ in range(RT)]
                        fpb = 512 // RB_
                        for fc in range(4):
                            pg_, pkg_ = pgs[fc // fpb]
                            for k in range(8):
                                mm(pg_[:, (fc % fpb) * RB_:(fc % fpb + 1) * RB_], WG[:, k, fc * 128:(fc + 1) * 128], xst[:, k, :],
                                   k == 0, k == 7, [kg, xtk], [pkg_])
                        for fc in range(4):
                            pu_, pku_ = pus[fc // fpb]
                            for k in range(8):
                                mm(pu_[:, (fc % fpb) * RB_:(fc % fpb + 1) * RB_], WU[:, k, fc * 128:(fc + 1) * 128], xst[:, k, :],
                                   k == 0, k == 7, [ku, xtk], [pku_])
                        for i_ in range(RT):
                            sg = SG[i_ % 2]
                            act(sg[:, :], pgs[i_][0][:, :], AF.Silu, [pgs[i_][1]], [f"SG{i_ % 2}"])
                            tt(Hb[:, i_ * fpb:(i_ + 1) * fpb, :].rearrange("p f r -> p (f r)"), sg[:, :], pus[i_][0][:, :],
                               ALU.mult, [f"SG{i_ % 2}", pus[i_][1]], [hk])
                        load_item(j0 + 5)
                        for rt in range(RT):
                            ys, yk = YS[(bb % 2) * RT + rt], f"YS{(bb % 2) * RT + rt}"
                            for half in range(2):
                                py, pky = nb()
                                for fc in range(4):
                                    mm(py[:, :], Hb[:, fc, rt * 128:(rt + 1) * 128], WD[:, fc, half * 512:(half + 1) * 512],
                                       fc == 0, fc == 3, [kd, hk], [pky])
                                cp(ys[:, half * 512:(half + 1) * 512], py[:, :], [pky], [yk], eng="act" if half else "dve")
                            P.dma("act", ys_d[bb * RB_ + rt * 128:bb * RB_ + (rt + 1) * 128, :], ys[:, :], [yk], [f"ysd{bb}_{rt}"])
                        load_item(j0 + 6)
                        load_item(j0 + 7)
                P.barrier()
                if MOE_STOP <= 3:
                    return
                with ExitStack() as s3:
                    SQ = [sb(f"SQ{j}", [128, 512], F32, s3) for j in range(2)]
                    MEAN = sb("MEAN", [128, 512], F32, s3)
                    RSTD = sb("RSTD", [128, 512], F32, s3)
                    GG = [[sb(f"G{j}_{i_}", [128, 1024], F32, s3) for j in range(2)] for i_ in range(2)]
                    ZZ = [sb(f"Z{i_}", [128, 1024], F32, s3) for i_ in range(2)]
                    for b in range(nblk):
                        t0, n, grp = BLOCKS[b]
                        for k in range(8):
                            xk = XT[:, k, t0:t0 + n]
                            if k % 2 == 0:
                                act(xk, xk, AF.Copy, [f"x{k}b{b}"], [f"x{k}b{b}"], scale=ALPHA)
                            else:
                                ts(xk, xk, ALPHA, ALU.mult, [f"x{k}b{b}"], [f"x{k}b{b}"])
                    ysk = [f"ysd{b_}_{r_}" for b_ in range(NB_) for r_ in range(RT)]
                    for gi in range(ntiles):
                        b = gi // 4 if gi < 16 else 4
                        grp = BLOCKS[b][2]
                        G0, G1 = GG[gi % 2]
                        Z = ZZ[gi % 2]
                        g0k, g1k, zk = f"G0_{gi % 2}", f"G1_{gi % 2}", f"Z{gi % 2}"
                        for kk, (G, gk) in enumerate(((G0, g0k), (G1, g1k))):
                            P.dma_fn("pool", lambda sem, G=G, gi=gi, kk=kk: nc.gpsimd.indirect_dma_start(
                                out=G[:, :], out_offset=None, in_=ys_d,
                                in_offset=bass.IndirectOffsetOnAxis(ap=DEST[:, gi, kk:kk + 1], axis=0)).then_inc(sem, 16),
                                ysk + ["DEST"], [gk])
                        ts(Z[:, :], G0[:, :], WSEL[:, gi, 0:1], ALU.mult, [g0k, "WSEL"], [zk])
                        stt(Z[:, :], G1[:, :], WSEL[:, gi, 1:2], Z[:, :], ALU.mult, ALU.add, [g1k, "WSEL", zk], [zk])
                        for half in range(2):
                            ps, pk = nb()
                            for kk in range(4):
                                k = half * 4 + kk
                                P.op("pe", lambda k=k, kk=kk, ps=ps, Z=Z: nc.tensor.transpose(
                                    out=ps[:, kk * 128:(kk + 1) * 128], in_=Z[:, k * 128:(k + 1) * 128], identity=IDF),
                                    [zk, "CST"], [pk])
                            for kk in range(4):
                                k = half * 4 + kk
                                xk = XT[:, k, gi * 128:(gi + 1) * 128]
                                stt(xk, ps[:, kk * 128:(kk + 1) * 128], mod(l, 5, k, grp), xk, ALU.mult, ALU.add,
                                    [pk, "MOD", f"x{k}b{b}"], [f"x{k}b{b}"])
                    for b in range(nblk):
                        ln_block(l, 1, b, (SQ, MEAN, RSTD))
            P.barrier()

        for l in range(n_layers):
            if stop == "ada":
                break
            lastl = l == DEPTH - 1
            need_ctx = not lastl
            is_dbg_last = (l == n_layers - 1)
            if l % 2 == 0:
                mixer_ab(l, need_ctx)
                wo = ab_w_out_d[l // 2]
            else:
                mixer_c(l, need_ctx)
                wo = c_w_out_d[l // 2]
            raw = is_dbg_last and stop == "mix"
            if is_dbg_last and stop == "y":
                for b in range(5):
                    t0, n, grp = BLOCKS[b]
                    for k in range(8):
                        cp(XT[:, k, t0:t0 + n], UY[:, k, t0:t0 + n], [f"u{k}b{b}"], [f"x{k}b{b}"])
                break
            proj_ln(l, wo, 5 if need_ctx else 4, raw)
            if is_dbg_last and stop in ("mix", "ln1"):
                break
            if SPARSE:
                moe_sparse(l, need_ctx)
            else:
                moe(l, need_ctx)

        for k in range(8):
            P.dma("sp", out_d[k], XT[:, k, :], xkeys(ks=[k]), [f"out{k}"])
        P.finish("sp", [f"out{k}" for k in range(8)])
        print("bass ops:", P.nops, P.cnt)
    return nc


def _fm(v):
    v = np.asarray(v, np.float32)
    lead = v.shape[:-1]
    r = v.reshape(lead + (8, 128))
    r = np.moveaxis(r, -1, 0)
    return np.ascontiguousarray(r)


def _rope_tables(head_dim, per, qscale):
    quarter = head_dim // 4
    row = np.repeat(np.arange(T // 64, dtype=np.float32), 64)
    col = np.tile(np.arange(64, dtype=np.float32), T // 64)
    inv = (np.float32(10000.0) ** (-np.arange(quarter, dtype=np.float32) / np.float32(quarter))).astype(np.float32)
    ang_r = row[:, None] * inv
    ang_c = col[:, None] * inv
    ang = np.concatenate([ang_r, ang_r, ang_c, ang_c], axis=-1)
    cos = np.cos(ang).astype(np.float32).T
    sin = np.sin(ang).astype(np.float32).T
    sign = np.ones((head_dim, 1), np.float32)
    sign[0:quarter] = -1.0
    sign[2 * quarter:3 * quarter] = -1.0
    sin = sin * sign
    rep = 128 // head_dim
    cos = np.tile(cos, (rep, 1))
    sin = np.tile(sin, (rep, 1))
    return np.ascontiguousarray(np.stack([cos * qscale, sin * qscale, cos, sin]).astype(np.float32))


_CONSTS = None


def _consts():
    global _CONSTS
    if _CONSTS is not None:
        return _CONSTS
    c = {}
    c["ropeA"] = _rope_tables(128, 128, np.float32(128 ** -0.5))
    c["ropeC"] = _rope_tables(64, 64, np.float32(0.125))
    ii = np.arange(128)
    cst = np.zeros((128, 1024), np.float32)
    cst[:, 0:128] = np.eye(128, dtype=np.float32)
    cst[:, 128:256] = 1.0 / 1024.0
    cst[:, 256:384] = 1.0 / 128.0
    cst[:, 384:512] = np.where(ii[:, None] <= ii[None, :], -1.0 / 16.0, 0.0)
    cst[:, 512:640] = 1.0
    c["cst"] = cst
    cst2 = np.zeros((128, 256), np.float32)
    cst2[:, 0:128] = np.where(ii[:, None] >= ii[None, :], -1.0 / 16.0, 0.0)
    cst2[:, 128:256] = np.where(ii[:, None] > ii[None, :], -1.0 / 16.0, 0.0)
    c["cst2"] = cst2
    cstb = np.zeros((128, 256), np.float32)
    cstb[:, 0:128] = np.eye(128)
    cstb[:, 128:256] = 1.0
    c["cstb"] = cstb.astype(ml_dtypes.bfloat16)
    m = (ii[:, None] > ii[None, :]).astype(np.uint8)
    c["mask"] = np.ascontiguousarray(np.tile(m, (1, 4)))
    cst3 = np.zeros((128, 256), np.float32)
    cst3[:, 0:36] = (np.arange(36, dtype=np.float32) * 128.0)[None, :]
    e32 = np.arange(32)
    cst3[0:32, 64:96] = (e32[:, None] < e32[None, :]).astype(np.float32)
    cst3[0:32, 96:128] = (e32[:, None] <= e32[None, :]).astype(np.float32)
    cst3[:, 128] = np.arange(128, dtype=np.float32) * 128.0
    cst3[:, 129] = np.arange(128, dtype=np.float32)
    cst3[:, 136:208] = (np.arange(72, dtype=np.float32) * 128.0)[None, :]
    c["cst3"] = cst3
    c["cstb2"] = (ii[:, None] < ii[None, :]).astype(np.float32).astype(ml_dtypes.bfloat16)
    sele = np.zeros((32, 32, 128), np.float32)
    for e in range(32):
        sele[e, e, :] = 1.0
    c["sele"] = sele.reshape(32, 32 * 128).astype(ml_dtypes.bfloat16)
    ret = np.zeros((128, 4, 6, 128), np.float32)
    pos = np.arange(128, dtype=np.float64)
    for h in range(4):
        lf = float(np.log1p(-np.exp2(-np.float32(5.0 + h)), dtype=np.float32))
        lb = float(np.log1p(-np.exp2(-np.float32(5.5 + h)), dtype=np.float32))
        ret[:, h, 0, :] = np.exp(lf * (pos + 1))
        ret[:, h, 1, :] = np.exp(-lf * (pos + 1))
        ret[:, h, 2, :] = np.exp(lf * (127 - pos))
        ret[:, h, 3, :] = np.exp(lb * (127 - pos))
        ret[:, h, 4, :] = np.exp(-lb * (128 - pos))
        ret[:, h, 5, :] = np.exp(lb * pos)
    c["ret"] = ret
    _CONSTS = c
    return c


def _prep_inputs(inputs):
    f = lambda a: np.ascontiguousarray(np.asarray(a, np.float32))
    shared = dict(_consts())
    shared["adab"] = np.ascontiguousarray(np.asarray(inputs["ada_b"], np.float32).reshape(4, 48, 128).transpose(2, 0, 1))
    lnp = np.stack([inputs["ln1_g"], inputs["ln1_b"], inputs["ln2_g"], inputs["ln2_b"]], axis=1)
    shared["lnp"] = np.ascontiguousarray(np.asarray(lnp, np.float32).reshape(4, 4, 8, 128).transpose(3, 0, 1, 2))
    gn = np.concatenate([inputs["ab_gn_a"], inputs["ab_gn_b"]], axis=1)
    shared["gn"] = np.ascontiguousarray(np.asarray(gn, np.float32).reshape(2, 8, 128).transpose(2, 0, 1))
    shared["subg"] = np.ascontiguousarray(np.asarray(inputs["c_subln_g"], np.float32).reshape(2, 8, 128).transpose(2, 0, 1))
    wlr = np.zeros((32, 2, 2, 256), np.float32)
    wlr[0:16, :, 0, :] = np.asarray(inputs["ab_w_lr_f"]).transpose(1, 0, 2)
    wlr[0:16, :, 1, :] = np.asarray(inputs["ab_w_lr_b"]).transpose(1, 0, 2)
    wlr[16, :, 0, :] = np.asarray(inputs["ab_b_lr_f"])
    wlr[16, :, 1, :] = np.asarray(inputs["ab_b_lr_b"])
    shared["wlr"] = wlr
    wr = np.concatenate([inputs["moe_w_grp"], inputs["moe_w_rexp"]], axis=2)
    shared["wr"] = np.ascontiguousarray(np.asarray(wr, np.float32).reshape(4, 8, 128, 36).transpose(2, 0, 1, 3))
    rb = np.concatenate([inputs["moe_b_grp"], inputs["moe_b_rexp"]], axis=1)
    shared["rb"] = np.ascontiguousarray(np.asarray(rb, np.float32)[None])
    lqk = np.stack([inputs["c_lq1"], inputs["c_lk1"], inputs["c_lq2"], inputs["c_lk2"]], axis=1)
    shared["lqk"] = np.ascontiguousarray(np.asarray(lqk, np.float32).transpose(3, 0, 1, 2))
    for nm in ("ada_w", "ab_w_in", "ab_w_out", "c_w_qkv", "c_w_out"):
        shared[nm] = f(inputs[nm])
    if SPARSE:
        for ci, nm in ((0, "moe_w_gate"), (2, "moe_w_up")):
            wp = np.asarray(inputs[nm], np.float32).reshape(4, 32, 8, 128, 512).transpose(0, 1, 3, 2, 4).reshape(4 * 32 * 128, 4096)
            shared[f"wc{ci}"] = np.ascontiguousarray(wp[:, 0:2048])
            shared[f"wc{ci + 1}"] = np.ascontiguousarray(wp[:, 2048:4096])
        wp = np.asarray(inputs["moe_w_down"], np.float32).reshape(4, 32, 4, 128, 1024).transpose(0, 1, 3, 2, 4).reshape(4 * 32 * 128, 4096)
        shared["wc4"] = np.ascontiguousarray(wp[:, 0:2048])
        shared["wc5"] = np.ascontiguousarray(wp[:, 2048:4096])
    else:
        for nm in ("moe_w_gate", "moe_w_up", "moe_w_down"):
            shared[nm] = f(inputs[nm])
    x = np.asarray(inputs["x"], np.float32)
    ctx = np.asarray(inputs["ctx"], np.float32)
    c = np.asarray(inputs["c"], np.float32)
    c_ctx = np.asarray(inputs["c_ctx"], np.float32)
    in_maps = []
    for b in range(8):
        tok = np.concatenate([x[b], ctx[b]], axis=0)
        xT = np.ascontiguousarray(tok.T.reshape(8, 128, NT))
        cc = np.stack([c[b].reshape(8, 128).T, c_ctx.reshape(8, 128).T], axis=-1)
        m = dict(shared)
        m["xT"] = xT
        m["cc"] = np.ascontiguousarray(cc.astype(np.float32))
        in_maps.append(m)
    return in_maps


_NC_CACHE = {}


def run(inputs, n_layers=DEPTH, stop=None, ncores=8):
    key = (n_layers, stop)
    if key not in _NC_CACHE:
        _NC_CACHE[key] = build(n_layers, stop)
    nc = _NC_CACHE[key]
    in_maps = _prep_inputs(inputs)[:ncores]
    if stop in ("ada", "mix", "ln1", "y") and n_layers <= 1:
        for m in in_maps:
            for nm in (["wc%d" % c for c in range(6)] if SPARSE else ["moe_w_gate", "moe_w_up", "moe_w_down"]):
                m.pop(nm)
    res = run_bass_kernel_spmd(nc, in_maps, core_ids=list(range(ncores)))
    outs = [np.asarray(r["outT"]).reshape(D, NT).T for r in res.results]
    return outs


def kernel(**inputs):
    outs = run(inputs)
    return np.ascontiguousarray(np.stack([o[:T] for o in outs], axis=0).astype(np.float32))
```

```python
import math
import numpy as np
import ml_dtypes
from contextlib import ExitStack
import concourse.bass as bass
import concourse.mybir as mybir
from concourse.bass_utils import run_bass_kernel_spmd

F32 = mybir.dt.float32
BF16 = mybir.dt.bfloat16
U8 = mybir.dt.uint8
U32 = mybir.dt.uint32
I32 = mybir.dt.int32
AF = mybir.ActivationFunctionType
ALU = mybir.AluOpType
AX = mybir.AxisListType

D = 1024
T = 2048
TC = 256
NT = T + TC
DEPTH = 4
ALPHA = (2 * DEPTH) ** 0.25
EPS = 1e-5
BLOCKS = [(0, 512, 0), (512, 512, 0), (1024, 512, 0), (1536, 512, 0), (2048, 256, 1)]
ORDER_F = [16, 17] + list(range(16))
ORDER_B = [17, 16] + list(range(15, -1, -1))
AB_IN = 3616
import os as _os
SPARSE = int(_os.environ.get("MOE_SPARSE", "1"))
MOE_STOP = int(_os.environ.get("MOE_STOP", "9"))
DBG_HEADS = int(_os.environ.get("DBG_HEADS", "8"))
DBG_B = int(_os.environ.get("DBG_B", "9"))
DBG_C = _os.environ.get("DBG_C", "z")


class Prog:
    def __init__(self, nc, es, ndma=28):
        self.nc = nc
        self.e = dict(pe=nc.tensor, act=nc.scalar, dve=nc.vector, pool=nc.gpsimd, sp=nc.sync)
        self.sem = {k: es.enter_context(nc.semaphore("s_" + k)) for k in self.e}
        self.cnt = {k: 0 for k in self.e}
        self.seen = {k: {} for k in self.e}
        self.st = {}
        self.dq = {}
        for q in ("sp", "pool", "act"):
            self.dq[q] = dict(sems=[es.enter_context(nc.semaphore(f"d_{q}{i}")) for i in range(ndma)],
                              vals=[0] * ndma, idx=0)
        self.nops = 0
        self.bar = es.enter_context(nc.sbuf_tensor("barrier_t", [128, 1], F32))

    def _wait(self, eng, ticks):
        need = {}
        for t in ticks:
            if t is None:
                continue
            key, sem, val = t
            if key == "pe" and eng == "pe":
                continue
            if need.get(key, (None, 0))[1] < val:
                need[key] = (sem, val)
        for key, (sem, val) in need.items():
            if self.seen[eng].get(key, 0) >= val:
                continue
            self.e[eng].wait_ge(sem, val)
            self.seen[eng][key] = val

    def _deps(self, R, W):
        ticks = []
        for k in R:
            s = self.st.get(k)
            if s is not None:
                ticks.append(s[0])
                if k.startswith("ps"):
                    ticks.extend(s[1].values())
        for k in W:
            s = self.st.get(k)
            if s is not None:
                ticks.append(s[0])
                ticks.extend(s[1].values())
        return ticks

    def _update(self, tick, R, W):
        for k in W:
            self.st[k] = [tick, {}]
        for k in R:
            s = self.st.get(k)
            if s is None:
                s = self.st[k] = [None, {}]
            s[1][tick[0]] = tick

    def op(self, eng, fn, R, W):
        self._wait(eng, self._deps(R, W))
        inst = fn()
        inst.then_inc(self.sem[eng], 1)
        self.cnt[eng] += 1
        self.nops += 1
        self._update((eng, self.sem[eng], self.cnt[eng]), R, W)

    def dma(self, q, out, in_, R, W):
        d = self.dq[q]
        i = d["idx"]
        d["idx"] = (i + 1) % len(d["sems"])
        key = ("d", q, i)
        ticks = self._deps(R, W)
        if d["vals"][i] > 0:
            ticks.append((key, d["sems"][i], d["vals"][i]))
        self._wait(q, ticks)
        self.e[q].dma_start(out=out, in_=in_).then_inc(d["sems"][i], 16)
        d["vals"][i] += 16
        self.nops += 1
        self._update((key, d["sems"][i], d["vals"][i]), R, W)

    def dma_fn(self, q, fn, R, W):
        d = self.dq[q]
        i = d["idx"]
        d["idx"] = (i + 1) % len(d["sems"])
        key = ("d", q, i)
        ticks = self._deps(R, W)
        if d["vals"][i] > 0:
            ticks.append((key, d["sems"][i], d["vals"][i]))
        self._wait(q, ticks)
        fn(d["sems"][i])
        d["vals"][i] += 16
        self.nops += 1
        self._update((key, d["sems"][i], d["vals"][i]), R, W)

    def barrier(self):
        ticks = []
        for e in self.e:
            if self.cnt[e] > 0:
                ticks.append((e, self.sem[e], self.cnt[e]))
        for q, d in self.dq.items():
            for i, v in enumerate(d["vals"]):
                if v > 0:
                    ticks.append((("d", q, i), d["sems"][i], v))
        self._wait("dve", ticks)
        inst = self.nc.vector.memset(self.bar[:], 0.0)
        inst.then_inc(self.sem["dve"], 1)
        self.cnt["dve"] += 1
        t = ("dve", self.sem["dve"], self.cnt["dve"])
        for e in ("pe", "act", "pool", "sp"):
            self._wait(e, [t])

    def finish(self, eng, keys):
        self._wait(eng, self._deps(keys, []))


def build(n_layers=DEPTH, stop=None):
    nc = bass.Bass("TRN2", target_bir_lowering=False)

    def din(name, shape, dt=F32):
        return nc.dram_tensor(name, list(shape), dt, kind="ExternalInput").ap()

    xT_d = din("xT", [8, 128, NT])
    cc_d = din("cc", [128, 8, 2])
    adab_d = din("adab", [128, 4, 48])
    lnp_d = din("lnp", [128, 4, 4, 8])
    gn_d = din("gn", [128, 2, 8])
    subg_d = din("subg", [128, 2, 8])
    wlr_d = din("wlr", [32, 2, 2, 256])
    wr_d = din("wr", [128, 4, 8, 36])
    rb_d = din("rb", [1, 4, 36])
    lqk_d = din("lqk", [64, 2, 4, 8])
    ret_d = din("ret", [128, 4, 6, 128])
    ropeA_d = din("ropeA", [4, 128, T])
    ropeC_d = din("ropeC", [4, 128, T])
    cst_d = din("cst", [128, 1024])
    cst2_d = din("cst2", [128, 256])
    cstb_d = din("cstb", [128, 256], BF16)
    mask_d = din("mask", [128, 512], U8)
    cst3_d = din("cst3", [128, 256])
    cstb2_d = din("cstb2", [128, 128], BF16)
    xs_d = nc.dram_tensor("xs_scr", [12800, 1024], BF16, kind="Internal").ap()
    ys_d = nc.dram_tensor("ys_scr", [12800, 1024], F32, kind="Internal").ap()
    sele_d = din("sele", [32, 32 * 128], BF16)
    ada_w_d = din("ada_w", [4, D, 6 * D])
    ab_w_in_d = din("ab_w_in", [2, D, AB_IN])
    ab_w_out_d = din("ab_w_out", [2, D, D])
    c_w_qkv_d = din("c_w_qkv", [2, D, 3 * D])
    c_w_out_d = din("c_w_out", [2, D, D])
    use_moe = not (stop in ("ada", "mix", "ln1", "y") and n_layers <= 1)
    if use_moe and SPARSE:
        wc_d = [din(f"wc{c}", [4 * 32 * 128, 2048]) for c in range(6)]
    elif use_moe:
        wg_d = din("moe_w_gate", [4, 32, D, 512])
        wu_d = din("moe_w_up", [4, 32, D, 512])
        wd_d = din("moe_w_down", [4, 32, 512, D])
    out_d = nc.dram_tensor("outT", [8, 128, NT], F32, kind="ExternalOutput").ap()

    es = ExitStack()
    with es:
        P = Prog(nc, es)

        uid = [0]

        def sb(name, shape, dt=F32, stack=es):
            uid[0] += 1
            return stack.enter_context(nc.sbuf_tensor(f"{name}_{uid[0]}", list(shape), dt))

        PS = [es.enter_context(nc.psum_tensor(f"ps{i}", [128, 512], F32)) for i in range(8)]
        psc = [0]

        def nb():
            i = psc[0]
            psc[0] = (i + 1) % 8
            return PS[i], f"ps{i}"

        def mm(out, lhsT, rhs, start, stop, R, W):
            P.op("pe", lambda: nc.tensor.matmul(out, lhsT=lhsT, rhs=rhs, start=start, stop=stop), R, W)

        def act(out, in_, func, R, W, scale=None, bias=None, accum_out=None):
            kw = {}
            if scale is not None:
                kw["scale"] = scale
            if bias is None and func != AF.Copy:
                sp_, np_ = in_.start_partition(), in_.partition_size()
                bias = ZEROC[sp_:sp_ + np_, 0:1]
                R = list(R) + ["ZEROC"]
            if bias is not None:
                kw["bias"] = bias
            if accum_out is not None:
                kw["accum_out"] = accum_out
            P.op("act", lambda: nc.scalar.activation(out=out, in_=in_, func=func, **kw), R, W)

        def tt(out, a, b, op, R, W, eng="dve"):
            e = nc.vector if eng == "dve" else nc.gpsimd
            P.op(eng, lambda: e.tensor_tensor(out=out, in0=a, in1=b, op=op), R, W)

        def ts(out, a, s1, op0, R, W, s2=None, op1=None, eng="dve"):
            e = nc.vector if eng == "dve" else nc.gpsimd
            if op1 is None:
                P.op(eng, lambda: e.tensor_scalar(out=out, in0=a, scalar1=s1, scalar2=None, op0=op0), R, W)
            else:
                P.op(eng, lambda: e.tensor_scalar(out=out, in0=a, scalar1=s1, scalar2=s2, op0=op0, op1=op1), R, W)

        def stt(out, a, s, b, op0, op1, R, W):
            P.op("dve", lambda: nc.vector.scalar_tensor_tensor(out=out, in0=a, scalar=s, in1=b, op0=op0, op1=op1), R, W)

        def cp(out, in_, R, W, eng="dve"):
            if eng == "act":
                act(out, in_, AF.Copy, R, W)
            else:
                e = nc.vector if eng == "dve" else nc.gpsimd
                P.op(eng, lambda: e.tensor_copy(out=out, in_=in_), R, W)

        XT = sb("XT", [128, 8, NT])
        UY = sb("UY", [128, 8, NT], BF16)
        MOD = sb("MOD", [128, 4, 6, 8, 2])
        LNP = sb("LNP", [128, 4, 4, 8])
        GN = sb("GN", [128, 2, 8])
        SUBG = sb("SUBG", [128, 2, 8])
        RB = sb("RB", [1, 4, 36])
        LAM = sb("LAM", [128, 2, 8])
        CST = sb("CST", [128, 1024])
        CST2 = sb("CST2", [128, 256])
        CSTB = sb("CSTB", [128, 256], BF16)
        MASK = sb("MASK", [128, 512], U8)
        CST3 = sb("CST3", [128, 256])
        CSTB2 = sb("CSTB2", [128, 128], BF16)
        TRIS = CSTB2[:, 0:128]
        EPSC = sb("EPSC", [128, 1])
        ONEC = sb("ONEC", [128, 1])
        ZEROC = sb("ZEROC", [128, 1])
        IDF = CST[:, 0:128]
        ONESD = CST[:, 128:256]
        ONESH = CST[:, 256:384]
        TRIF = CST[:, 384:512]
        ONES1 = CST[0:1, 512:640]
        TRIB = CST2[:, 0:128]
        TRIBP = CST2[:, 128:256]
        IDB = CSTB[:, 0:128]
        ONESB = CSTB[:, 128:256]

        def xkeys(blocks=range(5), ks=range(8)):
            return [f"x{k}b{b}" for k in ks for b in blocks]

        def ukeys(blocks=range(5), ks=range(8)):
            return [f"u{k}b{b}" for k in ks for b in blocks]

        for k in range(8):
            P.dma("sp", XT[:, k, :], xT_d[k], [], xkeys(ks=[k]))
        for (t_, d_, kname) in [(LNP, lnp_d, "LNP"), (GN, gn_d, "GN"), (SUBG, subg_d, "SUBG"),
                                (RB, rb_d, "RB"), (CST, cst_d, "CST"), (CST2, cst2_d, "CST2"), (CSTB, cstb_d, "CSTB"),
                                (MASK, mask_d, "MASK"), (CST3, cst3_d, "CST3"), (CSTB2, cstb2_d, "CSTB2")]:
            P.dma("sp", t_[:], d_, [], [kname])
        P.op("dve", lambda: nc.vector.memset(EPSC[:], EPS), [], ["EPSC"])
        P.op("dve", lambda: nc.vector.memset(ONEC[:], 1.0), [], ["ONEC"])
        P.op("dve", lambda: nc.vector.memset(ZEROC[:], 0.0), [], ["ZEROC"])

        with ExitStack() as s1:
            CC = sb("CC", [128, 8, 2], F32, s1)
            SCB = sb("SCB", [128, 8, 2], BF16, s1)
            ADAB = sb("ADAB", [128, 4, 48], F32, s1)
            WA = [sb(f"WA{i}", [128, 8, 1024], BF16, s1) for i in range(2)]
            LQK = sb("LQK", [64, 2, 4, 8], F32, s1)
            LQP = sb("LQP", [64, 2, 2, 8], F32, s1)
            P.dma("sp", CC[:], cc_d, [], ["CC"])
            P.dma("sp", ADAB[:], adab_d, [], ["ADAB"])
            P.dma("sp", LQK[:], lqk_d, [], ["LQK"])
            act(SCB[:], CC[:], AF.Silu, ["CC"], ["SCB"])
            it = 0
            for l in range(n_layers):
                for j6 in range(6):
                    w = WA[it % 2]
                    wk = f"WA{it % 2}"
                    it += 1
                    src = ada_w_d[l].rearrange("(k p) n -> p k n", p=128)[:, :, j6 * 1024:(j6 + 1) * 1024]
                    P.dma("pool", w[:], src, [], [wk])
                    ps, pk = nb()
                    for jj in range(8):
                        for k in range(8):
                            mm(ps[:, jj * 2:jj * 2 + 2], w[:, k, jj * 128:(jj + 1) * 128], SCB[:, k, :],
                               k == 0, k == 7, [wk, "SCB"], [pk])
                    tt(MOD[:, l, j6, :, :], ps[:, 0:16].rearrange("p (j g) -> p j g", g=2),
                       ADAB[:, l, j6 * 8:(j6 + 1) * 8].unsqueeze(2).broadcast_to([128, 8, 2]), ALU.add,
                       [pk, "ADAB"], ["MOD"])
                for j6 in (1, 4):
                    ts(MOD[:, l, j6, :, :], MOD[:, l, j6, :, :], 1.0, ALU.add, ["MOD"], ["MOD"])
            tt(LQP[:, :, 0, :], LQK[:, :, 0, :], LQK[:, :, 1, :], ALU.mult, ["LQK"], ["LQP"])
            tt(LQP[:, :, 1, :], LQK[:, :, 2, :], LQK[:, :, 3, :], ALU.mult, ["LQP", "LQK"], ["LQP"])
            ps, pk = nb()
            mm(ps[:, 0:32], CST[0:64, 512:640], LQP[:].rearrange("p a b c -> p (a b c)"), True, True,
               ["CST", "LQP"], [pk])
            LE = sb("LE", [128, 32], F32, s1)
            act(LE[:], ps[:, 0:32], AF.Exp, [pk], ["LE"])
            lev = LE[:].rearrange("p (a b c) -> p a b c", a=2, b=2)
            for i in range(2):
                lam_init = 0.8 - 0.6 * math.exp(-0.3 * (2 * i + 1))
                tt(LAM[:, i, :], lev[:, i, 1, :], lev[:, i, 0, :], ALU.subtract, ["LE", "LAM"], ["LAM"])
                ts(LAM[:, i, :], LAM[:, i, :], -lam_init, ALU.add, ["LAM"], ["LAM"])
                ts(SUBG[:, i, :], SUBG[:, i, :], 1.0 - lam_init, ALU.mult, ["SUBG"], ["SUBG"])

        P.barrier()

        def mod(l, which, k, grp):
            return MOD[:, l, which, k, grp:grp + 1]

        def ln_block(l, which_ln, b, scr):
            t0, n, grp = BLOCKS[b]
            SQ, MEAN, RSTD = scr
            psm, pkm = nb()
            pss, pks = nb()
            for k in range(8):
                xk = XT[:, k, t0:t0 + n]
                mm(psm[:, :n], ONESD, xk, k == 0, k == 7, [f"x{k}b{b}", "CST"], [pkm])
                sq = SQ[k % 2]
                act(sq[:, :n], xk, AF.Square, [f"x{k}b{b}"], [f"SQ{k % 2}"])
                mm(pss[:, :n], ONESD, sq[:, :n], k == 0, k == 7, [f"SQ{k % 2}", "CST"], [pks])
            cp(MEAN[:, :n], psm[:, :n], [pkm], ["MEAN"], eng="act")
            act(RSTD[:, :n], psm[:, :n], AF.Square, [pkm], ["RSTD"])
            tt(RSTD[:, :n], pss[:, :n], RSTD[:, :n], ALU.subtract, [pks, "RSTD"], ["RSTD"])
            act(RSTD[:, :n], RSTD[:, :n], AF.Ln, ["RSTD"], ["RSTD"], bias=EPSC[:, 0:1])
            act(RSTD[:, :n], RSTD[:, :n], AF.Exp, ["RSTD"], ["RSTD"], scale=-0.5)
            for k in range(8):
                xk = XT[:, k, t0:t0 + n]
                kk = [f"x{k}b{b}"]
                tt(xk, xk, MEAN[:, :n], ALU.subtract, kk + ["MEAN"], kk)
                tt(xk, xk, RSTD[:, :n], ALU.mult, kk + ["RSTD"], kk)
                act(xk, xk, AF.Identity, kk + ["LNP"], kk, scale=LNP[:, l, 2 * which_ln, k:k + 1],
                    bias=LNP[:, l, 2 * which_ln + 1, k:k + 1])

        def proj_ln(l, w_dram, nblocks, raw):
            with ExitStack() as s2:
                WO = sb("WO", [128, 8, 1024], BF16, s2)
                TMP = [sb(f"TMPO{i}", [128, 512], F32, s2) for i in range(2)]
                SQ = [sb(f"SQ{i}", [128, 512], F32, s2) for i in range(2)]
                MEAN = sb("MEAN", [128, 512], F32, s2)
                RSTD = sb("RSTD", [128, 512], F32, s2)
                P.dma("pool", WO[:], w_dram.rearrange("(k p) n -> p k n", p=128), [], ["WO"])
                for b in range(nblocks):
                    t0, n, grp = BLOCKS[b]
                    for c in range(8):
                        ps, pk = nb()
                        for h in range(8):
                            mm(ps[:, :n], WO[:, h, c * 128:(c + 1) * 128], UY[:, h, t0:t0 + n], h == 0, h == 7,
                               ["WO", f"u{h}b{b}"], [pk])
                        xk = XT[:, c, t0:t0 + n]
                        kk = [f"x{c}b{b}"]
                        if raw:
                            cp(xk, ps[:, :n], [pk], kk, eng="act")
                        else:
                            tmp = TMP[c % 2]
                            act(tmp[:, :n], ps[:, :n], AF.Identity, [pk, "MOD"], [f"TMPO{c % 2}"], scale=mod(l, 2, c, grp))
                            stt(xk, xk, ALPHA, tmp[:, :n], ALU.mult, ALU.add, kk + [f"TMPO{c % 2}"], kk)
                    if not raw:
                        ln_block(l, 0, b, (SQ, MEAN, RSTD))
            P.barrier()

        def make_u(l, b, UB, ub_i, which_s, which_sh):
            t0, n, grp = BLOCKS[b]
            for k in range(8):
                src = XT[:, k, t0:t0 + n]
                dst = UB[ub_i][:, k, :n]
                R = [f"x{k}b{b}", "MOD"]
                W = [f"UB{ub_i}k{k}"]
                if k % 2 == 0:
                    act(dst, src, AF.Identity, R, W, scale=mod(l, which_s, k, grp), bias=mod(l, which_sh, k, grp))
                else:
                    ts(dst, src, mod(l, which_s, k, grp), ALU.mult, R, W, s2=mod(l, which_sh, k, grp), op1=ALU.add)

        def fm_group(UBt, ub_i, n, Wt, wkey, c0, M):
            ps, pk = nb()
            for k in range(8):
                mm(ps[0:M, :n], Wt[:, k, c0:c0 + M], UBt[:, k, :n], k == 0, k == 7, [wkey, f"UB{ub_i}k{k}"], [pk])
            return ps, pk

        def rope_evac(dst, dkey, ps_a, pk_a, ps_b, pk_b, cos_t, sin_t, tkey, n, TR, ti):
            t1 = TR[0]
            t2 = TR[1]
            tt(t1[:, :n], ps_a[:, :n], cos_t, ALU.mult, [pk_a, tkey], ["TR0"])
            tt(t2[:, :n], ps_b[:, :n], sin_t, ALU.mult, [pk_b, tkey], ["TR1"])
            tt(dst, t1[:, :n], t2[:, :n], ALU.add, ["TR0", "TR1"], [dkey])

        def in_proj_head(l, hs, kind, wsrc, cols, Qf, Kf, V, SGN, gn_ap, rope_d, qscale, LRT=None, lr_cols=None,
                         after_block=None, nblocks=5):
            dk = 64 if kind == "B" else 128
            has_rope = kind in ("A", "C")
            has_g = kind in ("A", "B")
            wv = wsrc.rearrange("(k p) n -> p k n", p=128)
            W = hs["W"]
            ofs = {}
            o = 0
            names = ["q", "k", "v"] + (["g"] if has_g else [])
            for nm in names:
                w_ = dk if nm in ("q", "k") else 128
                P.dma("pool", W[:, :, o:o + w_], wv[:, :, cols[nm]:cols[nm] + w_], [], [f"W_{nm}"])
                ofs[nm] = o
                o += w_
            if LRT is not None:
                P.dma("pool", W[:, :, o:o + 32], wv[:, :, lr_cols:lr_cols + 32], [], ["W_lr"])
                ofs["lr"] = o
                o += 32
            if has_rope:
                blk = 32 if kind == "A" else 16
                for nm in ("q", "k"):
                    s_ = W[:, :, ofs[nm]:ofs[nm] + 128].rearrange("p k (a two b) -> p k a two b", two=2, b=blk)
                    d_ = W[:, :, o:o + 128].rearrange("p k (a two b) -> p k a two b", two=2, b=blk)
                    cp(d_[:, :, :, 0, :], s_[:, :, :, 1, :], [f"W_{nm}"], [f"W_{nm}p"])
                    cp(d_[:, :, :, 1, :], s_[:, :, :, 0, :], [f"W_{nm}", f"W_{nm}p"], [f"W_{nm}p"])
                    ofs[nm + "p"] = o
                    o += 128
            UB = hs["UB"]
            TR = hs["TR"]
            ROPE = hs["ROPE"]
            ti = 0
            for b in range(nblocks):
                t0, n, grp = BLOCKS[b]
                ub_i = b % len(UB)
                make_u(l, b, UB, ub_i, 1, 0)
                UBt = UB[ub_i]
                if has_rope and grp == 0:
                    P.dma("sp", ROPE[:, :, :], rope_d[:, :, t0:t0 + n].rearrange("f p t -> p f t"), [], ["ROPE"])
                for nm, dst_f in (("q", Qf), ("k", Kf)):
                    ps, pk = fm_group(UBt, ub_i, n, W, f"W_{nm}", ofs[nm], dk)
                    dst, dkey = dst_f(b)
                    if has_rope and grp == 0:
                        ps2, pk2 = fm_group(UBt, ub_i, n, W, f"W_{nm}p", ofs[nm + "p"], dk)
                        fi = 0 if nm == "q" else 2
                        rope_evac(dst, dkey, ps, pk, ps2, pk2, ROPE[:, fi, :n], ROPE[:, fi + 1, :n], "ROPE", n, TR, ti)
                        ti += 1
                    else:
                        sc = qscale if nm == "q" else 1.0
                        act(dst, ps[0:dk, :n], AF.Copy, [pk], [dkey], scale=sc)
                if has_g:
                    ps, pk = fm_group(UBt, ub_i, n, W, "W_g", ofs["g"], 128)
                    t1 = TR[ti % 2]
                    act(t1[:, :n], ps[:, :n], AF.Silu, [pk], [f"TR{ti % 2}"])
                    ts(SGN[:, t0:t0 + n], t1[:, :n], gn_ap, ALU.mult, [f"TR{ti % 2}", "GN"], [f"sgn_b{b}"])
                    ti += 1
                if LRT is not None:
                    for di in range(2):
                        ps, pk = fm_group(UBt, ub_i, n, W, "W_lr", ofs["lr"] + 16 * di, 16)
                        cp(LRT[di][0:16, :n], ps[0:16, :n], [pk], [f"lrt{di}"], eng="act")
                ps, pk = nb()
                ntile = n // 128
                for tI in range(ntile):
                    for k in range(8):
                        mm(ps[:, tI * 128:(tI + 1) * 128], UBt[:, k, tI * 128:(tI + 1) * 128],
                           W[:, k, ofs["v"]:ofs["v"] + 128], k == 0, k == 7, ["W_v", f"UB{ub_i}k{k}"], [pk])
                c0 = t0 // 128
                cp(V[:, c0:c0 + ntile, :], ps[:, :n].rearrange("p (c d) -> p c d", d=128), [pk], [f"vb{b}"], eng="act")
                if after_block is not None:
                    after_block(b)

        def head_norm(ps_o, pk_o, n, center, hs, out_ap, outkey, post_ap, postkeys, scale_ap=None, src_sb=None):
            OSB, SQh, RS = hs["OSB"], hs["SQh"], hs["RS"]
            M2 = SQh
            if src_sb is None:
                cp(OSB[:, :n], ps_o[:, :n], [pk_o], ["OSB"], eng="act")
                act(SQh[:, :n], ps_o[:, :n], AF.Square, [pk_o], ["SQh"])
            else:
                act(SQh[:, :n], OSB[:, :n], AF.Square, ["OSB"], ["SQh"])
            pss, pks = nb()
            mm(pss[:, :n], ONESH, SQh[:, :n], True, True, ["SQh", "CST"], [pks])
            if center:
                psm, pkm = nb()
                mm(psm[:, :n], ONESH, OSB[:, :n], True, True, ["OSB", "CST"], [pkm])
                act(M2[:, :n], psm[:, :n], AF.Square, [pkm, "SQh"], ["SQh"])
                tt(RS[:, :n], pss[:, :n], M2[:, :n], ALU.subtract, [pks, "SQh"], ["RS"])
                tt(OSB[:, :n], OSB[:, :n], psm[:, :n], ALU.subtract, ["OSB", pkm], ["OSB"])
                act(RS[:, :n], RS[:, :n], AF.Ln, ["RS"], ["RS"], bias=EPSC[:, 0:1])
            else:
                act(RS[:, :n], pss[:, :n], AF.Ln, [pks], ["RS"], bias=EPSC[:, 0:1])
            act(RS[:, :n], RS[:, :n], AF.Exp, ["RS"], ["RS"], scale=-0.5)
            if scale_ap is None:
                tt(OSB[:, :n], OSB[:, :n], RS[:, :n], ALU.mult, ["OSB", "RS"], ["OSB"])
                tt(out_ap, OSB[:, :n], post_ap, ALU.mult, ["OSB"] + postkeys, [outkey])
            else:
                stt(out_ap, OSB[:, :n], scale_ap, RS[:, :n], ALU.mult, ALU.mult, ["OSB", "RS"] + postkeys, [outkey])

        def mixer_ab(l, need_ctx):
            i = l // 2
            wsrc = ab_w_in_d[i]
            with ExitStack() as s2:
                hs = dict(
                    W=sb("Wh", [128, 8, 768], BF16, s2),
                    UB=[sb("UB0", [128, 8, 512], BF16, s2)],
                    TR=[sb(f"TR{j}", [128, 512], F32, s2) for j in range(2)],
                    OSB=sb("OSB", [128, 512], F32, s2), SQh=sb("SQh", [128, 512], F32, s2),
                    RS=sb("RS", [128, 512], F32, s2),
                )
                Qt = sb("Qt", [128, 512], BF16, s2)
                Kt = sb("Kt", [128, 512], BF16, s2)
                V = sb("V", [128, 18, 128], BF16, s2)
                SGN = sb("SGN", [128, NT], BF16, s2)
                QDF = sb("QDF", [128, NT], BF16, s2)
                QDB = sb("QDB", [128, NT], BF16, s2)
                ATT = sb("ATT", [128, NT], BF16, s2)
                SFb = sb("SFb", [128, 18, 128], BF16, s2)
                SBb = sb("SBb", [128, 18, 128], BF16, s2)
                CUR = [sb(f"CUR{j}", [128, 128], F32, s2) for j in range(4)]
                KD = [sb(f"KD{j}", [128, 512], BF16, s2) for j in range(4)]
                KTT = [sb(f"KTT{j}", [128, 4, 128], BF16, s2) for j in range(2)]

                def Qf(b):
                    return Qt[:, :BLOCKS[b][1]], "qt"

                def Kf(b):
                    return Kt[:, :BLOCKS[b][1]], "kt"

                def Qf64(b):
                    return Qt[0:64, :BLOCKS[b][1]], "qt"

                def Kf64(b):
                    return Kt[0:64, :BLOCKS[b][1]], "kt"

                def run_head(h, ph):
                    isA = h < 4
                    dk = 128 if isA else 64
                    hh = h if isA else h - 4
                    if isA:
                        RET = ph["RET"]
                        P.dma("sp", RET[:], ret_d[:, hh, :, :], [], ["RET"])
                    else:
                        LRT, WLRb, LS, LS2, EXS, TAB, TOT, DEC = (ph[k_] for k_ in ("LRT", "WLRb", "LS", "LS2", "EXS", "TAB", "TOT", "DEC"))
                    P.op("dve", lambda: nc.vector.memset(SFb[:, 16, :], 0.0), [], ["SFb"])
                    P.op("dve", lambda: nc.vector.memset(SBb[:, 17, :], 0.0), [], ["SBb"])

                    def pass1(b):
                        t0, n, grp = BLOCKS[b]
                        nch = n // 128
                        c0 = t0 // 128
                        if (not isA) and DBG_B < 1:
                            return
                        qv = Qt[0:dk, :n]
                        kv = Kt[0:dk, :n]
                        if isA:
                            tabs = [RET[:, j, :].unsqueeze(1).broadcast_to([128, nch, 128]) for j in range(6)]
                            tkeys = ["RET"]

                            def rr(ap):
                                return ap.rearrange("p (c d) -> p c d", d=128)
                        else:
                            psg, pkg = nb()
                            for c in range(nch):
                                for di in range(2):
                                    mm(psg[:, c * 128 + di * 64:c * 128 + di * 64 + 64],
                                       LRT[di][:, c * 128:(c + 1) * 128], WLRb[:, di, hh * 64:(hh + 1) * 64],
                                       True, True, [f"lrt{di}", "WLRb"], [pkg])
                            act(EXS[:, :n], psg[:, :n], AF.Exp, [pkg], ["EXS"], scale=-1.0)
                            ex4 = EXS[:, :n].rearrange("p (c a d) -> p c a d", a=2, d=64)
                            act(LS[:, 0:nch, :, :], ex4, AF.Ln, ["EXS"], ["LS"], bias=ONEC[:, 0:1])
                            act(LS2[:, 0:nch, 0, :], ex4[:, :, 1, :], AF.Ln, ["EXS"], ["LS2"], bias=ONEC[:, 0:1])
                            act(LS2[:, 0:nch, 1, :], ex4[:, :, 0, :], AF.Ln, ["EXS", "LS2"], ["LS2"], bias=ONEC[:, 0:1])
                            if DBG_C < "b":
                                return
                            psf, pkf = nb()
                            psb, pkb = nb()
                            psp, pkp = nb()
                            for c in range(nch):
                                mm(psf[:, c * 128:(c + 1) * 128], LS[:, c, :, :].rearrange("p a d -> p (a d)"), TRIF, True, True, ["LS", "CST"], [pkf])
                            for c in range(nch):
                                mm(psb[:, c * 128:(c + 1) * 128], LS2[:, c, :, :].rearrange("p a d -> p (a d)"), TRIB, True, True, ["LS2", "CST2"], [pkb])
                            for c in range(nch):
                                mm(psp[:, c * 128:(c + 1) * 128], LS2[:, c, :, :].rearrange("p a d -> p (a d)"), TRIBP, True, True, ["LS2", "CST2"], [pkp])
                            if DBG_C < "c":
                                return
                            f3 = psf[0:64, :n].rearrange("p (c d) -> p c d", d=128)
                            b3 = psb[0:64, :n].rearrange("p (c d) -> p c d", d=128)
                            cp(TOT[0:64, 0:nch, 0:1], f3[:, :, 127:128], [pkf], ["TOT"])
                            cp(TOT[0:64, 0:nch, 1:2], b3[:, :, 0:1], [pkb, "TOT"], ["TOT"])
                            if DBG_C < "d":
                                return
                            act(TAB[0][0:64, :n], psf[0:64, :n], AF.Exp, [pkf, "TOT"], ["TAB0"])
                            act(TAB[1][0:64, :n], psf[0:64, :n], AF.Exp, [pkf, "TOT"], ["TAB1"], scale=-1.0)
                            act(TAB[3][0:64, :n], psp[0:64, :n], AF.Exp, [pkp, "TOT"], ["TAB3"])
                            act(TAB[4][0:64, :n], psb[0:64, :n], AF.Exp, [pkb, "TOT"], ["TAB4"], scale=-1.0)
                            if DBG_C < "e":
                                return
                            for c in range(nch):
                                act(TAB[2][0:64, c * 128:(c + 1) * 128], psf[0:64, c * 128:(c + 1) * 128], AF.Exp,
                                    [pkf, "TOT"], ["TAB2"], scale=-1.0, bias=TOT[0:64, c, 0:1])
                                act(TAB[5][0:64, c * 128:(c + 1) * 128], psb[0:64, c * 128:(c + 1) * 128], AF.Exp,
                                    [pkb, "TOT"], ["TAB5"], scale=-1.0, bias=TOT[0:64, c, 1:2])
                            act(DEC[0:64, c0:c0 + nch, :], TOT[0:64, 0:nch, :], AF.Exp, ["TOT"], ["DEC"])
                            tabs = [TAB[j][0:64, :n] for j in range(6)]
                            tkeys = [f"TAB{j}" for j in range(6)]

                            def rr(ap):
                                return ap
                        if (not isA) and DBG_B < 2:
                            return
                        tt(rr(QDF[0:dk, t0:t0 + n]), rr(qv), tabs[0], ALU.mult, ["qt"] + tkeys, [f"qdf{b}"])
                        tt(rr(QDB[0:dk, t0:t0 + n]), rr(qv), tabs[3], ALU.mult, ["qt"] + tkeys, [f"qdb{b}"])
                        tt(rr(KD[0][0:dk, :n]), rr(kv), tabs[1], ALU.mult, ["kt"] + tkeys, ["KD0"])
                        tt(rr(KD[1][0:dk, :n]), rr(kv), tabs[4], ALU.mult, ["kt"] + tkeys, ["KD1"])
                        tt(rr(KD[2][0:dk, :n]), rr(kv), tabs[2], ALU.mult, ["kt"] + tkeys, ["KD2"])
                        tt(rr(KD[3][0:dk, :n]), rr(kv), tabs[5], ALU.mult, ["kt"] + tkeys, ["KD3"])
                        psa, pka = nb()
                        psb2, pkb2 = nb()
                        for c in range(nch):
                            sl = slice(c * 128, (c + 1) * 128)
                            gsl = slice(t0 + c * 128, t0 + (c + 1) * 128)
                            mm(psa[:, sl], KD[0][0:dk, sl], QDF[0:dk, gsl], True, True, ["KD0", f"qdf{b}"], [pka])
                            mm(psb2[:, sl], KD[1][0:dk, sl], QDB[0:dk, gsl], True, True, ["KD1", f"qdb{b}"], [pkb2])
                        cp(ATT[:, t0:t0 + n], psa[:, :n], [pka], [f"att{b}"], eng="act")
                        P.op("dve", lambda: nc.vector.copy_predicated(out=ATT[:, t0:t0 + n], mask=MASK[:, :n],
                                                                       data=psb2[:, :n]),
                             [pkb2, "MASK", f"att{b}"], [f"att{b}"])
                        if (not isA) and DBG_B < 3:
                            return
                        for di in range(2):
                            psk, pkk = nb()
                            for c in range(nch):
                                mm(psk[:, c * 128:c * 128 + dk], KD[2 + di][0:dk, c * 128:(c + 1) * 128], IDB[0:dk, 0:dk],
                                   True, True, [f"KD{2 + di}", "CSTB"], [pkk])
                            cp(KTT[di][:, 0:nch, 0:dk], psk[:, :n].rearrange("p (c d) -> p c d", d=128)[:, :, 0:dk],
                               [pkk], [f"KTT{di}"], eng="act")
                            psd, pkd = nb()
                            for c in range(nch):
                                mm(psd[0:dk, c * 128:(c + 1) * 128], KTT[di][:, c, 0:dk], V[:, c0 + c, :], True, True,
                                   [f"KTT{di}", f"vb{b}"], [pkd])
                            d3 = psd[0:dk, :n].rearrange("p (c d) -> p c d", d=128)
                            if di == 0:
                                if grp == 0:
                                    m = nch if c0 + nch < 16 else nch - 1
                                    cp(SFb[0:dk, c0 + 1:c0 + 1 + m, :], d3[:, 0:m, :], [pkd], ["SFb"])
                                else:
                                    cp(SFb[0:dk, 17, :], d3[:, 0, :], [pkd], ["SFb"])
                                    cp(SFb[0:dk, 0, :], d3[:, 1, :], [pkd], ["SFb"])
                            else:
                                if c0 == 0:
                                    cp(SBb[0:dk, 0:nch - 1, :], d3[:, 1:nch, :], [pkd], ["SBb"])
                                else:
                                    cp(SBb[0:dk, c0 - 1:c0 - 1 + nch, :], d3[:, :, :], [pkd], ["SBb"])

                    if isA:
                        cols = dict(q=hh * 128, k=512 + hh * 128, v=1024 + hh * 128, g=1536 + hh * 128)
                        hs["ROPE"] = ph["ROPE"]
                        in_proj_head(l, hs, "A", wsrc, cols, Qf, Kf, V, SGN, GN[:, i, h:h + 1], ropeA_d, dk ** -0.5,
                                     after_block=pass1)
                    else:
                        cols = dict(q=2048 + hh * 64, k=2304 + hh * 64, v=2560 + hh * 128, g=3072 + hh * 128)
                        hs["ROPE"] = None
                        in_proj_head(l, hs, "B", wsrc, cols, Qf64, Kf64, V, SGN, GN[:, i, h:h + 1], None, dk ** -0.5,
                                     LRT=LRT, lr_cols=3584, after_block=pass1)
                    if (not isA) and DBG_B < 4:
                        return
                    for di, (ST, order, skey) in enumerate(((SFb, ORDER_F, "SFb"), (SBb, ORDER_B, "SBb"))):
                        c_a, c_b = CUR[2 * di], CUR[2 * di + 1]
                        ka, kb_ = f"CUR{2 * di}", f"CUR{2 * di + 1}"
                        P.op("dve", lambda c_a=c_a: nc.vector.memset(c_a[:], 0.0), [], [ka])
                        for oi in range(17):
                            nn, nx = order[oi], order[oi + 1]
                            if isA:
                                g_ = 1.0 - 2.0 ** (-((5.0 if di == 0 else 5.5) + hh))
                                sc_ = float(np.float32(g_) ** 128)
                            else:
                                sc_ = DEC[0:64, nn, di:di + 1]
                            stt(c_b[0:dk, :], c_a[0:dk, :], sc_, ST[0:dk, nx, :], ALU.mult, ALU.add,
                                [ka, skey] + ([] if isA else ["DEC"]), [kb_])
                            cp(ST[0:dk, nx, :], c_b[0:dk, :], [kb_], [skey], eng="act")
                            c_a, c_b, ka, kb_ = c_b, c_a, kb_, ka
                    if (not isA) and DBG_B < 5:
                        return
                    for b in range(5 if need_ctx else 4):
                        t0, n, grp = BLOCKS[b]
                        nch = n // 128
                        c0 = t0 // 128
                        pso, pko = nb()
                        for c in range(nch):
                            sl = slice(c * 128, (c + 1) * 128)
                            gsl = slice(t0 + c * 128, t0 + (c + 1) * 128)
                            mm(pso[:, sl], V[:, c0 + c, :], ATT[:, gsl], True, False, [f"vb{b}", f"att{b}"], [pko])
                            mm(pso[:, sl], SFb[0:dk, c0 + c, :], QDF[0:dk, gsl], False, False, ["SFb", f"qdf{b}"], [pko])
                            mm(pso[:, sl], SBb[0:dk, c0 + c, :], QDB[0:dk, gsl], False, True, ["SBb", f"qdb{b}"], [pko])
                        head_norm(pso, pko, n, isA, hs, UY[:, h, t0:t0 + n], f"u{h}b{b}", SGN[:, t0:t0 + n], [f"sgn_b{b}"])

                with ExitStack() as s3:
                    ph = dict(ROPE=sb("ROPE", [128, 4, 512], F32, s3), RET=sb("RET", [128, 6, 128], F32, s3))
                    for h in range(min(4, DBG_HEADS)):
                        run_head(h, ph)
                P.barrier()
                with ExitStack() as s3:
                    WLR = sb("WLR", [32, 2, 256], F32, s3)
                    ph = dict(
                        LRT=[sb(f"LRT{j}", [32, 512], BF16, s3) for j in range(2)],
                        WLRb=sb("WLRb", [32, 2, 256], BF16, s3),
                        LS=sb("LS", [128, 4, 2, 64], F32, s3),
                        LS2=sb("LS2", [128, 4, 2, 64], F32, s3),
                        EXS=sb("EXS", [128, 512], F32, s3),
                        TAB=[sb(f"TAB{j}", [128, 512], BF16, s3) for j in range(6)],
                        TOT=sb("TOT", [128, 4, 2], F32, s3),
                        DEC=sb("DEC", [128, 18, 2], F32, s3),
                    )
                    P.dma("sp", WLR[:], wlr_d[:, i, :, :], [], ["WLR"])
                    cp(ph["WLRb"][:], WLR[:], ["WLR"], ["WLRb"])
                    for di in range(2):
                        P.op("dve", lambda di=di: nc.vector.memset(ph["LRT"][di][:], 1.0), [], [f"lrt{di}"])
                    for h in range(4, min(8, DBG_HEADS)):
                        run_head(h, ph)
            P.barrier()

        def mixer_c(l, need_ctx):
            i = l // 2
            wsrc = c_w_qkv_d[i]
            with ExitStack() as s2:
                hs = dict(
                    W=sb("Wh", [128, 8, 640], BF16, s2),
                    UB=[sb(f"UB{j}", [128, 8, 512], BF16, s2) for j in range(2)],
                    TR=[sb(f"TR{j}", [128, 512], F32, s2) for j in range(2)],
                    ROPE=sb("ROPE", [128, 4, 512], F32, s2),
                    OSB=sb("OSB", [128, 512], F32, s2), SQh=sb("SQh", [128, 512], F32, s2),
                    RS=sb("RS", [128, 512], F32, s2),
                )
                Q = sb("Q", [128, NT], BF16, s2)
                K = sb("K", [128, NT], BF16, s2)
                V = sb("V", [128, 18, 128], BF16, s2)
                E = [sb(f"E{j}", [128, 512], BF16, s2) for j in range(4)]
                R1 = sb("R1", [128, 512], F32, s2)
                R2 = sb("R2", [128, 512], F32, s2)
                A1 = sb("A1", [128, 512], F32, s2)
                for h in range(8):
                    cols = dict(q=h * 128, k=1024 + h * 128, v=2048 + h * 128)
                    in_proj_head(l, hs, "C", wsrc, cols,
                                 lambda b: (Q[:, BLOCKS[b][0]:BLOCKS[b][0] + BLOCKS[b][1]], f"qb{b}"),
                                 lambda b: (K[:, BLOCKS[b][0]:BLOCKS[b][0] + BLOCKS[b][1]], f"kb{b}"),
                                 V, None, None, ropeC_d, 0.125)
                    for b in range(5 if need_ctx else 4):
                        t0, n, grp = BLOCKS[b]
                        kchunks = list(range(18)) if grp == 0 else [16, 17]
                        acc = [(PS[j], f"ps{j}") for j in range(4)]
                        def issue_S(ci):
                            c = kchunks[ci]
                            kb_ = c // 4 if c < 16 else 4
                            ksl = slice(c * 128, (c + 1) * 128)
                            for comp in range(2):
                                sj = 4 + 2 * (ci % 2) + comp
                                rsl = slice(comp * 64, (comp + 1) * 64)
                                mm(PS[sj][:, :n], K[rsl, ksl], Q[rsl, t0:t0 + n], True, True, [f"kb{kb_}", f"qb{b}"], [f"ps{sj}"])

                        def issue_rest(ci):
                            c = kchunks[ci]
                            first, last = ci == 0, ci == len(kchunks) - 1
                            kb_ = c // 4 if c < 16 else 4
                            for comp in range(2):
                                sj = 4 + 2 * (ci % 2) + comp
                                ej = 2 * (ci % 2) + comp
                                act(E[ej][:, :n], PS[sj][:, :n], AF.Exp, [f"ps{sj}"], [f"E{ej}"])
                            for comp in range(2):
                                ej = 2 * (ci % 2) + comp
                                po, pko = acc[2 * comp]
                                pd, pkd = acc[2 * comp + 1]
                                mm(po[:, :n], V[:, c, :], E[ej][:, :n], first, last, [f"vb{kb_}", f"E{ej}"], [pko])
                                mm(pd[:, :n], ONESB, E[ej][:, :n], first, last, ["CSTB", f"E{ej}"], [pkd])

                        issue_S(0)
                        for ci in range(len(kchunks)):
                            if ci + 1 < len(kchunks):
                                issue_S(ci + 1)
                            issue_rest(ci)
                        act(R1[:, :n], acc[1][0][:, :n], AF.Ln, [acc[1][1]], ["R1"])
                        act(R1[:, :n], R1[:, :n], AF.Exp, ["R1"], ["R1"], scale=-1.0)
                        act(R2[:, :n], acc[3][0][:, :n], AF.Ln, [acc[3][1]], ["R2"])
                        act(R2[:, :n], R2[:, :n], AF.Exp, ["R2"], ["R2"], scale=-1.0)
                        tt(A1[:, :n], acc[0][0][:, :n], R1[:, :n], ALU.mult, [acc[0][1], "R1"], ["A1"])
                        tt(R2[:, :n], acc[2][0][:, :n], R2[:, :n], ALU.mult, [acc[2][1], "R2"], ["R2"])
                        stt(hs["OSB"][:, :n], R2[:, :n], LAM[:, i, h:h + 1], A1[:, :n], ALU.mult, ALU.add,
                            ["R2", "A1", "LAM"], ["OSB"])
                        psc[0] = 4
                        head_norm(None, None, n, False, hs, UY[:, h, t0:t0 + n], f"u{h}b{b}", None, ["SUBG"],
                                  scale_ap=SUBG[:, i, h:h + 1], src_sb=True)
                        psc[0] = 4
            P.barrier()

        def moe(l, need_ctx):
            nblk = 5 if need_ctx else 4
            with ExitStack() as s2:
                WB = [sb(f"WB{j}", [128, 4096], BF16, s2) for j in range(4)]
                WCT = sb("WCT", [32, NT], BF16, s2)
                SELE = sb("SELE", [32, 32 * 128], BF16, s2)
                BC = [sb(f"BC{j}", [128, NT], BF16, s2) for j in range(2)]
                H = [sb(f"H{j}", [128, 4, 512], BF16, s2) for j in range(2)]
                SG = [sb(f"SG{j}", [128, 512], F32, s2) for j in range(2)]
                TF = [sb(f"TF{j}", [128, 512], F32, s2) for j in range(3)]
                SQ = [sb(f"SQ{j}", [128, 512], F32, s2) for j in range(2)]
                MEAN = sb("MEAN", [128, 512], F32, s2)
                RSTD = sb("RSTD", [128, 512], F32, s2)
                LG = sb("LG", [128, 36], F32, s2)
                SM = sb("SM", [128, 16], F32, s2)
                GOH = sb("GOH", [128, 4], F32, s2)
                GE = sb("GE", [128, 4], F32, s2)
                ET = sb("ET", [128, 32], F32, s2)
                ES = sb("ES", [128, 8], F32, s2)
                T8 = sb("T8", [128, 8], F32, s2)
                SEL = sb("SEL", [128, 8], F32, s2)
                EX = sb("EX", [128, 8], F32, s2)
                WC = sb("WC", [128, 32], F32, s2)
                P.dma("sp", SELE[:], sele_d, [], ["SELE"])
                WR = sb("WRl", [128, 8, 36], F32, s2)
                P.dma("sp", WR[:], wr_d[:, l, :, :], [], ["WR"])
                items = []
                for e in range(32):
                    items.append(("g", e, wg_d[l, e].rearrange("(k p) n -> p k n", p=128)))
                    items.append(("u", e, wu_d[l, e].rearrange("(k p) n -> p k n", p=128)))
                    items.append(("d", e, wd_d[l, e].rearrange("(k p) n -> p k n", p=128)))

                def load_item(j):
                    if j >= len(items):
                        return
                    kind, e, src = items[j]
                    if kind == "d":
                        dst = WB[j % 4][:, :].rearrange("p (k n) -> p k n", n=1024)
                    else:
                        dst = WB[j % 4][:, :].rearrange("p (k n) -> p k n", n=512)
                    P.dma("pool", dst, src, [], [f"WB{j % 4}"])

                for j in range(4):
                    load_item(j)
                ti = 0
                for b in range(nblk):
                    t0, n, grp = BLOCKS[b]
                    ntile = n // 128
                    pr = [nb() for _ in range(ntile)]
                    for k in range(8):
                        tf = TF[ti % 3]
                        tk = f"TF{ti % 3}"
                        ti += 1
                        act(tf[:, :n], XT[:, k, t0:t0 + n], AF.Identity, [f"x{k}b{b}", "MOD"], [tk],
                            scale=mod(l, 4, k, grp), bias=mod(l, 3, k, grp))
                        cp(UY[:, k, t0:t0 + n], tf[:, :n], [tk], [f"u{k}b{b}"])
                        for tI in range(ntile):
                            mm(pr[tI][0][:, 0:36], tf[:, tI * 128:(tI + 1) * 128], WR[:, k, :], k == 0, False,
                               [tk, "WR"], [pr[tI][1]])
                    for tI in range(ntile):
                        ps, pk = pr[tI]
                        mm(ps[:, 0:36], ONES1, RB[0:1, l, :], False, True, ["CST", "RB"], [pk])
                        cp(LG[:], ps[:, 0:36], [pk], ["LG"])
                        R_ = ["LG", "SM", "GOH", "GE", "ET", "ES", "T8", "SEL", "EX", "WC"]

                        def dv(fn):
                            P.op("dve", fn, R_, R_)
                        dv(lambda: nc.vector.tensor_reduce(out=SM[:, 0:1], in_=LG[:, 0:4], axis=AX.X, op=ALU.max))
                        dv(lambda: nc.vector.tensor_scalar(out=GOH[:], in0=LG[:, 0:4], scalar1=SM[:, 0:1], scalar2=None,
                                                           op0=ALU.is_equal))
                        dv(lambda: nc.vector.tensor_scalar(out=SM[:, 1:2], in0=SM[:, 0:1], scalar1=-1.0, scalar2=None,
                                                           op0=ALU.mult))
                        act(GE[:], LG[:, 0:4], AF.Exp, R_, R_, bias=SM[:, 1:2])
                        dv(lambda: nc.vector.tensor_reduce(out=SM[:, 2:3], in_=GE[:], axis=AX.X, op=ALU.add))
                        dv(lambda: nc.vector.reciprocal(out=SM[:, 3:4], in_=SM[:, 2:3]))
                        dv(lambda: nc.vector.tensor_tensor(
                            out=ET[:].rearrange("p (g e) -> p g e", e=8),
                            in0=LG[:, 4:36].rearrange("p (g e) -> p g e", e=8),
                            in1=GOH[:].unsqueeze(2).broadcast_to([128, 4, 8]), op=ALU.mult))
                        dv(lambda: nc.vector.tensor_reduce(out=ES[:], in_=ET[:].rearrange("p (g e) -> p e g", e=8),
                                                           axis=AX.X, op=ALU.add))
                        dv(lambda: nc.vector.max(out=T8[:], in_=ES[:]))
                        dv(lambda: nc.vector.tensor_scalar(out=SEL[:], in0=ES[:], scalar1=T8[:, 1:2], scalar2=None,
                                                           op0=ALU.is_ge))
                        dv(lambda: nc.vector.tensor_scalar(out=SM[:, 4:5], in0=T8[:, 0:1], scalar1=-1.0, scalar2=None,
                                                           op0=ALU.mult))
                        act(EX[:], ES[:], AF.Exp, R_, R_, bias=SM[:, 4:5])
                        dv(lambda: nc.vector.tensor_tensor(out=EX[:], in0=EX[:], in1=SEL[:], op=ALU.mult))
                        dv(lambda: nc.vector.tensor_reduce(out=SM[:, 5:6], in_=EX[:], axis=AX.X, op=ALU.add))
                        dv(lambda: nc.vector.reciprocal(out=SM[:, 6:7], in_=SM[:, 5:6]))
                        dv(lambda: nc.vector.tensor_tensor(out=SM[:, 7:8], in0=SM[:, 6:7], in1=SM[:, 3:4], op=ALU.mult))
                        dv(lambda: nc.vector.tensor_scalar(out=EX[:], in0=EX[:], scalar1=SM[:, 7:8], scalar2=None,
                                                           op0=ALU.mult))
                        dv(lambda: nc.vector.tensor_tensor(
                            out=WC[:].rearrange("p (g e) -> p g e", e=8),
                            in0=GOH[:].unsqueeze(2).broadcast_to([128, 4, 8]),
                            in1=EX[:].unsqueeze(1).broadcast_to([128, 4, 8]), op=ALU.mult))
                        pt, pkt = nb()
                        P.op("pe", lambda: nc.tensor.transpose(out=pt[0:32, 0:128], in_=WC[:], identity=IDF),
                             R_ + ["CST"], [pkt])
                        cp(WCT[:, t0 + tI * 128:t0 + (tI + 1) * 128], pt[0:32, 0:128], [pkt], [f"wct{b}"], eng="act")
                for b in range(nblk):
                    t0, n, grp = BLOCKS[b]
                    for k in range(8):
                        xk = XT[:, k, t0:t0 + n]
                        if k % 2 == 0:
                            act(xk, xk, AF.Copy, [f"x{k}b{b}"], [f"x{k}b{b}"], scale=ALPHA)
                        else:
                            ts(xk, xk, ALPHA, ALU.mult, [f"x{k}b{b}"], [f"x{k}b{b}"])
                hi = 0
                for e in range(32):
                    j0 = 3 * e
                    WG = WB[j0 % 4][:, :].rearrange("p (k n) -> p k n", n=512)
                    WU = WB[(j0 + 1) % 4][:, :].rearrange("p (k n) -> p k n", n=512)
                    WD = WB[(j0 + 2) % 4][:, :].rearrange("p (k n) -> p k n", n=1024)
                    kg, ku, kd = f"WB{j0 % 4}", f"WB{(j0 + 1) % 4}", f"WB{(j0 + 2) % 4}"
                    bc = BC[e % 2]
                    bck = f"BC{e % 2}"
                    for b in range(nblk):
                        t0, n, grp = BLOCKS[b]
                        ps, pk = nb()
                        mm(ps[:, :n], SELE[:, e * 128:(e + 1) * 128], WCT[:, t0:t0 + n], True, True, ["SELE", f"wct{b}"], [pk])
                        cp(bc[:, t0:t0 + n], ps[:, :n], [pk], [bck + f"b{b}"], eng="act")
                    for b in range(nblk):
                        t0, n, grp = BLOCKS[b]
                        Hb = H[hi % 2]
                        hk = f"H{hi % 2}"
                        hi += 1
                        for fc in range(4):
                            pg, pkg = nb()
                            pu, pku = nb()
                            for k in range(8):
                                mm(pg[:, :n], WG[:, k, fc * 128:(fc + 1) * 128], UY[:, k, t0:t0 + n], k == 0, k == 7,
                                   [kg, f"u{k}b{b}"], [pkg])
                            for k in range(8):
                                mm(pu[:, :n], WU[:, k, fc * 128:(fc + 1) * 128], UY[:, k, t0:t0 + n], k == 0, k == 7,
                                   [ku, f"u{k}b{b}"], [pku])
                            sg = SG[fc % 2]
                            act(sg[:, :n], pg[:, :n], AF.Silu, [pkg], [f"SG{fc % 2}"])
                            tt(sg[:, :n], sg[:, :n], pu[:, :n], ALU.mult, [f"SG{fc % 2}", pku], [f"SG{fc % 2}"])
                            tt(Hb[:, fc, :n], sg[:, :n], bc[:, t0:t0 + n], ALU.mult, [f"SG{fc % 2}", bck + f"b{b}"],
                               [hk + f"f{fc}"])
                        if b == nblk - 1:
                            load_item(j0 + 4)
                            load_item(j0 + 5)
                        for oc in range(8):
                            py, pky = nb()
                            for fc in range(4):
                                mm(py[:, :n], WD[:, fc, oc * 128:(oc + 1) * 128], Hb[:, fc, :n], fc == 0, fc == 3,
                                   [kd, hk + f"f{fc}"], [pky])
                            xk = XT[:, oc, t0:t0 + n]
                            stt(xk, py[:, :n], mod(l, 5, oc, grp), xk, ALU.mult, ALU.add, [pky, "MOD", f"x{oc}b{b}"],
                                [f"x{oc}b{b}"])
                    load_item(j0 + 6)
                for b in range(nblk):
                    ln_block(l, 1, b, (SQ, MEAN, RSTD))
            P.barrier()

        BREG = []

        def moe_sparse(l, need_ctx):
            if not BREG:
                BREG.append(nc.gpsimd.to_reg(4 * 32 * 128 - 1))
            nblk = 5 if need_ctx else 4
            ntiles = 18 if need_ctx else 16
            RB_ = 256
            RT = RB_ // 128
            NB_ = (ntiles * 128 * 2) // RB_ + 32
            NZ = NB_ * RT
            POOL = (mybir.EngineType.Pool,)
            with ExitStack() as s2:
                AST = sb("AST", [128, 18, 32], BF16, s2)
                WCS = sb("WCS", [128, 18, 32], F32, s2)
                CSS = sb("CSS", [128, 18, 32], F32, s2)
                PRE = sb("PRE", [128, 19, 32], F32, s2)
                PST = sb("PST", [128, 32], F32, s2)
                PEN = sb("PEN", [128, 32], F32, s2)
                DESTF = sb("DESTF", [128, 18, 2], F32, s2)
                DEST = sb("DEST", [128, 18, 2], U32, s2)
                WSEL = sb("WSEL", [128, 18, 2], F32, s2)
                IDXW = sb("IDXW", [128, 72], U32, s2)
                SQ = [sb(f"SQ{j}", [128, 512], F32, s2) for j in range(2)]
                MEAN = sb("MEAN", [128, 512], F32, s2)
                RSTD = sb("RSTD", [128, 512], F32, s2)
                with ExitStack() as s3:
                    TF = [sb(f"TF{j}", [128, 512], F32, s3) for j in range(3)]
                    WR = sb("WRl", [128, 8, 36], F32, s3)
                    LG = sb("LG", [128, 36], F32, s3)
                    SM = sb("SM", [128, 16], F32, s3)
                    GOH = sb("GOH", [128, 4], F32, s3)
                    GE = sb("GE", [128, 4], F32, s3)
                    ET = sb("ET", [128, 32], F32, s3)
                    ES = sb("ES", [128, 8], F32, s3)
                    T8 = sb("T8", [128, 8], F32, s3)
                    SEL = sb("SEL", [128, 8], F32, s3)
                    EX = sb("EX", [128, 8], F32, s3)
                    NBK = sb("NBK", [128, 32, 36], F32, s3)
                    CNT = sb("CNT", [128, 32], F32, s3)
                    PADB = sb("PADB", [32, 128], F32, s3)
                    DP1 = sb("DP1", [128, 32], F32, s3)
                    EQ = sb("EQ", [128, 32], F32, s3)
                    BLE = sb("BLE", [128, 72, 32], F32, s3)
                    BLKB = sb("BLKB", [128, 72], F32, s3)
                    P.dma("sp", WR[:], wr_d[:, l, :, :], [], ["WR"])
                    ti = 0
                    for b in range(nblk):
                        t0, n, grp = BLOCKS[b]
                        ntile = n // 128
                        pr = [nb() for _ in range(ntile)]
                        for k in range(8):
                            tf = TF[ti % 3]
                            tk = f"TF{ti % 3}"
                            ti += 1
                            act(tf[:, :n], XT[:, k, t0:t0 + n], AF.Identity, [f"x{k}b{b}", "MOD"], [tk],
                                scale=mod(l, 4, k, grp), bias=mod(l, 3, k, grp))
                            cp(UY[:, k, t0:t0 + n], tf[:, :n], [tk], [f"u{k}b{b}"])
                            for tI in range(ntile):
                                mm(pr[tI][0][:, 0:36], tf[:, tI * 128:(tI + 1) * 128], WR[:, k, :], k == 0, False,
                                   [tk, "WR"], [pr[tI][1]])
                        for tI in range(ntile):
                            gi = t0 // 128 + tI
                            ps, pk = pr[tI]
                            mm(ps[:, 0:36], ONES1, RB[0:1, l, :], False, True, ["CST", "RB"], [pk])
                            cp(LG[:], ps[:, 0:36], [pk], ["LG"])
                            R_ = ["LG", "SM", "GOH", "GE", "ET", "ES", "T8", "SEL", "EX"]

                            def dv(fn, extra_w=()):
                                P.op("dve", fn, R_, R_ + list(extra_w))
                            dv(lambda: nc.vector.tensor_reduce(out=SM[:, 0:1], in_=LG[:, 0:4], axis=AX.X, op=ALU.max))
                            dv(lambda: nc.vector.tensor_scalar(out=GOH[:], in0=LG[:, 0:4], scalar1=SM[:, 0:1], scalar2=None,
                                                               op0=ALU.is_equal))
                            dv(lambda: nc.vector.tensor_scalar(out=SM[:, 1:2], in0=SM[:, 0:1], scalar1=-1.0, scalar2=None,
                                                               op0=ALU.mult))
                            act(GE[:], LG[:, 0:4], AF.Exp, R_, R_, bias=SM[:, 1:2])
                            dv(lambda: nc.vector.tensor_reduce(out=SM[:, 2:3], in_=GE[:], axis=AX.X, op=ALU.add))
                            dv(lambda: nc.vector.reciprocal(out=SM[:, 3:4], in_=SM[:, 2:3]))
                            dv(lambda: nc.vector.tensor_tensor(
                                out=ET[:].rearrange("p (g e) -> p g e", e=8),
                                in0=LG[:, 4:36].rearrange("p (g e) -> p g e", e=8),
                                in1=GOH[:].unsqueeze(2).broadcast_to([128, 4, 8]), op=ALU.mult))
                            dv(lambda: nc.vector.tensor_reduce(out=ES[:], in_=ET[:].rearrange("p (g e) -> p e g", e=8),
                                                               axis=AX.X, op=ALU.add))
                            dv(lambda: nc.vector.max(out=T8[:], in_=ES[:]))
                            dv(lambda: nc.vector.tensor_scalar(out=SEL[:], in0=ES[:], scalar1=T8[:, 1:2], scalar2=None,
                                                               op0=ALU.is_ge))
                            dv(lambda: nc.vector.tensor_scalar(out=SM[:, 4:5], in0=T8[:, 0:1], scalar1=-1.0, scalar2=None,
                                                               op0=ALU.mult))
                            act(EX[:], ES[:], AF.Exp, R_, R_, bias=SM[:, 4:5])
                            dv(lambda: nc.vector.tensor_tensor(out=EX[:], in0=EX[:], in1=SEL[:], op=ALU.mult))
                            dv(lambda: nc.vector.tensor_reduce(out=SM[:, 5:6], in_=EX[:], axis=AX.X, op=ALU.add))
                            dv(lambda: nc.vector.reciprocal(out=SM[:, 6:7], in_=SM[:, 5:6]))
                            dv(lambda: nc.vector.tensor_tensor(out=SM[:, 7:8], in0=SM[:, 6:7], in1=SM[:, 3:4], op=ALU.mult))
                            dv(lambda: nc.vector.tensor_scalar(out=EX[:], in0=EX[:], scalar1=SM[:, 7:8], scalar2=None,
                                                               op0=ALU.mult))
                            dv(lambda: nc.vector.tensor_tensor(
                                out=WCS[:, gi, :].rearrange("p (g e) -> p g e", e=8),
                                in0=GOH[:].unsqueeze(2).broadcast_to([128, 4, 8]),
                                in1=EX[:].unsqueeze(1).broadcast_to([128, 4, 8]), op=ALU.mult), ["WCS"])
                            dv(lambda: nc.vector.tensor_tensor(
                                out=AST[:, gi, :].rearrange("p (g e) -> p g e", e=8),
                                in0=GOH[:].unsqueeze(2).broadcast_to([128, 4, 8]),
                                in1=SEL[:].unsqueeze(1).broadcast_to([128, 4, 8]), op=ALU.mult), ["AST"])
                    for g4 in range(0, ntiles, 4):
                        m4 = min(4, ntiles - g4)
                        ps, pk = nb()
                        for j in range(m4):
                            mm(ps[:, j * 32:(j + 1) * 32], ONESB, AST[:, g4 + j, :], True, True, ["CSTB", "AST"], [pk])
                        cp(CSS[:, g4:g4 + m4, :], ps[:, 0:m4 * 32].rearrange("p (j e) -> p j e", e=32), [pk], ["CSS"])
                    P.op("dve", lambda: nc.vector.memset(PRE[:, 0, :], 0.0), [], ["PRE"])
                    for j in range(ntiles):
                        tt(PRE[:, j + 1, :], PRE[:, j, :], CSS[:, j, :], ALU.add, ["PRE", "CSS"], ["PRE"])
                    ps, pk = nb()
                    for j in range(ntiles):
                        mm(ps[0:32, 0:2], AST[:, j, :], ONESB[:, 0:2], j == 0, j == ntiles - 1, ["AST", "CSTB"], [pk])
                    cp(CNT[0:32, 0:1], ps[0:32, 0:1], [pk], ["CNT"])
                    ts(CNT[0:32, 3:4], CNT[0:32, 0:1], 128.0 / RB_, ALU.mult, ["CNT"], ["CNT"])
                    tt(NBK[0:32, 0, :], CNT[0:32, 3:4].broadcast_to([32, 36]), CST3[0:32, 0:36], ALU.is_gt,
                       ["CNT", "CST3"], ["NBK"])
                    P.op("dve", lambda: nc.vector.tensor_reduce(out=CNT[0:32, 1:2], in_=NBK[0:32, 0, :], axis=AX.X, op=ALU.add),
                         ["NBK", "CNT"], ["CNT"])
                    ts(CNT[0:32, 2:3], CNT[0:32, 1:2], float(RB_), ALU.mult, ["CNT"], ["CNT"])
                    cp(PADB[:, :], CNT[0:32, 2:3].broadcast_to([32, 128]), ["CNT"], ["PADB"])
                    ps, pk = nb()
                    mm(ps[:, 0:32], PADB[:, :], CST3[0:32, 64:96], True, True, ["PADB", "CST3"], [pk])
                    mm(ps[:, 32:64], PADB[:, :], CST3[0:32, 96:128], True, True, ["PADB", "CST3"], [pk])
                    cp(PST[:], ps[:, 0:32], [pk], ["PST"])
                    cp(PEN[:], ps[:, 32:64], [pk], ["PEN"])
                    ts(EQ[:], PEN[:], 128.0 / RB_, ALU.mult, ["PEN", "EQ"], ["EQ"])
                    tt(BLE[:, 0:NB_, :], EQ[:].unsqueeze(1).broadcast_to([128, NB_, 32]),
                       CST3[:, 136:136 + NB_].unsqueeze(2).broadcast_to([128, NB_, 32]), ALU.is_le, ["EQ", "CST3"], ["BLE"])
                    P.op("dve", lambda: nc.vector.tensor_reduce(out=BLKB[:, 0:NB_], in_=BLE[:, 0:NB_, :], axis=AX.X, op=ALU.add),
                         ["BLE"], ["BLKB"])
                    ts(BLKB[:, 0:NB_], BLKB[:, 0:NB_], 31.0, ALU.min, ["BLKB"], ["BLKB"], s2=128.0, op1=ALU.mult)
                    ts(BLKB[:, 0:NB_], BLKB[:, 0:NB_], CST3[:, 129:130], ALU.add, ["BLKB", "CST3"], ["BLKB"],
                       s2=float(l * 4096), op1=ALU.add)
                    blef = BLE[:, 0:3, :].rearrange("p a b -> p (a b)")[:, 0:NB_]
                    ts(blef, CST3[:, 136:136 + NB_], EQ[:, 31:32], ALU.is_ge, ["CST3", "EQ", "BLE"], ["BLE"])
                    stt(BLKB[:, 0:NB_], blef, 1.0e6, BLKB[:, 0:NB_], ALU.mult, ALU.add, ["BLE", "BLKB"], ["BLKB"])
                    cp(IDXW[:, 0:NB_], BLKB[:, 0:NB_], ["BLKB"], ["IDXW"])
                    for g4 in range(0, ntiles, 4):
                        m4 = min(4, ntiles - g4)
                        ps, pk = nb()
                        for j in range(m4):
                            mm(ps[:, j * 32:(j + 1) * 32], TRIS, AST[:, g4 + j, :], True, True, ["CSTB2", "AST"], [pk])
                        for j in range(m4):
                            gi = g4 + j
                            R2 = ["DP1", "EQ", "T8"]
                            tt(DP1[:], ps[:, j * 32:(j + 1) * 32], PRE[:, gi, :], ALU.add, [pk, "PRE"] + R2, R2)
                            tt(DP1[:], DP1[:], PST[:], ALU.add, R2 + ["PST"], R2)
                            stt(DP1[:], DP1[:], 1.0, AST[:, gi, :], ALU.add, ALU.mult, R2 + ["AST"], R2)
                            P.op("dve", lambda: nc.vector.max(out=T8[:], in_=DP1[:]), R2 + ["LG"], R2 + ["LG"])
                            ts(DESTF[:, gi, :], T8[:, 0:2], -1.0, ALU.add, R2, ["DESTF"])
                            for kk in range(2):
                                ts(EQ[:], DP1[:], T8[:, kk:kk + 1], ALU.is_equal, R2, R2)
                                tt(EQ[:], EQ[:], WCS[:, gi, :], ALU.mult, R2 + ["WCS"], R2)
                                P.op("dve", lambda kk=kk, gi=gi: nc.vector.tensor_reduce(
                                    out=WSEL[:, gi, kk:kk + 1], in_=EQ[:], axis=AX.X, op=ALU.add), R2 + ["WSEL"], R2 + ["WSEL"])
                    cp(DEST[:, 0:ntiles, :], DESTF[:, 0:ntiles, :], ["DESTF"], ["DEST"])
                P.barrier()
                if MOE_STOP <= 1:
                    return
                with ExitStack() as s3:
                    WB = [sb(f"WB{j}", [128, 4096], BF16, s3) for j in range(4)]
                    UT = [sb(f"UT{j}", [128, 1024], BF16, s3) for j in range(2)]
                    XS = [sb(f"XS{j}", [128, RT, 1024], BF16, s3) for j in range(2)]
                    XST = [sb(f"XST{j}", [128, 8, RB_], BF16, s3) for j in range(2)]
                    H = [sb(f"H{j}", [128, 4, RB_], BF16, s3) for j in range(2)]
                    SG = [sb(f"SG{j}", [128, 512], F32, s3) for j in range(2)]
                    YS = [sb(f"YS{j}", [128, 1024], F32, s3) for j in range(2)]
                    ZT = sb("ZT", [128, 1024], BF16, s3)
                    P.op("dve", lambda: nc.vector.memset(ZT[:], 0.0), [], ["ZT"])
                    for bb in range(NZ):
                        P.dma("sp", xs_d[bb * 128:(bb + 1) * 128, :], ZT[:, :], ["ZT"], [f"xsz{bb}"])
                    for gi in range(ntiles):
                        ut = UT[gi % 2]
                        uk = f"UT{gi % 2}"
                        b = gi // 4 if gi < 16 else 4
                        for half in range(2):
                            ps, pk = nb()
                            for kk in range(4):
                                k = half * 4 + kk
                                mm(ps[:, kk * 128:(kk + 1) * 128], UY[:, k, gi * 128:(gi + 1) * 128], IDB, True, True,
                                   [f"u{k}b{b}", "CSTB"], [pk])
                            cp(ut[:, half * 512:(half + 1) * 512], ps[:, :], [pk], [uk], eng="act" if half else "dve")
                        for kk in range(2):
                            P.dma_fn("pool", lambda sem, ut=ut, gi=gi, kk=kk: nc.gpsimd.indirect_dma_start(
                                out=xs_d, out_offset=bass.IndirectOffsetOnAxis(ap=DEST[:, gi, kk:kk + 1], axis=0),
                                in_=ut[:, :], in_offset=None).then_inc(sem, 16),
                                [uk, "DEST"] + [f"xsz{b_}" for b_ in range(NZ)], [f"xss{gi}_{kk}"])
                    items = []
                    for bb in range(NB_ if MOE_STOP > 2 else 0):
                        items += [("g", bb), ("u", bb), ("d", bb)]
                    def load_item(j):
                        if j >= len(items):
                            return
                        kind, bb = items[j]
                        c0_ = {"g": 0, "u": 2, "d": 4}[kind]
                        for hh_ in range(2):
                            P.dma_fn("pool", lambda sem, j=j, bb=bb, c=c0_ + hh_, hh_=hh_: nc.gpsimd.indirect_dma_start(
                                out=WB[j % 4][:, hh_ * 2048:(hh_ + 1) * 2048], out_offset=None, in_=wc_d[c],
                                in_offset=bass.IndirectOffsetOnAxis(ap=IDXW[:, bb:bb + 1], axis=0),
                                bounds_check=BREG[0], oob_is_err=False).then_inc(sem, 16),
                                ["IDXW"], [f"WB{j % 4}"])

                    for j in range(4):
                        load_item(j)
                    for bb in range(NB_ if MOE_STOP > 2 else 0):
                        j0 = 3 * bb
                        WG = WB[j0 % 4][:, :].rearrange("p (k n) -> p k n", n=512)
                        WU = WB[(j0 + 1) % 4][:, :].rearrange("p (k n) -> p k n", n=512)
                        WD = WB[(j0 + 2) % 4][:, :].rearrange("p (k n) -> p k n", n=1024)
                        kg, ku, kd = f"WB{j0 % 4}", f"WB{(j0 + 1) % 4}", f"WB{(j0 + 2) % 4}"
                        xs, xk = XS[bb % 2], f"XS{bb % 2}"
                        xst, xtk = XST[bb % 2], f"XST{bb % 2}"
                        Hb, hk = H[bb % 2], f"H{bb % 2}"
                        P.dma("sp", xs[:, :, :], xs_d[bb * RB_:(bb + 1) * RB_, :].rearrange("(t p) n -> p t n", p=128),
                              [f"xsz{bb * RT + r_}" for r_ in range(RT)]
                              + [f"xss{g_}_{k_}" for g_ in range(ntiles) for k_ in range(2)], [xk])
                        for rt in range(RT):
                            for half in range(2):
                                ps, pk = nb()
                                for kk in range(4):
                                    k = half * 4 + kk
                                    mm(ps[:, kk * 128:(kk + 1) * 128], xs[:, rt, k * 128:(k + 1) * 128], IDB, True, True,
                                       [xk, "CSTB"], [pk])
                                cp(xst[:, half * 4:(half + 1) * 4, rt * 128:(rt + 1) * 128],
                                   ps[:, :].rearrange("p (k r) -> p k r", r=128), [pk], [xtk], eng="act" if half else "dve")
                        pgs = [nb() for _ in range(RT)]
                        pus = [nb() for _ in range(RT)]
                        fpb = 512 // RB_
                        for fc in range(4):
                            pg_, pkg_ = pgs[fc // fpb]
                            for k in range(8):
                                mm(pg_[:, (fc % fpb) * RB_:(fc % fpb + 1) * RB_], WG[:, k, fc * 128:(fc + 1) * 128], xst[:, k, :],
                                   k == 0, k == 7, [kg, xtk], [pkg_])
                        for fc in range(4):
                            pu_, pku_ = pus[fc // fpb]
                            for k in range(8):
                                mm(pu_[:, (fc % fpb) * RB_:(fc % fpb + 1) * RB_], WU[:, k, fc * 128:(fc + 1) * 128], xst[:, k, :],
                                   k == 0, k == 7, [ku, xtk], [pku_])
                        for i_ in range(RT):
                            sg = SG[i_ % 2]
                            act(sg[:, :], pgs[i_][0][:, :], AF.Silu, [pgs[i_][1]], [f"SG{i_ % 2}"])
                            tt(Hb[:, i_ * fpb:(i_ + 1) * fpb, :].rearrange("p f r -> p (f r)"), sg[:, :], pus[i_][0][:, :],
                               ALU.mult, [f"SG{i_ % 2}", pus[i_][1]], [hk])
                        load_item(j0 + 4)
                        load_item(j0 + 5)
                        for rt in range(RT):
                            ys, yk = YS[rt % 2], f"YS{rt % 2}"
                            for half in range(2):
                                py, pky = nb()
                                for fc in range(4):
                                    mm(py[:, :], Hb[:, fc, rt * 128:(rt + 1) * 128], WD[:, fc, half * 512:(half + 1) * 512],
                                       fc == 0, fc == 3, [kd, hk], [pky])
                                cp(ys[:, half * 512:(half + 1) * 512], py[:, :], [pky], [yk], eng="act" if half else "dve")
                            P.dma("act", ys_d[bb * RB_ + rt * 128:bb * RB_ + (rt + 1) * 128, :], ys[:, :], [yk], [f"ysd{bb}_{rt}"])
                        load_item(j0 + 6)
                P.barrier()
                if MOE_STOP <= 3:
                    return
                with ExitStack() as s3:
                    GG = [[sb(f"G{j}_{i_}", [128, 1024], F32, s3) for j in range(2)] for i_ in range(2)]
                    ZZ = [sb(f"Z{i_}", [128, 1024], F32, s3) for i_ in range(2)]
                    for b in range(nblk):
                        t0, n, grp = BLOCKS[b]
                        for k in range(8):
                            xk = XT[:, k, t0:t0 + n]
                            if k % 2 == 0:
                                act(xk, xk, AF.Copy, [f"x{k}b{b}"], [f"x{k}b{b}"], scale=ALPHA)
                            else:
                                ts(xk, xk, ALPHA, ALU.mult, [f"x{k}b{b}"], [f"x{k}b{b}"])
                    ysk = [f"ysd{b_}_{r_}" for b_ in range(NB_) for r_ in range(RT)]
                    for gi in range(ntiles):
                        b = gi // 4 if gi < 16 else 4
                        grp = BLOCKS[b][2]
                        G0, G1 = GG[gi % 2]
                        Z = ZZ[gi % 2]
                        g0k, g1k, zk = f"G0_{gi % 2}", f"G1_{gi % 2}", f"Z{gi % 2}"
                        for kk, (G, gk) in enumerate(((G0, g0k), (G1, g1k))):
                            P.dma_fn("pool", lambda sem, G=G, gi=gi, kk=kk: nc.gpsimd.indirect_dma_start(
                                out=G[:, :], out_offset=None, in_=ys_d,
                                in_offset=bass.IndirectOffsetOnAxis(ap=DEST[:, gi, kk:kk + 1], axis=0)).then_inc(sem, 16),
                                ysk + ["DEST"], [gk])
                        ts(Z[:, :], G0[:, :], WSEL[:, gi, 0:1], ALU.mult, [g0k, "WSEL"], [zk])
                        stt(Z[:, :], G1[:, :], WSEL[:, gi, 1:2], Z[:, :], ALU.mult, ALU.add, [g1k, "WSEL", zk], [zk])
                        for half in range(2):
                            ps, pk = nb()
                            for kk in range(4):
                                k = half * 4 + kk
                                P.op("pe", lambda k=k, kk=kk, ps=ps, Z=Z: nc.tensor.transpose(
                                    out=ps[:, kk * 128:(kk + 1) * 128], in_=Z[:, k * 128:(k + 1) * 128], identity=IDF),
                                    [zk, "CST"], [pk])
                            for kk in range(4):
                                k = half * 4 + kk
                                xk = XT[:, k, gi * 128:(gi + 1) * 128]
                                stt(xk, ps[:, kk * 128:(kk + 1) * 128], mod(l, 5, k, grp), xk, ALU.mult, ALU.add,
                                    [pk, "MOD", f"x{k}b{b}"], [f"x{k}b{b}"])
                    for b in range(nblk):
                        ln_block(l, 1, b, (SQ, MEAN, RSTD))
            P.barrier()

        for l in range(n_layers):
            if stop == "ada":
                break
            lastl = l == DEPTH - 1
            need_ctx = not lastl
            is_dbg_last = (l == n_layers - 1)
            if l % 2 == 0:
                mixer_ab(l, need_ctx)
                wo = ab_w_out_d[l // 2]
            else:
                mixer_c(l, need_ctx)
                wo = c_w_out_d[l // 2]
            raw = is_dbg_last and stop == "mix"
            if is_dbg_last and stop == "y":
                for b in range(5):
                    t0, n, grp = BLOCKS[b]
                    for k in range(8):
                        cp(XT[:, k, t0:t0 + n], UY[:, k, t0:t0 + n], [f"u{k}b{b}"], [f"x{k}b{b}"])
                break
            proj_ln(l, wo, 5 if need_ctx else 4, raw)
            if is_dbg_last and stop in ("mix", "ln1"):
                break
            if SPARSE:
                moe_sparse(l, need_ctx)
            else:
                moe(l, need_ctx)

        for k in range(8):
            P.dma("sp", out_d[k], XT[:, k, :], xkeys(ks=[k]), [f"out{k}"])
        P.finish("sp", [f"out{k}" for k in range(8)])
        print("bass ops:", P.nops, P.cnt)
    return nc


def _fm(v):
    v = np.asarray(v, np.float32)
    lead = v.shape[:-1]
    r = v.reshape(lead + (8, 128))
    r = np.moveaxis(r, -1, 0)
    return np.ascontiguousarray(r)


def _rope_tables(head_dim, per, qscale):
    quarter = head_dim // 4
    row = np.repeat(np.arange(T // 64, dtype=np.float32), 64)
    col = np.tile(np.arange(64, dtype=np.float32), T // 64)
    inv = (np.float32(10000.0) ** (-np.arange(quarter, dtype=np.float32) / np.float32(quarter))).astype(np.float32)
    ang_r = row[:, None] * inv
    ang_c = col[:, None] * inv
    ang = np.concatenate([ang_r, ang_r, ang_c, ang_c], axis=-1)
    cos = np.cos(ang).astype(np.float32).T
    sin = np.sin(ang).astype(np.float32).T
    sign = np.ones((head_dim, 1), np.float32)
    sign[0:quarter] = -1.0
    sign[2 * quarter:3 * quarter] = -1.0
    sin = sin * sign
    rep = 128 // head_dim
    cos = np.tile(cos, (rep, 1))
    sin = np.tile(sin, (rep, 1))
    return np.ascontiguousarray(np.stack([cos * qscale, sin * qscale, cos, sin]).astype(np.float32))


_CONSTS = None


def _consts():
    global _CONSTS
    if _CONSTS is not None:
        return _CONSTS
    c = {}
    c["ropeA"] = _rope_tables(128, 128, np.float32(128 ** -0.5))
    c["ropeC"] = _rope_tables(64, 64, np.float32(0.125))
    ii = np.arange(128)
    cst = np.zeros((128, 1024), np.float32)
    cst[:, 0:128] = np.eye(128, dtype=np.float32)
    cst[:, 128:256] = 1.0 / 1024.0
    cst[:, 256:384] = 1.0 / 128.0
    cst[:, 384:512] = np.where(ii[:, None] <= ii[None, :], -1.0 / 16.0, 0.0)
    cst[:, 512:640] = 1.0
    c["cst"] = cst
    cst2 = np.zeros((128, 256), np.float32)
    cst2[:, 0:128] = np.where(ii[:, None] >= ii[None, :], -1.0 / 16.0, 0.0)
    cst2[:, 128:256] = np.where(ii[:, None] > ii[None, :], -1.0 / 16.0, 0.0)
    c["cst2"] = cst2
    cstb = np.zeros((128, 256), np.float32)
    cstb[:, 0:128] = np.eye(128)
    cstb[:, 128:256] = 1.0
    c["cstb"] = cstb.astype(ml_dtypes.bfloat16)
    m = (ii[:, None] > ii[None, :]).astype(np.uint8)
    c["mask"] = np.ascontiguousarray(np.tile(m, (1, 4)))
    cst3 = np.zeros((128, 256), np.float32)
    cst3[:, 0:36] = (np.arange(36, dtype=np.float32) * 128.0)[None, :]
    e32 = np.arange(32)
    cst3[0:32, 64:96] = (e32[:, None] < e32[None, :]).astype(np.float32)
    cst3[0:32, 96:128] = (e32[:, None] <= e32[None, :]).astype(np.float32)
    cst3[:, 128] = np.arange(128, dtype=np.float32) * 128.0
    cst3[:, 129] = np.arange(128, dtype=np.float32)
    cst3[:, 136:208] = (np.arange(72, dtype=np.float32) * 128.0)[None, :]
    c["cst3"] = cst3
    c["cstb2"] = (ii[:, None] < ii[None, :]).astype(np.float32).astype(ml_dtypes.bfloat16)
    sele = np.zeros((32, 32, 128), np.float32)
    for e in range(32):
        sele[e, e, :] = 1.0
    c["sele"] = sele.reshape(32, 32 * 128).astype(ml_dtypes.bfloat16)
    ret = np.zeros((128, 4, 6, 128), np.float32)
    pos = np.arange(128, dtype=np.float64)
    for h in range(4):
        lf = float(np.log1p(-np.exp2(-np.float32(5.0 + h)), dtype=np.float32))
        lb = float(np.log1p(-np.exp2(-np.float32(5.5 + h)), dtype=np.float32))
        ret[:, h, 0, :] = np.exp(lf * (pos + 1))
        ret[:, h, 1, :] = np.exp(-lf * (pos + 1))
        ret[:, h, 2, :] = np.exp(lf * (127 - pos))
        ret[:, h, 3, :] = np.exp(lb * (127 - pos))
        ret[:, h, 4, :] = np.exp(-lb * (128 - pos))
        ret[:, h, 5, :] = np.exp(lb * pos)
    c["ret"] = ret
    _CONSTS = c
    return c


def _prep_inputs(inputs):
    f = lambda a: np.ascontiguousarray(np.asarray(a, np.float32))
    shared = dict(_consts())
    shared["adab"] = np.ascontiguousarray(np.asarray(inputs["ada_b"], np.float32).reshape(4, 48, 128).transpose(2, 0, 1))
    lnp = np.stack([inputs["ln1_g"], inputs["ln1_b"], inputs["ln2_g"], inputs["ln2_b"]], axis=1)
    shared["lnp"] = np.ascontiguousarray(np.asarray(lnp, np.float32).reshape(4, 4, 8, 128).transpose(3, 0, 1, 2))
    gn = np.concatenate([inputs["ab_gn_a"], inputs["ab_gn_b"]], axis=1)
    shared["gn"] = np.ascontiguousarray(np.asarray(gn, np.float32).reshape(2, 8, 128).transpose(2, 0, 1))
    shared["subg"] = np.ascontiguousarray(np.asarray(inputs["c_subln_g"], np.float32).reshape(2, 8, 128).transpose(2, 0, 1))
    wlr = np.zeros((32, 2, 2, 256), np.float32)
    wlr[0:16, :, 0, :] = np.asarray(inputs["ab_w_lr_f"]).transpose(1, 0, 2)
    wlr[0:16, :, 1, :] = np.asarray(inputs["ab_w_lr_b"]).transpose(1, 0, 2)
    wlr[16, :, 0, :] = np.asarray(inputs["ab_b_lr_f"])
    wlr[16, :, 1, :] = np.asarray(inputs["ab_b_lr_b"])
    shared["wlr"] = wlr
    wr = np.concatenate([inputs["moe_w_grp"], inputs["moe_w_rexp"]], axis=2)
    shared["wr"] = np.ascontiguousarray(np.asarray(wr, np.float32).reshape(4, 8, 128, 36).transpose(2, 0, 1, 3))
    rb = np.concatenate([inputs["moe_b_grp"], inputs["moe_b_rexp"]], axis=1)
    shared["rb"] = np.ascontiguousarray(np.asarray(rb, np.float32)[None])
    lqk = np.stack([inputs["c_lq1"], inputs["c_lk1"], inputs["c_lq2"], inputs["c_lk2"]], axis=1)
    shared["lqk"] = np.ascontiguousarray(np.asarray(lqk, np.float32).transpose(3, 0, 1, 2))
    for nm in ("ada_w", "ab_w_in", "ab_w_out", "c_w_qkv", "c_w_out"):
        shared[nm] = f(inputs[nm])
    if SPARSE:
        for ci, nm in ((0, "moe_w_gate"), (2, "moe_w_up")):
            wp = np.asarray(inputs[nm], np.float32).reshape(4, 32, 8, 128, 512).transpose(0, 1, 3, 2, 4).reshape(4 * 32 * 128, 4096)
            shared[f"wc{ci}"] = np.ascontiguousarray(wp[:, 0:2048])
            shared[f"wc{ci + 1}"] = np.ascontiguousarray(wp[:, 2048:4096])
        wp = np.asarray(inputs["moe_w_down"], np.float32).reshape(4, 32, 4, 128, 1024).transpose(0, 1, 3, 2, 4).reshape(4 * 32 * 128, 4096)
        shared["wc4"] = np.ascontiguousarray(wp[:, 0:2048])
        shared["wc5"] = np.ascontiguousarray(wp[:, 2048:4096])
    else:
        for nm in ("moe_w_gate", "moe_w_up", "moe_w_down"):
            shared[nm] = f(inputs[nm])
    x = np.asarray(inputs["x"], np.float32)
    ctx = np.asarray(inputs["ctx"], np.float32)
    c = np.asarray(inputs["c"], np.float32)
    c_ctx = np.asarray(inputs["c_ctx"], np.float32)
    in_maps = []
    for b in range(8):
        tok = np.concatenate([x[b], ctx[b]], axis=0)
        xT = np.ascontiguousarray(tok.T.reshape(8, 128, NT))
        cc = np.stack([c[b].reshape(8, 128).T, c_ctx.reshape(8, 128).T], axis=-1)
        m = dict(shared)
        m["xT"] = xT
        m["cc"] = np.ascontiguousarray(cc.astype(np.float32))
        in_maps.append(m)
    return in_maps


_NC_CACHE = {}


def run(inputs, n_layers=DEPTH, stop=None, ncores=8):
    key = (n_layers, stop)
    if key not in _NC_CACHE:
        _NC_CACHE[key] = build(n_layers, stop)
    nc = _NC_CACHE[key]
    in_maps = _prep_inputs(inputs)[:ncores]
    if stop in ("ada", "mix", "ln1", "y") and n_layers <= 1:
        for m in in_maps:
            for nm in (["wc%d" % c for c in range(6)] if SPARSE else ["moe_w_gate", "moe_w_up", "moe_w_down"]):
                m.pop(nm)
    res = run_bass_kernel_spmd(nc, in_maps, core_ids=list(range(ncores)))
    outs = [np.asarray(r["outT"]).reshape(D, NT).T for r in res.results]
    return outs


def kernel(**inputs):
    outs = run(inputs)
    return np.ascontiguousarray(np.stack([o[:T] for o in outs], axis=0).astype(np.float32))
```

```python
import math
import numpy as np
import ml_dtypes
from contextlib import ExitStack
import concourse.bass as bass
import concourse.mybir as mybir
from concourse.bass_utils import run_bass_kernel_spmd

F32 = mybir.dt.float32
BF16 = mybir.dt.bfloat16
U8 = mybir.dt.uint8
U32 = mybir.dt.uint32
I32 = mybir.dt.int32
AF = mybir.ActivationFunctionType
ALU = mybir.AluOpType
AX = mybir.AxisListType

D = 1024
T = 2048
TC = 256
NT = T + TC
DEPTH = 4
ALPHA = (2 * DEPTH) ** 0.25
EPS = 1e-5
BLOCKS = [(0, 512, 0), (512, 512, 0), (1024, 512, 0), (1536, 512, 0), (2048, 256, 1)]
ORDER_F = [16, 17] + list(range(16))
ORDER_B = [17, 16] + list(range(15, -1, -1))
AB_IN = 3616
import os as _os
SPARSE = int(_os.environ.get("MOE_SPARSE", "1"))
MOE_STOP = int(_os.environ.get("MOE_STOP", "9"))
DBG_HEADS = int(_os.environ.get("DBG_HEADS", "8"))
DBG_B = int(_os.environ.get("DBG_B", "9"))
DBG_C = _os.environ.get("DBG_C", "z")


class Prog:
    def __init__(self, nc, es, ndma=28):
        self.nc = nc
        self.e = dict(pe=nc.tensor, act=nc.scalar, dve=nc.vector, pool=nc.gpsimd, sp=nc.sync)
        self.sem = {k: es.enter_context(nc.semaphore("s_" + k)) for k in self.e}
        self.cnt = {k: 0 for k in self.e}
        self.seen = {k: {} for k in self.e}
        self.st = {}
        self.dq = {}
        for q in ("sp", "pool", "act"):
            self.dq[q] = dict(sems=[es.enter_context(nc.semaphore(f"d_{q}{i}")) for i in range(ndma)],
                              vals=[0] * ndma, idx=0)
        self.nops = 0
        self.bar = es.enter_context(nc.sbuf_tensor("barrier_t", [128, 1], F32))

    def _wait(self, eng, ticks):
        need = {}
        for t in ticks:
            if t is None:
                continue
            key, sem, val = t
            if key == "pe" and eng == "pe":
                continue
            if need.get(key, (None, 0))[1] < val:
                need[key] = (sem, val)
        for key, (sem, val) in need.items():
            if self.seen[eng].get(key, 0) >= val:
                continue
            self.e[eng].wait_ge(sem, val)
            self.seen[eng][key] = val

    def _deps(self, R, W):
        ticks = []
        for k in R:
            s = self.st.get(k)
            if s is not None:
                ticks.append(s[0])
                if k.startswith("ps"):
                    ticks.extend(s[1].values())
        for k in W:
            s = self.st.get(k)
            if s is not None:
                ticks.append(s[0])
                ticks.extend(s[1].values())
        return ticks

    def _update(self, tick, R, W):
        for k in W:
            self.st[k] = [tick, {}]
        for k in R:
            s = self.st.get(k)
            if s is None:
                s = self.st[k] = [None, {}]
            s[1][tick[0]] = tick

    def op(self, eng, fn, R, W):
        self._wait(eng, self._deps(R, W))
        inst = fn()
        inst.then_inc(self.sem[eng], 1)
        self.cnt[eng] += 1
        self.nops += 1
        self._update((eng, self.sem[eng], self.cnt[eng]), R, W)

    def dma(self, q, out, in_, R, W):
        d = self.dq[q]
        i = d["idx"]
        d["idx"] = (i + 1) % len(d["sems"])
        key = ("d", q, i)
        ticks = self._deps(R, W)
        if d["vals"][i] > 0:
            ticks.append((key, d["sems"][i], d["vals"][i]))
        self._wait(q, ticks)
        self.e[q].dma_start(out=out, in_=in_).then_inc(d["sems"][i], 16)
        d["vals"][i] += 16
        self.nops += 1
        self._update((key, d["sems"][i], d["vals"][i]), R, W)

    def dma_fn(self, q, fn, R, W):
        d = self.dq[q]
        i = d["idx"]
        d["idx"] = (i + 1) % len(d["sems"])
        key = ("d", q, i)
        ticks = self._deps(R, W)
        if d["vals"][i] > 0:
            ticks.append((key, d["sems"][i], d["vals"][i]))
        self._wait(q, ticks)
        fn(d["sems"][i])
        d["vals"][i] += 16
        self.nops += 1
        self._update((key, d["sems"][i], d["vals"][i]), R, W)

    def barrier(self):
        ticks = []
        for e in self.e:
            if self.cnt[e] > 0:
                ticks.append((e, self.sem[e], self.cnt[e]))
        for q, d in self.dq.items():
            for i, v in enumerate(d["vals"]):
                if v > 0:
                    ticks.append((("d", q, i), d["sems"][i], v))
        self._wait("dve", ticks)
        inst = self.nc.vector.memset(self.bar[:], 0.0)
        inst.then_inc(self.sem["dve"], 1)
        self.cnt["dve"] += 1
        t = ("dve", self.sem["dve"], self.cnt["dve"])
        for e in ("pe", "act", "pool", "sp"):
            self._wait(e, [t])

    def finish(self, eng, keys):
        self._wait(eng, self._deps(keys, []))


def build(n_layers=DEPTH, stop=None):
    nc = bass.Bass("TRN2", target_bir_lowering=False)

    def din(name, shape, dt=F32):
        return nc.dram_tensor(name, list(shape), dt, kind="ExternalInput").ap()

    xT_d = din("xT", [8, 128, NT])
    cc_d = din("cc", [128, 8, 2])
    adab_d = din("adab", [128, 4, 48])
    lnp_d = din("lnp", [128, 4, 4, 8])
    gn_d = din("gn", [128, 2, 8])
    subg_d = din("subg", [128, 2, 8])
    wlr_d = din("wlr", [32, 2, 2, 256])
    wr_d = din("wr", [128, 4, 8, 36])
    rb_d = din("rb", [1, 4, 36])
    lqk_d = din("lqk", [64, 2, 4, 8])
    ret_d = din("ret", [128, 4, 6, 128])
    ropeA_d = din("ropeA", [4, 128, T])
    ropeC_d = din("ropeC", [4, 128, T])
    cst_d = din("cst", [128, 1024])
    cst2_d = din("cst2", [128, 256])
    cstb_d = din("cstb", [128, 256], BF16)
    mask_d = din("mask", [128, 512], U8)
    cst3_d = din("cst3", [128, 256])
    cstb2_d = din("cstb2", [128, 128], BF16)
    xs_d = nc.dram_tensor("xs_scr", [12800, 1024], BF16, kind="Internal").ap()
    ys_d = nc.dram_tensor("ys_scr", [12800, 1024], F32, kind="Internal").ap()
    sele_d = din("sele", [32, 32 * 128], BF16)
    ada_w_d = din("ada_w", [4, D, 6 * D])
    ab_w_in_d = din("ab_w_in", [2, D, AB_IN])
    ab_w_out_d = din("ab_w_out", [2, D, D])
    c_w_qkv_d = din("c_w_qkv", [2, D, 3 * D])
    c_w_out_d = din("c_w_out", [2, D, D])
    use_moe = not (stop in ("ada", "mix", "ln1", "y") and n_layers <= 1)
    if use_moe and SPARSE:
        wc_d = [din(f"wc{c}", [4 * 32 * 128, 2048]) for c in range(6)]
    elif use_moe:
        wg_d = din("moe_w_gate", [4, 32, D, 512])
        wu_d = din("moe_w_up", [4, 32, D, 512])
        wd_d = din("moe_w_down", [4, 32, 512, D])
    out_d = nc.dram_tensor("outT", [8, 128, NT], F32, kind="ExternalOutput").ap()

    es = ExitStack()
    with es:
        P = Prog(nc, es)

        uid = [0]

        def sb(name, shape, dt=F32, stack=es):
            uid[0] += 1
            return stack.enter_context(nc.sbuf_tensor(f"{name}_{uid[0]}", list(shape), dt))

        PS = [es.enter_context(nc.psum_tensor(f"ps{i}", [128, 512], F32)) for i in range(8)]
        psc = [0]

        def nb():
            i = psc[0]
            psc[0] = (i + 1) % 8
            return PS[i], f"ps{i}"

        def mm(out, lhsT, rhs, start, stop, R, W):
            P.op("pe", lambda: nc.tensor.matmul(out, lhsT=lhsT, rhs=rhs, start=start, stop=stop), R, W)

        def act(out, in_, func, R, W, scale=None, bias=None, accum_out=None):
            kw = {}
            if scale is not None:
                kw["scale"] = scale
            if bias is None and func != AF.Copy:
                sp_, np_ = in_.start_partition(), in_.partition_size()
                bias = ZEROC[sp_:sp_ + np_, 0:1]
                R = list(R) + ["ZEROC"]
            if bias is not None:
                kw["bias"] = bias
            if accum_out is not None:
                kw["accum_out"] = accum_out
            P.op("act", lambda: nc.scalar.activation(out=out, in_=in_, func=func, **kw), R, W)

        def tt(out, a, b, op, R, W, eng="dve"):
            e = nc.vector if eng == "dve" else nc.gpsimd
            P.op(eng, lambda: e.tensor_tensor(out=out, in0=a, in1=b, op=op), R, W)

        def ts(out, a, s1, op0, R, W, s2=None, op1=None, eng="dve"):
            e = nc.vector if eng == "dve" else nc.gpsimd
            if op1 is None:
                P.op(eng, lambda: e.tensor_scalar(out=out, in0=a, scalar1=s1, scalar2=None, op0=op0), R, W)
            else:
                P.op(eng, lambda: e.tensor_scalar(out=out, in0=a, scalar1=s1, scalar2=s2, op0=op0, op1=op1), R, W)

        def stt(out, a, s, b, op0, op1, R, W):
            P.op("dve", lambda: nc.vector.scalar_tensor_tensor(out=out, in0=a, scalar=s, in1=b, op0=op0, op1=op1), R, W)

        def cp(out, in_, R, W, eng="dve"):
            if eng == "act":
                act(out, in_, AF.Copy, R, W)
            else:
                e = nc.vector if eng == "dve" else nc.gpsimd
                P.op(eng, lambda: e.tensor_copy(out=out, in_=in_), R, W)

        XT = sb("XT", [128, 8, NT])
        UY = sb("UY", [128, 8, NT], BF16)
        MOD = sb("MOD", [128, 4, 6, 8, 2])
        LNP = sb("LNP", [128, 4, 4, 8])
        GN = sb("GN", [128, 2, 8])
        SUBG = sb("SUBG", [128, 2, 8])
        RB = sb("RB", [1, 4, 36])
        LAM = sb("LAM", [128, 2, 8])
        CST = sb("CST", [128, 1024])
        CST2 = sb("CST2", [128, 256])
        CSTB = sb("CSTB", [128, 256], BF16)
        MASK = sb("MASK", [128, 512], U8)
        CST3 = sb("CST3", [128, 256])
        CSTB2 = sb("CSTB2", [128, 128], BF16)
        TRIS = CSTB2[:, 0:128]
        EPSC = sb("EPSC", [128, 1])
        ONEC = sb("ONEC", [128, 1])
        ZEROC = sb("ZEROC", [128, 1])
        IDF = CST[:, 0:128]
        ONESD = CST[:, 128:256]
        ONESH = CST[:, 256:384]
        TRIF = CST[:, 384:512]
        ONES1 = CST[0:1, 512:640]
        TRIB = CST2[:, 0:128]
        TRIBP = CST2[:, 128:256]
        IDB = CSTB[:, 0:128]
        ONESB = CSTB[:, 128:256]

        def xkeys(blocks=range(5), ks=range(8)):
            return [f"x{k}b{b}" for k in ks for b in blocks]

        def ukeys(blocks=range(5), ks=range(8)):
            return [f"u{k}b{b}" for k in ks for b in blocks]

        for k in range(8):
            P.dma("sp", XT[:, k, :], xT_d[k], [], xkeys(ks=[k]))
        for (t_, d_, kname) in [(LNP, lnp_d, "LNP"), (GN, gn_d, "GN"), (SUBG, subg_d, "SUBG"),
                                (RB, rb_d, "RB"), (CST, cst_d, "CST"), (CST2, cst2_d, "CST2"), (CSTB, cstb_d, "CSTB"),
                                (MASK, mask_d, "MASK"), (CST3, cst3_d, "CST3"), (CSTB2, cstb2_d, "CSTB2")]:
            P.dma("sp", t_[:], d_, [], [kname])
        P.op("dve", lambda: nc.vector.memset(EPSC[:], EPS), [], ["EPSC"])
        P.op("dve", lambda: nc.vector.memset(ONEC[:], 1.0), [], ["ONEC"])
        P.op("dve", lambda: nc.vector.memset(ZEROC[:], 0.0), [], ["ZEROC"])

        with ExitStack() as s1:
            CC = sb("CC", [128, 8, 2], F32, s1)
            SCB = sb("SCB", [128, 8, 2], BF16, s1)
            ADAB = sb("ADAB", [128, 4, 48], F32, s1)
            WA = [sb(f"WA{i}", [128, 8, 1024], BF16, s1) for i in range(2)]
            LQK = sb("LQK", [64, 2, 4, 8], F32, s1)
            LQP = sb("LQP", [64, 2, 2, 8], F32, s1)
            P.dma("sp", CC[:], cc_d, [], ["CC"])
            P.dma("sp", ADAB[:], adab_d, [], ["ADAB"])
            P.dma("sp", LQK[:], lqk_d, [], ["LQK"])
            act(SCB[:], CC[:], AF.Silu, ["CC"], ["SCB"])
            it = 0
            for l in range(n_layers):
                for j6 in range(6):
                    w = WA[it % 2]
                    wk = f"WA{it % 2}"
                    it += 1
                    src = ada_w_d[l].rearrange("(k p) n -> p k n", p=128)[:, :, j6 * 1024:(j6 + 1) * 1024]
                    P.dma("pool", w[:], src, [], [wk])
                    ps, pk = nb()
                    for jj in range(8):
                        for k in range(8):
                            mm(ps[:, jj * 2:jj * 2 + 2], w[:, k, jj * 128:(jj + 1) * 128], SCB[:, k, :],
                               k == 0, k == 7, [wk, "SCB"], [pk])
                    tt(MOD[:, l, j6, :, :], ps[:, 0:16].rearrange("p (j g) -> p j g", g=2),
                       ADAB[:, l, j6 * 8:(j6 + 1) * 8].unsqueeze(2).broadcast_to([128, 8, 2]), ALU.add,
                       [pk, "ADAB"], ["MOD"])
                for j6 in (1, 4):
                    ts(MOD[:, l, j6, :, :], MOD[:, l, j6, :, :], 1.0, ALU.add, ["MOD"], ["MOD"])
            tt(LQP[:, :, 0, :], LQK[:, :, 0, :], LQK[:, :, 1, :], ALU.mult, ["LQK"], ["LQP"])
            tt(LQP[:, :, 1, :], LQK[:, :, 2, :], LQK[:, :, 3, :], ALU.mult, ["LQP", "LQK"], ["LQP"])
            ps, pk = nb()
            mm(ps[:, 0:32], CST[0:64, 512:640], LQP[:].rearrange("p a b c -> p (a b c)"), True, True,
               ["CST", "LQP"], [pk])
            LE = sb("LE", [128, 32], F32, s1)
            act(LE[:], ps[:, 0:32], AF.Exp, [pk], ["LE"])
            lev = LE[:].rearrange("p (a b c) -> p a b c", a=2, b=2)
            for i in range(2):
                lam_init = 0.8 - 0.6 * math.exp(-0.3 * (2 * i + 1))
                tt(LAM[:, i, :], lev[:, i, 1, :], lev[:, i, 0, :], ALU.subtract, ["LE", "LAM"], ["LAM"])
                ts(LAM[:, i, :], LAM[:, i, :], -lam_init, ALU.add, ["LAM"], ["LAM"])
                ts(SUBG[:, i, :], SUBG[:, i, :], 1.0 - lam_init, ALU.mult, ["SUBG"], ["SUBG"])

        P.barrier()

        def mod(l, which, k, grp):
            return MOD[:, l, which, k, grp:grp + 1]

        def ln_block(l, which_ln, b, scr):
            t0, n, grp = BLOCKS[b]
            SQ, MEAN, RSTD = scr
            psm, pkm = nb()
            pss, pks = nb()
            for k in range(8):
                xk = XT[:, k, t0:t0 + n]
                mm(psm[:, :n], ONESD, xk, k == 0, k == 7, [f"x{k}b{b}", "CST"], [pkm])
                sq = SQ[k % 2]
                act(sq[:, :n], xk, AF.Square, [f"x{k}b{b}"], [f"SQ{k % 2}"])
                mm(pss[:, :n], ONESD, sq[:, :n], k == 0, k == 7, [f"SQ{k % 2}", "CST"], [pks])
            cp(MEAN[:, :n], psm[:, :n], [pkm], ["MEAN"], eng="act")
            act(RSTD[:, :n], psm[:, :n], AF.Square, [pkm], ["RSTD"])
            tt(RSTD[:, :n], pss[:, :n], RSTD[:, :n], ALU.subtract, [pks, "RSTD"], ["RSTD"])
            act(RSTD[:, :n], RSTD[:, :n], AF.Ln, ["RSTD"], ["RSTD"], bias=EPSC[:, 0:1])
            act(RSTD[:, :n], RSTD[:, :n], AF.Exp, ["RSTD"], ["RSTD"], scale=-0.5)
            for k in range(8):
                xk = XT[:, k, t0:t0 + n]
                kk = [f"x{k}b{b}"]
                tt(xk, xk, MEAN[:, :n], ALU.subtract, kk + ["MEAN"], kk)
                tt(xk, xk, RSTD[:, :n], ALU.mult, kk + ["RSTD"], kk)
                act(xk, xk, AF.Identity, kk + ["LNP"], kk, scale=LNP[:, l, 2 * which_ln, k:k + 1],
                    bias=LNP[:, l, 2 * which_ln + 1, k:k + 1])

        def proj_ln(l, w_dram, nblocks, raw):
            with ExitStack() as s2:
                WO = sb("WO", [128, 8, 1024], BF16, s2)
                TMP = [sb(f"TMPO{i}", [128, 512], F32, s2) for i in range(2)]
                SQ = [sb(f"SQ{i}", [128, 512], F32, s2) for i in range(2)]
                MEAN = sb("MEAN", [128, 512], F32, s2)
                RSTD = sb("RSTD", [128, 512], F32, s2)
                P.dma("pool", WO[:], w_dram.rearrange("(k p) n -> p k n", p=128), [], ["WO"])
                for b in range(nblocks):
                    t0, n, grp = BLOCKS[b]
                    for c in range(8):
                        ps, pk = nb()
                        for h in range(8):
                            mm(ps[:, :n], WO[:, h, c * 128:(c + 1) * 128], UY[:, h, t0:t0 + n], h == 0, h == 7,
                               ["WO", f"u{h}b{b}"], [pk])
                        xk = XT[:, c, t0:t0 + n]
                        kk = [f"x{c}b{b}"]
                        if raw:
                            cp(xk, ps[:, :n], [pk], kk, eng="act")
                        else:
                            tmp = TMP[c % 2]
                            act(tmp[:, :n], ps[:, :n], AF.Identity, [pk, "MOD"], [f"TMPO{c % 2}"], scale=mod(l, 2, c, grp))
                            stt(xk, xk, ALPHA, tmp[:, :n], ALU.mult, ALU.add, kk + [f"TMPO{c % 2}"], kk)
                    if not raw:
                        ln_block(l, 0, b, (SQ, MEAN, RSTD))
            P.barrier()

        def make_u(l, b, UB, ub_i, which_s, which_sh):
            t0, n, grp = BLOCKS[b]
            for k in range(8):
                src = XT[:, k, t0:t0 + n]
                dst = UB[ub_i][:, k, :n]
                R = [f"x{k}b{b}", "MOD"]
                W = [f"UB{ub_i}k{k}"]
                if k % 2 == 0:
                    act(dst, src, AF.Identity, R, W, scale=mod(l, which_s, k, grp), bias=mod(l, which_sh, k, grp))
                else:
                    ts(dst, src, mod(l, which_s, k, grp), ALU.mult, R, W, s2=mod(l, which_sh, k, grp), op1=ALU.add)

        def fm_group(UBt, ub_i, n, Wt, wkey, c0, M):
            ps, pk = nb()
            for k in range(8):
                mm(ps[0:M, :n], Wt[:, k, c0:c0 + M], UBt[:, k, :n], k == 0, k == 7, [wkey, f"UB{ub_i}k{k}"], [pk])
            return ps, pk

        def rope_evac(dst, dkey, ps_a, pk_a, ps_b, pk_b, cos_t, sin_t, tkey, n, TR, ti):
            t1 = TR[0]
            t2 = TR[1]
            tt(t1[:, :n], ps_a[:, :n], cos_t, ALU.mult, [pk_a, tkey], ["TR0"])
            tt(t2[:, :n], ps_b[:, :n], sin_t, ALU.mult, [pk_b, tkey], ["TR1"])
            tt(dst, t1[:, :n], t2[:, :n], ALU.add, ["TR0", "TR1"], [dkey])

        def in_proj_head(l, hs, kind, wsrc, cols, Qf, Kf, V, SGN, gn_ap, rope_d, qscale, LRT=None, lr_cols=None,
                         after_block=None, nblocks=5):
            dk = 64 if kind == "B" else 128
            has_rope = kind in ("A", "C")
            has_g = kind in ("A", "B")
            wv = wsrc.rearrange("(k p) n -> p k n", p=128)
            W = hs["W"]
            ofs = {}
            o = 0
            names = ["q", "k", "v"] + (["g"] if has_g else [])
            for nm in names:
                w_ = dk if nm in ("q", "k") else 128
                P.dma("pool", W[:, :, o:o + w_], wv[:, :, cols[nm]:cols[nm] + w_], [], [f"W_{nm}"])
                ofs[nm] = o
                o += w_
            if LRT is not None:
                P.dma("pool", W[:, :, o:o + 32], wv[:, :, lr_cols:lr_cols + 32], [], ["W_lr"])
                ofs["lr"] = o
                o += 32
            if has_rope:
                blk = 32 if kind == "A" else 16
                for nm in ("q", "k"):
                    s_ = W[:, :, ofs[nm]:ofs[nm] + 128].rearrange("p k (a two b) -> p k a two b", two=2, b=blk)
                    d_ = W[:, :, o:o + 128].rearrange("p k (a two b) -> p k a two b", two=2, b=blk)
                    cp(d_[:, :, :, 0, :], s_[:, :, :, 1, :], [f"W_{nm}"], [f"W_{nm}p"])
                    cp(d_[:, :, :, 1, :], s_[:, :, :, 0, :], [f"W_{nm}", f"W_{nm}p"], [f"W_{nm}p"])
                    ofs[nm + "p"] = o
                    o += 128
            UB = hs["UB"]
            TR = hs["TR"]
            ROPE = hs["ROPE"]
            ti = 0
            for b in range(nblocks):
                t0, n, grp = BLOCKS[b]
                ub_i = b % len(UB)
                make_u(l, b, UB, ub_i, 1, 0)
                UBt = UB[ub_i]
                if has_rope and grp == 0:
                    P.dma("sp", ROPE[:, :, :], rope_d[:, :, t0:t0 + n].rearrange("f p t -> p f t"), [], ["ROPE"])
                for nm, dst_f in (("q", Qf), ("k", Kf)):
                    ps, pk = fm_group(UBt, ub_i, n, W, f"W_{nm}", ofs[nm], dk)
                    dst, dkey = dst_f(b)
                    if has_rope and grp == 0:
                        ps2, pk2 = fm_group(UBt, ub_i, n, W, f"W_{nm}p", ofs[nm + "p"], dk)
                        fi = 0 if nm == "q" else 2
                        rope_evac(dst, dkey, ps, pk, ps2, pk2, ROPE[:, fi, :n], ROPE[:, fi + 1, :n], "ROPE", n, TR, ti)
                        ti += 1
                    else:
                        sc = qscale if nm == "q" else 1.0
                        act(dst, ps[0:dk, :n], AF.Copy, [pk], [dkey], scale=sc)
                if has_g:
                    ps, pk = fm_group(UBt, ub_i, n, W, "W_g", ofs["g"], 128)
                    t1 = TR[ti % 2]
                    act(t1[:, :n], ps[:, :n], AF.Silu, [pk], [f"TR{ti % 2}"])
                    ts(SGN[:, t0:t0 + n], t1[:, :n], gn_ap, ALU.mult, [f"TR{ti % 2}", "GN"], [f"sgn_b{b}"])
                    ti += 1
                if LRT is not None:
                    for di in range(2):
                        ps, pk = fm_group(UBt, ub_i, n, W, "W_lr", ofs["lr"] + 16 * di, 16)
                        cp(LRT[di][0:16, :n], ps[0:16, :n], [pk], [f"lrt{di}"], eng="act")
                ps, pk = nb()
                ntile = n // 128
                for tI in range(ntile):
                    for k in range(8):
                        mm(ps[:, tI * 128:(tI + 1) * 128], UBt[:, k, tI * 128:(tI + 1) * 128],
                           W[:, k, ofs["v"]:ofs["v"] + 128], k == 0, k == 7, ["W_v", f"UB{ub_i}k{k}"], [pk])
                c0 = t0 // 128
                cp(V[:, c0:c0 + ntile, :], ps[:, :n].rearrange("p (c d) -> p c d", d=128), [pk], [f"vb{b}"], eng="act")
                if after_block is not None:
                    after_block(b)

        def head_norm(ps_o, pk_o, n, center, hs, out_ap, outkey, post_ap, postkeys, scale_ap=None, src_sb=None):
            OSB, SQh, RS = hs["OSB"], hs["SQh"], hs["RS"]
            M2 = SQh
            if src_sb is None:
                cp(OSB[:, :n], ps_o[:, :n], [pk_o], ["OSB"], eng="act")
                act(SQh[:, :n], ps_o[:, :n], AF.Square, [pk_o], ["SQh"])
            else:
                act(SQh[:, :n], OSB[:, :n], AF.Square, ["OSB"], ["SQh"])
            pss, pks = nb()
            mm(pss[:, :n], ONESH, SQh[:, :n], True, True, ["SQh", "CST"], [pks])
            if center:
                psm, pkm = nb()
                mm(psm[:, :n], ONESH, OSB[:, :n], True, True, ["OSB", "CST"], [pkm])
                act(M2[:, :n], psm[:, :n], AF.Square, [pkm, "SQh"], ["SQh"])
                tt(RS[:, :n], pss[:, :n], M2[:, :n], ALU.subtract, [pks, "SQh"], ["RS"])
                tt(OSB[:, :n], OSB[:, :n], psm[:, :n], ALU.subtract, ["OSB", pkm], ["OSB"])
                act(RS[:, :n], RS[:, :n], AF.Ln, ["RS"], ["RS"], bias=EPSC[:, 0:1])
            else:
                act(RS[:, :n], pss[:, :n], AF.Ln, [pks], ["RS"], bias=EPSC[:, 0:1])
            act(RS[:, :n], RS[:, :n], AF.Exp, ["RS"], ["RS"], scale=-0.5)
            if scale_ap is None:
                tt(OSB[:, :n], OSB[:, :n], RS[:, :n], ALU.mult, ["OSB", "RS"], ["OSB"])
                tt(out_ap, OSB[:, :n], post_ap, ALU.mult, ["OSB"] + postkeys, [outkey])
            else:
                stt(out_ap, OSB[:, :n], scale_ap, RS[:, :n], ALU.mult, ALU.mult, ["OSB", "RS"] + postkeys, [outkey])

        def mixer_ab(l, need_ctx):
            i = l // 2
            wsrc = ab_w_in_d[i]
            with ExitStack() as s2:
                hs = dict(
                    W=sb("Wh", [128, 8, 768], BF16, s2),
                    UB=[sb("UB0", [128, 8, 512], BF16, s2)],
                    TR=[sb(f"TR{j}", [128, 512], F32, s2) for j in range(2)],
                    OSB=sb("OSB", [128, 512], F32, s2), SQh=sb("SQh", [128, 512], F32, s2),
                    RS=sb("RS", [128, 512], F32, s2),
                )
                Qt = sb("Qt", [128, 512], BF16, s2)
                Kt = sb("Kt", [128, 512], BF16, s2)
                V = sb("V", [128, 18, 128], BF16, s2)
                SGN = sb("SGN", [128, NT], BF16, s2)
                QDF = sb("QDF", [128, NT], BF16, s2)
                QDB = sb("QDB", [128, NT], BF16, s2)
                ATT = sb("ATT", [128, NT], BF16, s2)
                SFb = sb("SFb", [128, 18, 128], BF16, s2)
                SBb = sb("SBb", [128, 18, 128], BF16, s2)
                CUR = [sb(f"CUR{j}", [128, 128], F32, s2) for j in range(4)]
                KD = [sb(f"KD{j}", [128, 512], BF16, s2) for j in range(4)]
                KTT = [sb(f"KTT{j}", [128, 4, 128], BF16, s2) for j in range(2)]

                def Qf(b):
                    return Qt[:, :BLOCKS[b][1]], "qt"

                def Kf(b):
                    return Kt[:, :BLOCKS[b][1]], "kt"

                def Qf64(b):
                    return Qt[0:64, :BLOCKS[b][1]], "qt"

                def Kf64(b):
                    return Kt[0:64, :BLOCKS[b][1]], "kt"

                def run_head(h, ph):
                    isA = h < 4
                    dk = 128 if isA else 64
                    hh = h if isA else h - 4
                    if isA:
                        RET = ph["RET"]
                        P.dma("sp", RET[:], ret_d[:, hh, :, :], [], ["RET"])
                    else:
                        LRT, WLRb, LS, LS2, EXS, TAB, TOT, DEC = (ph[k_] for k_ in ("LRT", "WLRb", "LS", "LS2", "EXS", "TAB", "TOT", "DEC"))
                    P.op("dve", lambda: nc.vector.memset(SFb[:, 16, :], 0.0), [], ["SFb"])
                    P.op("dve", lambda: nc.vector.memset(SBb[:, 17, :], 0.0), [], ["SBb"])

                    def pass1(b):
                        t0, n, grp = BLOCKS[b]
                        nch = n // 128
                        c0 = t0 // 128
                        if (not isA) and DBG_B < 1:
                            return
                        qv = Qt[0:dk, :n]
                        kv = Kt[0:dk, :n]
                        if isA:
                            tabs = [RET[:, j, :].unsqueeze(1).broadcast_to([128, nch, 128]) for j in range(6)]
                            tkeys = ["RET"]

                            def rr(ap):
                                return ap.rearrange("p (c d) -> p c d", d=128)
                        else:
                            psg, pkg = nb()
                            for c in range(nch):
                                for di in range(2):
                                    mm(psg[:, c * 128 + di * 64:c * 128 + di * 64 + 64],
                                       LRT[di][:, c * 128:(c + 1) * 128], WLRb[:, di, hh * 64:(hh + 1) * 64],
                                       True, True, [f"lrt{di}", "WLRb"], [pkg])
                            act(EXS[:, :n], psg[:, :n], AF.Exp, [pkg], ["EXS"], scale=-1.0)
                            ex4 = EXS[:, :n].rearrange("p (c a d) -> p c a d", a=2, d=64)
                            act(LS[:, 0:nch, :, :], ex4, AF.Ln, ["EXS"], ["LS"], bias=ONEC[:, 0:1])
                            act(LS2[:, 0:nch, 0, :], ex4[:, :, 1, :], AF.Ln, ["EXS"], ["LS2"], bias=ONEC[:, 0:1])
                            act(LS2[:, 0:nch, 1, :], ex4[:, :, 0, :], AF.Ln, ["EXS", "LS2"], ["LS2"], bias=ONEC[:, 0:1])
                            if DBG_C < "b":
                                return
                            psf, pkf = nb()
                            psb, pkb = nb()
                            psp, pkp = nb()
                            for c in range(nch):
                                mm(psf[:, c * 128:(c + 1) * 128], LS[:, c, :, :].rearrange("p a d -> p (a d)"), TRIF, True, True, ["LS", "CST"], [pkf])
                            for c in range(nch):
                                mm(psb[:, c * 128:(c + 1) * 128], LS2[:, c, :, :].rearrange("p a d -> p (a d)"), TRIB, True, True, ["LS2", "CST2"], [pkb])
                            for c in range(nch):
                                mm(psp[:, c * 128:(c + 1) * 128], LS2[:, c, :, :].rearrange("p a d -> p (a d)"), TRIBP, True, True, ["LS2", "CST2"], [pkp])
                            if DBG_C < "c":
                                return
                            f3 = psf[0:64, :n].rearrange("p (c d) -> p c d", d=128)
                            b3 = psb[0:64, :n].rearrange("p (c d) -> p c d", d=128)
                            cp(TOT[0:64, 0:nch, 0:1], f3[:, :, 127:128], [pkf], ["TOT"])
                            cp(TOT[0:64, 0:nch, 1:2], b3[:, :, 0:1], [pkb, "TOT"], ["TOT"])
                            if DBG_C < "d":
                                return
                            act(TAB[0][0:64, :n], psf[0:64, :n], AF.Exp, [pkf, "TOT"], ["TAB0"])
                            act(TAB[1][0:64, :n], psf[0:64, :n], AF.Exp, [pkf, "TOT"], ["TAB1"], scale=-1.0)
                            act(TAB[3][0:64, :n], psp[0:64, :n], AF.Exp, [pkp, "TOT"], ["TAB3"])
                            act(TAB[4][0:64, :n], psb[0:64, :n], AF.Exp, [pkb, "TOT"], ["TAB4"], scale=-1.0)
                            if DBG_C < "e":
                                return
                            for c in range(nch):
                                act(TAB[2][0:64, c * 128:(c + 1) * 128], psf[0:64, c * 128:(c + 1) * 128], AF.Exp,
                                    [pkf, "TOT"], ["TAB2"], scale=-1.0, bias=TOT[0:64, c, 0:1])
                                act(TAB[5][0:64, c * 128:(c + 1) * 128], psb[0:64, c * 128:(c + 1) * 128], AF.Exp,
                                    [pkb, "TOT"], ["TAB5"], scale=-1.0, bias=TOT[0:64, c, 1:2])
                            act(DEC[0:64, c0:c0 + nch, :], TOT[0:64, 0:nch, :], AF.Exp, ["TOT"], ["DEC"])
                            tabs = [TAB[j][0:64, :n] for j in range(6)]
                            tkeys = [f"TAB{j}" for j in range(6)]

                            def rr(ap):
                                return ap
                        if (not isA) and DBG_B < 2:
                            return
                        tt(rr(QDF[0:dk, t0:t0 + n]), rr(qv), tabs[0], ALU.mult, ["qt"] + tkeys, [f"qdf{b}"])
                        tt(rr(QDB[0:dk, t0:t0 + n]), rr(qv), tabs[3], ALU.mult, ["qt"] + tkeys, [f"qdb{b}"])
                        tt(rr(KD[0][0:dk, :n]), rr(kv), tabs[1], ALU.mult, ["kt"] + tkeys, ["KD0"])
                        tt(rr(KD[1][0:dk, :n]), rr(kv), tabs[4], ALU.mult, ["kt"] + tkeys, ["KD1"])
                        tt(rr(KD[2][0:dk, :n]), rr(kv), tabs[2], ALU.mult, ["kt"] + tkeys, ["KD2"])
                        tt(rr(KD[3][0:dk, :n]), rr(kv), tabs[5], ALU.mult, ["kt"] + tkeys, ["KD3"])
                        psa, pka = nb()
                        psb2, pkb2 = nb()
                        for c in range(nch):
                            sl = slice(c * 128, (c + 1) * 128)
                            gsl = slice(t0 + c * 128, t0 + (c + 1) * 128)
                            mm(psa[:, sl], KD[0][0:dk, sl], QDF[0:dk, gsl], True, True, ["KD0", f"qdf{b}"], [pka])
                            mm(psb2[:, sl], KD[1][0:dk, sl], QDB[0:dk, gsl], True, True, ["KD1", f"qdb{b}"], [pkb2])
                        cp(ATT[:, t0:t0 + n], psa[:, :n], [pka], [f"att{b}"], eng="act")
                        P.op("dve", lambda: nc.vector.copy_predicated(out=ATT[:, t0:t0 + n], mask=MASK[:, :n],
                                                                       data=psb2[:, :n]),
                             [pkb2, "MASK", f"att{b}"], [f"att{b}"])
                        if (not isA) and DBG_B < 3:
                            return
                        for di in range(2):
                            psk, pkk = nb()
                            for c in range(nch):
                                mm(psk[:, c * 128:c * 128 + dk], KD[2 + di][0:dk, c * 128:(c + 1) * 128], IDB[0:dk, 0:dk],
                                   True, True, [f"KD{2 + di}", "CSTB"], [pkk])
                            cp(KTT[di][:, 0:nch, 0:dk], psk[:, :n].rearrange("p (c d) -> p c d", d=128)[:, :, 0:dk],
                               [pkk], [f"KTT{di}"], eng="act")
                            psd, pkd = nb()
                            for c in range(nch):
                                mm(psd[0:dk, c * 128:(c + 1) * 128], KTT[di][:, c, 0:dk], V[:, c0 + c, :], True, True,
                                   [f"KTT{di}", f"vb{b}"], [pkd])
                            d3 = psd[0:dk, :n].rearrange("p (c d) -> p c d", d=128)
                            if di == 0:
                                if grp == 0:
                                    m = nch if c0 + nch < 16 else nch - 1
                                    cp(SFb[0:dk, c0 + 1:c0 + 1 + m, :], d3[:, 0:m, :], [pkd], ["SFb"])
                                else:
                                    cp(SFb[0:dk, 17, :], d3[:, 0, :], [pkd], ["SFb"])
                                    cp(SFb[0:dk, 0, :], d3[:, 1, :], [pkd], ["SFb"])
                            else:
                                if c0 == 0:
                                    cp(SBb[0:dk, 0:nch - 1, :], d3[:, 1:nch, :], [pkd], ["SBb"])
                                else:
                                    cp(SBb[0:dk, c0 - 1:c0 - 1 + nch, :], d3[:, :, :], [pkd], ["SBb"])

                    if isA:
                        cols = dict(q=hh * 128, k=512 + hh * 128, v=1024 + hh * 128, g=1536 + hh * 128)
                        hs["ROPE"] = ph["ROPE"]
                        in_proj_head(l, hs, "A", wsrc, cols, Qf, Kf, V, SGN, GN[:, i, h:h + 1], ropeA_d, dk ** -0.5,
                                     after_block=pass1)
                    else:
                        cols = dict(q=2048 + hh * 64, k=2304 + hh * 64, v=2560 + hh * 128, g=3072 + hh * 128)
                        hs["ROPE"] = None
                        in_proj_head(l, hs, "B", wsrc, cols, Qf64, Kf64, V, SGN, GN[:, i, h:h + 1], None, dk ** -0.5,
                                     LRT=LRT, lr_cols=3584, after_block=pass1)
                    if (not isA) and DBG_B < 4:
                        return
                    for di, (ST, order, skey) in enumerate(((SFb, ORDER_F, "SFb"), (SBb, ORDER_B, "SBb"))):
                        c_a, c_b = CUR[2 * di], CUR[2 * di + 1]
                        ka, kb_ = f"CUR{2 * di}", f"CUR{2 * di + 1}"
                        P.op("dve", lambda c_a=c_a: nc.vector.memset(c_a[:], 0.0), [], [ka])
                        for oi in range(17):
                            nn, nx = order[oi], order[oi + 1]
                            if isA:
                                g_ = 1.0 - 2.0 ** (-((5.0 if di == 0 else 5.5) + hh))
                                sc_ = float(np.float32(g_) ** 128)
                            else:
                                sc_ = DEC[0:64, nn, di:di + 1]
                            stt(c_b[0:dk, :], c_a[0:dk, :], sc_, ST[0:dk, nx, :], ALU.mult, ALU.add,
                                [ka, skey] + ([] if isA else ["DEC"]), [kb_])
                            cp(ST[0:dk, nx, :], c_b[0:dk, :], [kb_], [skey], eng="act")
                            c_a, c_b, ka, kb_ = c_b, c_a, kb_, ka
                    if (not isA) and DBG_B < 5:
                        return
                    for b in range(5 if need_ctx else 4):
                        t0, n, grp = BLOCKS[b]
                        nch = n // 128
                        c0 = t0 // 128
                        pso, pko = nb()
                        for c in range(nch):
                            sl = slice(c * 128, (c + 1) * 128)
                            gsl = slice(t0 + c * 128, t0 + (c + 1) * 128)
                            mm(pso[:, sl], V[:, c0 + c, :], ATT[:, gsl], True, False, [f"vb{b}", f"att{b}"], [pko])
                            mm(pso[:, sl], SFb[0:dk, c0 + c, :], QDF[0:dk, gsl], False, False, ["SFb", f"qdf{b}"], [pko])
                            mm(pso[:, sl], SBb[0:dk, c0 + c, :], QDB[0:dk, gsl], False, True, ["SBb", f"qdb{b}"], [pko])
                        head_norm(pso, pko, n, isA, hs, UY[:, h, t0:t0 + n], f"u{h}b{b}", SGN[:, t0:t0 + n], [f"sgn_b{b}"])

                with ExitStack() as s3:
                    ph = dict(ROPE=sb("ROPE", [128, 4, 512], F32, s3), RET=sb("RET", [128, 6, 128], F32, s3))
                    for h in range(min(4, DBG_HEADS)):
                        run_head(h, ph)
                P.barrier()
                with ExitStack() as s3:
                    WLR = sb("WLR", [32, 2, 256], F32, s3)
                    ph = dict(
                        LRT=[sb(f"LRT{j}", [32, 512], BF16, s3) for j in range(2)],
                        WLRb=sb("WLRb", [32, 2, 256], BF16, s3),
                        LS=sb("LS", [128, 4, 2, 64], F32, s3),
                        LS2=sb("LS2", [128, 4, 2, 64], F32, s3),
                        EXS=sb("EXS", [128, 512], F32, s3),
                        TAB=[sb(f"TAB{j}", [128, 512], BF16, s3) for j in range(6)],
                        TOT=sb("TOT", [128, 4, 2], F32, s3),
                        DEC=sb("DEC", [128, 18, 2], F32, s3),
                    )
                    P.dma("sp", WLR[:], wlr_d[:, i, :, :], [], ["WLR"])
                    cp(ph["WLRb"][:], WLR[:], ["WLR"], ["WLRb"])
                    for di in range(2):
                        P.op("dve", lambda di=di: nc.vector.memset(ph["LRT"][di][:], 1.0), [], [f"lrt{di}"])
                    for h in range(4, min(8, DBG_HEADS)):
                        run_head(h, ph)
            P.barrier()

        def mixer_c(l, need_ctx):
            i = l // 2
            wsrc = c_w_qkv_d[i]
            with ExitStack() as s2:
                hs = dict(
                    W=sb("Wh", [128, 8, 640], BF16, s2),
                    UB=[sb(f"UB{j}", [128, 8, 512], BF16, s2) for j in range(2)],
                    TR=[sb(f"TR{j}", [128, 512], F32, s2) for j in range(2)],
                    ROPE=sb("ROPE", [128, 4, 512], F32, s2),
                    OSB=sb("OSB", [128, 512], F32, s2), SQh=sb("SQh", [128, 512], F32, s2),
                    RS=sb("RS", [128, 512], F32, s2),
                )
                Q = sb("Q", [128, NT], BF16, s2)
                K = sb("K", [128, NT], BF16, s2)
                V = sb("V", [128, 18, 128], BF16, s2)
                E = [sb(f"E{j}", [128, 512], BF16, s2) for j in range(4)]
                R1 = sb("R1", [128, 512], F32, s2)
                R2 = sb("R2", [128, 512], F32, s2)
                A1 = sb("A1", [128, 512], F32, s2)
                for h in range(8):
                    cols = dict(q=h * 128, k=1024 + h * 128, v=2048 + h * 128)
                    in_proj_head(l, hs, "C", wsrc, cols,
                                 lambda b: (Q[:, BLOCKS[b][0]:BLOCKS[b][0] + BLOCKS[b][1]], f"qb{b}"),
                                 lambda b: (K[:, BLOCKS[b][0]:BLOCKS[b][0] + BLOCKS[b][1]], f"kb{b}"),
                                 V, None, None, ropeC_d, 0.125)
                    for b in range(5 if need_ctx else 4):
                        t0, n, grp = BLOCKS[b]
                        kchunks = list(range(18)) if grp == 0 else [16, 17]
                        acc = [(PS[j], f"ps{j}") for j in range(4)]
                        def issue_S(ci):
                            c = kchunks[ci]
                            kb_ = c // 4 if c < 16 else 4
                            ksl = slice(c * 128, (c + 1) * 128)
                            for comp in range(2):
                                sj = 4 + 2 * (ci % 2) + comp
                                rsl = slice(comp * 64, (comp + 1) * 64)
                                mm(PS[sj][:, :n], K[rsl, ksl], Q[rsl, t0:t0 + n], True, True, [f"kb{kb_}", f"qb{b}"], [f"ps{sj}"])

                        def issue_rest(ci):
                            c = kchunks[ci]
                            first, last = ci == 0, ci == len(kchunks) - 1
                            kb_ = c // 4 if c < 16 else 4
                            for comp in range(2):
                                sj = 4 + 2 * (ci % 2) + comp
                                ej = 2 * (ci % 2) + comp
                                act(E[ej][:, :n], PS[sj][:, :n], AF.Exp, [f"ps{sj}"], [f"E{ej}"])
                            for comp in range(2):
                                ej = 2 * (ci % 2) + comp
                                po, pko = acc[2 * comp]
                                pd, pkd = acc[2 * comp + 1]
                                mm(po[:, :n], V[:, c, :], E[ej][:, :n], first, last, [f"vb{kb_}", f"E{ej}"], [pko])
                                mm(pd[:, :n], ONESB, E[ej][:, :n], first, last, ["CSTB", f"E{ej}"], [pkd])

                        issue_S(0)
                        for ci in range(len(kchunks)):
                            if ci + 1 < len(kchunks):
                                issue_S(ci + 1)
                            issue_rest(ci)
                        act(R1[:, :n], acc[1][0][:, :n], AF.Ln, [acc[1][1]], ["R1"])
                        act(R1[:, :n], R1[:, :n], AF.Exp, ["R1"], ["R1"], scale=-1.0)
                        act(R2[:, :n], acc[3][0][:, :n], AF.Ln, [acc[3][1]], ["R2"])
                        act(R2[:, :n], R2[:, :n], AF.Exp, ["R2"], ["R2"], scale=-1.0)
                        tt(A1[:, :n], acc[0][0][:, :n], R1[:, :n], ALU.mult, [acc[0][1], "R1"], ["A1"])
                        tt(R2[:, :n], acc[2][0][:, :n], R2[:, :n], ALU.mult, [acc[2][1], "R2"], ["R2"])
                        stt(hs["OSB"][:, :n], R2[:, :n], LAM[:, i, h:h + 1], A1[:, :n], ALU.mult, ALU.add,
                            ["R2", "A1", "LAM"], ["OSB"])
                        psc[0] = 4
                        head_norm(None, None, n, False, hs, UY[:, h, t0:t0 + n], f"u{h}b{b}", None, ["SUBG"],
                                  scale_ap=SUBG[:, i, h:h + 1], src_sb=True)
                        psc[0] = 4
            P.barrier()

        def moe(l, need_ctx):
            nblk = 5 if need_ctx else 4
            with ExitStack() as s2:
                WB = [sb(f"WB{j}", [128, 4096], BF16, s2) for j in range(4)]
                WCT = sb("WCT", [32, NT], BF16, s2)
                SELE = sb("SELE", [32, 32 * 128], BF16, s2)
                BC = [sb(f"BC{j}", [128, NT], BF16, s2) for j in range(2)]
                H = [sb(f"H{j}", [128, 4, 512], BF16, s2) for j in range(2)]
                SG = [sb(f"SG{j}", [128, 512], F32, s2) for j in range(2)]
                TF = [sb(f"TF{j}", [128, 512], F32, s2) for j in range(3)]
                SQ = [sb(f"SQ{j}", [128, 512], F32, s2) for j in range(2)]
                MEAN = sb("MEAN", [128, 512], F32, s2)
                RSTD = sb("RSTD", [128, 512], F32, s2)
                LG = sb("LG", [128, 36], F32, s2)
                SM = sb("SM", [128, 16], F32, s2)
                GOH = sb("GOH", [128, 4], F32, s2)
                GE = sb("GE", [128, 4], F32, s2)
                ET = sb("ET", [128, 32], F32, s2)
                ES = sb("ES", [128, 8], F32, s2)
                T8 = sb("T8", [128, 8], F32, s2)
                SEL = sb("SEL", [128, 8], F32, s2)
                EX = sb("EX", [128, 8], F32, s2)
                WC = sb("WC", [128, 32], F32, s2)
                P.dma("sp", SELE[:], sele_d, [], ["SELE"])
                WR = sb("WRl", [128, 8, 36], F32, s2)
                P.dma("sp", WR[:], wr_d[:, l, :, :], [], ["WR"])
                items = []
                for e in range(32):
                    items.append(("g", e, wg_d[l, e].rearrange("(k p) n -> p k n", p=128)))
                    items.append(("u", e, wu_d[l, e].rearrange("(k p) n -> p k n", p=128)))
                    items.append(("d", e, wd_d[l, e].rearrange("(k p) n -> p k n", p=128)))

                def load_item(j):
                    if j >= len(items):
                        return
                    kind, e, src = items[j]
                    if kind == "d":
                        dst = WB[j % 4][:, :].rearrange("p (k n) -> p k n", n=1024)
                    else:
                        dst = WB[j % 4][:, :].rearrange("p (k n) -> p k n", n=512)
                    P.dma("pool", dst, src, [], [f"WB{j % 4}"])

                for j in range(4):
                    load_item(j)
                ti = 0
                for b in range(nblk):
                    t0, n, grp = BLOCKS[b]
                    ntile = n // 128
                    pr = [nb() for _ in range(ntile)]
                    for k in range(8):
                        tf = TF[ti % 3]
                        tk = f"TF{ti % 3}"
                        ti += 1
                        act(tf[:, :n], XT[:, k, t0:t0 + n], AF.Identity, [f"x{k}b{b}", "MOD"], [tk],
                            scale=mod(l, 4, k, grp), bias=mod(l, 3, k, grp))
                        cp(UY[:, k, t0:t0 + n], tf[:, :n], [tk], [f"u{k}b{b}"])
                        for tI in range(ntile):
                            mm(pr[tI][0][:, 0:36], tf[:, tI * 128:(tI + 1) * 128], WR[:, k, :], k == 0, False,
                               [tk, "WR"], [pr[tI][1]])
                    for tI in range(ntile):
                        ps, pk = pr[tI]
                        mm(ps[:, 0:36], ONES1, RB[0:1, l, :], False, True, ["CST", "RB"], [pk])
                        cp(LG[:], ps[:, 0:36], [pk], ["LG"])
                        R_ = ["LG", "SM", "GOH", "GE", "ET", "ES", "T8", "SEL", "EX", "WC"]

                        def dv(fn):
                            P.op("dve", fn, R_, R_)
                        dv(lambda: nc.vector.tensor_reduce(out=SM[:, 0:1], in_=LG[:, 0:4], axis=AX.X, op=ALU.max))
                        dv(lambda: nc.vector.tensor_scalar(out=GOH[:], in0=LG[:, 0:4], scalar1=SM[:, 0:1], scalar2=None,
                                                           op0=ALU.is_equal))
                        dv(lambda: nc.vector.tensor_scalar(out=SM[:, 1:2], in0=SM[:, 0:1], scalar1=-1.0, scalar2=None,
                                                           op0=ALU.mult))
                        act(GE[:], LG[:, 0:4], AF.Exp, R_, R_, bias=SM[:, 1:2])
                        dv(lambda: nc.vector.tensor_reduce(out=SM[:, 2:3], in_=GE[:], axis=AX.X, op=ALU.add))
                        dv(lambda: nc.vector.reciprocal(out=SM[:, 3:4], in_=SM[:, 2:3]))
                        dv(lambda: nc.vector.tensor_tensor(
                            out=ET[:].rearrange("p (g e) -> p g e", e=8),
                            in0=LG[:, 4:36].rearrange("p (g e) -> p g e", e=8),
                            in1=GOH[:].unsqueeze(2).broadcast_to([128, 4, 8]), op=ALU.mult))
                        dv(lambda: nc.vector.tensor_reduce(out=ES[:], in_=ET[:].rearrange("p (g e) -> p e g", e=8),
                                                           axis=AX.X, op=ALU.add))
                        dv(lambda: nc.vector.max(out=T8[:], in_=ES[:]))
                        dv(lambda: nc.vector.tensor_scalar(out=SEL[:], in0=ES[:], scalar1=T8[:, 1:2], scalar2=None,
                                                           op0=ALU.is_ge))
                        dv(lambda: nc.vector.tensor_scalar(out=SM[:, 4:5], in0=T8[:, 0:1], scalar1=-1.0, scalar2=None,
                                                           op0=ALU.mult))
                        act(EX[:], ES[:], AF.Exp, R_, R_, bias=SM[:, 4:5])
                        dv(lambda: nc.vector.tensor_tensor(out=EX[:], in0=EX[:], in1=SEL[:], op=ALU.mult))
                        dv(lambda: nc.vector.tensor_reduce(out=SM[:, 5:6], in_=EX[:], axis=AX.X, op=ALU.add))
                        dv(lambda: nc.vector.reciprocal(out=SM[:, 6:7], in_=SM[:, 5:6]))
                        dv(lambda: nc.vector.tensor_tensor(out=SM[:, 7:8], in0=SM[:, 6:7], in1=SM[:, 3:4], op=ALU.mult))
                        dv(lambda: nc.vector.tensor_scalar(out=EX[:], in0=EX[:], scalar1=SM[:, 7:8], scalar2=None,
                                                           op0=ALU.mult))
                        dv(lambda: nc.vector.tensor_tensor(
                            out=WC[:].rearrange("p (g e) -> p g e", e=8),
                            in0=GOH[:].unsqueeze(2).broadcast_to([128, 4, 8]),
                            in1=EX[:].unsqueeze(1).broadcast_to([128, 4, 8]), op=ALU.mult))
                        pt, pkt = nb()
                        P.op("pe", lambda: nc.tensor.transpose(out=pt[0:32, 0:128], in_=WC[:], identity=IDF),
                             R_ + ["CST"], [pkt])
                        cp(WCT[:, t0 + tI * 128:t0 + (tI + 1) * 128], pt[0:32, 0:128], [pkt], [f"wct{b}"], eng="act")
                for b in range(nblk):
                    t0, n, grp = BLOCKS[b]
                    for k in range(8):
                        xk = XT[:, k, t0:t0 + n]
                        if k % 2 == 0:
                            act(xk, xk, AF.Copy, [f"x{k}b{b}"], [f"x{k}b{b}"], scale=ALPHA)
                        else:
                            ts(xk, xk, ALPHA, ALU.mult, [f"x{k}b{b}"], [f"x{k}b{b}"])
                hi = 0
                for e in range(32):
                    j0 = 3 * e
                    WG = WB[j0 % 4][:, :].rearrange("p (k n) -> p k n", n=512)
                    WU = WB[(j0 + 1) % 4][:, :].rearrange("p (k n) -> p k n", n=512)
                    WD = WB[(j0 + 2) % 4][:, :].rearrange("p (k n) -> p k n", n=1024)
                    kg, ku, kd = f"WB{j0 % 4}", f"WB{(j0 + 1) % 4}", f"WB{(j0 + 2) % 4}"
                    bc = BC[e % 2]
                    bck = f"BC{e % 2}"
                    for b in range(nblk):
                        t0, n, grp = BLOCKS[b]
                        ps, pk = nb()
                        mm(ps[:, :n], SELE[:, e * 128:(e + 1) * 128], WCT[:, t0:t0 + n], True, True, ["SELE", f"wct{b}"], [pk])
                        cp(bc[:, t0:t0 + n], ps[:, :n], [pk], [bck + f"b{b}"], eng="act")
                    for b in range(nblk):
                        t0, n, grp = BLOCKS[b]
                        Hb = H[hi % 2]
                        hk = f"H{hi % 2}"
                        hi += 1
                        for fc in range(4):
                            pg, pkg = nb()
                            pu, pku = nb()
                            for k in range(8):
                                mm(pg[:, :n], WG[:, k, fc * 128:(fc + 1) * 128], UY[:, k, t0:t0 + n], k == 0, k == 7,
                                   [kg, f"u{k}b{b}"], [pkg])
                            for k in range(8):
                                mm(pu[:, :n], WU[:, k, fc * 128:(fc + 1) * 128], UY[:, k, t0:t0 + n], k == 0, k == 7,
                                   [ku, f"u{k}b{b}"], [pku])
                            sg = SG[fc % 2]
                            act(sg[:, :n], pg[:, :n], AF.Silu, [pkg], [f"SG{fc % 2}"])
                            tt(sg[:, :n], sg[:, :n], pu[:, :n], ALU.mult, [f"SG{fc % 2}", pku], [f"SG{fc % 2}"])
                            tt(Hb[:, fc, :n], sg[:, :n], bc[:, t0:t0 + n], ALU.mult, [f"SG{fc % 2}", bck + f"b{b}"],
                               [hk + f"f{fc}"])
                        if b == nblk - 1:
                            load_item(j0 + 4)
                            load_item(j0 + 5)
                        for oc in range(8):
                            py, pky = nb()
                            for fc in range(4):
                                mm(py[:, :n], WD[:, fc, oc * 128:(oc + 1) * 128], Hb[:, fc, :n], fc == 0, fc == 3,
                                   [kd, hk + f"f{fc}"], [pky])
                            xk = XT[:, oc, t0:t0 + n]
                            stt(xk, py[:, :n], mod(l, 5, oc, grp), xk, ALU.mult, ALU.add, [pky, "MOD", f"x{oc}b{b}"],
                                [f"x{oc}b{b}"])
                    load_item(j0 + 6)
                for b in range(nblk):
                    ln_block(l, 1, b, (SQ, MEAN, RSTD))
            P.barrier()

        BREG = []

        def moe_sparse(l, need_ctx):
            if not BREG:
                BREG.append(nc.gpsimd.to_reg(4 * 32 * 128 - 1))
            nblk = 5 if need_ctx else 4
            ntiles = 18 if need_ctx else 16
            RB_ = 256
            RT = RB_ // 128
            NB_ = (ntiles * 128 * 2) // RB_ + 32
            NZ = NB_ * RT
            POOL = (mybir.EngineType.Pool,)
            with ExitStack() as s2:
                AST = sb("AST", [128, 18, 32], BF16, s2)
                WCS = sb("WCS", [128, 18, 32], F32, s2)
                CSS = sb("CSS", [128, 18, 32], F32, s2)
                PRE = sb("PRE", [128, 19, 32], F32, s2)
                PST = sb("PST", [128, 32], F32, s2)
                PEN = sb("PEN", [128, 32], F32, s2)
                DESTF = sb("DESTF", [128, 18, 2], F32, s2)
                DEST = sb("DEST", [128, 18, 2], U32, s2)
                WSEL = sb("WSEL", [128, 18, 2], F32, s2)
                IDXW = sb("IDXW", [128, 72], U32, s2)
                SQ = [sb(f"SQ{j}", [128, 512], F32, s2) for j in range(2)]
                MEAN = sb("MEAN", [128, 512], F32, s2)
                RSTD = sb("RSTD", [128, 512], F32, s2)
                with ExitStack() as s3:
                    TF = [sb(f"TF{j}", [128, 512], F32, s3) for j in range(3)]
                    WR = sb("WRl", [128, 8, 36], F32, s3)
                    LG = sb("LG", [128, 36], F32, s3)
                    SM = sb("SM", [128, 16], F32, s3)
                    GOH = sb("GOH", [128, 4], F32, s3)
                    GE = sb("GE", [128, 4], F32, s3)
                    ET = sb("ET", [128, 32], F32, s3)
                    ES = sb("ES", [128, 8], F32, s3)
                    T8 = sb("T8", [128, 8], F32, s3)
                    SEL = sb("SEL", [128, 8], F32, s3)
                    EX = sb("EX", [128, 8], F32, s3)
                    NBK = sb("NBK", [128, 32, 36], F32, s3)
                    CNT = sb("CNT", [128, 32], F32, s3)
                    PADB = sb("PADB", [32, 128], F32, s3)
                    DP1 = sb("DP1", [128, 32], F32, s3)
                    EQ = sb("EQ", [128, 32], F32, s3)
                    BLE = sb("BLE", [128, 72, 32], F32, s3)
                    BLKB = sb("BLKB", [128, 72], F32, s3)
                    P.dma("sp", WR[:], wr_d[:, l, :, :], [], ["WR"])
                    ti = 0
                    for b in range(nblk):
                        t0, n, grp = BLOCKS[b]
                        ntile = n // 128
                        pr = [nb() for _ in range(ntile)]
                        for k in range(8):
                            tf = TF[ti % 3]
                            tk = f"TF{ti % 3}"
                            ti += 1
                            act(tf[:, :n], XT[:, k, t0:t0 + n], AF.Identity, [f"x{k}b{b}", "MOD"], [tk],
                                scale=mod(l, 4, k, grp), bias=mod(l, 3, k, grp))
                            cp(UY[:, k, t0:t0 + n], tf[:, :n], [tk], [f"u{k}b{b}"])
                            for tI in range(ntile):
                                mm(pr[tI][0][:, 0:36], tf[:, tI * 128:(tI + 1) * 128], WR[:, k, :], k == 0, False,
                                   [tk, "WR"], [pr[tI][1]])
                        for tI in range(ntile):
                            gi = t0 // 128 + tI
                            ps, pk = pr[tI]
                            mm(ps[:, 0:36], ONES1, RB[0:1, l, :], False, True, ["CST", "RB"], [pk])
                            cp(LG[:], ps[:, 0:36], [pk], ["LG"])
                            R_ = ["LG", "SM", "GOH", "GE", "ET", "ES", "T8", "SEL", "EX"]

                            def dv(fn, extra_w=()):
                                P.op("dve", fn, R_, R_ + list(extra_w))
                            dv(lambda: nc.vector.tensor_reduce(out=SM[:, 0:1], in_=LG[:, 0:4], axis=AX.X, op=ALU.max))
                            dv(lambda: nc.vector.tensor_scalar(out=GOH[:], in0=LG[:, 0:4], scalar1=SM[:, 0:1], scalar2=None,
                                                               op0=ALU.is_equal))
                            dv(lambda: nc.vector.tensor_scalar(out=SM[:, 1:2], in0=SM[:, 0:1], scalar1=-1.0, scalar2=None,
                                                               op0=ALU.mult))
                            act(GE[:], LG[:, 0:4], AF.Exp, R_, R_, bias=SM[:, 1:2])
                            dv(lambda: nc.vector.tensor_reduce(out=SM[:, 2:3], in_=GE[:], axis=AX.X, op=ALU.add))
                            dv(lambda: nc.vector.reciprocal(out=SM[:, 3:4], in_=SM[:, 2:3]))
                            dv(lambda: nc.vector.tensor_tensor(
                                out=ET[:].rearrange("p (g e) -> p g e", e=8),
                                in0=LG[:, 4:36].rearrange("p (g e) -> p g e", e=8),
                                in1=GOH[:].unsqueeze(2).broadcast_to([128, 4, 8]), op=ALU.mult))
                            dv(lambda: nc.vector.tensor_reduce(out=ES[:], in_=ET[:].rearrange("p (g e) -> p e g", e=8),
                                                               axis=AX.X, op=ALU.add))
                            dv(lambda: nc.vector.max(out=T8[:], in_=ES[:]))
                            dv(lambda: nc.vector.tensor_scalar(out=SEL[:], in0=ES[:], scalar1=T8[:, 1:2], scalar2=None,
                                                               op0=ALU.is_ge))
                            dv(lambda: nc.vector.tensor_scalar(out=SM[:, 4:5], in0=T8[:, 0:1], scalar1=-1.0, scalar2=None,
                                                               op0=ALU.mult))
                            act(EX[:], ES[:], AF.Exp, R_, R_, bias=SM[:, 4:5])
                            dv(lambda: nc.vector.tensor_tensor(out=EX[:], in0=EX[:], in1=SEL[:], op=ALU.mult))
                            dv(lambda: nc.vector.tensor_reduce(out=SM[:, 5:6], in_=EX[:], axis=AX.X, op=ALU.add))
                            dv(lambda: nc.vector.reciprocal(out=SM[:, 6:7], in_=SM[:, 5:6]))
                            dv(lambda: nc.vector.tensor_tensor(out=SM[:, 7:8], in0=SM[:, 6:7], in1=SM[:, 3:4], op=ALU.mult))
                            dv(lambda: nc.vector.tensor_scalar(out=EX[:], in0=EX[:], scalar1=SM[:, 7:8], scalar2=None,
                                                               op0=ALU.mult))
                            dv(lambda: nc.vector.tensor_tensor(
                                out=WCS[:, gi, :].rearrange("p (g e) -> p g e", e=8),
                                in0=GOH[:].unsqueeze(2).broadcast_to([128, 4, 8]),
                                in1=EX[:].unsqueeze(1).broadcast_to([128, 4, 8]), op=ALU.mult), ["WCS"])
                            dv(lambda: nc.vector.tensor_tensor(
                                out=AST[:, gi, :].rearrange("p (g e) -> p g e", e=8),
                                in0=GOH[:].unsqueeze(2).broadcast_to([128, 4, 8]),
                                in1=SEL[:].unsqueeze(1).broadcast_to([128, 4, 8]), op=ALU.mult), ["AST"])
                    for g4 in range(0, ntiles, 4):
                        m4 = min(4, ntiles - g4)
                        ps, pk = nb()
                        for j in range(m4):
                            mm(ps[:, j * 32:(j + 1) * 32], ONESB, AST[:, g4 + j, :], True, True, ["CSTB", "AST"], [pk])
                        cp(CSS[:, g4:g4 + m4, :], ps[:, 0:m4 * 32].rearrange("p (j e) -> p j e", e=32), [pk], ["CSS"])
                    P.op("dve", lambda: nc.vector.memset(PRE[:, 0, :], 0.0), [], ["PRE"])
                    for j in range(ntiles):
                        tt(PRE[:, j + 1, :], PRE[:, j, :], CSS[:, j, :], ALU.add, ["PRE", "CSS"], ["PRE"])
                    ps, pk = nb()
                    for j in range(ntiles):
                        mm(ps[0:32, 0:2], AST[:, j, :], ONESB[:, 0:2], j == 0, j == ntiles - 1, ["AST", "CSTB"], [pk])
                    cp(CNT[0:32, 0:1], ps[0:32, 0:1], [pk], ["CNT"])
                    ts(CNT[0:32, 3:4], CNT[0:32, 0:1], 128.0 / RB_, ALU.mult, ["CNT"], ["CNT"])
                    tt(NBK[0:32, 0, :], CNT[0:32, 3:4].broadcast_to([32, 36]), CST3[0:32, 0:36], ALU.is_gt,
                       ["CNT", "CST3"], ["NBK"])
                    P.op("dve", lambda: nc.vector.tensor_reduce(out=CNT[0:32, 1:2], in_=NBK[0:32, 0, :], axis=AX.X, op=ALU.add),
                         ["NBK", "CNT"], ["CNT"])
                    ts(CNT[0:32, 2:3], CNT[0:32, 1:2], float(RB_), ALU.mult, ["CNT"], ["CNT"])
                    cp(PADB[:, :], CNT[0:32, 2:3].broadcast_to([32, 128]), ["CNT"], ["PADB"])
                    ps, pk = nb()
                    mm(ps[:, 0:32], PADB[:, :], CST3[0:32, 64:96], True, True, ["PADB", "CST3"], [pk])
                    mm(ps[:, 32:64], PADB[:, :], CST3[0:32, 96:128], True, True, ["PADB", "CST3"], [pk])
                    cp(PST[:], ps[:, 0:32], [pk], ["PST"])
                    cp(PEN[:], ps[:, 32:64], [pk], ["PEN"])
                    ts(EQ[:], PEN[:], 128.0 / RB_, ALU.mult, ["PEN", "EQ"], ["EQ"])
                    tt(BLE[:, 0:NB_, :], EQ[:].unsqueeze(1).broadcast_to([128, NB_, 32]),
                       CST3[:, 136:136 + NB_].unsqueeze(2).broadcast_to([128, NB_, 32]), ALU.is_le, ["EQ", "CST3"], ["BLE"])
                    P.op("dve", lambda: nc.vector.tensor_reduce(out=BLKB[:, 0:NB_], in_=BLE[:, 0:NB_, :], axis=AX.X, op=ALU.add),
                         ["BLE"], ["BLKB"])
                    ts(BLKB[:, 0:NB_], BLKB[:, 0:NB_], 31.0, ALU.min, ["BLKB"], ["BLKB"], s2=128.0, op1=ALU.mult)
                    ts(BLKB[:, 0:NB_], BLKB[:, 0:NB_], CST3[:, 129:130], ALU.add, ["BLKB", "CST3"], ["BLKB"],
                       s2=float(l * 4096), op1=ALU.add)
                    blef = BLE[:, 0:3, :].rearrange("p a b -> p (a b)")[:, 0:NB_]
                    ts(blef, CST3[:, 136:136 + NB_], EQ[:, 31:32], ALU.is_ge, ["CST3", "EQ", "BLE"], ["BLE"])
                    stt(BLKB[:, 0:NB_], blef, 1.0e6, BLKB[:, 0:NB_], ALU.mult, ALU.add, ["BLE", "BLKB"], ["BLKB"])
                    cp(IDXW[:, 0:NB_], BLKB[:, 0:NB_], ["BLKB"], ["IDXW"])
                    for g4 in range(0, ntiles, 4):
                        m4 = min(4, ntiles - g4)
                        ps, pk = nb()
                        for j in range(m4):
                            mm(ps[:, j * 32:(j + 1) * 32], TRIS, AST[:, g4 + j, :], True, True, ["CSTB2", "AST"], [pk])
                        for j in range(m4):
                            gi = g4 + j
                            R2 = ["DP1", "EQ", "T8"]
                            tt(DP1[:], ps[:, j * 32:(j + 1) * 32], PRE[:, gi, :], ALU.add, [pk, "PRE"] + R2, R2)
                            tt(DP1[:], DP1[:], PST[:], ALU.add, R2 + ["PST"], R2)
                            stt(DP1[:], DP1[:], 1.0, AST[:, gi, :], ALU.add, ALU.mult, R2 + ["AST"], R2)
                            P.op("dve", lambda: nc.vector.max(out=T8[:], in_=DP1[:]), R2 + ["LG"], R2 + ["LG"])
                            ts(DESTF[:, gi, :], T8[:, 0:2], -1.0, ALU.add, R2, ["DESTF"])
                            for kk in range(2):
                                ts(EQ[:], DP1[:], T8[:, kk:kk + 1], ALU.is_equal, R2, R2)
                                tt(EQ[:], EQ[:], WCS[:, gi, :], ALU.mult, R2 + ["WCS"], R2)
                                P.op("dve", lambda kk=kk, gi=gi: nc.vector.tensor_reduce(
                                    out=WSEL[:, gi, kk:kk + 1], in_=EQ[:], axis=AX.X, op=ALU.add), R2 + ["WSEL"], R2 + ["WSEL"])
                    cp(DEST[:, 0:ntiles, :], DESTF[:, 0:ntiles, :], ["DESTF"], ["DEST"])
                P.barrier()
                if MOE_STOP <= 1:
                    return
                with ExitStack() as s3:
                    WB = [sb(f"WB{j}", [128, 4096], BF16, s3) for j in range(4)]
                    UT = [sb(f"UT{j}", [128, 1024], BF16, s3) for j in range(2)]
                    XS = [sb(f"XS{j}", [128, RT, 1024], BF16, s3) for j in range(2)]
                    XST = [sb(f"XST{j}", [128, 8, RB_], BF16, s3) for j in range(2)]
                    H = [sb(f"H{j}", [128, 4, RB_], BF16, s3) for j in range(2)]
                    SG = [sb(f"SG{j}", [128, 512], F32, s3) for j in range(2)]
                    YS = [sb(f"YS{j}", [128, 1024], F32, s3) for j in range(2)]
                    ZT = sb("ZT", [128, 1024], BF16, s3)
                    P.op("dve", lambda: nc.vector.memset(ZT[:], 0.0), [], ["ZT"])
                    for bb in range(NZ):
                        P.dma("sp", xs_d[bb * 128:(bb + 1) * 128, :], ZT[:, :], ["ZT"], [f"xsz{bb}"])
                    for gi in range(ntiles):
                        ut = UT[gi % 2]
                        uk = f"UT{gi % 2}"
                        b = gi // 4 if gi < 16 else 4
                        for half in range(2):
                            ps, pk = nb()
                            for kk in range(4):
                                k = half * 4 + kk
                                mm(ps[:, kk * 128:(kk + 1) * 128], UY[:, k, gi * 128:(gi + 1) * 128], IDB, True, True,
                                   [f"u{k}b{b}", "CSTB"], [pk])
                            cp(ut[:, half * 512:(half + 1) * 512], ps[:, :], [pk], [uk], eng="act" if half else "dve")
                        for kk in range(2):
                            P.dma_fn("pool", lambda sem, ut=ut, gi=gi, kk=kk: nc.gpsimd.indirect_dma_start(
                                out=xs_d, out_offset=bass.IndirectOffsetOnAxis(ap=DEST[:, gi, kk:kk + 1], axis=0),
                                in_=ut[:, :], in_offset=None).then_inc(sem, 16),
                                [uk, "DEST"] + [f"xsz{b_}" for b_ in range(NZ)], [f"xss{gi}_{kk}"])
                    items = []
                    for bb in range(NB_ if MOE_STOP > 2 else 0):
                        items += [("g", bb), ("u", bb), ("d", bb)]
                    def load_item(j):
                        if j >= len(items):
                            return
                        kind, bb = items[j]
                        c0_ = {"g": 0, "u": 2, "d": 4}[kind]
                        for hh_ in range(2):
                            P.dma_fn("pool", lambda sem, j=j, bb=bb, c=c0_ + hh_, hh_=hh_: nc.gpsimd.indirect_dma_start(
                                out=WB[j % 4][:, hh_ * 2048:(hh_ + 1) * 2048], out_offset=None, in_=wc_d[c],
                                in_offset=bass.IndirectOffsetOnAxis(ap=IDXW[:, bb:bb + 1], axis=0),
                                bounds_check=BREG[0], oob_is_err=False).then_inc(sem, 16),
                                ["IDXW"], [f"WB{j % 4}"])

                    def emit_T(bb):
                        xs, xk = XS[bb % 2], f"XS{bb % 2}"
                        xst, xtk = XST[bb % 2], f"XST{bb % 2}"
                        P.dma("sp", xs[:, :, :], xs_d[bb * RB_:(bb + 1) * RB_, :].rearrange("(t p) n -> p t n", p=128),
                              [f"xsz{bb * RT + r_}" for r_ in range(RT)]
                              + [f"xss{g_}_{k_}" for g_ in range(ntiles) for k_ in range(2)], [xk])
                        for rt in range(RT):
                            for half in range(2):
                                ps, pk = nb()
                                for kk in range(4):
                                    k = half * 4 + kk
                                    mm(ps[:, kk * 128:(kk + 1) * 128], xs[:, rt, k * 128:(k + 1) * 128], IDB, True, True,
                                       [xk, "CSTB"], [pk])
                                cp(xst[:, half * 4:(half + 1) * 4, rt * 128:(rt + 1) * 128],
                                   ps[:, :].rearrange("p (k r) -> p k r", r=128), [pk], [xtk], eng="act" if half else "dve")

                    for j in range(4):
                        load_item(j)
                    for bb in range(NB_ if MOE_STOP > 2 else 0):
                        j0 = 3 * bb
                        WG = WB[j0 % 4][:, :].rearrange("p (k n) -> p k n", n=512)
                        WU = WB[(j0 + 1) % 4][:, :].rearrange("p (k n) -> p k n", n=512)
                        WD = WB[(j0 + 2) % 4][:, :].rearrange("p (k n) -> p k n", n=1024)
                        kg, ku, kd = f"WB{j0 % 4}", f"WB{(j0 + 1) % 4}", f"WB{(j0 + 2) % 4}"
                        xst, xtk = XST[bb % 2], f"XST{bb % 2}"
                        Hb, hk = H[bb % 2], f"H{bb % 2}"
                        if bb == 0:
                            emit_T(0)
                        pgs = [nb() for _ in range(RT)]
                        pus = [nb() for _ in range(RT)]
                        fpb = 512 // RB_
                        for fc in range(4):
                            pg_, pkg_ = pgs[fc // fpb]
                            for k in range(8):
                                mm(pg_[:, (fc % fpb) * RB_:(fc % fpb + 1) * RB_], WG[:, k, fc * 128:(fc + 1) * 128], xst[:, k, :],
                                   k == 0, k == 7, [kg, xtk], [pkg_])
                        for fc in range(4):
                            pu_, pku_ = pus[fc // fpb]
                            for k in range(8):
                                mm(pu_[:, (fc % fpb) * RB_:(fc % fpb + 1) * RB_], WU[:, k, fc * 128:(fc + 1) * 128], xst[:, k, :],
                                   k == 0, k == 7, [ku, xtk], [pku_])
                        for i_ in range(RT):
                            sg = SG[i_ % 2]
                            act(sg[:, :], pgs[i_][0][:, :], AF.Silu, [pgs[i_][1]], [f"SG{i_ % 2}"])
                            tt(Hb[:, i_ * fpb:(i_ + 1) * fpb, :].rearrange("p f r -> p (f r)"), sg[:, :], pus[i_][0][:, :],
                               ALU.mult, [f"SG{i_ % 2}", pus[i_][1]], [hk])
                        load_item(j0 + 4)
                        load_item(j0 + 5)
                        if bb + 1 < NB_:
                            emit_T(bb + 1)
                        for rt in range(RT):
                            ys, yk = YS[rt % 2], f"YS{rt % 2}"
                            for half in range(2):
                                py, pky = nb()
                                for fc in range(4):
                                    mm(py[:, :], Hb[:, fc, rt * 128:(rt + 1) * 128], WD[:, fc, half * 512:(half + 1) * 512],
                                       fc == 0, fc == 3, [kd, hk], [pky])
                                cp(ys[:, half * 512:(half + 1) * 512], py[:, :], [pky], [yk], eng="act" if half else "dve")
                            P.dma("act", ys_d[bb * RB_ + rt * 128:bb * RB_ + (rt + 1) * 128, :], ys[:, :], [yk], [f"ysd{bb}_{rt}"])
                        load_item(j0 + 6)
                P.barrier()
                if MOE_STOP <= 3:
                    return
                with ExitStack() as s3:
                    GG = [[sb(f"G{j}_{i_}", [128, 1024], F32, s3) for j in range(2)] for i_ in range(2)]
                    ZZ = [sb(f"Z{i_}", [128, 1024], F32, s3) for i_ in range(2)]
                    for b in range(nblk):
                        t0, n, grp = BLOCKS[b]
                        for k in range(8):
                            xk = XT[:, k, t0:t0 + n]
                            if k % 2 == 0:
                                act(xk, xk, AF.Copy, [f"x{k}b{b}"], [f"x{k}b{b}"], scale=ALPHA)
                            else:
                                ts(xk, xk, ALPHA, ALU.mult, [f"x{k}b{b}"], [f"x{k}b{b}"])
                    ysk = [f"ysd{b_}_{r_}" for b_ in range(NB_) for r_ in range(RT)]
                    for gi in range(ntiles):
                        b = gi // 4 if gi < 16 else 4
                        grp = BLOCKS[b][2]
                        G0, G1 = GG[gi % 2]
                        Z = ZZ[gi % 2]
                        g0k, g1k, zk = f"G0_{gi % 2}", f"G1_{gi % 2}", f"Z{gi % 2}"
                        for kk, (G, gk) in enumerate(((G0, g0k), (G1, g1k))):
                            P.dma_fn("pool", lambda sem, G=G, gi=gi, kk=kk: nc.gpsimd.indirect_dma_start(
                                out=G[:, :], out_offset=None, in_=ys_d,
                                in_offset=bass.IndirectOffsetOnAxis(ap=DEST[:, gi, kk:kk + 1], axis=0)).then_inc(sem, 16),
                                ysk + ["DEST"], [gk])
                        ts(Z[:, :], G0[:, :], WSEL[:, gi, 0:1], ALU.mult, [g0k, "WSEL"], [zk])
                        stt(Z[:, :], G1[:, :], WSEL[:, gi, 1:2], Z[:, :], ALU.mult, ALU.add, [g1k, "WSEL", zk], [zk])
                        for half in range(2):
                            ps, pk = nb()
                            for kk in range(4):
                                k = half * 4 + kk
                                P.op("pe", lambda k=k, kk=kk, ps=ps, Z=Z: nc.tensor.transpose(
                                    out=ps[:, kk * 128:(kk + 1) * 128], in_=Z[:, k * 128:(k + 1) * 128], identity=IDF),
                                    [zk, "CST"], [pk])
                            for kk in range(4):
                                k = half * 4 + kk
                                xk = XT[:, k, gi * 128:(gi + 1) * 128]
                                stt(xk, ps[:, kk * 128:(kk + 1) * 128], mod(l, 5, k, grp), xk, ALU.mult, ALU.add,
                                    [pk, "MOD", f"x{k}b{b}"], [f"x{k}b{b}"])
                    for b in range(nblk):
                        ln_block(l, 1, b, (SQ, MEAN, RSTD))
            P.barrier()

        for l in range(n_layers):
            if stop == "ada":
                break
            lastl = l == DEPTH - 1
            need_ctx = not lastl
            is_dbg_last = (l == n_layers - 1)
            if l % 2 == 0:
                mixer_ab(l, need_ctx)
                wo = ab_w_out_d[l // 2]
            else:
                mixer_c(l, need_ctx)
                wo = c_w_out_d[l // 2]
            raw = is_dbg_last and stop == "mix"
            if is_dbg_last and stop == "y":
                for b in range(5):
                    t0, n, grp = BLOCKS[b]
                    for k in range(8):
                        cp(XT[:, k, t0:t0 + n], UY[:, k, t0:t0 + n], [f"u{k}b{b}"], [f"x{k}b{b}"])
                break
            proj_ln(l, wo, 5 if need_ctx else 4, raw)
            if is_dbg_last and stop in ("mix", "ln1"):
                break
            if SPARSE:
                moe_sparse(l, need_ctx)
            else:
                moe(l, need_ctx)

        for k in range(8):
            P.dma("sp", out_d[k], XT[:, k, :], xkeys(ks=[k]), [f"out{k}"])
        P.finish("sp", [f"out{k}" for k in range(8)])
        print("bass ops:", P.nops, P.cnt)
    return nc


def _fm(v):
    v = np.asarray(v, np.float32)
    lead = v.shape[:-1]
    r = v.reshape(lead + (8, 128))
    r = np.moveaxis(r, -1, 0)
    return np.ascontiguousarray(r)


def _rope_tables(head_dim, per, qscale):
    quarter = head_dim // 4
    row = np.repeat(np.arange(T // 64, dtype=np.float32), 64)
    col = np.tile(np.arange(64, dtype=np.float32), T // 64)
    inv = (np.float32(10000.0) ** (-np.arange(quarter, dtype=np.float32) / np.float32(quarter))).astype(np.float32)
    ang_r = row[:, None] * inv
    ang_c = col[:, None] * inv
    ang = np.concatenate([ang_r, ang_r, ang_c, ang_c], axis=-1)
    cos = np.cos(ang).astype(np.float32).T
    sin = np.sin(ang).astype(np.float32).T
    sign = np.ones((head_dim, 1), np.float32)
    sign[0:quarter] = -1.0
    sign[2 * quarter:3 * quarter] = -1.0
    sin = sin * sign
    rep = 128 // head_dim
    cos = np.tile(cos, (rep, 1))
    sin = np.tile(sin, (rep, 1))
    return np.ascontiguousarray(np.stack([cos * qscale, sin * qscale, cos, sin]).astype(np.float32))


_CONSTS = None


def _consts():
    global _CONSTS
    if _CONSTS is not None:
        return _CONSTS
    c = {}
    c["ropeA"] = _rope_tables(128, 128, np.float32(128 ** -0.5))
    c["ropeC"] = _rope_tables(64, 64, np.float32(0.125))
    ii = np.arange(128)
    cst = np.zeros((128, 1024), np.float32)
    cst[:, 0:128] = np.eye(128, dtype=np.float32)
    cst[:, 128:256] = 1.0 / 1024.0
    cst[:, 256:384] = 1.0 / 128.0
    cst[:, 384:512] = np.where(ii[:, None] <= ii[None, :], -1.0 / 16.0, 0.0)
    cst[:, 512:640] = 1.0
    c["cst"] = cst
    cst2 = np.zeros((128, 256), np.float32)
    cst2[:, 0:128] = np.where(ii[:, None] >= ii[None, :], -1.0 / 16.0, 0.0)
    cst2[:, 128:256] = np.where(ii[:, None] > ii[None, :], -1.0 / 16.0, 0.0)
    c["cst2"] = cst2
    cstb = np.zeros((128, 256), np.float32)
    cstb[:, 0:128] = np.eye(128)
    cstb[:, 128:256] = 1.0
    c["cstb"] = cstb.astype(ml_dtypes.bfloat16)
    m = (ii[:, None] > ii[None, :]).astype(np.uint8)
    c["mask"] = np.ascontiguousarray(np.tile(m, (1, 4)))
    cst3 = np.zeros((128, 256), np.float32)
    cst3[:, 0:36] = (np.arange(36, dtype=np.float32) * 128.0)[None, :]
    e32 = np.arange(32)
    cst3[0:32, 64:96] = (e32[:, None] < e32[None, :]).astype(np.float32)
    cst3[0:32, 96:128] = (e32[:, None] <= e32[None, :]).astype(np.float32)
    cst3[:, 128] = np.arange(128, dtype=np.float32) * 128.0
    cst3[:, 129] = np.arange(128, dtype=np.float32)
    cst3[:, 136:208] = (np.arange(72, dtype=np.float32) * 128.0)[None, :]
    c["cst3"] = cst3
    c["cstb2"] = (ii[:, None] < ii[None, :]).astype(np.float32).astype(ml_dtypes.bfloat16)
    sele = np.zeros((32, 32, 128), np.float32)
    for e in range(32):
        sele[e, e, :] = 1.0
    c["sele"] = sele.reshape(32, 32 * 128).astype(ml_dtypes.bfloat16)
    ret = np.zeros((128, 4, 6, 128), np.float32)
    pos = np.arange(128, dtype=np.float64)
    for h in range(4):
        lf = float(np.log1p(-np.exp2(-np.float32(5.0 + h)), dtype=np.float32))
        lb = float(np.log1p(-np.exp2(-np.float32(5.5 + h)), dtype=np.float32))
        ret[:, h, 0, :] = np.exp(lf * (pos + 1))
        ret[:, h, 1, :] = np.exp(-lf * (pos + 1))
        ret[:, h, 2, :] = np.exp(lf * (127 - pos))
        ret[:, h, 3, :] = np.exp(lb * (127 - pos))
        ret[:, h, 4, :] = np.exp(-lb * (128 - pos))
        ret[:, h, 5, :] = np.exp(lb * pos)
    c["ret"] = ret
    _CONSTS = c
    return c


def _prep_inputs(inputs):
    f = lambda a: np.ascontiguousarray(np.asarray(a, np.float32))
    shared = dict(_consts())
    shared["adab"] = np.ascontiguousarray(np.asarray(inputs["ada_b"], np.float32).reshape(4, 48, 128).transpose(2, 0, 1))
    lnp = np.stack([inputs["ln1_g"], inputs["ln1_b"], inputs["ln2_g"], inputs["ln2_b"]], axis=1)
    shared["lnp"] = np.ascontiguousarray(np.asarray(lnp, np.float32).reshape(4, 4, 8, 128).transpose(3, 0, 1, 2))
    gn = np.concatenate([inputs["ab_gn_a"], inputs["ab_gn_b"]], axis=1)
    shared["gn"] = np.ascontiguousarray(np.asarray(gn, np.float32).reshape(2, 8, 128).transpose(2, 0, 1))
    shared["subg"] = np.ascontiguousarray(np.asarray(inputs["c_subln_g"], np.float32).reshape(2, 8, 128).transpose(2, 0, 1))
    wlr = np.zeros((32, 2, 2, 256), np.float32)
    wlr[0:16, :, 0, :] = np.asarray(inputs["ab_w_lr_f"]).transpose(1, 0, 2)
    wlr[0:16, :, 1, :] = np.asarray(inputs["ab_w_lr_b"]).transpose(1, 0, 2)
    wlr[16, :, 0, :] = np.asarray(inputs["ab_b_lr_f"])
    wlr[16, :, 1, :] = np.asarray(inputs["ab_b_lr_b"])
    shared["wlr"] = wlr
    wr = np.concatenate([inputs["moe_w_grp"], inputs["moe_w_rexp"]], axis=2)
    shared["wr"] = np.ascontiguousarray(np.asarray(wr, np.float32).reshape(4, 8, 128, 36).transpose(2, 0, 1, 3))
    rb = np.concatenate([inputs["moe_b_grp"], inputs["moe_b_rexp"]], axis=1)
    shared["rb"] = np.ascontiguousarray(np.asarray(rb, np.float32)[None])
    lqk = np.stack([inputs["c_lq1"], inputs["c_lk1"], inputs["c_lq2"], inputs["c_lk2"]], axis=1)
    shared["lqk"] = np.ascontiguousarray(np.asarray(lqk, np.float32).transpose(3, 0, 1, 2))
    for nm in ("ada_w", "ab_w_in", "ab_w_out", "c_w_qkv", "c_w_out"):
        shared[nm] = f(inputs[nm])
    if SPARSE:
        for ci, nm in ((0, "moe_w_gate"), (2, "moe_w_up")):
            wp = np.asarray(inputs[nm], np.float32).reshape(4, 32, 8, 128, 512).transpose(0, 1, 3, 2, 4).reshape(4 * 32 * 128, 4096)
            shared[f"wc{ci}"] = np.ascontiguousarray(wp[:, 0:2048])
            shared[f"wc{ci + 1}"] = np.ascontiguousarray(wp[:, 2048:4096])
        wp = np.asarray(inputs["moe_w_down"], np.float32).reshape(4, 32, 4, 128, 1024).transpose(0, 1, 3, 2, 4).reshape(4 * 32 * 128, 4096)
        shared["wc4"] = np.ascontiguousarray(wp[:, 0:2048])
        shared["wc5"] = np.ascontiguousarray(wp[:, 2048:4096])
    else:
        for nm in ("moe_w_gate", "moe_w_up", "moe_w_down"):
            shared[nm] = f(inputs[nm])
    x = np.asarray(inputs["x"], np.float32)
    ctx = np.asarray(inputs["ctx"], np.float32)
    c = np.asarray(inputs["c"], np.float32)
    c_ctx = np.asarray(inputs["c_ctx"], np.float32)
    in_maps = []
    for b in range(8):
        tok = np.concatenate([x[b], ctx[b]], axis=0)
        xT = np.ascontiguousarray(tok.T.reshape(8, 128, NT))
        cc = np.stack([c[b].reshape(8, 128).T, c_ctx.reshape(8, 128).T], axis=-1)
        m = dict(shared)
        m["xT"] = xT
        m["cc"] = np.ascontiguousarray(cc.astype(np.float32))
        in_maps.append(m)
    return in_maps


_NC_CACHE = {}


def run(inputs, n_layers=DEPTH, stop=None, ncores=8):
    key = (n_layers, stop)
    if key not in _NC_CACHE:
        _NC_CACHE[key] = build(n_layers, stop)
    nc = _NC_CACHE[key]
    in_maps = _prep_inputs(inputs)[:ncores]
    if stop in ("ada", "mix", "ln1", "y") and n_layers <= 1:
        for m in in_maps:
            for nm in (["wc%d" % c for c in range(6)] if SPARSE else ["moe_w_gate", "moe_w_up", "moe_w_down"]):
                m.pop(nm)
    res = run_bass_kernel_spmd(nc, in_maps, core_ids=list(range(ncores)))
    outs = [np.asarray(r["outT"]).reshape(D, NT).T for r in res.results]
    return outs


def kernel(**inputs):
    outs = run(inputs)
    return np.ascontiguousarray(np.stack([o[:T] for o in outs], axis=0).astype(np.float32))
```
